# Optimizing a Trainium2 kernel written in Bass

```python
import math
import jax
import jax.numpy as jnp
from jax import lax
import numpy as np

D_MODEL = 1024
BATCH = 8
SEQ = 2048
DEPTH = 4

CTX_LEN = 256
GRID_W = 64
N_MIXERS = 4
NORM_EPS = 1e-6
ATTN_QBLOCK = 128
ROPE_BASE = 10000.0

NA_HEADS = 16
NA_HEAD_DIM = D_MODEL // NA_HEADS
NA_WIN_H = 8
NA_WIN_W = 16
NA_QBLOCK_W = 16
NA_KBLOCK_W = 2 * NA_WIN_W

MLA_HEADS = 16
MLA_Q_RANK = 3 * D_MODEL // 8
MLA_KV_RANK = D_MODEL // 4
MLA_NOPE_DIM = D_MODEL // MLA_HEADS
MLA_ROPE_DIM = MLA_NOPE_DIM // 2
MLA_V_DIM = D_MODEL // MLA_HEADS

HY_SHORT_CONV = 3
HY_EMB_DIM = 33
HY_FILTER_HIDDEN = 64
HY_DECAY_TARGET = 1e-2
HY_FAST_DECAY = 0.3
HY_SLOW_DECAY = 1.5
HY_MOD_SHIFT = 0.05

ML_HEADS = 8
ML_V_DIM = D_MODEL // ML_HEADS
ML_QK_DIM = ML_V_DIM // 2
ML_CHUNK = 64
ML_SHORT_CONV = 3
ML_FORGET_BIAS = 3.0

N_EXPERTS = 16
EXPERT_FF = 2816
EC_CAPACITY_FACTOR = 2

kernel_name = 'hybrid_dit_na_mla_hyena_mlstm_ecmoe'


def rmsnorm(x, g):
    x32 = x.astype(jnp.float32)
    y = x32 * lax.rsqrt(jnp.mean(x32 * x32, axis=-1, keepdims=True) + NORM_EPS)
    return (y * g.astype(jnp.float32)).astype(x.dtype)


def modulate(h, shift, scale):
    return h * (1 + scale) + shift


def short_conv(z, w, b):
    taps = w.shape[0]
    length = z.shape[1]
    pad = taps // 2
    zp = jnp.pad(z, ((0, 0), (pad, taps - 1 - pad), (0, 0)))
    y = b
    for t in range(taps):
        y = y + zp[:, t:t + length] * w[t]
    return y


def dense_attention(q, k, v, scale):
    bsz, lq, nh, dk = q.shape
    dv = v.shape[-1]
    nb = lq // ATTN_QBLOCK
    qb = jnp.moveaxis(q.reshape(bsz, nb, ATTN_QBLOCK, nh, dk), 1, 0)

    def block(qi):
        s = jnp.einsum('bqhd,bkhd->bhqk', qi, k).astype(jnp.float32) * scale
        p = jax.nn.softmax(s, axis=-1).astype(v.dtype)
        return jnp.einsum('bhqk,bkhd->bqhd', p, v)

    o = lax.map(block, qb)
    return jnp.moveaxis(o, 0, 1).reshape(bsz, lq, nh * dv)


def axial_rope(length):
    t = jnp.arange(length)
    row = (t // GRID_W).astype(jnp.float32)
    col = (t % GRID_W).astype(jnp.float32)
    n_freq = MLA_ROPE_DIM // 4
    inv = ROPE_BASE ** (-jnp.arange(n_freq, dtype=jnp.float32) / n_freq)
    ang = jnp.concatenate([row[:, None] * inv, col[:, None] * inv], axis=-1)
    return jnp.cos(ang), jnp.sin(ang)


def apply_rope(x, cos, sin):
    x32 = x.astype(jnp.float32)
    half = x.shape[-1] // 2
    x1, x2 = x32[..., :half], x32[..., half:]
    return jnp.concatenate([x1 * cos - x2 * sin, x2 * cos + x1 * sin], axis=-1).astype(x.dtype)


def na_indices(rows):
    kh = min(NA_WIN_H, rows)
    r = np.arange(rows)
    rs = np.clip(r - kh // 2, 0, rows - kh)
    key_rows = rs[:, None] + np.arange(kh)[None, :]
    dr = key_rows - r[:, None] + (NA_WIN_H - 1)
    ncb = GRID_W // NA_QBLOCK_W
    qcol = np.arange(GRID_W).reshape(ncb, NA_QBLOCK_W)
    cs = np.clip(qcol - NA_WIN_W // 2, 0, GRID_W - NA_WIN_W)
    kc0 = np.clip(np.arange(ncb) * NA_QBLOCK_W - NA_WIN_W // 2, 0, GRID_W - NA_KBLOCK_W)
    kcol = kc0[:, None] + np.arange(NA_KBLOCK_W)[None, :]
    col_ok = (kcol[:, None, :] >= cs[:, :, None]) & (kcol[:, None, :] < cs[:, :, None] + NA_WIN_W)
    dc = np.clip(kcol[:, None, :] - qcol[:, :, None], -(NA_WIN_W - 1), NA_WIN_W - 1) + NA_WIN_W - 1
    key_idx = key_rows[:, None, :, None] * GRID_W + kcol[None, :, None, :]
    return (kh, key_idx.reshape(rows, ncb, kh * NA_KBLOCK_W).astype(np.int32), dr.astype(np.int32),
            col_ok, dc.astype(np.int32))


def neighbourhood_attention(h_lat, h_ctx, w_qkv, rpb, w_o):
    bsz, length, _ = h_lat.shape
    rows = length // GRID_W
    nh, hd = NA_HEADS, NA_HEAD_DIM
    scale = hd ** -0.5
    qkv = (h_lat @ w_qkv).reshape(bsz, length, 3, nh, hd)
    q, k, v = qkv[:, :, 0], qkv[:, :, 1], qkv[:, :, 2]
    qkv_c = (h_ctx @ w_qkv).reshape(bsz, h_ctx.shape[1], 3, nh, hd)
    qc, kc, vc = qkv_c[:, :, 0], qkv_c[:, :, 1], qkv_c[:, :, 2]
    lc = kc.shape[1]
    y_ctx = dense_attention(qc, kc, vc, scale)

    kh, key_idx, dr, col_ok, dc = na_indices(rows)
    ncb = GRID_W // NA_QBLOCK_W
    nk = kh * NA_KBLOCK_W
    mask = np.broadcast_to(col_ok[:, :, None, :], (ncb, NA_QBLOCK_W, kh, NA_KBLOCK_W)).reshape(ncb, NA_QBLOCK_W, nk)
    dc_b = dc[:, :, None, :]
    q_rows = jnp.moveaxis(q.reshape(bsz, rows, GRID_W, nh, hd), 1, 0)

    def row_block(args):
        q_r, kidx, dr_r = args
        qr = q_r.reshape(bsz, ncb, NA_QBLOCK_W, nh, hd)
        kg = k[:, kidx]
        vg = v[:, kidx]
        bias = rpb[:, dr_r[None, None, :, None], dc_b].reshape(nh, ncb, NA_QBLOCK_W, nk)
        s_loc = jnp.einsum('bjuhd,bjkhd->bhjuk', qr, kg).astype(jnp.float32) * scale + bias.astype(jnp.float32)[None]
        s_loc = jnp.where(mask, s_loc, -jnp.inf)
        s_ctx = jnp.einsum('bjuhd,bchd->bhjuc', qr, kc).astype(jnp.float32) * scale
        p = jax.nn.softmax(jnp.concatenate([s_ctx, s_loc], axis=-1), axis=-1).astype(v.dtype)
        o = (jnp.einsum('bhjuc,bchd->bjuhd', p[..., :lc], vc)
             + jnp.einsum('bhjuk,bjkhd->bjuhd', p[..., lc:], vg))
        return o.reshape(bsz, GRID_W, nh, hd)

    o = lax.map(row_block, (q_rows, jnp.asarray(key_idx), jnp.asarray(dr)))
    y_lat = jnp.moveaxis(o, 0, 1).reshape(bsz, length, nh * hd)
    return y_lat @ w_o, y_ctx @ w_o


def mla_project(u, w_in, q_norm_g, w_q_b, kv_norm_g, w_kv_b, rope):
    bsz, length, _ = u.shape
    nh = MLA_HEADS
    z = u @ w_in
    cq = z[..., :MLA_Q_RANK]
    ckv = z[..., MLA_Q_RANK:MLA_Q_RANK + MLA_KV_RANK]
    k_rope = z[..., MLA_Q_RANK + MLA_KV_RANK:]
    q = (rmsnorm(cq, q_norm_g) @ w_q_b).reshape(bsz, length, nh, MLA_NOPE_DIM + MLA_ROPE_DIM)
    kv = (rmsnorm(ckv, kv_norm_g) @ w_kv_b).reshape(bsz, length, nh, MLA_NOPE_DIM + MLA_V_DIM)
    q_nope, q_rope = q[..., :MLA_NOPE_DIM], q[..., MLA_NOPE_DIM:]
    k_nope, v = kv[..., :MLA_NOPE_DIM], kv[..., MLA_NOPE_DIM:]
    if rope is not None:
        cos, sin = rope
        q_rope = apply_rope(q_rope, cos[:, None, :], sin[:, None, :])
        k_rope = apply_rope(k_rope, cos, sin)
    k = jnp.concatenate([k_nope, jnp.broadcast_to(k_rope[:, :, None, :], (bsz, length, nh, MLA_ROPE_DIM))], axis=-1)
    q = jnp.concatenate([q_nope, q_rope], axis=-1)
    return q, k, v


def mla_attention(h_lat, h_ctx, w_in, q_norm_g, w_q_b, kv_norm_g, w_kv_b, w_o):
    rope = axial_rope(h_lat.shape[1])
    ql, kl, vl = mla_project(h_lat, w_in, q_norm_g, w_q_b, kv_norm_g, w_kv_b, rope)
    qc, kc, vc = mla_project(h_ctx, w_in, q_norm_g, w_q_b, kv_norm_g, w_kv_b, None)
    scale = (MLA_NOPE_DIM + MLA_ROPE_DIM) ** -0.5
    y_lat = dense_attention(ql, jnp.concatenate([kc, kl], axis=1), jnp.concatenate([vc, vl], axis=1), scale)
    y_ctx = dense_attention(qc, kc, vc, scale)
    return y_lat @ w_o, y_ctx @ w_o


def hyena_filter(length, w1, b1, w2, b2, w3, b3, sin_freq):
    f32 = jnp.float32
    t = jnp.linspace(0.0, 1.0, length, dtype=f32)[:, None]
    bands = (HY_EMB_DIM - 1) // 2
    w = (2.0 * math.pi / length) * jnp.arange(length, dtype=f32)[:, None]
    f = jnp.linspace(1e-4, bands - 1, bands, dtype=f32)[None, :]
    z = jnp.concatenate([t, jnp.cos(f * w), -jnp.sin(f * w)], axis=-1)
    sf = sin_freq.astype(f32)
    h = jnp.sin(sf[0] * (z @ w1.astype(f32) + b1.astype(f32)))
    h = jnp.sin(sf[1] * (h @ w2.astype(f32) + b2.astype(f32)))
    h = h @ w3.astype(f32) + b3.astype(f32)
    max_decay = math.log(HY_DECAY_TARGET) / HY_FAST_DECAY
    min_decay = math.log(HY_DECAY_TARGET) / HY_SLOW_DECAY
    deltas = jnp.abs(jnp.linspace(min_decay, max_decay, h.shape[-1] // 2, dtype=f32))
    deltas = jnp.tile(deltas, 2)
    return h * (jnp.exp(-t * deltas) + HY_MOD_SHIFT)


def bidir_long_conv(u, h):
    length, d = u.shape[1], u.shape[2]
    hf, hb = h[:, :d], h[:, d:]
    k = jnp.concatenate([hf, jnp.zeros((1, d), jnp.float32), hb[1:][::-1]], axis=0)
    kf = jnp.fft.rfft(k, n=2 * length, axis=0)
    uf = jnp.fft.rfft(u, n=2 * length, axis=1)
    return jnp.fft.irfft(uf * kf[None], n=2 * length, axis=1)[:, :length]


def hyena_mixer(h_lat, h_ctx, w_in, conv_w, conv_b, f_w1, f_b1, f_w2, f_b2, f_w3, f_b3, sin_freq, skip, w_o):
    def one(u):
        z = short_conv(u @ w_in, conv_w, conv_b)
        x0, x1, v = jnp.split(z, 3, axis=-1)
        filt = hyena_filter(u.shape[1], f_w1, f_b1, f_w2, f_b2, f_w3, f_b3, sin_freq)
        g = (v * x1).astype(jnp.float32)
        y = bidir_long_conv(g, filt) + g * skip.astype(jnp.float32)
        return (y.astype(u.dtype) * x0) @ w_o
    return one(h_lat), one(h_ctx)


def mlstm_scan(q, k, v, ig, lf, state):
    bsz, nh, length, dk = q.shape
    dv = v.shape[-1]
    nc = length // ML_CHUNK

    def chunks(a):
        return jnp.moveaxis(a.reshape(a.shape[:2] + (nc, ML_CHUNK) + a.shape[3:]), 2, 0)

    causal = jnp.tril(jnp.ones((ML_CHUNK, ML_CHUNK), dtype=bool))

    def step(carry, xs):
        c_st, n_st, m_st = carry
        qc, kc, vc, ic, fc = xs
        b = jnp.cumsum(fc, axis=-1)
        log_d = jnp.where(causal, b[..., :, None] - b[..., None, :] + ic[..., None, :], -jnp.inf)
        m_inter = b + m_st[..., None]
        m_t = jnp.maximum(jnp.max(log_d, axis=-1), m_inter)
        s = jnp.einsum('bhtd,bhsd->bhts', qc, kc) * jnp.exp(log_d - m_t[..., None])
        inter = jnp.exp(m_inter - m_t)
        num = jnp.einsum('bhts,bhsv->bhtv', s, vc) + inter[..., None] * jnp.einsum('bhvd,bhtd->bhtv', c_st, qc)
        qn = jnp.sum(s, axis=-1) + inter * jnp.einsum('bhd,bhtd->bht', n_st, qc)
        h = num / jnp.maximum(jnp.abs(qn), jnp.exp(-m_t))[..., None]
        b_end = b[..., -1]
        w_log = b_end[..., None] - b + ic
        m_new = jnp.maximum(b_end + m_st, jnp.max(w_log, axis=-1))
        w = jnp.exp(w_log - m_new[..., None])
        decay = jnp.exp(b_end + m_st - m_new)
        c_new = decay[..., None, None] * c_st + jnp.einsum('bht,bhtv,bhtd->bhvd', w, vc, kc)
        n_new = decay[..., None] * n_st + jnp.einsum('bht,bhtd->bhd', w, kc)
        return (c_new, n_new, m_new), h

    state, hs = lax.scan(step, state, (chunks(q), chunks(k), chunks(v), chunks(ig), chunks(lf)))
    return state, jnp.moveaxis(hs, 0, 2).reshape(bsz, nh, length, dv)


def mlstm_mixer(h_lat, h_ctx, w_in, conv_w, conv_b, gate_b, out_norm_g, w_o):
    nh, dk, dv = ML_HEADS, ML_QK_DIM, ML_V_DIM
    f32 = jnp.float32
    nqk = 2 * nh * dk

    def project(u):
        bsz, length, _ = u.shape
        z = u @ w_in
        qk = jax.nn.silu(short_conv(z[..., :nqk], conv_w, conv_b))
        v = z[..., nqk:nqk + nh * dv]
        o = z[..., nqk + nh * dv:nqk + 2 * nh * dv]
        g = (z[..., nqk + 2 * nh * dv:] + gate_b).astype(f32)

        def heads(a, d):
            return a.reshape(bsz, length, nh, d).transpose(0, 2, 1, 3).astype(f32)

        q = heads(qk[..., :nh * dk], dk)
        k = heads(qk[..., nh * dk:], dk) * (dk ** -0.5)
        vh = heads(v, dv)
        g = g.reshape(bsz, length, 4, nh).transpose(2, 0, 3, 1)
        gates = ((g[0], jax.nn.log_sigmoid(g[1])), (g[2], jax.nn.log_sigmoid(g[3])))
        return q, k, vh, o, gates

    ql, kl, vl, ol, gl = project(h_lat)
    qc, kc, vc, oc, gc = project(h_ctx)
    bsz = ql.shape[0]
    zero = (jnp.zeros((bsz, nh, dv, dk), f32), jnp.zeros((bsz, nh, dk), f32), jnp.zeros((bsz, nh), f32))

    def flip(a):
        return jnp.flip(a, axis=2)

    st_f, hc_f = mlstm_scan(qc, kc, vc, gc[0][0], gc[0][1], zero)
    _, hl_f = mlstm_scan(ql, kl, vl, gl[0][0], gl[0][1], st_f)
    st_b, hc_b = mlstm_scan(flip(qc), flip(kc), flip(vc), flip(gc[1][0]), flip(gc[1][1]), zero)
    _, hl_b = mlstm_scan(flip(ql), flip(kl), flip(vl), flip(gl[1][0]), flip(gl[1][1]), st_b)

    def finish(h, o):
        b_, _, length, _ = h.shape
        h = h * lax.rsqrt(jnp.mean(h * h, axis=-1, keepdims=True) + NORM_EPS)
        h = h.transpose(0, 2, 1, 3).reshape(b_, length, nh * dv) * out_norm_g.astype(f32)
        return (h.astype(o.dtype) * jax.nn.sigmoid(o)) @ w_o

    return finish(hl_f + flip(hl_b), ol), finish(hc_f + flip(hc_b), oc)


def ec_moe(h, router_w, w_gate, w_up, w_down):
    bsz, length, _ = h.shape
    cap = max(1, EC_CAPACITY_FACTOR * length // N_EXPERTS)
    aff = jax.nn.softmax((h @ router_w).astype(jnp.float32), axis=-1)
    gval, tok = lax.top_k(jnp.swapaxes(aff, 1, 2), cap)
    bidx = jnp.arange(bsz)[:, None, None]
    xg = h[bidx, tok]
    a = jnp.einsum('becd,edf->becf', xg, w_gate)
    u = jnp.einsum('becd,edf->becf', xg, w_up)
    y = jnp.einsum('becf,efd->becd', jax.nn.silu(a) * u, w_down) * gval[..., None].astype(h.dtype)
    return jnp.zeros_like(h).at[bidx, tok].add(y)


def setup_inputs(seed: int = 0) -> dict:
    key = jax.random.key(seed)
    ks = iter(jax.random.split(key, 48))

    def nrm(shape, scale):
        return scale * jax.random.normal(next(ks), shape, jnp.float32)

    d = D_MODEL
    e, ff = N_EXPERTS, EXPERT_FF
    n_a, n_b, n_c, n_d = [len(range(m, DEPTH, N_MIXERS)) for m in range(N_MIXERS)]
    hid = HY_FILTER_HIDDEN
    ml_in = 2 * ML_HEADS * ML_QK_DIM + 2 * ML_HEADS * ML_V_DIM + 4 * ML_HEADS
    gate_off = jnp.tile(jnp.repeat(jnp.array([0.0, ML_FORGET_BIAS], jnp.float32), ML_HEADS), 2)
    return {
        'x': nrm((BATCH, SEQ, d), 1.0),
        'c': nrm((BATCH, d), 1.0),
        'ctx': nrm((BATCH, CTX_LEN, d), 1.0),
        'c_ctx': nrm((d,), 1.0),
        'mod_w': nrm((DEPTH, d, 6 * d), 0.5 * d ** -0.5),
        'mod_b': nrm((DEPTH, 6 * d), 0.02),
        'norm_mix_g': 1.0 + nrm((DEPTH, d), 0.02),
        'norm_ffn_g': 1.0 + nrm((DEPTH, d), 0.02),
        'router_w': nrm((DEPTH, d, e), d ** -0.5),
        'moe_w_gate': nrm((DEPTH, e, d, ff), d ** -0.5),
        'moe_w_up': nrm((DEPTH, e, d, ff), d ** -0.5),
        'moe_w_down': nrm((DEPTH, e, ff, d), ff ** -0.5),
        'na_w_qkv': nrm((n_a, d, 3 * d), d ** -0.5),
        'na_rpb': nrm((n_a, NA_HEADS, 2 * NA_WIN_H - 1, 2 * NA_WIN_W - 1), 0.02),
        'na_w_o': nrm((n_a, d, d), d ** -0.5),
        'mla_w_in': nrm((n_b, d, MLA_Q_RANK + MLA_KV_RANK + MLA_ROPE_DIM), d ** -0.5),
        'mla_q_norm_g': 1.0 + nrm((n_b, MLA_Q_RANK), 0.02),
        'mla_w_q_b': nrm((n_b, MLA_Q_RANK, MLA_HEADS * (MLA_NOPE_DIM + MLA_ROPE_DIM)), MLA_Q_RANK ** -0.5),
        'mla_kv_norm_g': 1.0 + nrm((n_b, MLA_KV_RANK), 0.02),
        'mla_w_kv_b': nrm((n_b, MLA_KV_RANK, MLA_HEADS * (MLA_NOPE_DIM + MLA_V_DIM)), MLA_KV_RANK ** -0.5),
        'mla_w_o': nrm((n_b, MLA_HEADS * MLA_V_DIM, d), (MLA_HEADS * MLA_V_DIM) ** -0.5),
        'hy_w_in': nrm((n_c, d, 3 * d), d ** -0.5),
        'hy_conv_w': nrm((n_c, HY_SHORT_CONV, 3 * d), HY_SHORT_CONV ** -0.5),
        'hy_conv_b': nrm((n_c, 3 * d), 0.02),
        'hy_f_w1': nrm((n_c, HY_EMB_DIM, hid), HY_EMB_DIM ** -0.5),
        'hy_f_b1': nrm((n_c, hid), 0.02),
        'hy_f_w2': nrm((n_c, hid, hid), hid ** -0.5),
        'hy_f_b2': nrm((n_c, hid), 0.02),
        'hy_f_w3': nrm((n_c, hid, 2 * d), 0.2 * hid ** -0.5),
        'hy_f_b3': nrm((n_c, 2 * d), 0.02),
        'hy_sin_freq': 1.0 + nrm((n_c, 2, hid), 0.02),
        'hy_skip': nrm((n_c, d), 0.5),
        'hy_w_o': nrm((n_c, d, d), d ** -0.5),
        'ml_w_in': nrm((n_d, d, ml_in), d ** -0.5),
        'ml_conv_w': nrm((n_d, ML_SHORT_CONV, 2 * ML_HEADS * ML_QK_DIM), ML_SHORT_CONV ** -0.5),
        'ml_conv_b': nrm((n_d, 2 * ML_HEADS * ML_QK_DIM), 0.02),
        'ml_gate_b': gate_off + nrm((n_d, 4 * ML_HEADS), 0.1),
        'ml_out_norm_g': 1.0 + nrm((n_d, ML_HEADS * ML_V_DIM), 0.02),
        'ml_w_o': nrm((n_d, ML_HEADS * ML_V_DIM, d), (ML_HEADS * ML_V_DIM) ** -0.5),
        'final_norm_g': 1.0 + nrm((d,), 0.02),
    }


def reference(x, c, ctx, c_ctx, mod_w, mod_b, norm_mix_g, norm_ffn_g, router_w, moe_w_gate, moe_w_up,
              moe_w_down, na_w_qkv, na_rpb, na_w_o, mla_w_in, mla_q_norm_g, mla_w_q_b, mla_kv_norm_g,
              mla_w_kv_b, mla_w_o, hy_w_in, hy_conv_w, hy_conv_b, hy_f_w1, hy_f_b1, hy_f_w2, hy_f_b2, hy_f_w3,
              hy_f_b3, hy_sin_freq, hy_skip, hy_w_o, ml_w_in, ml_conv_w, ml_conv_b, ml_gate_b, ml_out_norm_g,
              ml_w_o, final_norm_g):
    silu_c = jax.nn.silu(c)
    silu_cc = jax.nn.silu(c_ctx)
    for i in range(DEPTH):
        last = i == DEPTH - 1
        mod = (silu_c @ mod_w[i] + mod_b[i])[:, None, :]
        mod_c = silu_cc @ mod_w[i] + mod_b[i]
        sh1, sc1, g1, sh2, sc2, g2 = jnp.split(mod, 6, axis=-1)
        csh1, csc1, cg1, csh2, csc2, cg2 = jnp.split(mod_c, 6, axis=-1)
        h = modulate(rmsnorm(x, norm_mix_g[i]), sh1, sc1)
        hc = modulate(rmsnorm(ctx, norm_mix_g[i]), csh1, csc1)
        kind, j = i % N_MIXERS, i // N_MIXERS
        if kind == 0:
            y, yc = neighbourhood_attention(h, hc, na_w_qkv[j], na_rpb[j], na_w_o[j])
        elif kind == 1:
            y, yc = mla_attention(h, hc, mla_w_in[j], mla_q_norm_g[j], mla_w_q_b[j], mla_kv_norm_g[j],
                                  mla_w_kv_b[j], mla_w_o[j])
        elif kind == 2:
            y, yc = hyena_mixer(h, hc, hy_w_in[j], hy_conv_w[j], hy_conv_b[j], hy_f_w1[j], hy_f_b1[j],
                                hy_f_w2[j], hy_f_b2[j], hy_f_w3[j], hy_f_b3[j], hy_sin_freq[j], hy_skip[j],
                                hy_w_o[j])
        else:
            y, yc = mlstm_mixer(h, hc, ml_w_in[j], ml_conv_w[j], ml_conv_b[j], ml_gate_b[j],
                                ml_out_norm_g[j], ml_w_o[j])
        x = x + g1 * y
        h = modulate(rmsnorm(x, norm_ffn_g[i]), sh2, sc2)
        x = x + g2 * ec_moe(h, router_w[i], moe_w_gate[i], moe_w_up[i], moe_w_down[i])
        if not last:
            ctx = ctx + cg1 * yc
            hc = modulate(rmsnorm(ctx, norm_ffn_g[i]), csh2, csc2)
            ctx = ctx + cg2 * ec_moe(hc, router_w[i], moe_w_gate[i], moe_w_up[i], moe_w_down[i])
    return rmsnorm(x, final_norm_g)
```

```python
import contextlib
import math
import numpy as np
import concourse.bass as bass
import concourse.mybir as mybir
from concourse.bass_utils import run_bass_kernel_spmd

F32 = mybir.dt.float32
BF16 = mybir.dt.bfloat16
I32 = mybir.dt.int32
ALU = mybir.AluOpType
AF = mybir.ActivationFunctionType
AX = mybir.AxisListType
NPBF = mybir.dt.np(BF16)


class Buf:
    __slots__ = ("name", "w", "rs", "excl")

    def __init__(self, name="", excl=False):
        self.name = name
        self.w = None
        self.rs = []
        self.excl = excl


class Prog:
    EPOCH = 30000
    NRING = 6

    def __init__(self):
        self.nc = bass.Bass("TRN2", target_bir_lowering=False)
        nc = self.nc
        self.engs = ["pe", "act", "dve", "pool", "sp"]
        self.lists = {e: [] for e in self.engs}
        self.cnt = {e: 0 for e in self.engs}
        self.nsem = 0
        self.sem = {e: self._newsem(e) for e in ("pe", "act", "dve", "pool")}
        self.seen = {e: {} for e in self.engs}
        self.ring = {q: [self._newsem(f"r{q}{i}") for i in range(self.NRING)] for q in ("sp", "pool", "act")}
        self.ring_n = {q: [0] * self.NRING for q in self.ring}
        self.ring_i = {q: 0 for q in self.ring}
        self.nbuf = 0
        self.ninstr = 0
        self.limit = 1 << 60

    def _newsem(self, name):
        self.nsem += 1
        return self.nc.alloc_semaphore(name=f"s{self.nsem}_{name}")

    def dram(self, name, shape, dtype, kind="Internal"):
        return self.nc.dram_tensor(name, list(shape), dtype, kind=kind).ap()

    def sb(self, stack, name, shape, dtype):
        self.nbuf += 1
        return stack.enter_context(self.nc.sbuf_tensor(f"{name}_{self.nbuf}", list(shape), dtype))

    def ps(self, stack, name, shape=(128, 512), dtype=F32):
        self.nbuf += 1
        return stack.enter_context(self.nc.psum_tensor(f"{name}_{self.nbuf}", list(shape), dtype))

    def _deps(self, eng, reads, writes):
        evs = {}

        def add(ev, kind):
            if ev is None:
                return
            s, v, src = ev
            if src == eng:
                if eng == "pe":
                    return
            k = id(s)
            if self.seen[eng].get(k, -1) >= v:
                return
            if k not in evs or evs[k][1] < v:
                evs[k] = (s, v)

        for b in reads:
            add(b.w, "raw")
        for b in writes:
            add(b.w, "waw")
            for r in b.rs:
                add(r, "war")
        out = []
        for k, (s, v) in evs.items():
            self.seen[eng][k] = v
            out.append((s, v))
        return out

    def _commit(self, ev, reads, writes):
        for b in reads:
            b.rs.append(ev)
            if len(b.rs) > 64:
                best = {}
                for (s, v, src) in b.rs:
                    k = (id(s), src)
                    if k not in best or best[k][1] < v:
                        best[k] = (s, v, src)
                b.rs = list(best.values())
        for b in writes:
            b.w = ev
            b.rs = []

    def op(self, eng, fn, reads=(), writes=()):
        if self.ninstr >= self.limit:
            return
        ex = [b for b in reads if b.excl]
        if ex:
            writes = list(writes) + [b for b in ex if b not in writes]
        waits = self._deps(eng, reads, writes)
        if self.cnt[eng] >= self.EPOCH:
            self.sem[eng] = self._newsem(eng)
            self.cnt[eng] = 0
        self.cnt[eng] += 1
        s = self.sem[eng]
        ev = (s, self.cnt[eng], eng)
        self.lists[eng].append((waits, fn, s, 1))
        self._commit(ev, reads, writes)
        self.ninstr += 1

    def dma(self, out, in_, reads=(), writes=(), q="sp", **kw):
        if self.ninstr >= self.limit:
            return
        eng = q
        i = self.ring_i[q]
        self.ring_i[q] = (i + 1) % self.NRING
        s = self.ring[q][i]
        n = self.ring_n[q][i]
        waits = []
        if n > 0 and self.seen[eng].get(id(s), -1) < 16 * n:
            waits.append((s, 16 * n))
            self.seen[eng][id(s)] = 16 * n
        waits += self._deps(eng, reads, writes)
        self.ring_n[q][i] = n + 1
        ev = (s, 16 * (n + 1), "dma")
        self.lists[eng].append((waits, (lambda e, out=out, in_=in_, kw=kw: e.dma_start(out=out, in_=in_, **kw)), s, 16))
        self._commit(ev, reads, writes)
        self.ninstr += 1

    def barrier(self):
        targets = []
        for e in ("pe", "act", "dve", "pool"):
            if self.cnt[e] > 0:
                targets.append((self.sem[e], self.cnt[e], e))
        for q in self.ring:
            for i, s in enumerate(self.ring[q]):
                n = self.ring_n[q][i]
                if n > 0:
                    targets.append((s, 16 * n, "dma"))
        for eng in self.engs:
            waits = []
            for (s, v, src) in targets:
                if self.seen[eng].get(id(s), -1) >= v:
                    continue
                self.seen[eng][id(s)] = v
                waits.append((s, v))
            if waits:
                self.lists[eng].append((waits, None, None, 0))

    def flush(self, final=False):
        nc = self.nc
        self.barrier()
        if final:
            for q in self.ring:
                for i, s in enumerate(self.ring[q]):
                    n = self.ring_n[q][i]
                    if n > 0 and self.seen[q].get(id(s), -1) < 16 * n:
                        self.lists[q].append(([(s, 16 * n)], None, None, 0))
                        self.seen[q][id(s)] = 16 * n
        lists = self.lists
        self.lists = {e: [] for e in self.engs}

        def emit(e, items):
            for waits, fn, s, inc in items:
                for (ws, wv) in waits:
                    e.wait_ge(ws, wv)
                if fn is not None:
                    fn(e).then_inc(s, inc)

        with nc.Block() as block:
            @block.tensor
            def _(e):
                emit(e, lists["pe"])

            @block.scalar
            def _(e):
                emit(e, lists["act"])

            @block.vector
            def _(e):
                emit(e, lists["dve"])

            @block.gpsimd
            def _(e):
                emit(e, lists["pool"])

            @block.sync
            def _(e):
                emit(e, lists["sp"])


def run(prog, in_maps, trace=False):
    res = run_bass_kernel_spmd(prog.nc, in_maps, core_ids=list(range(len(in_maps))), trace=trace)
    return res


T = 2304
NT = 18
D = 1024
NE = 16
NSLOT = 288
EPS = 1e-6


def load_consts(P, st, C):
    K = {}
    K["idf"] = P.sb(st, "idf", [128, 128], F32); K["b_idf"] = Buf("idf")
    K["idb"] = P.sb(st, "idb", [128, 128], BF16); K["b_idb"] = Buf("idb")
    K["eps"] = P.sb(st, "epsc", [128, 1], F32); K["b_eps"] = Buf("eps")
    ci = Buf("cin")
    P.dma(K["idf"][:, :], C["idf"][:, :], reads=[ci], writes=[K["b_idf"]])
    P.op("dve", lambda v: v.tensor_copy(out=K["idb"][:, :], in_=K["idf"][:, :]), reads=[K["b_idf"]], writes=[K["b_idb"]])
    P.op("dve", lambda v: v.memset(K["eps"][:, :], EPS), writes=[K["b_eps"]])
    return K


def phase_norm(P, K, X, bX, MODS, sh_off, norm_g, HT, bHT, HTM=None, bHTM=None, tiles=range(NT)):
    with contextlib.ExitStack() as st:
        G = P.sb(st, "n_G", [128, D], F32); bG = Buf()
        SC = P.sb(st, "n_SC", [128, 2, D], F32); bSC = Buf()
        AV = P.sb(st, "n_AV", [128, 2, D], F32); bAV = Buf()
        BV = P.sb(st, "n_BV", [128, 2, D], F32); bBV = Buf()
        XT = [P.sb(st, f"n_XT{i}", [128, D], F32) for i in range(2)]; bXT = [Buf() for _ in range(2)]
        JK = P.sb(st, "n_JK", [128, D], F32); bJK = Buf()
        TMP = [P.sb(st, f"n_TMP{i}", [128, D], F32) for i in range(2)]; bTMP = [Buf() for _ in range(2)]
        SS = P.sb(st, "n_SS", [128, 3 * NT], F32); bSS = [Buf() for _ in range(NT)]
        HL = [P.sb(st, f"n_HL{i}", [128, D], BF16) for i in range(2)]; bHL = [Buf() for _ in range(2)]
        PT = [P.ps(st, f"n_PT{i}", [128, 8, 128], BF16) for i in range(2)]; bPT = [Buf(excl=True) for _ in range(2)]
        cin = Buf()
        P.dma(G[:, :], norm_g.partition_broadcast(128), reads=[cin], writes=[bG])
        for r in range(2):
            P.dma(SC[:, r, :], MODS[r, sh_off + D:sh_off + 2 * D].partition_broadcast(128), reads=[cin], writes=[bSC])
            P.dma(BV[:, r, :], MODS[r, sh_off:sh_off + D].partition_broadcast(128), reads=[cin], writes=[bBV])
        for r in range(2):
            P.op("dve", lambda v, r=r: v.scalar_tensor_tensor(out=AV[:, r, :], in0=SC[:, r, :], scalar=1.0, in1=G[:, :], op0=ALU.add, op1=ALU.mult),
                 reads=[bSC, bG], writes=[bAV])
        for i, tt in enumerate(tiles):
            r = 1 if tt < 2 else 0
            b = i % 2
            xt = XT[b]
            P.dma(xt[:, :], X[tt * 128:(tt + 1) * 128, :], reads=[bX[tt]], writes=[bXT[b]])
            P.op("act", lambda a, xt=xt, tt=tt: a.activation(out=JK[:, :], in_=xt[:, :], func=AF.Square, accum_out=SS[:, 3 * tt:3 * tt + 1]),
                 reads=[bXT[b]], writes=[bJK, bSS[tt]])
            P.op("act", lambda a, tt=tt: a.activation(out=SS[:, 3 * tt + 1:3 * tt + 2], in_=SS[:, 3 * tt:3 * tt + 1], func=AF.Sqrt, scale=1.0 / D, bias=K["eps"][:, :]),
                 reads=[bSS[tt], K["b_eps"]], writes=[bSS[tt]])
            P.op("dve", lambda v, tt=tt: v.reciprocal(out=SS[:, 3 * tt + 2:3 * tt + 3], in_=SS[:, 3 * tt + 1:3 * tt + 2]), reads=[bSS[tt]], writes=[bSS[tt]])
            P.op("dve", lambda v, xt=xt, tt=tt, r=r, b=b: v.scalar_tensor_tensor(out=TMP[b][:, :], in0=xt[:, :], scalar=SS[:, 3 * tt + 2:3 * tt + 3], in1=AV[:, r, :], op0=ALU.mult, op1=ALU.mult),
                 reads=[bXT[b], bSS[tt], bAV], writes=[bTMP[b]])
            if HTM is not None:
                hdst = HTM[:, tt, :]; bh = bHTM[tt]
            else:
                hdst = HL[b][:, :]; bh = bHL[b]
            P.op("dve", lambda g, hdst=hdst, r=r, b=b: g.tensor_tensor(out=hdst, in0=TMP[b][:, :], in1=BV[:, r, :], op=ALU.add),
                 reads=[bTMP[b], bBV], writes=[bh])
            for k in range(8):
                P.op("pe", lambda t, k=k, hdst=hdst, b=b: t.transpose(PT[b][:, k, :], hdst[:, k * 128:(k + 1) * 128], K["idb"][:, :]),
                     reads=[bh, K["b_idb"]], writes=[bPT[b]])
            if i % 2 == 0:
                P.op("act", lambda a, tt=tt, b=b: a.copy(out=HT[:, :, tt * 128:(tt + 1) * 128], in_=PT[b][:, :, :]), reads=[bPT[b]], writes=[bHT[tt]])
            else:
                P.op("dve", lambda v, tt=tt, b=b: v.tensor_copy(out=HT[:, :, tt * 128:(tt + 1) * 128], in_=PT[b][:, :, :]), reads=[bPT[b]], writes=[bHT[tt]])
        P.flush()


def phase_outproj(P, K, src, WO, X, bX, MODS, g_off, tiles=range(NT), mode="tm"):
    with contextlib.ExitStack() as st:
        WOb = P.sb(st, "o_WO", [128, 8, D], BF16); bWO = [Buf() for _ in range(8)]
        STG = [P.sb(st, f"o_stg{i}", [128, D], F32) for i in range(2)]; bSTG = [Buf() for _ in range(2)]
        G1 = P.sb(st, "o_G1", [128, 2, D], F32); bG1 = Buf()
        XT = [P.sb(st, f"o_XT{i}", [128, D], F32) for i in range(2)]; bXT = [Buf() for _ in range(2)]
        TMP = [P.sb(st, f"o_TMP{i}", [128, 512], F32) for i in range(2)]; bTMP = [Buf() for _ in range(2)]
        YL = [P.sb(st, f"o_YL{i}", [128, D], BF16) for i in range(2)]; bYL = [Buf() for _ in range(2)]
        YTt = [P.sb(st, f"o_YTt{i}", [128, 8, 128], BF16) for i in range(2)]; bYTt = [Buf() for _ in range(2)]
        PT = [P.ps(st, f"o_PT{i}", [128, 8, 128], BF16) for i in range(2)]; bPT = [Buf(excl=True) for _ in range(2)]
        PO = [P.ps(st, f"o_PO{i}") for i in range(2)]; bPO = [Buf(excl=True) for _ in range(2)]
        cin = Buf()
        for k in range(8):
            s = k % 2
            P.dma(STG[s][:, :], WO[k * 128:(k + 1) * 128, :], reads=[cin], writes=[bSTG[s]])
            if k % 2 == 0:
                P.op("dve", lambda g, k=k, s=s: g.tensor_copy(out=WOb[:, k, :], in_=STG[s][:, :]), reads=[bSTG[s]], writes=[bWO[k]])
            else:
                P.op("act", lambda a, k=k, s=s: a.copy(out=WOb[:, k, :], in_=STG[s][:, :]), reads=[bSTG[s]], writes=[bWO[k]])
        for r in range(2):
            P.dma(G1[:, r, :], MODS[r, g_off:g_off + D].partition_broadcast(128), reads=[cin], writes=[bG1])
        pi = 0
        for i, tt in enumerate(tiles):
            r = 1 if tt < 2 else 0
            b = i % 2
            if mode == "tm":
                src(tt, YL[b], bYL[b])
                for k in range(8):
                    P.op("pe", lambda t, k=k, b=b: t.transpose(PT[b][:, k, :], YL[b][:, k * 128:(k + 1) * 128], K["idb"][:, :]),
                         reads=[bYL[b], K["b_idb"]], writes=[bPT[b]])
                P.op("act", lambda a, b=b: a.copy(out=YTt[b][:, :, :], in_=PT[b][:, :, :]), reads=[bPT[b]], writes=[bYTt[b]])
                lhs = lambda k, b=b: YTt[b][:, k, :]
                blhs = [bYTt[b]]
            else:
                YT, bYT = src
                lhs = lambda k, tt=tt: YT[:, k, tt * 128:(tt + 1) * 128]
                blhs = [bYT[tt]]
            P.dma(XT[b][:, :], X[tt * 128:(tt + 1) * 128, :], reads=[bX[tt]], writes=[bXT[b]])
            for dh in range(2):
                p = pi % 2; pi += 1
                for k in range(8):
                    P.op("pe", lambda t, k=k, p=p, dh=dh, lhs=lhs: t.matmul(PO[p][:, :], lhs(k), WOb[:, k, dh * 512:(dh + 1) * 512], start=(k == 0), stop=(k == 7)),
                         reads=blhs + [bWO[k]], writes=[bPO[p]])
                P.op("dve", lambda v, p=p, r=r, dh=dh: v.tensor_tensor(out=TMP[p][:, :], in0=PO[p][:, :], in1=G1[:, r, dh * 512:(dh + 1) * 512], op=ALU.mult),
                     reads=[bPO[p], bG1], writes=[bTMP[p]])
                P.op("dve", lambda g, p=p, b=b, dh=dh: g.tensor_tensor(out=XT[b][:, dh * 512:(dh + 1) * 512], in0=TMP[p][:, :], in1=XT[b][:, dh * 512:(dh + 1) * 512], op=ALU.add),
                     reads=[bTMP[p], bXT[b]], writes=[bXT[b]])
            P.dma(X[tt * 128:(tt + 1) * 128, :], XT[b][:, :], reads=[bXT[b]], writes=[bX[tt]])
        P.flush()


def phase_route(P, K, C, HT, bHT, HTM, bHTM, RW, XG, GV, POSMT_out, dbg=None):
    with contextlib.ExitStack() as st:
        RWf = P.sb(st, "r_RWf", [128, 8, NE], F32); bRWf = Buf()
        RWb = P.sb(st, "r_RWb", [128, 8, NE], BF16); bRWb = Buf()
        AFF = P.sb(st, "r_AFF", [128, NT, NE], F32); bAFF = [Buf() for _ in range(NT)]
        AHI = P.sb(st, "r_AHI", [128, NT, NE], BF16)
        ALO = P.sb(st, "r_ALO", [128, NT, NE], BF16)
        AT32 = P.sb(st, "r_AT32", [128, NT, NE], F32)
        SM = P.sb(st, "r_SM", [128, NT, 4], F32); bSM = [Buf() for _ in range(NT)]
        EX = P.sb(st, "r_EX", [128, NT, NE], F32)
        AFFT = P.sb(st, "r_AFFT", [NE, T], F32); bAFFT = Buf()
        W = P.sb(st, "r_W", [NE, T], F32); bW = [Buf(), Buf()]
        M8 = P.sb(st, "r_M8", [NE, 8 * 36], F32); bM8 = [Buf(), Buf()]
        CA = P.sb(st, "r_CA", [NE, T], F32); bCA = [Buf(), Buf()]
        CB = P.sb(st, "r_CB", [NE, T], F32); bCB = [Buf(), Buf()]
        MASK = P.sb(st, "r_MASK", [NE, T], F32); bMASK = [Buf(), Buf()]
        POSM = P.sb(st, "r_POSM", [128, NT, NE], F32); bPOSM = [Buf() for _ in range(NT)]
        IOTA = P.sb(st, "r_IOTA", [128, NSLOT], F32); bIOTA = Buf()
        PSEL = [P.sb(st, f"r_PSEL{i}", [128, NT, 256], BF16) for i in range(2)]; bPSEL = [[Buf() for _ in range(NT)] for _ in range(2)]
        XGT = [P.sb(st, f"r_XGT{i}", [128, 8, NSLOT], BF16) for i in range(2)]; bXGT = [Buf() for _ in range(2)]
        GVR = [P.sb(st, f"r_GVR{i}", [1, NSLOT], F32) for i in range(2)]; bGVR = [Buf() for _ in range(2)]
        PL = [P.ps(st, f"r_PL{i}") for i in range(2)]; bPL = [Buf(excl=True) for _ in range(2)]
        PG = [P.ps(st, f"r_PG{i}") for i in range(3)]; bPG = [Buf(excl=True) for _ in range(3)]
        PV = [P.ps(st, f"r_PV{i}") for i in range(2)]; bPV = [Buf(excl=True) for _ in range(2)]
        cin = Buf(); cout = Buf()
        P.dma(RWf[:, :, :], RW.rearrange("(c p) e -> p c e", p=128), reads=[cin], writes=[bRWf])
        P.op("dve", lambda v: v.tensor_copy(out=RWb[:, :, :], in_=RWf[:, :, :]), reads=[bRWf], writes=[bRWb])
        P.dma(IOTA[:, :], C["iota"][0:NSLOT].partition_broadcast(128), reads=[cin], writes=[bIOTA])
        for tt in range(NT):
            p = tt % 2
            for k in range(8):
                P.op("pe", lambda t, k=k, tt=tt, p=p: t.matmul(PL[p][:, 0:NE], HT[:, k, tt * 128:(tt + 1) * 128], RWb[:, k, :], start=(k == 0), stop=(k == 7)),
                     reads=[bHT[tt], bRWb], writes=[bPL[p]])
            P.op("dve", lambda v, tt=tt, p=p: v.reduce_max(out=SM[:, tt, 0:1], in_=PL[p][:, 0:NE], axis=AX.X), reads=[bPL[p]], writes=[bSM[tt]])
            P.op("dve", lambda v, tt=tt: v.tensor_scalar(out=SM[:, tt, 1:2], in0=SM[:, tt, 0:1], scalar1=-1.0, scalar2=None, op0=ALU.mult), reads=[bSM[tt]], writes=[bSM[tt]])
            P.op("act", lambda a, tt=tt, p=p: a.activation(out=EX[:, tt, :], in_=PL[p][:, 0:NE], func=AF.Exp, bias=SM[:, tt, 1:2], accum_out=SM[:, tt, 2:3]),
                 reads=[bPL[p], bSM[tt]], writes=[bAFF[tt], bSM[tt]])
            P.op("dve", lambda v, tt=tt: v.reciprocal(out=SM[:, tt, 3:4], in_=SM[:, tt, 2:3]), reads=[bSM[tt]], writes=[bSM[tt]])
            P.op("dve", lambda v, tt=tt: v.tensor_scalar(out=AFF[:, tt, :], in0=EX[:, tt, :], scalar1=SM[:, tt, 3:4], scalar2=None, op0=ALU.mult), reads=[bSM[tt], bAFF[tt]], writes=[bAFF[tt]])
            P.op("dve", lambda v, tt=tt: v.tensor_copy(out=AHI[:, tt, :], in_=AFF[:, tt, :]), reads=[bAFF[tt]], writes=[bAFF[tt]])
            P.op("dve", lambda v, tt=tt: v.tensor_copy(out=AT32[:, tt, :], in_=AHI[:, tt, :]), reads=[bAFF[tt]], writes=[bAFF[tt]])
            P.op("dve", lambda v, tt=tt: v.tensor_tensor(out=ALO[:, tt, :], in0=AFF[:, tt, :], in1=AT32[:, tt, :], op=ALU.subtract), reads=[bAFF[tt]], writes=[bAFF[tt]])
            q = tt % 3
            P.op("pe", lambda t, tt=tt, q=q: t.transpose(PG[q][0:NE, 0:128], AFF[:, tt, :], K["idf"][:, :]), reads=[bAFF[tt], K["b_idf"]], writes=[bPG[q]])
            P.op("act", lambda a, tt=tt, q=q: a.copy(out=AFFT[:, tt * 128:(tt + 1) * 128], in_=PG[q][0:NE, 0:128]), reads=[bPG[q]], writes=[bAFFT])
        segs = [(0, 256, 32, 0.0), (256, T, 256, 32.0)]
        for si, (a0, a1, kk, base) in enumerate(segs):
            eng = "dve"
            P.op(eng, lambda v, a0=a0, a1=a1: v.tensor_copy(out=W[:, a0:a1], in_=AFFT[:, a0:a1]), reads=[bAFFT], writes=[bW[si]])
            nr = kk // 8
            for rnd in range(nr):
                mo = (si * 32 + rnd) * 8 if si == 0 else (4 + rnd) * 8
                P.op("dve", lambda v, a0=a0, a1=a1, mo=mo: v.max(out=M8[:, mo:mo + 8], in_=W[:, a0:a1]), reads=[bW[si]], writes=[bM8[si]])
                if rnd < nr - 1:
                    P.op("dve", lambda v, a0=a0, a1=a1, mo=mo: v.match_replace(out=W[:, a0:a1], in_to_replace=M8[:, mo:mo + 8], in_values=W[:, a0:a1], imm_value=-1.0),
                         reads=[bW[si], bM8[si]], writes=[bW[si]])
            thr = M8[:, mo + 7:mo + 8]
            P.op("dve", lambda v, a0=a0, a1=a1, thr=thr: v.tensor_scalar(out=MASK[:, a0:a1], in0=AFFT[:, a0:a1], scalar1=thr, scalar2=None, op0=ALU.is_ge),
                 reads=[bAFFT, bM8[si]], writes=[bMASK[si]])
            n = a1 - a0
            src, bsrc, dst, bdst = MASK, bMASK, CA, bCA
            sh = 1
            first = True
            while sh < n:
                P.op("dve", lambda v, src=src, dst=dst, sh=sh, a0=a0, a1=a1: v.tensor_tensor(out=dst[:, a0 + sh:a1], in0=src[:, a0 + sh:a1], in1=src[:, a0:a1 - sh], op=ALU.add),
                     reads=[bsrc[si]], writes=[bdst[si]])
                P.op("pool", lambda g, src=src, dst=dst, sh=sh, a0=a0: g.tensor_copy(out=dst[:, a0:a0 + sh], in_=src[:, a0:a0 + sh]),
                     reads=[bsrc[si]], writes=[bdst[si]])
                if first:
                    src, bsrc, dst, bdst = CA, bCA, CB, bCB
                    first = False
                else:
                    src, bsrc, dst, bdst = dst, bdst, src, bsrc
                sh *= 2
            incl, bincl = src, bsrc
            other, bother = (CB, bCB) if incl is CA else (CA, bCA)
            P.op("dve", lambda v, incl=incl, other=other, a0=a0, a1=a1, base=base: v.scalar_tensor_tensor(out=other[:, a0:a1], in0=incl[:, a0:a1], scalar=base, in1=MASK[:, a0:a1], op0=ALU.add, op1=ALU.mult),
                 reads=[bincl[si], bMASK[si]], writes=[bother[si]])
            P.op("dve", lambda v, other=other, a0=a0, a1=a1: v.tensor_scalar(out=W[:, a0:a1], in0=other[:, a0:a1], scalar1=-1.0, scalar2=None, op0=ALU.add),
                 reads=[bother[si]], writes=[bW[si]])
        P.dma(POSMT_out[:, :], W[:, :], reads=[bW[0], bW[1]], writes=[cout])
        if dbg is not None:
            P.dma(dbg["afft"][:, :], AFFT[:, :], reads=[bAFFT], writes=[cout])
        for tt in range(NT):
            q = tt % 3
            si = 0 if tt < 2 else 1
            P.op("pe", lambda t, tt=tt, q=q: t.transpose(PG[q][:, 0:NE], W[:, tt * 128:(tt + 1) * 128], K["idf"][0:NE, 0:NE]), reads=[bW[si], K["b_idf"]], writes=[bPG[q]])
            P.op("act", lambda a, tt=tt, q=q: a.copy(out=POSM[:, tt, :], in_=PG[q][:, 0:NE]), reads=[bPG[q]], writes=[bPOSM[tt]])
        for e in range(NE):
            b = e % 2
            for tt in range(NT):
                c0, nc_ = (0, 32) if tt < 2 else (32, 256)
                eng = "dve"
                P.op(eng, lambda v, tt=tt, e=e, b=b, c0=c0, nc_=nc_: v.tensor_scalar(out=PSEL[b][:, tt, 0:nc_], in0=IOTA[:, c0:c0 + nc_], scalar1=POSM[:, tt, e:e + 1], scalar2=None, op0=ALU.is_equal),
                     reads=[bIOTA, bPOSM[tt]], writes=[bPSEL[b][tt]])
            for dc in range(8):
                p = dc % 3
                for tt in range(NT):
                    c0, nc_ = (0, 32) if tt < 2 else (32, 256)
                    st_ = tt in (0, 2); sp_ = tt in (1, NT - 1)
                    P.op("pe", lambda t, tt=tt, dc=dc, p=p, b=b, c0=c0, nc_=nc_, st_=st_, sp_=sp_: t.matmul(PG[p][:, c0:c0 + nc_], HTM[:, tt, dc * 128:(dc + 1) * 128], PSEL[b][:, tt, 0:nc_], start=st_, stop=sp_),
                         reads=[bHTM[tt], bPSEL[b][tt]], writes=[bPG[p]])
                if dc % 2 == 0:
                    P.op("act", lambda a, dc=dc, p=p, b=b: a.copy(out=XGT[b][:, dc, :], in_=PG[p][:, 0:NSLOT]), reads=[bPG[p]], writes=[bXGT[b]])
                else:
                    P.op("dve", lambda v, dc=dc, p=p, b=b: v.tensor_copy(out=XGT[b][:, dc, :], in_=PG[p][:, 0:NSLOT]), reads=[bPG[p]], writes=[bXGT[b]])
            P.dma(XG[e].rearrange("(c p) s -> p c s", p=128), XGT[b][:, :, :], reads=[bXGT[b]], writes=[cout])
            for tt in range(NT):
                c0, nc_ = (0, 32) if tt < 2 else (32, 256)
                for hl, A_ in enumerate((AHI, ALO)):
                    st_ = (tt in (0, 2)) and hl == 0; sp_ = (tt in (1, NT - 1)) and hl == 1
                    P.op("pe", lambda t, tt=tt, e=e, b=b, c0=c0, nc_=nc_, st_=st_, sp_=sp_, A_=A_: t.matmul(PV[b][0:1, c0:c0 + nc_], A_[:, tt, e:e + 1], PSEL[b][:, tt, 0:nc_], start=st_, stop=sp_),
                         reads=[bAFF[tt], bPSEL[b][tt]], writes=[bPV[b]])
            P.op("act", lambda a, b=b: a.copy(out=GVR[b][:, :], in_=PV[b][0:1, 0:NSLOT]), reads=[bPV[b]], writes=[bGVR[b]])
            P.dma(GV[e:e + 1, :], GVR[b][:, :], reads=[bGVR[b]], writes=[cout])
        P.flush()


def phase_pro(P, K, C, Y, POSMT, MODS_prev, X_in, X, bX):
    with contextlib.ExitStack() as st:
        YG = P.sb(st, "p_YG", [128, NE * 2, D], BF16); bYG = [Buf() for _ in range(NE)]
        YGC = P.sb(st, "p_YGC", [128, 4, D], BF16); bYGC = Buf()
        PM = P.sb(st, "p_PM", [NE, T], F32); bPM = Buf()
        SEL = P.sb(st, "p_SEL", [NE, NE, 128], F32); bSEL = Buf()
        SELQ = P.sb(st, "p_SELQ", [NE, 4, 128], F32)
        SID = P.sb(st, "p_SID", [128, 4], F32); bSID = Buf()
        G2 = P.sb(st, "p_G2", [128, 2, D], F32); bG2 = Buf()
        PTt = [P.sb(st, f"p_PT{i}", [128, 384], BF16) for i in range(3)]; bPTt = [Buf() for _ in range(3)]
        XT = [P.sb(st, f"p_XT{i}", [128, D], F32) for i in range(2)]; bXT = [Buf() for _ in range(2)]
        TMP = [P.sb(st, f"p_TMP{i}", [128, 512], F32) for i in range(2)]; bTMP = [Buf() for _ in range(2)]
        ACC = [P.ps(st, f"p_ACC{i}") for i in range(6)]; bACC = [Buf(excl=True) for _ in range(6)]
        PB = [P.ps(st, f"p_PB{i}") for i in range(2)]; bPB = [Buf(excl=True) for _ in range(2)]
        cin = Buf()
        for e in range(NE):
            P.dma(YG[:, e * 2:e * 2 + 2, :], Y[e, 32:288, :].rearrange("(k p) d -> p k d", p=128), reads=[cin], writes=[bYG[e]])
            P.dma(YGC[(e % 4) * 32:(e % 4) * 32 + 32, e // 4, :], Y[e, 0:32, :], reads=[cin], writes=[bYGC])
        P.dma(PM[:, :], POSMT[:, :], reads=[cin], writes=[bPM])
        P.dma(SEL[:, :, :], C["sel16"][:, :, :], reads=[cin], writes=[bSEL])
        P.dma(SELQ[:, :, :], C["selq"][:, :, :], reads=[cin], writes=[bSEL])
        P.dma(SID[:, :], C["slotid"][:, :], reads=[cin], writes=[bSID])
        for r in range(2):
            P.dma(G2[:, r, :], MODS_prev[r, 5 * D:6 * D].partition_broadcast(128), reads=[cin], writes=[bG2])
        state = {"pti": 0, "xi": 0, "ti": 0}

        def finish(tiles_):
            for j, tt in enumerate(tiles_):
                r = 1 if tt < 2 else 0
                b = state["xi"] % 2; state["xi"] += 1
                P.dma(XT[b][:, :], X_in[tt * 128:(tt + 1) * 128, :], reads=[cin], writes=[bXT[b]])
                for dh in range(2):
                    a = j * 2 + dh
                    p = state["ti"] % 2; state["ti"] += 1
                    P.op("dve", lambda v, a=a, p=p, r=r, dh=dh: v.tensor_tensor(out=TMP[p][:, :], in0=ACC[a][:, :], in1=G2[:, r, dh * 512:(dh + 1) * 512], op=ALU.mult),
                         reads=[bACC[a], bG2], writes=[bTMP[p]])
                    P.op("dve", lambda g_, p=p, b=b, dh=dh: g_.tensor_tensor(out=XT[b][:, dh * 512:(dh + 1) * 512], in0=TMP[p][:, :], in1=XT[b][:, dh * 512:(dh + 1) * 512], op=ALU.add),
                         reads=[bTMP[p], bXT[b]], writes=[bXT[b]])
                P.dma(X[tt * 128:(tt + 1) * 128, :], XT[b][:, :], reads=[bXT[b]], writes=[bX[tt]])

        ntok = 256
        for g4 in range(4):
            pb = g4 % 2
            P.op("pe", lambda t, g4=g4, pb=pb: t.matmul(PB[pb][:, 0:ntok], SELQ[:, g4, :], PM[:, 0:ntok], start=True, stop=True), reads=[bSEL, bPM], writes=[bPB[pb]])
            pt = state["pti"] % 3; state["pti"] += 1
            P.op("dve", lambda v, pt=pt, pb=pb: v.tensor_scalar(out=PTt[pt][:, 0:ntok], in0=PB[pb][:, 0:ntok], scalar1=SID[:, 3:4], scalar2=None, op0=ALU.is_equal), reads=[bPB[pb], bSID], writes=[bPTt[pt]])
            for j in range(2):
                for dh in range(2):
                    a = j * 2 + dh
                    P.op("pe", lambda t, a=a, pt=pt, j=j, dh=dh, g4=g4: t.matmul(ACC[a][:, :], PTt[pt][:, j * 128:(j + 1) * 128], YGC[:, g4, dh * 512:(dh + 1) * 512], start=(g4 == 0), stop=(g4 == 3)),
                         reads=[bPTt[pt], bYGC], writes=[bACC[a]])
        finish([0, 1])
        groups = [list(range(2 + 3 * g, min(2 + 3 * g + 3, NT))) for g in range(6)]
        for tiles_ in groups:
            t0 = tiles_[0] * 128; ntk = len(tiles_) * 128

            def emit_sel(e, t0=t0, ntk=ntk):
                pb = e % 2
                P.op("pe", lambda t, e=e, pb=pb, t0=t0, ntk=ntk: t.matmul(PB[pb][:, 0:ntk], SEL[:, e, :], PM[:, t0:t0 + ntk], start=True, stop=True), reads=[bSEL, bPM], writes=[bPB[pb]])
            emit_sel(0)
            for e in range(NE):
                pb = e % 2
                if e + 1 < NE:
                    emit_sel(e + 1)
                for k in range(2):
                    pt = state["pti"] % 3; state["pti"] += 1
                    P.op("dve", lambda v, pt=pt, pb=pb, k=k, ntk=ntk: v.tensor_scalar(out=PTt[pt][:, 0:ntk], in0=PB[pb][:, 0:ntk], scalar1=SID[:, k:k + 1], scalar2=None, op0=ALU.is_equal),
                         reads=[bPB[pb], bSID], writes=[bPTt[pt]])
                    for j in range(len(tiles_)):
                        for dh in range(2):
                            a = j * 2 + dh
                            first = (e == 0 and k == 0); last = (e == NE - 1 and k == 1)
                            P.op("pe", lambda t, a=a, pt=pt, j=j, dh=dh, e=e, k=k, first=first, last=last: t.matmul(ACC[a][:, :], PTt[pt][:, j * 128:(j + 1) * 128], YG[:, e * 2 + k, dh * 512:(dh + 1) * 512], start=first, stop=last),
                                 reads=[bPTt[pt], bYG[e]], writes=[bACC[a]])
            finish(tiles_)
        P.flush()


NS = 2304
FF = 2816
D = 1024

def build_B(P, xgT, gv, wg, wu, wd, y, nexp=2):
    nc = P.nc
    with contextlib.ExitStack() as st:
        XT = P.sb(st, "XT", [128, 8, NS], BF16); bXT = Buf("XT")
        GV = P.sb(st, "GV", [128, 18], F32); bGV = Buf("GV")
        WD = P.sb(st, "WD", [128, 22, D], BF16); bWD = [Buf(f"WD{f}") for f in range(22)]
        ACTT = P.sb(st, "ACTT", [128, 22, NS // 2], BF16); bACTT = [Buf(f"ACTT{f}") for f in range(22)]
        stg = [P.sb(st, f"stg{i}", [128, 1024], F32) for i in range(4)]; bstg = [Buf(f"stg{i}") for i in range(4)]
        WG = [P.sb(st, f"WG{i}", [128, 8, 128], BF16) for i in range(2)]; bWG = [Buf() for _ in range(2)]
        WU = [P.sb(st, f"WU{i}", [128, 8, 128], BF16) for i in range(2)]; bWU = [Buf() for _ in range(2)]
        SA = [P.sb(st, f"SA{i}", [128, 384], F32) for i in range(2)]; bSA = [Buf() for _ in range(2)]
        YT = [P.sb(st, f"YT{i}", [128, D], BF16) for i in range(2)]; bYT = [Buf() for _ in range(2)]
        PA = [P.ps(st, f"PA{i}") for i in range(2)]; bPA = [Buf(excl=True) for _ in range(2)]
        PU = [P.ps(st, f"PU{i}") for i in range(2)]; bPU = [Buf(excl=True) for _ in range(2)]
        PY = [P.ps(st, f"PY{i}") for i in range(2)]; bPY = [Buf(excl=True) for _ in range(2)]
        bin_ = Buf("in"); bout = Buf("out")
        sgi = 0; ci = 0; pi = 0; yi = 0; pyi = 0
        for e in range(nexp):
            P.dma(XT[:, :, :], xgT[e].rearrange("(c p) s -> p c s", p=128), reads=[bin_], writes=[bXT])
            P.dma(GV[:, :], gv[e], reads=[bin_], writes=[bGV])
            for f in range(22):
                s = sgi % 4; sgi += 1
                P.dma(stg[s][:, :], wd[e, f * 128:(f + 1) * 128, :], reads=[bin_], writes=[bstg[s]])
                eng = "pool" if ci % 2 == 0 else "act"; ci += 1
                if eng == "pool":
                    P.op("pool", lambda g, o=WD[:, f, :], i=stg[s][:, :]: g.tensor_copy(out=o, in_=i), reads=[bstg[s]], writes=[bWD[f]])
                else:
                    P.op("act", lambda g, o=WD[:, f, :], i=stg[s][:, :]: g.copy(out=o, in_=i), reads=[bstg[s]], writes=[bWD[f]])
            for sh in range(2):
                for f in range(22):
                    w = f % 2
                    for (src, dst, bdst) in ((wg, WG[w], bWG[w]), (wu, WU[w], bWU[w])):
                        s = sgi % 4; sgi += 1
                        sv = stg[s][:, :].rearrange("p (c f) -> p c f", c=8)
                        P.dma(sv, src[e].rearrange("(c p) f -> p c f", p=128)[:, :, f * 128:(f + 1) * 128], reads=[bin_], writes=[bstg[s]])
                        eng = "pool" if ci % 2 == 0 else "act"; ci += 1
                        if eng == "pool":
                            P.op("pool", lambda g, o=dst[:, :, :], i=sv: g.tensor_copy(out=o, in_=i), reads=[bstg[s]], writes=[bdst])
                        else:
                            P.op("act", lambda g, o=dst[:, :, :], i=sv: g.copy(out=o, in_=i), reads=[bstg[s]], writes=[bdst])
                    for nb in range(3):
                        s0 = sh * (NS // 2) + nb * 384
                        p = pi % 2; pi += 1
                        for k in range(8):
                            P.op("pe", lambda t, o=PA[p][:, 0:384], l=WG[w][:, k, :], r=XT[:, k, s0:s0 + 384], k=k: t.matmul(o, l, r, start=(k == 0), stop=(k == 7)),
                                 reads=[bWG[w], bXT], writes=[bPA[p]])
                        for k in range(8):
                            P.op("pe", lambda t, o=PU[p][:, 0:384], l=WU[w][:, k, :], r=XT[:, k, s0:s0 + 384], k=k: t.matmul(o, l, r, start=(k == 0), stop=(k == 7)),
                                 reads=[bWU[w], bXT], writes=[bPU[p]])
                        P.op("act", lambda a, o=SA[p][:, :], i=PA[p][:, 0:384]: a.activation(out=o, in_=i, func=AF.Silu), reads=[bPA[p]], writes=[bSA[p]])
                        P.op("dve", lambda v, o=ACTT[:, f, nb * 384:(nb + 1) * 384], a=SA[p][:, :], b=PU[p][:, 0:384]: v.tensor_tensor(out=o, in0=a, in1=b, op=ALU.mult),
                             reads=[bSA[p], bPU[p]], writes=[bACTT[f]])
                for sc in range(9):
                    yb = yi % 2; yi += 1
                    chunk = sh * 9 + sc
                    for dh in range(2):
                        p = pyi % 2; pyi += 1
                        for f in range(22):
                            P.op("pe", lambda t, o=PY[p][:, :], l=ACTT[:, f, sc * 128:(sc + 1) * 128], r=WD[:, f, dh * 512:(dh + 1) * 512], f=f: t.matmul(o, l, r, start=(f == 0), stop=(f == 21)),
                                 reads=[bACTT[f], bWD[f]], writes=[bPY[p]])
                        if dh == 0:
                            P.op("dve", lambda v, o=YT[yb][:, 0:512], i=PY[p][:, :], s=GV[:, chunk:chunk + 1]: v.tensor_scalar(out=o, in0=i, scalar1=s, scalar2=None, op0=ALU.mult),
                                 reads=[bPY[p], bGV], writes=[bYT[yb]])
                        else:
                            P.op("act", lambda a, o=YT[yb][:, 512:1024], i=PY[p][:, :], s=GV[:, chunk:chunk + 1]: a.activation(out=o, in_=i, func=AF.Copy, scale=s),
                                 reads=[bPY[p], bGV], writes=[bYT[yb]])
                    P.dma(y[e, chunk * 128:(chunk + 1) * 128, :], YT[yb][:, :], reads=[bYT[yb]], writes=[bout])
        P.flush(final=True)


NEG = -30000.0
GW = 64

def na_tables(rpb):
    nh = rpb.shape[0]
    qc = np.arange(64); kc = np.arange(64)
    cs = np.clip(qc - 8, 0, 48)
    ok = (kc[:, None] >= cs[None, :]) & (kc[:, None] < cs[None, :] + 16)
    idx = np.clip(kc[:, None] - qc[None, :], -15, 15) + 15
    Tb = np.where(ok[None, None], rpb[:, :, idx], np.float32(NEG)).astype(np.float32)
    mask = np.full((nh, 64, 64), NEG, np.float32)
    tte = np.zeros((128, nh, 14, 64), np.float32)
    for d in range(14):
        tte[0:64, :, d, :] = Tb[:, d].transpose(1, 0, 2)
        tte[64:128, :, d, :] = Tb[:, d + 1].transpose(1, 0, 2)
    tto = np.zeros((128, nh, 5, 64), np.float32)
    pairs = [(None, 3), (4, 5), (6, 7), (8, 9), (10, None)]
    for j, (a, b) in enumerate(pairs):
        tto[0:64, :, j, :] = (mask if a is None else Tb[:, a]).transpose(1, 0, 2)
        tto[64:128, :, j, :] = (mask if b is None else Tb[:, b]).transpose(1, 0, 2)
    return tte, tto


def mixer_na(P, K, run_norm, WQKV, TTE, TTO, YATT, bY, last=False):
    with contextlib.ExitStack() as st:
        QT = P.sb(st, "a_QT", [128, 8, T], BF16); bQT = [Buf() for _ in range(8)]
        KT = P.sb(st, "a_KT", [128, 8, T], BF16); bKT = [Buf() for _ in range(8)]
        VA = P.sb(st, "a_VA", [128, NT, 16, 65], BF16); bVA = [Buf() for _ in range(NT)]
        cin = Buf()
        with contextlib.ExitStack() as st2:
            HT = P.sb(st2, "HT", [128, 8, T], BF16); bHT = [Buf() for _ in range(NT)]
            run_norm(HT, bHT)
            WB = P.sb(st2, "a_WB", [128, 8, 1024], BF16); bWB = [Buf() for _ in range(8)]
            STG = [P.sb(st2, f"a_stg{i}", [128, 1024], F32) for i in range(2)]; bSTG = [Buf() for _ in range(2)]
            PP = [P.ps(st2, f"a_PP{i}") for i in range(4)]; bPP = [Buf(excl=True) for _ in range(4)]
            P.op("dve", lambda v: v.memset(VA[:, :, :, 64:65], 1.0), writes=bVA)
            pi = 0
            for which in range(3):
                for k in range(8):
                    s = k % 2
                    P.dma(STG[s][:, :], WQKV[k * 128:(k + 1) * 128, which * 1024:(which + 1) * 1024], reads=[cin], writes=[bSTG[s]])
                    if k % 2 == 0:
                        P.op("dve", lambda g, k=k, s=s: g.tensor_copy(out=WB[:, k, :], in_=STG[s][:, :]), reads=[bSTG[s]], writes=[bWB[k]])
                    else:
                        P.op("act", lambda a, k=k, s=s: a.copy(out=WB[:, k, :], in_=STG[s][:, :]), reads=[bSTG[s]], writes=[bWB[k]])
                if which < 2:
                    DST, bD = (QT, bQT) if which == 0 else (KT, bKT)
                    for c in range(8):
                        for tb in range(6):
                            p = pi % 4; pi += 1
                            for k in range(8):
                                P.op("pe", lambda t, k=k, p=p, c=c, tb=tb: t.matmul(PP[p][:, 0:384], WB[:, k, c * 128:(c + 1) * 128], HT[:, k, tb * 384:(tb + 1) * 384], start=(k == 0), stop=(k == 7)),
                                     reads=[bWB[k]] + bHT[tb * 3:tb * 3 + 3], writes=[bPP[p]])
                            if which == 0:
                                P.op("act", lambda a, p=p, c=c, tb=tb: a.activation(out=QT[:, c, tb * 384:(tb + 1) * 384], in_=PP[p][:, 0:384], func=AF.Copy, scale=0.125), reads=[bPP[p]], writes=[bQT[c]])
                            else:
                                P.op("dve", lambda v, p=p, c=c, tb=tb: v.tensor_copy(out=KT[:, c, tb * 384:(tb + 1) * 384], in_=PP[p][:, 0:384]), reads=[bPP[p]], writes=[bKT[c]])
                else:
                    for tt in range(NT):
                        for dh in range(2):
                            p = pi % 4; pi += 1
                            for k in range(8):
                                P.op("pe", lambda t, k=k, p=p, tt=tt, dh=dh: t.matmul(PP[p][:, :], HT[:, k, tt * 128:(tt + 1) * 128], WB[:, k, dh * 512:(dh + 1) * 512], start=(k == 0), stop=(k == 7)),
                                     reads=[bWB[k], bHT[tt]], writes=[bPP[p]])
                            if dh == 0:
                                P.op("act", lambda a, p=p, tt=tt, dh=dh: a.copy(out=VA[:, tt, dh * 8:(dh + 1) * 8, 0:64], in_=PP[p][:, :].rearrange("p (h d) -> p h d", h=8)), reads=[bPP[p]], writes=[bVA[tt]])
                            else:
                                P.op("dve", lambda v, p=p, tt=tt, dh=dh: v.tensor_copy(out=VA[:, tt, dh * 8:(dh + 1) * 8, 0:64], in_=PP[p][:, :].rearrange("p (h d) -> p h d", h=8)), reads=[bPP[p]], writes=[bVA[tt]])
            P.flush()
        with contextlib.ExitStack() as st2:
            TTEb = P.sb(st2, "a_TTE", [128, 16, 14, 64], BF16); bTTE = Buf()
            TTOb = P.sb(st2, "a_TTO", [128, 16, 5, 64], BF16); bTTO = Buf()
            STG = [P.sb(st2, f"a_tstg{i}", [128, 14 * 64], F32) for i in range(2)]; bSTG = [Buf() for _ in range(2)]
            for h in range(16):
                s = h % 2
                P.dma(STG[s][:, :], TTE[:, h, :, :].rearrange("p a b -> p (a b)"), reads=[cin], writes=[bSTG[s]])
                P.op("dve", lambda g, h=h, s=s: g.tensor_copy(out=TTEb[:, h, :, :].rearrange("p a b -> p (a b)"), in_=STG[s][:, :]), reads=[bSTG[s]], writes=[bTTE])
            for h in range(16):
                s = h % 2
                P.dma(STG[s][:, 0:320], TTO[:, h, :, :].rearrange("p a b -> p (a b)"), reads=[cin], writes=[bSTG[s]])
                P.op("dve", lambda g, h=h, s=s: g.tensor_copy(out=TTOb[:, h, :, :].rearrange("p a b -> p (a b)"), in_=STG[s][:, 0:320]), reads=[bSTG[s]], writes=[bTTO])
            PS = [P.ps(st2, f"a_PS{i}") for i in range(4)]; bPS = [Buf(excl=True) for _ in range(4)]
            PO = [P.ps(st2, f"a_PO{i}") for i in range(4)]; bPO = [Buf(excl=True) for _ in range(4)]
            PT = [P.sb(st2, f"a_PT{i}", [128, 512], BF16) for i in range(4)]; bPT = [Buf() for _ in range(4)]
            REC = [P.sb(st2, f"a_REC{i}", [128, 2], F32) for i in range(4)]; bREC = [Buf() for _ in range(4)]
            YR = [P.sb(st2, f"a_YR{i}", [128, 2, D], BF16) for i in range(2)]; bYR = [Buf() for _ in range(2)]
            si = 0
            if not last:
                yb = 0
                for h in range(16):
                    c = h // 2; hp = (h % 2) * 64
                    s = si % 3; si += 1
                    for kc in range(2):
                        P.op("pe", lambda t, s=s, kc=kc, c=c, hp=hp: t.matmul(PS[s][:, kc * 256:(kc + 1) * 256], KT[hp:hp + 64, c, kc * 128:(kc + 1) * 128], QT[hp:hp + 64, c, 0:256], start=True, stop=True),
                             reads=[bKT[c], bQT[c]], writes=[bPS[s]])
                    P.op("act", lambda a, s=s: a.activation(out=PT[s][:, :], in_=PS[s][:, :], func=AF.Exp), reads=[bPS[s]], writes=[bPT[s]])
                    for qt in range(2):
                        for kc in range(2):
                            P.op("pe", lambda t, s=s, qt=qt, kc=kc, h=h: t.matmul(PO[s][:, qt * 65:(qt + 1) * 65], PT[s][:, kc * 256 + qt * 128:kc * 256 + (qt + 1) * 128], VA[:, kc, h, :], start=(kc == 0), stop=(kc == 1)),
                                 reads=[bPT[s], bVA[kc]], writes=[bPO[s]])
                    for qt in range(2):
                        P.op("dve", lambda v, s=s, qt=qt: v.reciprocal(out=REC[s][:, qt:qt + 1], in_=PO[s][:, qt * 65 + 64:qt * 65 + 65]), reads=[bPO[s]], writes=[bREC[s]])
                        P.op("act", lambda a, s=s, qt=qt, h=h: a.activation(out=YR[yb][:, qt, h * 64:(h + 1) * 64], in_=PO[s][:, qt * 65:qt * 65 + 64], func=AF.Copy, scale=REC[s][:, qt:qt + 1]),
                             reads=[bPO[s], bREC[s]], writes=[bYR[yb]])
                for qt in range(2):
                    P.dma(YATT[qt * 128:(qt + 1) * 128, :], YR[yb][:, qt, :], reads=[bYR[yb]], writes=[bY[qt]])
            LOOK = 3
            steps = []
            for r in range(32):
                rs = min(max(r - 4, 0), 24)
                if rs % 2 == 0:
                    m0 = rs // 2; nloc = 4; dr0 = rs - r + 7
                else:
                    m0 = (rs - 1) // 2; nloc = 5; dr0 = None
                tiles_ = [0, 1] + [2 + m0 + c for c in range(nloc)]
                for h in range(16):
                    steps.append((r, h, tiles_, nloc, dr0))
            def emit_S(idx):
                r, h, tiles_, nloc, dr0 = steps[idx]
                s = idx % 4; c = h // 2; hp = (h % 2) * 64; q0 = 256 + r * 64
                for ci, tile in enumerate(tiles_):
                    P.op("pe", lambda t, s=s, ci=ci, tile=tile, c=c, hp=hp, q0=q0: t.matmul(PS[s][:, ci * 64:(ci + 1) * 64], KT[hp:hp + 64, c, tile * 128:(tile + 1) * 128], QT[hp:hp + 64, c, q0:q0 + 64], start=True, stop=True),
                         reads=[bKT[c], bQT[c]], writes=[bPS[s]])
            for i0 in range(min(LOOK, len(steps))):
                emit_S(i0)
            for idx, (r, h, tiles_, nloc, dr0) in enumerate(steps):
                s = idx % 4
                yb = (r + 1) % 2
                nch = len(tiles_)
                q0 = 256 + r * 64
                if dr0 is not None:
                    tab = TTEb[:, h, dr0:dr0 + 7:2, :]; btab = bTTE
                else:
                    tab = TTOb[:, h, 0:5, :]; btab = bTTO
                P.op("dve", lambda v, s=s, nloc=nloc, tab=tab: v.tensor_tensor(out=PS[s][:, 128:128 + nloc * 64].rearrange("p (a b) -> p a b", b=64), in0=PS[s][:, 128:128 + nloc * 64].rearrange("p (a b) -> p a b", b=64), in1=tab, op=ALU.add),
                     reads=[bPS[s], btab], writes=[bPS[s]])
                P.op("act", lambda a, s=s, nch=nch: a.activation(out=PT[s][:, 0:nch * 64], in_=PS[s][:, 0:nch * 64], func=AF.Exp), reads=[bPS[s]], writes=[bPT[s]])
                if idx + LOOK < len(steps):
                    emit_S(idx + LOOK)
                for ci, tile in enumerate(tiles_):
                    P.op("pe", lambda t, s=s, ci=ci, tile=tile, h=h, nch=nch: t.matmul(PO[s][0:64, 0:65], PT[s][:, ci * 64:(ci + 1) * 64], VA[:, tile, h, :], start=(ci == 0), stop=(ci == nch - 1)),
                         reads=[bPT[s], bVA[tile]], writes=[bPO[s]])
                P.op("dve", lambda v, s=s: v.reciprocal(out=REC[s][0:64, 0:1], in_=PO[s][0:64, 64:65]), reads=[bPO[s]], writes=[bREC[s]])
                P.op("act", lambda a, s=s, h=h, yb=yb: a.activation(out=YR[yb][0:64, 0, h * 64:(h + 1) * 64], in_=PO[s][0:64, 0:64], func=AF.Copy, scale=REC[s][0:64, 0:1]),
                     reads=[bPO[s], bREC[s]], writes=[bYR[yb]])
                if h == 15:
                    P.dma(YATT[q0:q0 + 64, :], YR[yb][0:64, 0, :], reads=[bYR[yb]], writes=[bY[q0 // 128]])
            P.flush()


QR, KVR, RD = 384, 256, 32
SCALE = 96 ** -0.5

def mla_host(w_in, w_q_b, qg, kvg):
    wks = np.concatenate([w_in[:, 576:640], w_in[:, 656:672], w_in[:, 640:656]], 1).copy()
    wq = w_q_b.reshape(QR, 16, 96)
    wqs = np.concatenate([wq[:, :, 0:64], wq[:, :, 80:96], wq[:, :, 64:80]], 2).reshape(QR, 16 * 96).copy()
    gq = qg.reshape(3, 128).T.copy(); gkv = kvg.reshape(2, 128).T.copy()
    t = np.arange(2048)
    row = (t // 64).astype(np.float32); col = (t % 64).astype(np.float32)
    inv = (10000.0 ** (-np.arange(8, dtype=np.float32) / 8)).astype(np.float32)
    ang = np.concatenate([row[:, None] * inv, col[:, None] * inv], -1)
    cos = np.cos(ang).astype(np.float32); sin = np.sin(ang).astype(np.float32)
    cs = np.zeros((32, 2, T), np.float32)
    cs[:, 0, :256] = 1.0
    cs[0:16, 0, 256:] = cos.T; cs[16:32, 0, 256:] = cos.T
    cs[0:16, 1, 256:] = -sin.T; cs[16:32, 1, 256:] = sin.T
    return wks, wqs, gq, gkv, cs


STOP = 99
def mixer_mla(P, K, run_norm, WIN, WKS, GQ, GKV, WQB, WQS, WKVB, CS, YTM, bYTM):
    with contextlib.ExitStack() as st:
        CQN = P.sb(st, "m_CQN", [128, 3, T], BF16); bCQN = [Buf() for _ in range(6)]
        CKVN = P.sb(st, "m_CKVN", [128, 2, T], BF16); bCKVN = [Buf() for _ in range(6)]
        KRT = P.sb(st, "m_KRT", [128, T], BF16); bKRT = [Buf() for _ in range(6)]
        CSs = P.sb(st, "m_CS", [128, 2, T], F32); bCS = Buf()
        cin = Buf()
        P.dma(CSs[64:96, :, :], CS[:, :, :], reads=[cin], writes=[bCS])
        with contextlib.ExitStack() as st2:
            HT = P.sb(st2, "HT", [128, 8, T], BF16); bHT = [Buf() for _ in range(NT)]
            run_norm(HT, bHT)
            WINb = P.sb(st2, "m_WIN", [128, 8, 672], BF16); bWIN = [Buf() for _ in range(8)]
            WKSb = P.sb(st2, "m_WKS", [128, 8, 96], BF16); bWKS = Buf()
            STG = [P.sb(st2, f"m_stg{i}", [128, 672], F32) for i in range(2)]; bSTG = [Buf() for _ in range(2)]
            STK = P.sb(st2, "m_stk", [128, 8, 96], F32); bSTK = Buf()
            GQs = P.sb(st2, "m_GQ", [128, 3], F32); GKVs = P.sb(st2, "m_GKV", [128, 2], F32); bG = Buf()
            ONES = P.sb(st2, "m_ONES", [128, 128], BF16); bONES = Buf()
            ZF = [P.sb(st2, f"m_ZF{i}", [128, 5, 384], F32) for i in range(2)]; bZF = [Buf() for _ in range(2)]
            SQ = [P.sb(st2, f"m_SQ{i}", [128, 5, 384], BF16) for i in range(2)]; bSQ = [Buf() for _ in range(2)]
            RQ = [P.sb(st2, f"m_RQ{i}", [128, 2, 384], F32) for i in range(2)]; bRQ = [Buf() for _ in range(2)]
            T1 = [P.sb(st2, f"m_T1{i}", [128, 384], F32) for i in range(2)]; bT1 = [Buf() for _ in range(2)]
            T2 = [P.sb(st2, f"m_T2{i}", [128, 384], F32) for i in range(2)]; bT2 = [Buf() for _ in range(2)]
            PZ = [P.ps(st2, f"m_PZ{i}") for i in range(4)]; bPZ = [Buf(excl=True) for _ in range(4)]
            PSM = [P.ps(st2, f"m_PSM{i}") for i in range(2)]; bPSM = [Buf(excl=True) for _ in range(2)]
            PK = [P.ps(st2, f"m_PK{i}") for i in range(2)]; bPK = [Buf(excl=True) for _ in range(2)]
            for k in range(8):
                s = k % 2
                P.dma(STG[s][:, :], WIN[k * 128:(k + 1) * 128, :], reads=[cin], writes=[bSTG[s]])
                P.op("dve", lambda g, k=k, s=s: g.tensor_copy(out=WINb[:, k, :], in_=STG[s][:, :]), reads=[bSTG[s]], writes=[bWIN[k]])
            P.dma(STK[:, :, :], WKS.rearrange("(c p) f -> p c f", p=128), reads=[cin], writes=[bSTK])
            P.op("dve", lambda g: g.tensor_copy(out=WKSb[:, :, :], in_=STK[:, :, :]), reads=[bSTK], writes=[bWKS])
            P.dma(GQs[:, :], GQ[:, :], reads=[cin], writes=[bG])
            P.dma(GKVs[:, :], GKV[:, :], reads=[cin], writes=[bG])
            P.op("dve", lambda v: v.memset(ONES[:, :], 1.0), writes=[bONES])
            pz = 0
            for tb in range(6):
                b = tb % 2
                tsl = slice(tb * 384, (tb + 1) * 384)
                hts = bHT[tb * 3:tb * 3 + 3]
                for c in range(5):
                    p = pz % 4; pz += 1
                    for k in range(8):
                        P.op("pe", lambda t, k=k, p=p, c=c, tsl=tsl: t.matmul(PZ[p][:, 0:384], WINb[:, k, c * 128:(c + 1) * 128], HT[:, k, tsl], start=(k == 0), stop=(k == 7)),
                             reads=[bWIN[k]] + hts, writes=[bPZ[p]])
                    P.op("dve", lambda v, p=p, c=c, b=b: v.tensor_copy(out=ZF[b][:, c, :], in_=PZ[p][:, 0:384]), reads=[bPZ[p]], writes=[bZF[b]])
                    P.op("act", lambda a, p=p, c=c, b=b: a.activation(out=SQ[b][:, c, :], in_=PZ[p][:, 0:384], func=AF.Square), reads=[bPZ[p]], writes=[bSQ[b]])
                for which, cs_, n_ in ((0, (0, 1, 2), QR), (1, (3, 4), KVR)):
                    for i, c in enumerate(cs_):
                        P.op("pe", lambda t, which=which, c=c, b=b, i=i, cs_=cs_: t.matmul(PSM[which][:, 0:384], ONES[:, :], SQ[b][:, c, :], start=(i == 0), stop=(i == len(cs_) - 1)),
                             reads=[bONES, bSQ[b]], writes=[bPSM[which]])
                    P.op("act", lambda a, which=which, b=b, n_=n_: a.activation(out=RQ[b][:, which, :], in_=PSM[which][:, 0:384], func=AF.Sqrt, scale=1.0 / n_, bias=K["eps"][:, :]),
                         reads=[bPSM[which], K["b_eps"]], writes=[bRQ[b]])
                    P.op("dve", lambda v, which=which, b=b: v.reciprocal(out=RQ[b][:, which, :], in_=RQ[b][:, which, :]), reads=[bRQ[b]], writes=[bRQ[b]])
                for c in range(3):
                    P.op("dve", lambda v, c=c, b=b, tsl=tsl: v.scalar_tensor_tensor(out=CQN[:, c, tsl], in0=ZF[b][:, c, :], scalar=GQs[:, c:c + 1], in1=RQ[b][:, 0, :], op0=ALU.mult, op1=ALU.mult),
                         reads=[bZF[b], bG, bRQ[b]], writes=[bCQN[tb]])
                for c in range(2):
                    P.op("dve", lambda v, c=c, b=b, tsl=tsl: v.scalar_tensor_tensor(out=CKVN[:, c, tsl], in0=ZF[b][:, 3 + c, :], scalar=GKVs[:, c:c + 1], in1=RQ[b][:, 1, :], op0=ALU.mult, op1=ALU.mult),
                         reads=[bZF[b], bG, bRQ[b]], writes=[bCKVN[tb]])
                for k in range(8):
                    P.op("pe", lambda t, k=k, tsl=tsl: t.matmul(PK[0][0:96, 0:384], WINb[:, k, 576:672], HT[:, k, tsl], start=(k == 0), stop=(k == 7)), reads=[bWIN[k]] + hts, writes=[bPK[0]])
                for k in range(8):
                    P.op("pe", lambda t, k=k, tsl=tsl: t.matmul(PK[1][0:96, 0:384], WKSb[:, k, :], HT[:, k, tsl], start=(k == 0), stop=(k == 7)), reads=[bWKS] + hts, writes=[bPK[1]])
                P.op("dve", lambda v, b=b, tsl=tsl: v.tensor_tensor(out=T1[b][64:96, :], in0=PK[0][64:96, 0:384], in1=CSs[64:96, 0, tsl], op=ALU.mult), reads=[bPK[0], bCS], writes=[bT1[b]])
                P.op("dve", lambda v, b=b, tsl=tsl: v.tensor_tensor(out=T2[b][64:96, :], in0=PK[1][64:96, 0:384], in1=CSs[64:96, 1, tsl], op=ALU.mult), reads=[bPK[1], bCS], writes=[bT2[b]])
                P.op("dve", lambda g, b=b, tsl=tsl: g.tensor_tensor(out=KRT[64:96, tsl], in0=T1[b][64:96, :], in1=T2[b][64:96, :], op=ALU.add), reads=[bT1[b], bT2[b]], writes=[bKRT[tb]])
            P.flush()
        if STOP <= 1:
            return
        for hg in range(2):
            with contextlib.ExitStack() as st2:
                QT = P.sb(st2, "m_QT", [128, 8, T], BF16); bQT = [Buf() for _ in range(8)]
                KT = P.sb(st2, "m_KT", [128, 8, T], BF16); bKT = [Buf() for _ in range(8)]
                VA = P.sb(st2, "m_VA", [128, NT, 8, 65], BF16); bVA = [Buf() for _ in range(NT)]
                WQBb = P.sb(st2, "m_WQB", [128, 3, 768], BF16); WQSb = P.sb(st2, "m_WQS", [128, 3, 768], BF16); bWQ = Buf()
                WKVb = P.sb(st2, "m_WKV", [128, 2, 1024], BF16); bWKV = Buf()
                STG = [P.sb(st2, f"m_stg2{i}", [128, 1024], F32) for i in range(2)]; bSTG = [Buf() for _ in range(2)]
                T1 = [P.sb(st2, f"m_T1b{i}", [128, 384], F32) for i in range(2)]; bT1 = [Buf() for _ in range(2)]
                T2 = [P.sb(st2, f"m_T2b{i}", [128, 384], F32) for i in range(2)]; bT2 = [Buf() for _ in range(2)]
                PTs = [P.sb(st2, f"m_PT{i}", [128, 512], BF16) for i in range(4)]; bPT = [Buf() for _ in range(4)]
                REC = [P.sb(st2, f"m_REC{i}", [128, 4], F32) for i in range(2)]; bREC = [Buf() for _ in range(2)]
                PQ = [P.ps(st2, f"m_PQ{i}") for i in range(2)]; bPQ = [Buf(excl=True) for _ in range(2)]
                PQS = [P.ps(st2, f"m_PQS{i}") for i in range(2)]; bPQS = [Buf(excl=True) for _ in range(2)]
                PS = [P.ps(st2, f"m_PS{i}") for i in range(2)]; bPS = [Buf(excl=True) for _ in range(2)]
                PO = [P.ps(st2, f"m_PO{i}") for i in range(2)]; bPO = [Buf(excl=True) for _ in range(2)]
                si = 0
                for (SRC, DST) in ((WQB, WQBb), (WQS, WQSb)):
                    for k in range(3):
                        s = si % 2; si += 1
                        P.dma(STG[s][:, 0:768], SRC[k * 128:(k + 1) * 128, hg * 768:(hg + 1) * 768], reads=[cin], writes=[bSTG[s]])
                        P.op("dve", lambda g, k=k, s=s, DST=DST: g.tensor_copy(out=DST[:, k, :], in_=STG[s][:, 0:768]), reads=[bSTG[s]], writes=[bWQ])
                for k in range(2):
                    s = si % 2; si += 1
                    P.dma(STG[s][:, :], WKVB[k * 128:(k + 1) * 128, hg * 1024:(hg + 1) * 1024], reads=[cin], writes=[bSTG[s]])
                    P.op("dve", lambda g, k=k, s=s: g.tensor_copy(out=WKVb[:, k, :], in_=STG[s][:, :]), reads=[bSTG[s]], writes=[bWKV])
                P.op("dve", lambda v: v.memset(VA[:, :, :, 64:65], 1.0), writes=bVA)
                pq = 0
                for hl in range(8):
                    for tb in range(6):
                        tsl = slice(tb * 384, (tb + 1) * 384)
                        p = pq % 2; pq += 1
                        for k in range(3):
                            P.op("pe", lambda t, k=k, p=p, hl=hl, tsl=tsl: t.matmul(PQ[p][0:96, 0:384], WQBb[:, k, hl * 96:(hl + 1) * 96], CQN[:, k, tsl], start=(k == 0), stop=(k == 2)),
                                 reads=[bWQ, bCQN[tb]], writes=[bPQ[p]])
                        for k in range(3):
                            P.op("pe", lambda t, k=k, p=p, hl=hl, tsl=tsl: t.matmul(PQS[p][0:96, 0:384], WQSb[:, k, hl * 96:(hl + 1) * 96], CQN[:, k, tsl], start=(k == 0), stop=(k == 2)),
                                 reads=[bWQ, bCQN[tb]], writes=[bPQS[p]])
                        P.op("act", lambda a, p=p, hl=hl, tsl=tsl: a.copy(out=QT[0:64, hl, tsl], in_=PQ[p][0:64, 0:384]), reads=[bPQ[p]], writes=[bQT[hl]])
                        P.op("dve", lambda v, p=p, tsl=tsl: v.tensor_tensor(out=T1[p][64:96, :], in0=PQ[p][64:96, 0:384], in1=CSs[64:96, 0, tsl], op=ALU.mult), reads=[bPQ[p], bCS], writes=[bT1[p]])
                        P.op("dve", lambda v, p=p, tsl=tsl: v.tensor_tensor(out=T2[p][64:96, :], in0=PQS[p][64:96, 0:384], in1=CSs[64:96, 1, tsl], op=ALU.mult), reads=[bPQS[p], bCS], writes=[bT2[p]])
                        P.op("dve", lambda g, p=p, hl=hl, tsl=tsl: g.tensor_tensor(out=QT[64:96, hl, tsl], in0=T1[p][64:96, :], in1=T2[p][64:96, :], op=ALU.add), reads=[bT1[p], bT2[p]], writes=[bQT[hl]])
                        for k in range(2):
                            P.op("pe", lambda t, k=k, p=p, hl=hl, tsl=tsl: t.matmul(PS[p][0:64, 0:384], WKVb[:, k, hl * 128:hl * 128 + 64], CKVN[:, k, tsl], start=(k == 0), stop=(k == 1)),
                                 reads=[bWKV, bCKVN[tb]], writes=[bPS[p]])
                        P.op("act", lambda a, p=p, hl=hl, tsl=tsl: a.copy(out=KT[0:64, hl, tsl], in_=PS[p][0:64, 0:384]), reads=[bPS[p]], writes=[bKT[hl]])
                    P.op("dve", lambda g, hl=hl: g.tensor_copy(out=KT[64:96, hl, :], in_=KRT[64:96, :]), reads=bKRT, writes=[bKT[hl]])
                if STOP <= 2:
                    P.flush(); return
                for tt in range(NT):
                    p = tt % 2
                    for k in range(2):
                        P.op("pe", lambda t, k=k, p=p, tt=tt: t.matmul(PO[p][:, :], CKVN[:, k, tt * 128:(tt + 1) * 128], WKVb[:, k, :].rearrange("p (h d) -> p h d", d=128)[:, :, 64:128], start=(k == 0), stop=(k == 1)),
                             reads=[bWKV, bCKVN[tt // 3]], writes=[bPO[p]])
                    P.op("dve", lambda v, p=p, tt=tt: v.tensor_copy(out=VA[:, tt, :, 0:64], in_=PO[p][:, :].rearrange("p (h d) -> p h d", h=8)), reads=[bPO[p]], writes=[bVA[tt]])
                if STOP <= 3:
                    P.flush(); return
                POL = [PQ[0], PQ[1], PQS[0], PQS[1]]; bPOL = [bPQ[0], bPQ[1], bPQS[0], bPQS[1]]
                s3 = 0
                for hl in range(8):
                    h = hg * 8 + hl
                    s = s3 % 3; s3 += 1; ps = s % 2
                    for kc in range(2):
                        P.op("pe", lambda t, ps=ps, kc=kc, hl=hl: t.matmul(PS[ps][:, kc * 256:(kc + 1) * 256], KT[0:96, hl, kc * 128:(kc + 1) * 128], QT[0:96, hl, 0:256], start=True, stop=True),
                             reads=[bKT[hl], bQT[hl]], writes=[bPS[ps]])
                    P.op("act", lambda a, s=s, ps=ps: a.activation(out=PTs[s][:, :], in_=PS[ps][:, :], func=AF.Exp, scale=SCALE), reads=[bPS[ps]], writes=[bPT[s]])
                    for qt in range(2):
                        for kc in range(2):
                            P.op("pe", lambda t, s=s, qt=qt, kc=kc, hl=hl: t.matmul(POL[qt][:, 0:65], PTs[s][:, kc * 256 + qt * 128:kc * 256 + (qt + 1) * 128], VA[:, kc, hl, :], start=(kc == 0), stop=(kc == 1)),
                                 reads=[bPT[s], bVA[kc]], writes=[bPOL[qt]])
                    for qt in range(2):
                        P.op("dve", lambda v, qt=qt: v.reciprocal(out=REC[0][:, qt:qt + 1], in_=POL[qt][:, 64:65]), reads=[bPOL[qt]], writes=[bREC[0]])
                        P.op("act", lambda a, qt=qt, h=h: a.activation(out=YTM[:, qt, h * 64:(h + 1) * 64], in_=POL[qt][:, 0:64], func=AF.Copy, scale=REC[0][:, qt:qt + 1]),
                             reads=[bPOL[qt], bREC[0]], writes=[bYTM[qt]])
                steps = [(hl, qb, kc) for hl in range(8) for qb in range(4) for kc in range(NT)]
                PSA = [PS[0], PS[1], PO[0], PO[1]]; bPSA = [bPS[0], bPS[1], bPO[0], bPO[1]]
                LOOK = 2
                def emit_S(idx):
                    hl, qb, kc = steps[idx]; ps = idx % 4; q0 = 256 + qb * 512
                    P.op("pe", lambda t, ps=ps, kc=kc, hl=hl, q0=q0: t.matmul(PSA[ps][:, :], KT[0:96, hl, kc * 128:(kc + 1) * 128], QT[0:96, hl, q0:q0 + 512], start=True, stop=True),
                         reads=[bKT[hl], bQT[hl]], writes=[bPSA[ps]])
                for i0 in range(LOOK):
                    emit_S(i0)
                for idx, (hl, qb, kc) in enumerate(steps):
                    h = hg * 8 + hl
                    s = idx % 4; ps = idx % 4
                    if idx + LOOK < len(steps):
                        emit_S(idx + LOOK)
                    P.op("act", lambda a, s=s, ps=ps: a.activation(out=PTs[s][:, :], in_=PSA[ps][:, :], func=AF.Exp, scale=SCALE), reads=[bPSA[ps]], writes=[bPT[s]])
                    for j in range(4):
                        P.op("pe", lambda t, s=s, j=j, kc=kc, hl=hl: t.matmul(POL[j][:, 0:65], PTs[s][:, j * 128:(j + 1) * 128], VA[:, kc, hl, :], start=(kc == 0), stop=(kc == NT - 1)),
                             reads=[bPT[s], bVA[kc]], writes=[bPOL[j]])
                    if kc == NT - 1:
                        for j in range(4):
                            tile = 2 + qb * 4 + j
                            P.op("dve", lambda v, j=j: v.reciprocal(out=REC[1][:, j:j + 1], in_=POL[j][:, 64:65]), reads=[bPOL[j]], writes=[bREC[1]])
                            P.op("act", lambda a, j=j, h=h, tile=tile: a.activation(out=YTM[:, tile, h * 64:(h + 1) * 64], in_=POL[j][:, 0:64], func=AF.Copy, scale=REC[1][:, j:j + 1]),
                                 reads=[bPOL[j], bREC[1]], writes=[bYTM[tile]])
                P.flush()


PI = math.pi

def hy_host(conv_w, conv_b, b1, b2, b3, sin_freq, skip):
    H = {}
    H["cw"] = conv_w.reshape(3, 24, 128).transpose(2, 1, 0).copy()
    H["cb"] = conv_b.reshape(24, 128).T.copy()
    H["fb"] = np.stack([b1, b2, sin_freq[0], sin_freq[1]], 1).astype(np.float32).copy()
    max_decay = math.log(1e-2) / 0.3; min_decay = math.log(1e-2) / 1.5
    H["delta"] = np.abs(np.linspace(min_decay, max_decay, 1024, dtype=np.float32)).astype(np.float32)
    for L, tag in ((256, "c"), (2048, "l")):
        N = 2 * L
        t = np.linspace(0.0, 1.0, L, dtype=np.float32)
        w = (2.0 * np.float32(math.pi) / L) * np.arange(L, dtype=np.float32)
        f = np.linspace(1e-4, 15, 16, dtype=np.float32)
        ze = np.concatenate([t[:, None], np.cos(f[None, :] * w[:, None]), -np.sin(f[None, :] * w[:, None])], -1).astype(np.float32)
        H["ze" + tag] = ze.T.copy()
        nt = L // 128
        H["negt" + tag] = (-t).reshape(nt, 128).T.copy()
        idx = (np.arange(L, dtype=np.int64)[:, None] * np.arange(L, dtype=np.int64)[None, :]) % N
        ang = idx.astype(np.float64) * (2 * math.pi / N)
        Cm = np.cos(ang); Sm = -np.sin(ang)
        Sm[:, 0] = (-1.0) ** np.arange(L)
        H["C" + tag] = Cm.astype(NPBF); H["S" + tag] = Sm.astype(NPBF); H["ST" + tag] = Sm.T.copy().astype(NPBF)
        wsc = np.full((128, nt), 2.0 / N, np.float32); wsc[0, 0] = 1.0 / N
        H["wsc" + tag] = wsc
    return H


def mixer_hyena(P, K, run_norm, WIN, CW, CB, FW1, FW2, FW3, FB, FB3, SKIP, DELTA, TB, X0D, YTD, bYTD):
    cin = Buf(); bX0D = [Buf() for _ in range(8)]
    with contextlib.ExitStack() as st:
        GTM = P.sb(st, "h_GTM", [128, NT, D], BF16); bGTM = [Buf() for _ in range(NT)]
        with contextlib.ExitStack() as st2:
            HT = P.sb(st2, "HT", [128, 8, T], BF16); bHT = [Buf() for _ in range(NT)]
            run_norm(HT, bHT)
            X1T = P.sb(st2, "h_X1T", [128, 8, T], BF16); bX1T = [Buf() for _ in range(8)]
            WB = P.sb(st2, "h_WB", [128, 8, 1024], BF16); bWB = [Buf() for _ in range(8)]
            STG = [P.sb(st2, f"h_stg{i}", [128, 1024], F32) for i in range(2)]; bSTG = [Buf() for _ in range(2)]
            CWs = P.sb(st2, "h_CW", [128, 24, 3], F32); CBs = P.sb(st2, "h_CB", [128, 24], F32); bCW = Buf()
            ZC = P.sb(st2, "h_ZC", [128, T], F32); bZC = Buf()
            AC = P.sb(st2, "h_AC", [128, T], F32); bAC = Buf()
            OB = [P.sb(st2, f"h_OB{i}", [128, T], BF16) for i in range(2)]; bOB = [Buf() for _ in range(2)]
            PP = [P.ps(st2, f"h_PP{i}") for i in range(4)]; bPP = [Buf(excl=True) for _ in range(4)]
            PT = [P.ps(st2, f"h_PT{i}", [128, 8, 128], BF16) for i in range(2)]; bPT = [Buf(excl=True) for _ in range(2)]
            P.dma(CWs[:, :, :], CW[:, :, :], reads=[cin], writes=[bCW])
            P.dma(CBs[:, :], CB[:, :], reads=[cin], writes=[bCW])
            pi = 0
            for which in range(3):
                for k in range(8):
                    s = k % 2
                    P.dma(STG[s][:, :], WIN[k * 128:(k + 1) * 128, which * 1024:(which + 1) * 1024], reads=[cin], writes=[bSTG[s]])
                    if k % 2 == 0:
                        P.op("dve", lambda g, k=k, s=s: g.tensor_copy(out=WB[:, k, :], in_=STG[s][:, :]), reads=[bSTG[s]], writes=[bWB[k]])
                    else:
                        P.op("act", lambda a, k=k, s=s: a.copy(out=WB[:, k, :], in_=STG[s][:, :]), reads=[bSTG[s]], writes=[bWB[k]])
                for c in range(8):
                    cc = which * 8 + c
                    for tb in range(6):
                        p = pi % 4; pi += 1
                        for k in range(8):
                            P.op("pe", lambda t, k=k, p=p, c=c, tb=tb: t.matmul(PP[p][:, 0:384], WB[:, k, c * 128:(c + 1) * 128], HT[:, k, tb * 384:(tb + 1) * 384], start=(k == 0), stop=(k == 7)),
                                 reads=[bWB[k]] + bHT[tb * 3:tb * 3 + 3], writes=[bPP[p]])
                        P.op("act", lambda a, p=p, tb=tb: a.copy(out=ZC[:, tb * 384:(tb + 1) * 384], in_=PP[p][:, 0:384]), reads=[bPP[p]], writes=[bZC])
                    P.op("dve", lambda v, cc=cc: v.tensor_scalar(out=AC[:, :], in0=ZC[:, :], scalar1=CWs[:, cc, 1:2], scalar2=CBs[:, cc:cc + 1], op0=ALU.mult, op1=ALU.add), reads=[bZC, bCW], writes=[bAC])
                    for (o0, o1, i0, i1, tap) in ((1, 256, 0, 255, 0), (257, T, 256, T - 1, 0), (0, 255, 1, 256, 2), (256, T - 1, 257, T, 2)):
                        P.op("dve", lambda v, cc=cc, o0=o0, o1=o1, i0=i0, i1=i1, tap=tap: v.scalar_tensor_tensor(out=AC[:, o0:o1], in0=ZC[:, i0:i1], scalar=CWs[:, cc, tap:tap + 1], in1=AC[:, o0:o1], op0=ALU.mult, op1=ALU.add),
                             reads=[bZC, bAC, bCW], writes=[bAC])
                    if which == 0:
                        b = c % 2
                        P.op("act", lambda a, b=b: a.copy(out=OB[b][:, :], in_=AC[:, :]), reads=[bAC], writes=[bOB[b]])
                        P.dma(X0D[c * 128:(c + 1) * 128, :], OB[b][:, :], reads=[bOB[b]], writes=[bX0D[c]])
                    elif which == 1:
                        P.op("act", lambda a, c=c: a.copy(out=X1T[:, c, :], in_=AC[:, :]), reads=[bAC], writes=[bX1T[c]])
                    else:
                        b = c % 2
                        P.op("dve", lambda g, b=b, c=c: g.tensor_tensor(out=OB[b][:, :], in0=AC[:, :], in1=X1T[:, c, :], op=ALU.mult), reads=[bAC, bX1T[c]], writes=[bOB[b]])
                        for tt in range(NT):
                            q = tt // 8
                            P.op("pe", lambda t, b=b, tt=tt: t.transpose(PT[(tt // 8) % 2][:, tt % 8, :], OB[b][:, tt * 128:(tt + 1) * 128], K["idb"][:, :]), reads=[bOB[b], K["b_idb"]], writes=[bPT[q % 2]])
                            if tt % 8 == 7 or tt == NT - 1:
                                t0 = (tt // 8) * 8; n_ = tt - t0 + 1
                                P.op("act", lambda a, q=q, t0=t0, n_=n_, c=c: a.copy(out=GTM[:, t0:t0 + n_, c * 128:(c + 1) * 128], in_=PT[q % 2][:, 0:n_, :]), reads=[bPT[q % 2]], writes=bGTM[t0:t0 + n_])
            P.flush()
        with contextlib.ExitStack() as st2:
            W1 = P.sb(st2, "h_W1", [33, 64], F32); W2 = P.sb(st2, "h_W2", [64, 64], F32); W3 = P.sb(st2, "h_W3", [64, 2048], F32); bW = Buf()
            FBs = P.sb(st2, "h_FB", [64, 4], F32); B3 = P.sb(st2, "h_B3", [1, 2048], F32); ONES1 = P.sb(st2, "h_ON1", [1, 128], F32)
            SKs = P.sb(st2, "h_SK", [1, 1024], F32); DEL = P.sb(st2, "h_DEL", [128, 1024], F32)
            ARG = P.sb(st2, "h_ARG", [64, 512], F32); bARG = Buf(); MM = P.sb(st2, "h_MM", [64, 512], F32); bMM = Buf()
            H1 = P.sb(st2, "h_H1", [64, 2048], F32); bH1 = Buf(); H2 = P.sb(st2, "h_H2", [64, 2048], F32); bH2 = Buf()
            ZE = P.sb(st2, "h_ZE", [33, 2048], F32); bZE = Buf()
            NEGT = P.sb(st2, "h_NEGT", [128, 16], F32); WSC = P.sb(st2, "h_WSC", [128, 16], F32); bTBL = Buf()
            EE = P.sb(st2, "h_EE", [128, 512], F32); bEE = Buf()
            F0 = P.sb(st2, "h_F0", [128, 512], F32); bF0 = Buf()
            HF = P.sb(st2, "h_HF", [128, 16, 512], BF16); bHF = Buf(); HB = P.sb(st2, "h_HB", [128, 16, 512], BF16); bHB = Buf()
            YRE = P.sb(st2, "h_YRE", [128, 16, 512], BF16); bYRE = [Buf() for _ in range(16)]
            YIM = P.sb(st2, "h_YIM", [128, 16, 512], BF16); bYIM = [Buf() for _ in range(16)]
            CT = [P.sb(st2, f"h_CT{i}", [128, 16, 128], BF16) for i in range(1)] * 2; bCT = [Buf()] * 2
            STt = [P.sb(st2, f"h_ST{i}", [128, 16, 128], BF16) for i in range(1)] * 2; bST = [Buf()] * 2
            CI = P.sb(st2, "h_CI", [128, 16, 256], BF16); bCI = Buf(); SI = P.sb(st2, "h_SI", [128, 16, 256], BF16); bSI = Buf()
            TM = [P.sb(st2, f"h_TM{i}", [128, 512], F32) for i in range(9)]; bTM = [Buf() for _ in range(9)]
            X0L = [P.sb(st2, f"h_X0L{i}", [128, 512], BF16) for i in range(2)]; bX0L = [Buf() for _ in range(2)]
            YO = [P.sb(st2, f"h_YO{i}", [128, 512], BF16) for i in range(2)]; bYO = [Buf() for _ in range(2)]
            PF = [P.ps(st2, f"h_PF{i}") for i in range(6)]; bPF = [Buf(excl=True) for _ in range(6)]
            PI_ = [P.ps(st2, f"h_PI{i}") for i in range(2)]; bPI = [Buf(excl=True) for _ in range(2)]
            P.dma(W1[:, :], FW1[:, :], reads=[cin], writes=[bW]); P.dma(W2[:, :], FW2[:, :], reads=[cin], writes=[bW]); P.dma(W3[:, :], FW3[:, :], reads=[cin], writes=[bW])
            P.dma(FBs[:, :], FB[:, :], reads=[cin], writes=[bW]); P.dma(B3[:, :], FB3[:, :], reads=[cin], writes=[bW])
            P.dma(SKs[:, :], SKIP[:, :], reads=[cin], writes=[bW]); P.dma(DEL[:, :], DELTA.partition_broadcast(128), reads=[cin], writes=[bW])
            P.op("dve", lambda v: v.memset(ONES1[:, :], 1.0), writes=[bW])
            ci = 0; xi = 0
            for tag, L, tk0 in (("c", 256, 0), ("l", 2048, 256)):
                tb_ = TB[tag]; nt = L // 128; tile0 = tk0 // 128
                P.dma(ZE[:, 0:L], tb_["ze"][:, :], reads=[cin], writes=[bZE])
                P.dma(NEGT[:, 0:nt], tb_["negt"][:, :], reads=[cin], writes=[bTBL]); P.dma(WSC[:, 0:nt], tb_["wsc"][:, :], reads=[cin], writes=[bTBL])
                for (Wl, kin, SRC, bSRC, DST, bDST, bcol, scol) in ((W1, 33, ZE, bZE, H1, bH1, 0, 2), (W2, 64, H1, bH1, H2, bH2, 1, 3)):
                    for blk in range(0, L, 512):
                        n_ = min(512, L - blk)
                        P.op("pe", lambda t, Wl=Wl, kin=kin, SRC=SRC, blk=blk, n_=n_: t.matmul(PI_[0][0:64, 0:n_], Wl[0:kin, :], SRC[0:kin, blk:blk + n_], start=True, stop=True), reads=[bW, bSRC], writes=[bPI[0]])
                        P.op("dve", lambda v, n_=n_, bcol=bcol, scol=scol: v.tensor_scalar(out=ARG[:, 0:n_], in0=PI_[0][0:64, 0:n_], scalar1=FBs[:, bcol:bcol + 1], scalar2=FBs[:, scol:scol + 1], op0=ALU.add, op1=ALU.mult), reads=[bPI[0], bW], writes=[bARG])
                        P.op("dve", lambda v, n_=n_: v.tensor_scalar(out=MM[:, 0:n_], in0=ARG[:, 0:n_], scalar1=PI, scalar2=None, op0=ALU.is_gt), reads=[bARG], writes=[bMM])
                        P.op("dve", lambda v, n_=n_: v.scalar_tensor_tensor(out=ARG[:, 0:n_], in0=MM[:, 0:n_], scalar=-2 * PI, in1=ARG[:, 0:n_], op0=ALU.mult, op1=ALU.add), reads=[bMM, bARG], writes=[bARG])
                        P.op("dve", lambda v, n_=n_: v.tensor_scalar(out=MM[:, 0:n_], in0=ARG[:, 0:n_], scalar1=-PI, scalar2=None, op0=ALU.is_lt), reads=[bARG], writes=[bMM])
                        P.op("dve", lambda v, n_=n_: v.scalar_tensor_tensor(out=ARG[:, 0:n_], in0=MM[:, 0:n_], scalar=2 * PI, in1=ARG[:, 0:n_], op0=ALU.mult, op1=ALU.add), reads=[bMM, bARG], writes=[bARG])
                        P.op("act", lambda a, DST=DST, blk=blk, n_=n_: a.activation(out=DST[:, blk:blk + n_], in_=ARG[:, 0:n_], func=AF.Sin), reads=[bARG], writes=[bDST])
                for hh in range(2):
                    c0 = hh * 512
                    for tl in range(nt):
                        P.op("act", lambda a, tl=tl, c0=c0: a.activation(out=EE[:, 0:512], in_=DEL[:, c0:c0 + 512], func=AF.Exp, scale=NEGT[:, tl:tl + 1]), reads=[bW, bTBL], writes=[bEE])
                        for fi, (DSTF, bDSTF) in enumerate(((HF, bHF), (HB, bHB))):
                            w0 = fi * 1024 + c0
                            P.op("pe", lambda t, tl=tl, w0=w0: t.matmul(PI_[1][:, :], H2[:, tl * 128:(tl + 1) * 128], W3[:, w0:w0 + 512], start=True, stop=False), reads=[bH2, bW], writes=[bPI[1]])
                            P.op("pe", lambda t, w0=w0: t.matmul(PI_[1][:, :], ONES1[:, :], B3[:, w0:w0 + 512], start=False, stop=True), reads=[bW], writes=[bPI[1]])
                            if tl == 0:
                                P.op("dve", lambda v: v.scalar_tensor_tensor(out=F0[:, :], in0=EE[:, 0:512], scalar=0.05, in1=PI_[1][:, :], op0=ALU.add, op1=ALU.mult), reads=[bEE, bPI[1]], writes=[bF0])
                                if fi == 0:
                                    P.op("dve", lambda v, c0=c0: v.tensor_tensor(out=F0[0:1, :], in0=F0[0:1, :], in1=SKs[0:1, c0:c0 + 512], op=ALU.add), reads=[bF0, bW], writes=[bF0])
                                else:
                                    P.op("dve", lambda v: v.memset(F0[0:1, :], 0.0), reads=[bF0], writes=[bF0])
                                P.op("dve", lambda v, DSTF=DSTF: v.tensor_copy(out=DSTF[:, 0, :], in_=F0[:, :]), reads=[bF0], writes=[bDSTF])
                            else:
                                P.op("dve", lambda v, DSTF=DSTF, tl=tl: v.scalar_tensor_tensor(out=DSTF[:, tl, :], in0=EE[:, 0:512], scalar=0.05, in1=PI_[1][:, :], op0=ALU.add, op1=ALU.mult), reads=[bEE, bPI[1]], writes=[bDSTF])
                    for fc in range(nt):
                        cb_ = ci % 2; ci += 1
                        P.dma(CT[cb_][:, 0:nt, :], tb_["C"].rearrange("(tc p) f -> p tc f", p=128)[:, :, fc * 128:(fc + 1) * 128], reads=[cin], writes=[bCT[cb_]])
                        P.dma(STt[cb_][:, 0:nt, :], tb_["S"].rearrange("(tc p) f -> p tc f", p=128)[:, :, fc * 128:(fc + 1) * 128], reads=[cin], writes=[bST[cb_]])
                        srcs = [(lambda tc, tile0=tile0, c0=c0: GTM[:, tile0 + tc, c0:c0 + 512], lambda tc, tile0=tile0: [bGTM[tile0 + tc]]), (lambda tc: HF[:, tc, :], lambda tc: [bHF]), (lambda tc: HB[:, tc, :], lambda tc: [bHB])]
                        for si_, (sf_, bf_) in enumerate(srcs):
                            for ti_, (TAB, bTAB) in enumerate(((CT[cb_], bCT[cb_]), (STt[cb_], bST[cb_]))):
                                pf = si_ * 2 + ti_
                                for tc in range(nt):
                                    P.op("pe", lambda t, pf=pf, TAB=TAB, tc=tc, sf_=sf_, nt=nt: t.matmul(PF[pf][:, :], TAB[:, tc, :], sf_(tc), start=(tc == 0), stop=(tc == nt - 1)), reads=[bTAB] + bf_(tc), writes=[bPF[pf]])
                        Gc, Gs, Hfc, Hfs, Hbc, Hbs = PF; bGc, bGs, bHfc, bHfs, bHbc, bHbs = bPF
                        w_ = WSC[:, fc:fc + 1]
                        HbcW, HbsW, GsS, Kre, Kim, t1, t2, t3, t4 = TM; bHbcW, bHbsW, bGsS, bKre, bKim, bt1, bt2, bt3, bt4 = bTM
                        P.op("act", lambda a, w_=w_: a.activation(out=HbcW[:, :], in_=Hbc[:, :], func=AF.Copy, scale=w_), reads=[bHbc, bTBL], writes=[bHbcW])
                        P.op("act", lambda a, w_=w_: a.activation(out=HbsW[:, :], in_=Hbs[:, :], func=AF.Copy, scale=w_), reads=[bHbs, bTBL], writes=[bHbsW])
                        P.op("act", lambda a: a.copy(out=GsS[:, :], in_=Gs[:, :]), reads=[bGs], writes=[bGsS])
                        P.op("dve", lambda v, w_=w_: v.scalar_tensor_tensor(out=Kre[:, :], in0=Hfc[:, :], scalar=w_, in1=HbcW[:, :], op0=ALU.mult, op1=ALU.add), reads=[bHfc, bTBL, bHbcW], writes=[bKre])
                        P.op("dve", lambda v, w_=w_: v.scalar_tensor_tensor(out=Kim[:, :], in0=Hfs[:, :], scalar=w_, in1=HbsW[:, :], op0=ALU.mult, op1=ALU.subtract), reads=[bHfs, bTBL, bHbsW], writes=[bKim])
                        P.op("dve", lambda v: v.tensor_tensor(out=t1[:, :], in0=Gc[:, :], in1=Kre[:, :], op=ALU.mult), reads=[bGc, bKre], writes=[bt1])
                        P.op("dve", lambda g: g.tensor_tensor(out=t2[:, :], in0=GsS[:, :], in1=Kim[:, :], op=ALU.mult), reads=[bGsS, bKim], writes=[bt2])
                        P.op("dve", lambda g, fc=fc: g.tensor_tensor(out=YRE[:, fc, :], in0=t1[:, :], in1=t2[:, :], op=ALU.subtract), reads=[bt1, bt2], writes=[bYRE[fc]])
                        P.op("dve", lambda v: v.tensor_tensor(out=t3[:, :], in0=Gc[:, :], in1=Kim[:, :], op=ALU.mult), reads=[bGc, bKim], writes=[bt3])
                        P.op("dve", lambda g: g.tensor_tensor(out=t4[:, :], in0=GsS[:, :], in1=Kre[:, :], op=ALU.mult), reads=[bGsS, bKre], writes=[bt4])
                        P.op("dve", lambda g, fc=fc: g.tensor_tensor(out=YIM[:, fc, :], in0=t3[:, :], in1=t4[:, :], op=ALU.add), reads=[bt3, bt4], writes=[bYIM[fc]])
                        if fc == 0:
                            P.op("dve", lambda v: v.tensor_copy(out=YRE[0:1, 0, :], in_=t1[0:1, :]), reads=[bt1, bYRE[0]], writes=[bYRE[0]])
                            P.op("dve", lambda v, w_=w_: v.scalar_tensor_tensor(out=t2[0:1, :], in0=Hfs[0:1, :], scalar=WSC[0:1, 0:1], in1=HbsW[0:1, :], op0=ALU.mult, op1=ALU.add), reads=[bHfs, bTBL, bHbsW, bt2], writes=[bt2])
                            P.op("dve", lambda g: g.tensor_tensor(out=YIM[0:1, 0, :], in0=GsS[0:1, :], in1=t2[0:1, :], op=ALU.mult), reads=[bGsS, bt2, bYIM[0]], writes=[bYIM[0]])
                    for blk in range(0, L, 256):
                        n_ = min(256, L - blk)
                        P.dma(CI[:, 0:nt, 0:n_], tb_["C"].rearrange("(fc p) t -> p fc t", p=128)[:, :, blk:blk + n_], reads=[cin], writes=[bCI])
                        P.dma(SI[:, 0:nt, 0:n_], tb_["ST"].rearrange("(fc p) t -> p fc t", p=128)[:, :, blk:blk + n_], reads=[cin], writes=[bSI])
                        for cch in range(4):
                            pb = cch % 2
                            chan = hh * 4 + cch
                            xb = xi % 2; xi += 1
                            P.dma(X0L[xb][:, 0:n_], X0D[chan * 128:(chan + 1) * 128, tk0 + blk:tk0 + blk + n_], reads=[bX0D[chan]], writes=[bX0L[xb]])
                            for fc in range(nt):
                                P.op("pe", lambda t, pb=pb, fc=fc, cch=cch, n_=n_: t.matmul(PI_[pb][:, 0:n_], YRE[:, fc, cch * 128:(cch + 1) * 128], CI[:, fc, 0:n_], start=(fc == 0), stop=False), reads=[bYRE[fc], bCI], writes=[bPI[pb]])
                                P.op("pe", lambda t, pb=pb, fc=fc, cch=cch, n_=n_, nt=nt: t.matmul(PI_[pb][:, 0:n_], YIM[:, fc, cch * 128:(cch + 1) * 128], SI[:, fc, 0:n_], start=False, stop=(fc == nt - 1)), reads=[bYIM[fc], bSI], writes=[bPI[pb]])
                            P.op("dve", lambda v, pb=pb, xb=xb, n_=n_: v.tensor_tensor(out=YO[xb][:, 0:n_], in0=PI_[pb][:, 0:n_], in1=X0L[xb][:, 0:n_], op=ALU.mult), reads=[bPI[pb], bX0L[xb]], writes=[bYO[xb]])
                            P.dma(YTD[chan * 128:(chan + 1) * 128, tk0 + blk:tk0 + blk + n_], YO[xb][:, 0:n_], reads=[bYO[xb]], writes=[bYTD[chan]])
            P.flush()


def outproj_fm_dram(P, K, YTD, bYTD, WO, X, bX, MODS, g_off, tiles=range(NT)):
    with contextlib.ExitStack() as st:
        YT = P.sb(st, "ofm_YT", [128, 8, T], BF16); bYT = [Buf() for _ in range(NT)]
        for k in range(8):
            P.dma(YT[:, k, :], YTD[k * 128:(k + 1) * 128, :], reads=[bYTD[k]], writes=bYT)
        phase_outproj(P, K, (YT, bYT), WO, X, bX, MODS, g_off, tiles=tiles, mode="fm")


LN8 = math.log(0.125)

def ml_host(conv_w, conv_b):
    cw = conv_w.reshape(3, 8, 128).transpose(2, 1, 0).copy()
    cb = conv_b.reshape(8, 128).T.copy()
    sel8 = np.zeros((8, 8, 128), np.float32)
    for h in range(8): sel8[h, h, :] = 1
    s = np.arange(128)
    tri = np.zeros((128, 2, 128), np.float32)
    tri[:, 0, :] = (s[:, None] <= s[None, :])
    tri[:, 1, :] = (s[:, None] >= s[None, :])
    return cw, cb, sel8, tri


def hs_scan(P, src, bsrc, A, bA, B, bB, a0, a1, op, reverse, nparts=8):
    n = a1 - a0
    sh = 1
    cur, bcur = src, bsrc
    nxt = [(A, bA), (B, bB)]
    i = 0
    while sh < n:
        dst, bdst = nxt[i % 2]; i += 1
        if not reverse:
            P.op("dve", lambda v, cur=cur, dst=dst, sh=sh: v.tensor_tensor(out=dst[0:nparts, a0 + sh:a1], in0=cur[0:nparts, a0 + sh:a1], in1=cur[0:nparts, a0:a1 - sh], op=op), reads=[bcur], writes=[bdst])
            P.op("pool", lambda g, cur=cur, dst=dst, sh=sh: g.tensor_copy(out=dst[0:nparts, a0:a0 + sh], in_=cur[0:nparts, a0:a0 + sh]), reads=[bcur], writes=[bdst])
        else:
            P.op("dve", lambda v, cur=cur, dst=dst, sh=sh: v.tensor_tensor(out=dst[0:nparts, a0:a1 - sh], in0=cur[0:nparts, a0:a1 - sh], in1=cur[0:nparts, a0 + sh:a1], op=op), reads=[bcur], writes=[bdst])
            P.op("pool", lambda g, cur=cur, dst=dst, sh=sh: g.tensor_copy(out=dst[0:nparts, a1 - sh:a1], in_=cur[0:nparts, a1 - sh:a1]), reads=[bcur], writes=[bdst])
        cur, bcur = dst, bdst
        sh *= 2
    return cur, bcur


def mixer_mlstm(P, K, run_norm, WIN, CW, CB, GB, ONG, SEL8, TRI, SIGO, YTM, bYTM):
    cin = Buf(); bSIGO = [Buf() for _ in range(NT)]
    with contextlib.ExitStack() as st:
        HT = P.sb(st, "HT", [128, 8, T], BF16); bHT = [Buf() for _ in range(NT)]
        run_norm(HT, bHT)
        NEGM = [P.sb(st, f"l_NEGM{d}", [8, T], F32) for d in range(2)]; bNEGM = [Buf(), Buf()]
        ATM = P.sb(st, "l_ATM", [128, NT, 16], F32); bATM = Buf()
        EMT = P.sb(st, "l_EMT", [128, NT, 16], F32); bEMT = Buf()
        if True:
            with contextlib.ExitStack() as st3:
                WG = P.sb(st3, "l_WG", [128, 8, 32], BF16); bWG = Buf()
                WGf = P.sb(st3, "l_WGf", [128, 8, 32], F32); bWGf = Buf()
                GBs = P.sb(st3, "l_GB", [128, 32], F32); bGB = Buf()
                ONE = P.sb(st3, "l_ONE", [128, 1], F32); bONE = Buf()
                GT = P.sb(st3, "l_GT", [128, NT, 32], F32); bGT = [Buf() for _ in range(NT)]
                TMPg = P.sb(st3, "l_TMPg", [128, NT, 32], F32)
                SC = [P.sb(st3, f"l_SC{i}", [8, T], F32) for i in range(10)]; bSC = [Buf() for _ in range(10)]
                PGt = [P.ps(st3, f"l_PG{i}") for i in range(2)]; bPG = [Buf(excl=True) for _ in range(2)]
                PTr = [P.ps(st3, f"l_PTr{i}") for i in range(4)]; bPTr = [Buf(excl=True) for _ in range(4)]
                P.dma(WGf[:, :, :], WIN.rearrange("(c p) f -> p c f", p=128)[:, :, 3072:3104], reads=[cin], writes=[bWGf])
                P.op("dve", lambda v: v.tensor_copy(out=WG[:, :, :], in_=WGf[:, :, :]), reads=[bWGf], writes=[bWG])
                P.dma(GBs[:, :], GB.partition_broadcast(128), reads=[cin], writes=[bGB])
                P.op("dve", lambda v: v.memset(ONE[:, :], 1.0), writes=[bONE])
                for tt in range(NT):
                    p = tt % 2
                    for k in range(8):
                        P.op("pe", lambda t, k=k, p=p, tt=tt: t.matmul(PGt[p][:, 0:32], HT[:, k, tt * 128:(tt + 1) * 128], WG[:, k, :], start=(k == 0), stop=(k == 7)), reads=[bHT[tt], bWG], writes=[bPG[p]])
                    P.op("dve", lambda v, p=p, tt=tt: v.tensor_tensor(out=GT[:, tt, :], in0=PGt[p][:, 0:32], in1=GBs[:, :], op=ALU.add), reads=[bPG[p], bGB], writes=[bGT[tt]])
                    for j in (1, 3):
                        sl = slice(j * 8, (j + 1) * 8)
                        P.op("act", lambda a, tt=tt, sl=sl: a.activation(out=TMPg[:, tt, sl], in_=GT[:, tt, sl], func=AF.Exp, scale=-1.0), reads=[bGT[tt]], writes=[bGT[tt]])
                        P.op("act", lambda a, tt=tt, sl=sl: a.activation(out=TMPg[:, tt, sl], in_=TMPg[:, tt, sl], func=AF.Ln, bias=ONE[:, :]), reads=[bGT[tt], bONE], writes=[bGT[tt]])
                        P.op("dve", lambda v, tt=tt, sl=sl: v.tensor_scalar(out=GT[:, tt, sl], in0=TMPg[:, tt, sl], scalar1=-1.0, scalar2=None, op0=ALU.mult), reads=[bGT[tt]], writes=[bGT[tt]])
                    for j in range(4):
                        P.op("pe", lambda t, j=j, tt=tt: t.transpose(PTr[j][0:8, 0:128], GT[:, tt, j * 8:(j + 1) * 8], K["idf"][:, :]), reads=[bGT[tt], K["b_idf"]], writes=[bPTr[j]])
                        P.op("act", lambda a, j=j, tt=tt: a.copy(out=SC[j][:, tt * 128:(tt + 1) * 128], in_=PTr[j][0:8, 0:128]), reads=[bPTr[j]], writes=[bSC[j]])
                for d in range(2):
                    IG, bIG, LF, bLF = SC[2 * d], bSC[2 * d], SC[2 * d + 1], bSC[2 * d + 1]
                    w = [(SC[4 + i], bSC[4 + i]) for i in range(6)]
                    if d == 0:
                        F_, bF = hs_scan(P, LF, bLF, w[0][0], w[0][1], w[1][0], w[1][1], 0, T, ALU.add, False)
                    else:
                        Fc, bFc = hs_scan(P, LF, bLF, w[0][0], w[0][1], w[1][0], w[1][1], 0, 256, ALU.add, True)
                        Fl, bFl = hs_scan(P, LF, bLF, w[2][0], w[2][1], w[3][0], w[3][1], 256, T, ALU.add, True)
                        F_, bF = w[4]
                        P.op("dve", lambda v, Fc=Fc: v.tensor_copy(out=F_[:, 0:256], in_=Fc[:, 0:256]), reads=[bFc], writes=[bF])
                        P.op("dve", lambda v, Fl=Fl, Fc=Fc: v.tensor_scalar(out=F_[:, 256:T], in0=Fl[:, 256:T], scalar1=Fc[:, 0:1], scalar2=None, op0=ALU.add), reads=[bFl, bFc], writes=[bF])
                    P.op("dve", lambda v, IG=IG, F_=F_: v.tensor_tensor(out=IG[:, :], in0=IG[:, :], in1=F_[:, :], op=ALU.subtract), reads=[bIG, bF], writes=[bIG])
                    if d == 0:
                        free = [x for x in w if x[0] is not F_]
                        CM, bCM = hs_scan(P, IG, bIG, free[0][0], free[0][1], free[1][0], free[1][1], 0, T, ALU.max, False)
                        Mt, bM = free[2]
                        P.op("dve", lambda v, CM=CM, Mt=Mt: v.tensor_scalar(out=Mt[:, :], in0=CM[:, :], scalar1=0.0, scalar2=None, op0=ALU.max), reads=[bCM], writes=[bM])
                    else:
                        CMc, bCMc = hs_scan(P, IG, bIG, w[0][0], w[0][1], w[1][0], w[1][1], 0, 256, ALU.max, True)
                        CMl, bCMl = hs_scan(P, IG, bIG, w[2][0], w[2][1], w[3][0], w[3][1], 256, T, ALU.max, True)
                        Mt, bM = w[5]
                        P.op("dve", lambda v, CMc=CMc, Mt=Mt: v.tensor_scalar(out=Mt[:, 0:256], in0=CMc[:, 0:256], scalar1=0.0, scalar2=None, op0=ALU.max), reads=[bCMc], writes=[bM])
                        P.op("dve", lambda v, CMl=CMl, CMc=CMc, Mt=Mt: v.tensor_scalar(out=Mt[:, 256:T], in0=CMl[:, 256:T], scalar1=CMc[:, 0:1], scalar2=0.0, op0=ALU.max, op1=ALU.max), reads=[bCMl, bCMc], writes=[bM])
                    P.op("dve", lambda v, d=d, Mt=Mt: v.tensor_scalar(out=NEGM[d][:, :], in0=Mt[:, :], scalar1=-1.0, scalar2=None, op0=ALU.mult), reads=[bM], writes=[bNEGM[d]])
                    P.op("dve", lambda v, LF=LF, F_=F_, Mt=Mt: v.tensor_tensor(out=LF[:, :], in0=F_[:, :], in1=Mt[:, :], op=ALU.add), reads=[bF, bM], writes=[bLF])
                    P.op("act", lambda a, LF=LF: a.activation(out=LF[:, :], in_=LF[:, :], func=AF.Exp, scale=-1.0), reads=[bLF], writes=[bLF])
                    P.op("dve", lambda v, IG=IG: v.tensor_scalar(out=IG[:, :], in0=IG[:, :], scalar1=LN8, scalar2=None, op0=ALU.add), reads=[bIG], writes=[bIG])
                    for tt in range(NT):
                        j = tt % 2
                        P.op("pe", lambda t, j=j, tt=tt, IG=IG: t.transpose(PTr[j][:, 0:8], IG[:, tt * 128:(tt + 1) * 128], K["idf"][0:8, 0:8]), reads=[bIG, K["b_idf"]], writes=[bPTr[j]])
                        P.op("act", lambda a, j=j, tt=tt, d=d: a.copy(out=ATM[:, tt, d * 8:(d + 1) * 8], in_=PTr[j][:, 0:8]), reads=[bPTr[j]], writes=[bATM])
                        P.op("pe", lambda t, j=j, tt=tt, LF=LF: t.transpose(PTr[2 + j][:, 0:8], LF[:, tt * 128:(tt + 1) * 128], K["idf"][0:8, 0:8]), reads=[bLF, K["b_idf"]], writes=[bPTr[2 + j]])
                        P.op("act", lambda a, j=j, tt=tt, d=d: a.copy(out=EMT[:, tt, d * 8:(d + 1) * 8], in_=PTr[2 + j][:, 0:8]), reads=[bPTr[2 + j]], writes=[bEMT])
                P.flush()
            QKT = P.sb(st, "l_QKT", [128, 8, T], BF16); bQKT = [Buf() for _ in range(8)]
            VA = P.sb(st, "l_VA", [128, NT, 8, 129], BF16); bVA = [Buf() for _ in range(NT)]
            with contextlib.ExitStack() as st3:
                WB = P.sb(st3, "l_WB", [128, 8, 1024], BF16); bWB = [Buf() for _ in range(8)]
                STG = [P.sb(st3, f"l_stg{i}", [128, 1024], F32) for i in range(2)]; bSTG = [Buf() for _ in range(2)]
                CWs = P.sb(st3, "l_CW", [128, 8, 3], F32); CBs = P.sb(st3, "l_CB", [128, 8], F32); bCW = Buf()
                ZC = [P.sb(st3, f"l_ZC{i}", [128, T], F32) for i in range(1)] * 2; bZC = [Buf()] * 2
                AC = [P.sb(st3, f"l_AC{i}", [128, T], F32) for i in range(1)] * 2; bAC = [Buf()] * 2
                SO = [P.sb(st3, f"l_SO{i}", [128, 1024], BF16) for i in range(2)]; bSO = [Buf() for _ in range(2)]
                PP = [P.ps(st3, f"l_PP{i}") for i in range(4)]; bPP = [Buf(excl=True) for _ in range(4)]
                P.dma(CWs[:, :, :], CW[:, :, :], reads=[cin], writes=[bCW])
                P.dma(CBs[:, :], CB[:, :], reads=[cin], writes=[bCW])
                P.op("dve", lambda v: v.memset(VA[:, :, :, 128:129], 1.0), writes=bVA)
                pi = 0
                for which in range(3):
                    for k in range(8):
                        s = k % 2
                        P.dma(STG[s][:, :], WIN[k * 128:(k + 1) * 128, which * 1024:(which + 1) * 1024], reads=[cin], writes=[bSTG[s]])
                        if k % 2 == 0:
                            P.op("dve", lambda g, k=k, s=s: g.tensor_copy(out=WB[:, k, :], in_=STG[s][:, :]), reads=[bSTG[s]], writes=[bWB[k]])
                        else:
                            P.op("act", lambda a, k=k, s=s: a.copy(out=WB[:, k, :], in_=STG[s][:, :]), reads=[bSTG[s]], writes=[bWB[k]])
                    if which == 0:
                        for c in range(8):
                            b = c % 2
                            for tb in range(6):
                                p = pi % 4; pi += 1
                                for k in range(8):
                                    P.op("pe", lambda t, k=k, p=p, c=c, tb=tb: t.matmul(PP[p][:, 0:384], WB[:, k, c * 128:(c + 1) * 128], HT[:, k, tb * 384:(tb + 1) * 384], start=(k == 0), stop=(k == 7)),
                                         reads=[bWB[k]] + bHT[tb * 3:tb * 3 + 3], writes=[bPP[p]])
                                P.op("act", lambda a, p=p, b=b, tb=tb: a.copy(out=ZC[b][:, tb * 384:(tb + 1) * 384], in_=PP[p][:, 0:384]), reads=[bPP[p]], writes=[bZC[b]])
                            z = ZC[b]; ac = AC[b]
                            P.op("dve", lambda v, z=z, ac=ac, c=c: v.tensor_scalar(out=ac[:, :], in0=z[:, :], scalar1=CWs[:, c, 1:2], scalar2=CBs[:, c:c + 1], op0=ALU.mult, op1=ALU.add), reads=[bZC[b], bCW], writes=[bAC[b]])
                            for (o0, o1, i0, i1, tap) in ((1, 256, 0, 255, 0), (257, T, 256, T - 1, 0), (0, 255, 1, 256, 2), (256, T - 1, 257, T, 2)):
                                P.op("dve", lambda v, z=z, ac=ac, c=c, o0=o0, o1=o1, i0=i0, i1=i1, tap=tap: v.scalar_tensor_tensor(out=ac[:, o0:o1], in0=z[:, i0:i1], scalar=CWs[:, c, tap:tap + 1], in1=ac[:, o0:o1], op0=ALU.mult, op1=ALU.add),
                                     reads=[bZC[b], bAC[b], bCW], writes=[bAC[b]])
                            P.op("act", lambda a, ac=ac, c=c: a.activation(out=QKT[:, c, :], in_=ac[:, :], func=AF.Silu), reads=[bAC[b]], writes=[bQKT[c]])
                    elif which == 1:
                        for tt in range(NT):
                            for dh in range(2):
                                p = pi % 4; pi += 1
                                for k in range(8):
                                    P.op("pe", lambda t, k=k, p=p, tt=tt, dh=dh: t.matmul(PP[p][:, :], HT[:, k, tt * 128:(tt + 1) * 128], WB[:, k, dh * 512:(dh + 1) * 512], start=(k == 0), stop=(k == 7)),
                                         reads=[bWB[k], bHT[tt]], writes=[bPP[p]])
                                if dh == 0:
                                    P.op("act", lambda a, p=p, tt=tt, dh=dh: a.copy(out=VA[:, tt, dh * 4:(dh + 1) * 4, 0:128], in_=PP[p][:, :].rearrange("p (h d) -> p h d", h=4)), reads=[bPP[p]], writes=[bVA[tt]])
                                else:
                                    P.op("dve", lambda v, p=p, tt=tt, dh=dh: v.tensor_copy(out=VA[:, tt, dh * 4:(dh + 1) * 4, 0:128], in_=PP[p][:, :].rearrange("p (h d) -> p h d", h=4)), reads=[bPP[p]], writes=[bVA[tt]])
                    else:
                        for tt in range(2, NT):
                            b = tt % 2
                            for dh in range(2):
                                p = pi % 4; pi += 1
                                for k in range(8):
                                    P.op("pe", lambda t, k=k, p=p, tt=tt, dh=dh: t.matmul(PP[p][:, :], HT[:, k, tt * 128:(tt + 1) * 128], WB[:, k, dh * 512:(dh + 1) * 512], start=(k == 0), stop=(k == 7)),
                                         reads=[bWB[k], bHT[tt]], writes=[bPP[p]])
                                P.op("act", lambda a, p=p, b=b, dh=dh: a.activation(out=SO[b][:, dh * 512:(dh + 1) * 512], in_=PP[p][:, :], func=AF.Sigmoid), reads=[bPP[p]], writes=[bSO[b]])
                            P.dma(SIGO[tt * 128:(tt + 1) * 128, :], SO[b][:, :], reads=[bSO[b]], writes=[bSIGO[tt]])
                P.flush()
        with contextlib.ExitStack() as st2:
            SEL = P.sb(st2, "l_SEL", [8, 8, 128], F32); bSEL = Buf()
            TRf = P.sb(st2, "l_TRf", [128, 2, 128], F32); TRb = P.sb(st2, "l_TRb", [128, 2, 128], BF16); bTR = Buf()
            ONGs = P.sb(st2, "l_ONG", [128, 1024], F32); bONG = Buf()
            HS = P.sb(st2, "l_HS", [128, 4, 1024], F32); bHS = [Buf() for _ in range(4)]
            DT = [P.sb(st2, f"l_DT{i}", [128, 512], F32) for i in range(3)]; bDT = [Buf() for _ in range(3)]
            WT = [P.sb(st2, f"l_WT{i}", [128, 512], BF16) for i in range(3)]; bWT = [Buf() for _ in range(3)]
            SM = [P.sb(st2, f"l_SM{i}", [128, 8], F32) for i in range(2)]; bSM = [Buf() for _ in range(2)]
            SG = [P.sb(st2, f"l_SG{i}", [128, 1024], BF16) for i in range(2)]; bSG = [Buf() for _ in range(2)]
            JK = P.sb(st2, "l_JK", [128, 128], F32); bJK = Buf()
            RS = P.sb(st2, "l_RS", [128, 4, 24], F32); bRS = [Buf() for _ in range(4)]
            HN = [P.sb(st2, f"l_HN{i}", [128, 1024], F32) for i in range(1)] * 2; bHN = [Buf()] * 2
            PO = [P.ps(st2, f"l_PO{i}") for i in range(4)]; bPO = [Buf(excl=True) for _ in range(4)]
            PS = [P.ps(st2, f"l_PS{i}") for i in range(3)]; bPS = [Buf(excl=True) for _ in range(3)]
            NB = [P.ps(st2, f"l_NB{i}") for i in range(1)]; bNB = [Buf(excl=True) for _ in range(1)]
            P.dma(SEL[:, :, :], SEL8[:, :, :], reads=[cin], writes=[bSEL])
            P.dma(TRf[:, :, :], TRI[:, :, :], reads=[cin], writes=[bTR])
            P.op("dve", lambda v: v.tensor_copy(out=TRb[:, :, :], in_=TRf[:, :, :]), reads=[bTR], writes=[bTR])
            P.dma(ONGs[:, :], ONG.partition_broadcast(128), reads=[cin], writes=[bONG])
            def kcs_of(qb, d):
                tiles_ = [2 + qb * 4 + j for j in range(4)]
                return list(range(0, tiles_[-1] + 1)) if d == 0 else [0, 1] + list(range(tiles_[0], NT))
            groups = [(qb, h, d) for qb in range(4) for h in range(8) for d in range(2)]
            def emit_NB(gi):
                qb, h, d = groups[gi]; nb = 0; q0 = 256 + qb * 512
                P.op("pe", lambda t, nb=nb, h=h, d=d, q0=q0: t.matmul(NB[nb][:, :], SEL[:, h, :], NEGM[d][:, q0:q0 + 512], start=True, stop=True), reads=[bSEL, bNEGM[d]], writes=[bNB[nb]])
            steps = []
            for gi, (qb, h, d) in enumerate(groups):
                ks = kcs_of(qb, d)
                for ki, kc in enumerate(ks):
                    steps.append((gi, kc, ki == 0, ki == len(ks) - 1))
            def emit_S(idx):
                gi, kc, _, _ = steps[idx]
                qb, h, d = groups[gi]; b = idx % 3; q0 = 256 + qb * 512
                cq = h // 2; ck = 4 + h // 2; hp = (h % 2) * 64
                P.op("pe", lambda t, b=b, kc=kc, ck=ck, cq=cq, hp=hp, q0=q0: t.matmul(PS[b][:, :], QKT[hp:hp + 64, ck, kc * 128:(kc + 1) * 128], QKT[hp:hp + 64, cq, q0:q0 + 512], start=True, stop=True),
                     reads=[bQKT[ck], bQKT[cq]], writes=[bPS[b]])
            emit_NB(0)
            emit_S(0)
            emit_S(1)
            for idx, (gi, kc, gfirst, glast) in enumerate(steps):
                qb, h, d = groups[gi]
                tiles_ = [2 + qb * 4 + j for j in range(4)]
                nb = 0; b = idx % 3
                col = d * 8 + h
                if gfirst and gi > 0:
                    emit_NB(gi)
                if tiles_[0] <= kc <= tiles_[-1]:
                    P.op("dve", lambda v, b=b, nb=nb, kc=kc, col=col: v.tensor_scalar(out=DT[b][:, :], in0=NB[nb][:, :], scalar1=ATM[:, kc, col:col + 1], scalar2=0.0, op0=ALU.add, op1=ALU.min),
                         reads=[bNB[nb], bATM], writes=[bDT[b]])
                    P.op("act", lambda a, b=b: a.activation(out=DT[b][:, :], in_=DT[b][:, :], func=AF.Exp), reads=[bDT[b]], writes=[bDT[b]])
                else:
                    P.op("act", lambda a, b=b, nb=nb, kc=kc, col=col: a.activation(out=DT[b][:, :], in_=NB[nb][:, :], func=AF.Exp, bias=ATM[:, kc, col:col + 1]), reads=[bNB[nb], bATM], writes=[bDT[b]])
                if idx + 2 < len(steps):
                    emit_S(idx + 2)
                P.op("dve", lambda v, b=b: v.tensor_tensor(out=WT[b][:, :], in0=PS[b][:, :], in1=DT[b][:, :], op=ALU.mult), reads=[bPS[b], bDT[b]], writes=[bWT[b]])
                for j, tj in enumerate(tiles_):
                    if d == 0:
                        ok = kc <= tj; first = (kc == 0); last = (kc == tj)
                    else:
                        ok = kc < 2 or kc >= tj; first = (kc == 0); last = (kc == NT - 1)
                    if not ok:
                        continue
                    if kc == tj:
                        P.op("dve", lambda g, b=b, j=j, d=d: g.tensor_tensor(out=WT[b][:, j * 128:(j + 1) * 128], in0=WT[b][:, j * 128:(j + 1) * 128], in1=TRb[:, d, :], op=ALU.mult), reads=[bWT[b], bTR], writes=[bWT[b]])
                    P.op("pe", lambda t, b=b, j=j, kc=kc, h=h, first=first, last=last: t.matmul(PO[j][:, 0:129], WT[b][:, j * 128:(j + 1) * 128], VA[:, kc, h, :], start=first, stop=last),
                         reads=[bWT[b], bVA[kc]], writes=[bPO[j]])
                if glast:
                    for j, tj in enumerate(tiles_):
                        sm = j % 2
                        P.op("act", lambda a, j=j, sm=sm: a.activation(out=SM[sm][:, 2:3], in_=PO[j][:, 128:129], func=AF.Abs), reads=[bPO[j]], writes=[bSM[sm]])
                        P.op("dve", lambda v, j=j, tj=tj, col=col, sm=sm: v.tensor_scalar(out=SM[sm][:, 0:1], in0=SM[sm][:, 2:3], scalar1=EMT[:, tj, col:col + 1], scalar2=None, op0=ALU.max), reads=[bSM[sm], bEMT], writes=[bSM[sm]])
                        P.op("dve", lambda v, sm=sm: v.reciprocal(out=SM[sm][:, 1:2], in_=SM[sm][:, 0:1]), reads=[bSM[sm]], writes=[bSM[sm]])
                        if d == 0:
                            P.op("dve", lambda v, j=j, h=h, sm=sm: v.tensor_scalar(out=HS[:, j, h * 128:(h + 1) * 128], in0=PO[j][:, 0:128], scalar1=SM[sm][:, 1:2], scalar2=None, op0=ALU.mult), reads=[bPO[j], bSM[sm]], writes=[bHS[j]])
                        else:
                            P.op("dve", lambda v, j=j, h=h, sm=sm: v.scalar_tensor_tensor(out=HS[:, j, h * 128:(h + 1) * 128], in0=PO[j][:, 0:128], scalar=SM[sm][:, 1:2], in1=HS[:, j, h * 128:(h + 1) * 128], op0=ALU.mult, op1=ALU.add),
                                 reads=[bPO[j], bSM[sm], bHS[j]], writes=[bHS[j]])
                if not (glast and h == 7 and d == 1):
                    continue
                for j, tj in enumerate(tiles_):
                    b = j % 2
                    P.dma(SG[b][:, :], SIGO[tj * 128:(tj + 1) * 128, :], reads=[bSIGO[tj]], writes=[bSG[b]])
                    for h in range(8):
                        P.op("act", lambda a, j=j, h=h: a.activation(out=JK[:, :], in_=HS[:, j, h * 128:(h + 1) * 128], func=AF.Square, accum_out=RS[:, j, h:h + 1]), reads=[bHS[j]], writes=[bJK, bRS[j]])
                    P.op("act", lambda a, j=j: a.activation(out=RS[:, j, 8:16], in_=RS[:, j, 0:8], func=AF.Sqrt, scale=1.0 / 128, bias=K["eps"][:, :]), reads=[bRS[j], K["b_eps"]], writes=[bRS[j]])
                    P.op("dve", lambda v, j=j: v.reciprocal(out=RS[:, j, 16:24], in_=RS[:, j, 8:16]), reads=[bRS[j]], writes=[bRS[j]])
                    for h in range(8):
                        P.op("dve", lambda v, j=j, h=h, b=b: v.scalar_tensor_tensor(out=HN[b][:, h * 128:(h + 1) * 128], in0=HS[:, j, h * 128:(h + 1) * 128], scalar=RS[:, j, 16 + h:17 + h], in1=ONGs[:, h * 128:(h + 1) * 128], op0=ALU.mult, op1=ALU.mult),
                             reads=[bHS[j], bRS[j], bONG], writes=[bHN[b]])
                    P.op("dve", lambda g, b=b, tj=tj: g.tensor_tensor(out=YTM[:, tj - 2, :], in0=HN[b][:, :], in1=SG[b][:, :], op=ALU.mult), reads=[bHN[b], bSG[b]], writes=[bYTM[tj - 2]])
            P.flush()


NCORES = 8


def _consts_np():
    sel16 = np.zeros((NE, NE, 128), np.float32)
    for e in range(NE):
        sel16[e, e, :] = 1
    slotid = np.zeros((128, 4), np.float32)
    slotid[:, 0] = 32 + np.arange(128); slotid[:, 1] = 160 + np.arange(128); slotid[:, 3] = np.arange(128) % 32
    selq = np.zeros((NE, 4, 128), np.float32)
    for e in range(NE):
        selq[e, e // 4, (e % 4) * 32:(e % 4) * 32 + 32] = 1
    return {"c_idf": np.eye(128, dtype=np.float32), "c_iota": np.arange(512, dtype=np.float32), "c_sel16": sel16, "c_slotid": slotid, "c_selq": selq}


def build_mod():
    P = Prog()
    CT = P.dram("ct", [128, 8, 9], F32, "ExternalInput")
    MW = P.dram("mw", [4, D, 768], F32, "ExternalInput")
    MB = P.dram("mb", [4, 1, 768], F32, "ExternalInput")
    OUT = P.dram("modo", [4, 9, 768], F32, "ExternalOutput")
    with contextlib.ExitStack() as st:
        C_ = P.sb(st, "C_", [128, 8, 9], F32); bC = Buf()
        S_ = P.sb(st, "S_", [128, 8, 9], F32); bS = Buf()
        ON = P.sb(st, "ON", [1, 9], F32); bON = Buf()
        W = [P.sb(st, f"W{i}", [128, 768], F32) for i in range(3)]; bW = [Buf() for _ in range(3)]
        Bb = [P.sb(st, f"Bb{i}", [1, 768], F32) for i in range(2)]; bBb = [Buf() for _ in range(2)]
        O_ = [P.sb(st, f"O{i}", [9, 768], F32) for i in range(2)]; bO = [Buf() for _ in range(2)]
        PS_ = [P.ps(st, f"PS{i}") for i in range(4)]; bPS = [Buf(excl=True) for _ in range(4)]
        cin = Buf(); cout = Buf()
        P.dma(C_[:, :, :], CT[:, :, :], reads=[cin], writes=[bC])
        P.op("act", lambda a: a.activation(out=S_[:, :, :], in_=C_[:, :, :], func=AF.Silu), reads=[bC], writes=[bS])
        P.op("dve", lambda v: v.memset(ON[:, :], 1.0), writes=[bON])
        wi = 0
        for l in range(4):
            b = l % 2
            P.dma(Bb[b][:, :], MB[l], reads=[cin], writes=[bBb[b]])
            for k in range(8):
                w = wi % 3; wi += 1
                P.dma(W[w][:, :], MW[l, k * 128:(k + 1) * 128, :], reads=[cin], writes=[bW[w]])
                for cb, (c0, c1) in enumerate(((0, 512), (512, 768))):
                    P.op("pe", lambda t, k=k, w=w, b=b, cb=cb, c0=c0, c1=c1: t.matmul(PS_[b * 2 + cb][0:9, 0:c1 - c0], S_[:, k, :], W[w][:, c0:c1], start=(k == 0), stop=False),
                         reads=[bS, bW[w]], writes=[bPS[b * 2 + cb]])
            for cb, (c0, c1) in enumerate(((0, 512), (512, 768))):
                P.op("pe", lambda t, b=b, cb=cb, c0=c0, c1=c1: t.matmul(PS_[b * 2 + cb][0:9, 0:c1 - c0], ON[:, :], Bb[b][:, c0:c1], start=False, stop=True),
                     reads=[bON, bBb[b]], writes=[bPS[b * 2 + cb]])
                P.op("dve", lambda v, b=b, cb=cb, c0=c0, c1=c1: v.tensor_copy(out=O_[b][:, c0:c1], in_=PS_[b * 2 + cb][0:9, 0:c1 - c0]), reads=[bPS[b * 2 + cb]], writes=[bO[b]])
            P.dma(OUT[l], O_[b][:, :], reads=[bO[b]], writes=[cout])
        P.flush(final=True)
    return P


def phase_copy_x(P, Xin, X, bX):
    with contextlib.ExitStack() as st1:
        XC = [P.sb(st1, f"XC{i}", [128, D], F32) for i in range(2)]; bXC = [Buf(), Buf()]
        cin = Buf()
        for tt in range(NT):
            P.dma(XC[tt % 2][:, :], Xin[tt * 128:(tt + 1) * 128, :], reads=[cin], writes=[bXC[tt % 2]])
            P.dma(X[tt * 128:(tt + 1) * 128, :], XC[tt % 2][:, :], reads=[bXC[tt % 2]], writes=[bX[tt]])
        P.flush()


def build_A(i):
    P = Prog()
    C = {"idf": P.dram("c_idf", [128, 128], F32, "ExternalInput"), "iota": P.dram("c_iota", [512], F32, "ExternalInput")}
    Xin = P.dram("xin", [T, D], F32, "ExternalInput")
    MODS = P.dram("mods", [2, 6 * D], F32, "ExternalInput")
    NG1 = P.dram("ng1", [D], F32, "ExternalInput"); NG2 = P.dram("ng2", [D], F32, "ExternalInput")
    RW = P.dram("rw", [D, NE], F32, "ExternalInput")
    WO = P.dram("wo", [D, D], F32, "ExternalInput")
    X = P.dram("xmid", [T, D], F32, "ExternalOutput")
    XG = P.dram("xg", [NE, D, NSLOT], BF16, "ExternalOutput")
    GV = P.dram("gv", [NE, NSLOT], F32, "ExternalOutput")
    PM = P.dram("posmt", [NE, T], F32, "ExternalOutput")
    if i > 0:
        C["sel16"] = P.dram("c_sel16", [NE, NE, 128], F32, "ExternalInput"); C["slotid"] = P.dram("c_slotid", [128, 4], F32, "ExternalInput"); C["selq"] = P.dram("c_selq", [NE, 4, 128], F32, "ExternalInput")
        MODSP = P.dram("modsp", [2, 6 * D], F32, "ExternalInput")
        Yd = P.dram("y", [NE, NSLOT, D], BF16, "ExternalInput")
        PMin = P.dram("posmt_in", [NE, T], F32, "ExternalInput")
    with contextlib.ExitStack() as st:
        K = load_consts(P, st, C)
        bX = [Buf() for _ in range(NT)]
        if i == 0:
            phase_copy_x(P, Xin, X, bX)
        else:
            phase_pro(P, K, C, Yd, PMin, MODSP, Xin, X, bX)
        rn = lambda HT, bHT: phase_norm(P, K, X, bX, MODS, 0, NG1, HT, bHT)
        if i == 0:
            WQKV = P.dram("wqkv", [D, 3 * D], F32, "ExternalInput")
            TTE = P.dram("tte", [128, 16, 14, 64], F32, "ExternalInput"); TTO = P.dram("tto", [128, 16, 5, 64], F32, "ExternalInput")
            YATT = P.dram("yatt", [T, D], BF16); bY = [Buf() for _ in range(NT)]
            mixer_na(P, K, rn, WQKV, TTE, TTO, YATT, bY)
            def src(tt, dst, bdst):
                P.dma(dst[:, :], YATT[tt * 128:(tt + 1) * 128, :], reads=[bY[tt]], writes=[bdst])
            phase_outproj(P, K, src, WO, X, bX, MODS, 2 * D)
        elif i == 1:
            WIN = P.dram("win", [D, 672], F32, "ExternalInput"); WKS = P.dram("wks", [D, 96], F32, "ExternalInput")
            GQ = P.dram("gq", [128, 3], F32, "ExternalInput"); GKV = P.dram("gkv", [128, 2], F32, "ExternalInput")
            WQB = P.dram("wqb", [QR, 1536], F32, "ExternalInput"); WQS = P.dram("wqs", [QR, 1536], F32, "ExternalInput")
            WKVB = P.dram("wkvb", [KVR, 2048], F32, "ExternalInput"); CS = P.dram("cs", [32, 2, T], F32, "ExternalInput")
            with contextlib.ExitStack() as stm:
                YTM = P.sb(stm, "YTM", [128, NT, D], BF16); bYTM = [Buf() for _ in range(NT)]
                mixer_mla(P, K, rn, WIN, WKS, GQ, GKV, WQB, WQS, WKVB, CS, YTM, bYTM)
                def src(tt, dst, bdst):
                    P.op("pool", lambda g, tt=tt, dst=dst: g.tensor_copy(out=dst[:, :], in_=YTM[:, tt, :]), reads=[bYTM[tt]], writes=[bdst])
                phase_outproj(P, K, src, WO, X, bX, MODS, 2 * D)
        elif i == 2:
            WIN = P.dram("win", [D, 3072], F32, "ExternalInput")
            CW = P.dram("cw", [128, 24, 3], F32, "ExternalInput"); CB = P.dram("cb", [128, 24], F32, "ExternalInput")
            FW1 = P.dram("fw1", [33, 64], F32, "ExternalInput"); FW2 = P.dram("fw2", [64, 64], F32, "ExternalInput"); FW3 = P.dram("fw3", [64, 2048], F32, "ExternalInput")
            FB = P.dram("fb", [64, 4], F32, "ExternalInput"); FB3 = P.dram("fb3", [1, 2048], F32, "ExternalInput")
            SKIP = P.dram("skip", [1, 1024], F32, "ExternalInput"); DELTA = P.dram("delta", [1024], F32, "ExternalInput")
            TB = {}
            for tag, L in (("c", 256), ("l", 2048)):
                nt = L // 128
                TB[tag] = {"ze": P.dram("ze" + tag, [33, L], F32, "ExternalInput"), "negt": P.dram("negt" + tag, [128, nt], F32, "ExternalInput"), "wsc": P.dram("wsc" + tag, [128, nt], F32, "ExternalInput"),
                           "C": P.dram("C" + tag, [L, L], BF16, "ExternalInput"), "S": P.dram("S" + tag, [L, L], BF16, "ExternalInput"), "ST": P.dram("ST" + tag, [L, L], BF16, "ExternalInput")}
            X0D = P.dram("x0d", [D, T], BF16); YTD = P.dram("ytd", [D, T], BF16); bYTD = [Buf() for _ in range(8)]
            mixer_hyena(P, K, rn, WIN, CW, CB, FW1, FW2, FW3, FB, FB3, SKIP, DELTA, TB, X0D, YTD, bYTD)
            outproj_fm_dram(P, K, YTD, bYTD, WO, X, bX, MODS, 2 * D)
        else:
            WIN = P.dram("win", [D, 3104], F32, "ExternalInput")
            CW = P.dram("cw", [128, 8, 3], F32, "ExternalInput"); CB = P.dram("cb", [128, 8], F32, "ExternalInput")
            GB = P.dram("gb", [32], F32, "ExternalInput"); ONG = P.dram("ong", [D], F32, "ExternalInput")
            SEL8 = P.dram("sel8", [8, 8, 128], F32, "ExternalInput"); TRI = P.dram("tri", [128, 2, 128], F32, "ExternalInput")
            SIGO = P.dram("sigo", [T, D], BF16)
            with contextlib.ExitStack() as stm:
                YTM = P.sb(stm, "YTM", [128, 16, D], BF16); bYTM = [Buf() for _ in range(16)]
                mixer_mlstm(P, K, rn, WIN, CW, CB, GB, ONG, SEL8, TRI, SIGO, YTM, bYTM)
                def src(tt, dst, bdst):
                    P.op("pool", lambda g, tt=tt, dst=dst: g.tensor_copy(out=dst[:, :], in_=YTM[:, tt - 2, :]), reads=[bYTM[tt - 2]], writes=[bdst])
                phase_outproj(P, K, src, WO, X, bX, MODS, 2 * D, tiles=range(2, NT))
        with contextlib.ExitStack() as st2:
            HT = P.sb(st2, "HT2", [128, 8, T], BF16); bHT = [Buf() for _ in range(NT)]
            HTM = P.sb(st2, "HTM2", [128, NT, D], BF16); bHTM = [Buf() for _ in range(NT)]
            phase_norm(P, K, X, bX, MODS, 3 * D, NG2, HT, bHT, HTM, bHTM)
            phase_route(P, K, C, HT, bHT, HTM, bHTM, RW, XG, GV, PM)
        P.flush(final=True)
    return P


def build_Bprog():
    P = Prog()
    xgT = P.dram("xgT", [2, D, NS], BF16, "ExternalInput")
    gv = P.dram("gv", [2, 128, 18], F32, "ExternalInput")
    wg = P.dram("wg", [2, D, FF], F32, "ExternalInput")
    wu = P.dram("wu", [2, D, FF], F32, "ExternalInput")
    wd = P.dram("wd", [2, FF, D], F32, "ExternalInput")
    y = P.dram("y", [2, NS, D], BF16, "ExternalOutput")
    build_B(P, xgT, gv, wg, wu, wd, y, nexp=2)
    return P


def build_F():
    P = Prog()
    C = {"idf": P.dram("c_idf", [128, 128], F32, "ExternalInput"), "sel16": P.dram("c_sel16", [NE, NE, 128], F32, "ExternalInput"),
         "slotid": P.dram("c_slotid", [128, 4], F32, "ExternalInput"), "selq": P.dram("c_selq", [NE, 4, 128], F32, "ExternalInput")}
    Xin = P.dram("xin", [T, D], F32, "ExternalInput")
    MODSP = P.dram("modsp", [2, 6 * D], F32, "ExternalInput")
    Yd = P.dram("y", [NE, NSLOT, D], BF16, "ExternalInput")
    PMin = P.dram("posmt_in", [NE, T], F32, "ExternalInput")
    FNG = P.dram("fng", [D], F32, "ExternalInput")
    OUT = P.dram("out", [2048, D], F32, "ExternalOutput")
    X = P.dram("xfin", [T, D], F32)
    with contextlib.ExitStack() as st:
        K = load_consts(P, st, C)
        bX = [Buf() for _ in range(NT)]
        phase_pro(P, K, C, Yd, PMin, MODSP, Xin, X, bX)
        with contextlib.ExitStack() as st2:
            G = P.sb(st2, "f_G", [128, D], F32); bG = Buf()
            XT = [P.sb(st2, f"f_XT{i}", [128, D], F32) for i in range(2)]; bXT = [Buf() for _ in range(2)]
            OT = [P.sb(st2, f"f_OT{i}", [128, D], F32) for i in range(2)]; bOT = [Buf() for _ in range(2)]
            JK = P.sb(st2, "f_JK", [128, D], F32); bJK = Buf()
            SS = P.sb(st2, "f_SS", [128, 3 * NT], F32); bSS = [Buf() for _ in range(NT)]
            cin = Buf(); cout = Buf()
            P.dma(G[:, :], FNG.partition_broadcast(128), reads=[cin], writes=[bG])
            for tt in range(2, NT):
                b = tt % 2
                P.dma(XT[b][:, :], X[tt * 128:(tt + 1) * 128, :], reads=[bX[tt]], writes=[bXT[b]])
                P.op("act", lambda a, b=b, tt=tt: a.activation(out=JK[:, :], in_=XT[b][:, :], func=AF.Square, accum_out=SS[:, 3 * tt:3 * tt + 1]), reads=[bXT[b]], writes=[bJK, bSS[tt]])
                P.op("act", lambda a, tt=tt: a.activation(out=SS[:, 3 * tt + 1:3 * tt + 2], in_=SS[:, 3 * tt:3 * tt + 1], func=AF.Sqrt, scale=1.0 / D, bias=K["eps"][:, :]), reads=[bSS[tt], K["b_eps"]], writes=[bSS[tt]])
                P.op("dve", lambda v, tt=tt: v.reciprocal(out=SS[:, 3 * tt + 2:3 * tt + 3], in_=SS[:, 3 * tt + 1:3 * tt + 2]), reads=[bSS[tt]], writes=[bSS[tt]])
                P.op("dve", lambda v, b=b, tt=tt: v.scalar_tensor_tensor(out=OT[b][:, :], in0=XT[b][:, :], scalar=SS[:, 3 * tt + 2:3 * tt + 3], in1=G[:, :], op0=ALU.mult, op1=ALU.mult), reads=[bXT[b], bSS[tt], bG], writes=[bOT[b]])
                P.dma(OUT[(tt - 2) * 128:(tt - 1) * 128, :], OT[b][:, :], reads=[bOT[b]], writes=[cout])
        P.flush(final=True)
    return P


def _launch(P, maps):
    res = run_bass_kernel_spmd(P.nc, maps, core_ids=list(range(NCORES)))
    return res.results


def kernel(**inp):
    f32 = lambda a: np.ascontiguousarray(np.asarray(a, dtype=np.float32))
    x = f32(inp["x"]); c = f32(inp["c"]); ctx = f32(inp["ctx"]); c_ctx = f32(inp["c_ctx"])
    KC = _consts_np()
    cvec = np.concatenate([c, c_ctx[None, :]], 0)
    ct = np.ascontiguousarray(cvec.T.reshape(8, 128, 9).transpose(1, 0, 2))
    mod_w = inp["mod_w"]; mod_b = inp["mod_b"]
    Pm = build_mod()
    maps = [{"ct": ct, "mw": f32(mod_w[:, :, j * 768:(j + 1) * 768]), "mb": f32(mod_b[:, None, j * 768:(j + 1) * 768])} for j in range(NCORES)]
    r = _launch(Pm, maps)
    modall = np.concatenate([np.asarray(r[j]["modo"]) for j in range(NCORES)], axis=2)
    def mods_for(l, b):
        return np.ascontiguousarray(np.stack([modall[l, b], modall[l, 8]], 0))
    PB_ = build_Bprog()
    xcur = [np.ascontiguousarray(np.concatenate([ctx[b], x[b]], 0)) for b in range(NCORES)]
    ycur = None; pmcur = None
    for i in range(4):
        PA = build_A(i)
        maps = []
        if i == 0:
            tte, tto = na_tables(f32(inp["na_rpb"][0]))
            extra = {"wqkv": f32(inp["na_w_qkv"][0]), "tte": tte, "tto": tto, "wo": f32(inp["na_w_o"][0])}
        elif i == 1:
            wks, wqs, gq, gkv, cs = mla_host(f32(inp["mla_w_in"][0]), f32(inp["mla_w_q_b"][0]), f32(inp["mla_q_norm_g"][0]), f32(inp["mla_kv_norm_g"][0]))
            extra = {"win": f32(inp["mla_w_in"][0]), "wks": wks, "gq": gq, "gkv": gkv, "wqb": f32(inp["mla_w_q_b"][0]), "wqs": wqs,
                     "wkvb": f32(inp["mla_w_kv_b"][0]), "cs": cs, "wo": f32(inp["mla_w_o"][0])}
        elif i == 2:
            H = hy_host(f32(inp["hy_conv_w"][0]), f32(inp["hy_conv_b"][0]), f32(inp["hy_f_b1"][0]), f32(inp["hy_f_b2"][0]), f32(inp["hy_f_b3"][0]), f32(inp["hy_sin_freq"][0]), f32(inp["hy_skip"][0]))
            extra = {"win": f32(inp["hy_w_in"][0]), "cw": H["cw"], "cb": H["cb"], "fw1": f32(inp["hy_f_w1"][0]), "fw2": f32(inp["hy_f_w2"][0]), "fw3": f32(inp["hy_f_w3"][0]),
                     "fb": H["fb"], "fb3": f32(inp["hy_f_b3"][0])[None, :], "skip": f32(inp["hy_skip"][0])[None, :], "delta": H["delta"], "wo": f32(inp["hy_w_o"][0])}
            for tag in ("c", "l"):
                for kk in ("ze", "negt", "wsc", "C", "S", "ST"):
                    extra[kk + tag] = H[kk + tag]
        else:
            cw, cb, sel8, tri = ml_host(f32(inp["ml_conv_w"][0]), f32(inp["ml_conv_b"][0]))
            extra = {"win": f32(inp["ml_w_in"][0]), "cw": cw, "cb": cb, "gb": f32(inp["ml_gate_b"][0]), "ong": f32(inp["ml_out_norm_g"][0]), "sel8": sel8, "tri": tri, "wo": f32(inp["ml_w_o"][0])}
        for b in range(NCORES):
            m = {"c_idf": KC["c_idf"], "c_iota": KC["c_iota"], "xin": xcur[b], "mods": mods_for(i, b), "ng1": f32(inp["norm_mix_g"][i]), "ng2": f32(inp["norm_ffn_g"][i]),
                 "rw": f32(inp["router_w"][i])}
            m.update(extra)
            if i > 0:
                m.update({"c_sel16": KC["c_sel16"], "c_slotid": KC["c_slotid"], "c_selq": KC["c_selq"], "modsp": mods_for(i - 1, b), "y": ycur[b], "posmt_in": pmcur[b]})
            maps.append(m)
        r = _launch(PA, maps)
        xcur = [np.asarray(r[b]["xmid"]) for b in range(NCORES)]
        pmcur = [np.asarray(r[b]["posmt"]) for b in range(NCORES)]
        xg = [np.asarray(r[b]["xg"]) for b in range(NCORES)]
        gvv = [np.asarray(r[b]["gv"]) for b in range(NCORES)]
        maps = []
        for j in range(NCORES):
            xgT = np.ascontiguousarray(np.stack([np.concatenate([xg[b][2 * j + el] for b in range(NCORES)], axis=1) for el in range(2)], 0))
            gvj = np.stack([np.concatenate([gvv[b][2 * j + el] for b in range(NCORES)], 0) for el in range(2)], 0)
            gvl = np.ascontiguousarray(gvj.reshape(2, 18, 128).transpose(0, 2, 1))
            maps.append({"xgT": xgT, "gv": gvl, "wg": f32(inp["moe_w_gate"][i, 2 * j:2 * j + 2]), "wu": f32(inp["moe_w_up"][i, 2 * j:2 * j + 2]),
                         "wd": f32(inp["moe_w_down"][i, 2 * j:2 * j + 2])})
        r = _launch(PB_, maps)
        yb = [np.asarray(r[j]["y"]) for j in range(NCORES)]
        ycur = [np.ascontiguousarray(np.concatenate([yb[j][:, b * NSLOT:(b + 1) * NSLOT, :] for j in range(NCORES)], 0)) for b in range(NCORES)]
    PF_ = build_F()
    maps = [{"c_idf": KC["c_idf"], "c_sel16": KC["c_sel16"], "c_slotid": KC["c_slotid"], "c_selq": KC["c_selq"], "xin": xcur[b], "modsp": mods_for(3, b), "y": ycur[b], "posmt_in": pmcur[b],
             "fng": f32(inp["final_norm_g"])} for b in range(NCORES)]
    r = _launch(PF_, maps)
    return np.stack([np.asarray(r[b]["out"]) for b in range(NCORES)], 0).astype(np.float32)
```

```python
import contextlib
import math
import numpy as np
import concourse.bass as bass
import concourse.mybir as mybir
from concourse.bass_utils import run_bass_kernel_spmd

F32 = mybir.dt.float32
BF16 = mybir.dt.bfloat16
I32 = mybir.dt.int32
ALU = mybir.AluOpType
AF = mybir.ActivationFunctionType
AX = mybir.AxisListType
NPBF = mybir.dt.np(BF16)


class Buf:
    __slots__ = ("name", "w", "rs", "excl")

    def __init__(self, name="", excl=False):
        self.name = name
        self.w = None
        self.rs = []
        self.excl = excl


class Prog:
    EPOCH = 30000
    NRING = 6

    def __init__(self):
        self.nc = bass.Bass("TRN2", target_bir_lowering=False)
        nc = self.nc
        self.engs = ["pe", "act", "dve", "pool", "sp"]
        self.lists = {e: [] for e in self.engs}
        self.cnt = {e: 0 for e in self.engs}
        self.nsem = 0
        self.sem = {e: self._newsem(e) for e in ("pe", "act", "dve", "pool")}
        self.seen = {e: {} for e in self.engs}
        self.ring = {q: [self._newsem(f"r{q}{i}") for i in range(self.NRING)] for q in ("sp", "pool", "act")}
        self.ring_n = {q: [0] * self.NRING for q in self.ring}
        self.ring_i = {q: 0 for q in self.ring}
        self.nbuf = 0
        self.ninstr = 0
        self.limit = 1 << 60

    def _newsem(self, name):
        self.nsem += 1
        return self.nc.alloc_semaphore(name=f"s{self.nsem}_{name}")

    def dram(self, name, shape, dtype, kind="Internal"):
        return self.nc.dram_tensor(name, list(shape), dtype, kind=kind).ap()

    def sb(self, stack, name, shape, dtype):
        self.nbuf += 1
        return stack.enter_context(self.nc.sbuf_tensor(f"{name}_{self.nbuf}", list(shape), dtype))

    def ps(self, stack, name, shape=(128, 512), dtype=F32):
        self.nbuf += 1
        return stack.enter_context(self.nc.psum_tensor(f"{name}_{self.nbuf}", list(shape), dtype))

    def _deps(self, eng, reads, writes):
        evs = {}

        def add(ev, kind):
            if ev is None:
                return
            s, v, src = ev
            if src == eng:
                if eng == "pe":
                    return
            k = id(s)
            if self.seen[eng].get(k, -1) >= v:
                return
            if k not in evs or evs[k][1] < v:
                evs[k] = (s, v)

        for b in reads:
            add(b.w, "raw")
        for b in writes:
            add(b.w, "waw")
            for r in b.rs:
                add(r, "war")
        out = []
        for k, (s, v) in evs.items():
            self.seen[eng][k] = v
            out.append((s, v))
        return out

    def _commit(self, ev, reads, writes):
        for b in reads:
            b.rs.append(ev)
            if len(b.rs) > 64:
                best = {}
                for (s, v, src) in b.rs:
                    k = (id(s), src)
                    if k not in best or best[k][1] < v:
                        best[k] = (s, v, src)
                b.rs = list(best.values())
        for b in writes:
            b.w = ev
            b.rs = []

    def op(self, eng, fn, reads=(), writes=()):
        if self.ninstr >= self.limit:
            return
        ex = [b for b in reads if b.excl]
        if ex:
            writes = list(writes) + [b for b in ex if b not in writes]
        waits = self._deps(eng, reads, writes)
        if self.cnt[eng] >= self.EPOCH:
            self.sem[eng] = self._newsem(eng)
            self.cnt[eng] = 0
        self.cnt[eng] += 1
        s = self.sem[eng]
        ev = (s, self.cnt[eng], eng)
        self.lists[eng].append((waits, fn, s, 1))
        self._commit(ev, reads, writes)
        self.ninstr += 1

    def dma(self, out, in_, reads=(), writes=(), q="sp", **kw):
        if self.ninstr >= self.limit:
            return
        eng = q
        i = self.ring_i[q]
        self.ring_i[q] = (i + 1) % self.NRING
        s = self.ring[q][i]
        n = self.ring_n[q][i]
        waits = []
        if n > 0 and self.seen[eng].get(id(s), -1) < 16 * n:
            waits.append((s, 16 * n))
            self.seen[eng][id(s)] = 16 * n
        waits += self._deps(eng, reads, writes)
        self.ring_n[q][i] = n + 1
        ev = (s, 16 * (n + 1), "dma")
        self.lists[eng].append((waits, (lambda e, out=out, in_=in_, kw=kw: e.dma_start(out=out, in_=in_, **kw)), s, 16))
        self._commit(ev, reads, writes)
        self.ninstr += 1

    def barrier(self):
        targets = []
        for e in ("pe", "act", "dve", "pool"):
            if self.cnt[e] > 0:
                targets.append((self.sem[e], self.cnt[e], e))
        for q in self.ring:
            for i, s in enumerate(self.ring[q]):
                n = self.ring_n[q][i]
                if n > 0:
                    targets.append((s, 16 * n, "dma"))
        for eng in self.engs:
            waits = []
            for (s, v, src) in targets:
                if self.seen[eng].get(id(s), -1) >= v:
                    continue
                self.seen[eng][id(s)] = v
                waits.append((s, v))
            if waits:
                self.lists[eng].append((waits, None, None, 0))

    def flush(self, final=False):
        nc = self.nc
        self.barrier()
        if final:
            for q in self.ring:
                for i, s in enumerate(self.ring[q]):
                    n = self.ring_n[q][i]
                    if n > 0 and self.seen[q].get(id(s), -1) < 16 * n:
                        self.lists[q].append(([(s, 16 * n)], None, None, 0))
                        self.seen[q][id(s)] = 16 * n
        lists = self.lists
        self.lists = {e: [] for e in self.engs}

        def emit(e, items):
            for waits, fn, s, inc in items:
                for (ws, wv) in waits:
                    e.wait_ge(ws, wv)
                if fn is not None:
                    fn(e).then_inc(s, inc)

        with nc.Block() as block:
            @block.tensor
            def _(e):
                emit(e, lists["pe"])

            @block.scalar
            def _(e):
                emit(e, lists["act"])

            @block.vector
            def _(e):
                emit(e, lists["dve"])

            @block.gpsimd
            def _(e):
                emit(e, lists["pool"])

            @block.sync
            def _(e):
                emit(e, lists["sp"])


def run(prog, in_maps, trace=False):
    res = run_bass_kernel_spmd(prog.nc, in_maps, core_ids=list(range(len(in_maps))), trace=trace)
    return res


T = 2304
NT = 18
D = 1024
NE = 16
NSLOT = 288
EPS = 1e-6


def load_consts(P, st, C):
    K = {}
    K["idf"] = P.sb(st, "idf", [128, 128], F32); K["b_idf"] = Buf("idf")
    K["idb"] = P.sb(st, "idb", [128, 128], BF16); K["b_idb"] = Buf("idb")
    K["eps"] = P.sb(st, "epsc", [128, 1], F32); K["b_eps"] = Buf("eps")
    ci = Buf("cin")
    P.dma(K["idf"][:, :], C["idf"][:, :], reads=[ci], writes=[K["b_idf"]])
    P.op("dve", lambda v: v.tensor_copy(out=K["idb"][:, :], in_=K["idf"][:, :]), reads=[K["b_idf"]], writes=[K["b_idb"]])
    P.op("dve", lambda v: v.memset(K["eps"][:, :], EPS), writes=[K["b_eps"]])
    return K


def phase_norm(P, K, X, bX, MODS, sh_off, norm_g, HT, bHT, HTM=None, bHTM=None, tiles=range(NT)):
    with contextlib.ExitStack() as st:
        G = P.sb(st, "n_G", [128, D], F32); bG = Buf()
        SC = P.sb(st, "n_SC", [128, 2, D], F32); bSC = Buf()
        AV = P.sb(st, "n_AV", [128, 2, D], F32); bAV = Buf()
        BV = P.sb(st, "n_BV", [128, 2, D], F32); bBV = Buf()
        XT = [P.sb(st, f"n_XT{i}", [128, D], F32) for i in range(2)]; bXT = [Buf() for _ in range(2)]
        JK = P.sb(st, "n_JK", [128, D], F32); bJK = Buf()
        TMP = [P.sb(st, f"n_TMP{i}", [128, D], F32) for i in range(2)]; bTMP = [Buf() for _ in range(2)]
        SS = P.sb(st, "n_SS", [128, 3 * NT], F32); bSS = [Buf() for _ in range(NT)]
        HL = [P.sb(st, f"n_HL{i}", [128, D], BF16) for i in range(2)]; bHL = [Buf() for _ in range(2)]
        PT = [P.ps(st, f"n_PT{i}", [128, 8, 128], BF16) for i in range(2)]; bPT = [Buf(excl=True) for _ in range(2)]
        cin = Buf()
        P.dma(G[:, :], norm_g.partition_broadcast(128), reads=[cin], writes=[bG])
        for r in range(2):
            P.dma(SC[:, r, :], MODS[r, sh_off + D:sh_off + 2 * D].partition_broadcast(128), reads=[cin], writes=[bSC])
            P.dma(BV[:, r, :], MODS[r, sh_off:sh_off + D].partition_broadcast(128), reads=[cin], writes=[bBV])
        for r in range(2):
            P.op("dve", lambda v, r=r: v.scalar_tensor_tensor(out=AV[:, r, :], in0=SC[:, r, :], scalar=1.0, in1=G[:, :], op0=ALU.add, op1=ALU.mult),
                 reads=[bSC, bG], writes=[bAV])
        for i, tt in enumerate(tiles):
            r = 1 if tt < 2 else 0
            b = i % 2
            xt = XT[b]
            P.dma(xt[:, :], X[tt * 128:(tt + 1) * 128, :], reads=[bX[tt]], writes=[bXT[b]])
            P.op("act", lambda a, xt=xt, tt=tt: a.activation(out=JK[:, :], in_=xt[:, :], func=AF.Square, accum_out=SS[:, 3 * tt:3 * tt + 1]),
                 reads=[bXT[b]], writes=[bJK, bSS[tt]])
            P.op("act", lambda a, tt=tt: a.activation(out=SS[:, 3 * tt + 1:3 * tt + 2], in_=SS[:, 3 * tt:3 * tt + 1], func=AF.Sqrt, scale=1.0 / D, bias=K["eps"][:, :]),
                 reads=[bSS[tt], K["b_eps"]], writes=[bSS[tt]])
            P.op("dve", lambda v, tt=tt: v.reciprocal(out=SS[:, 3 * tt + 2:3 * tt + 3], in_=SS[:, 3 * tt + 1:3 * tt + 2]), reads=[bSS[tt]], writes=[bSS[tt]])
            P.op("dve", lambda v, xt=xt, tt=tt, r=r, b=b: v.scalar_tensor_tensor(out=TMP[b][:, :], in0=xt[:, :], scalar=SS[:, 3 * tt + 2:3 * tt + 3], in1=AV[:, r, :], op0=ALU.mult, op1=ALU.mult),
                 reads=[bXT[b], bSS[tt], bAV], writes=[bTMP[b]])
            if HTM is not None:
                hdst = HTM[:, tt, :]; bh = bHTM[tt]
            else:
                hdst = HL[b][:, :]; bh = bHL[b]
            P.op("dve", lambda g, hdst=hdst, r=r, b=b: g.tensor_tensor(out=hdst, in0=TMP[b][:, :], in1=BV[:, r, :], op=ALU.add),
                 reads=[bTMP[b], bBV], writes=[bh])
            for k in range(8):
                P.op("pe", lambda t, k=k, hdst=hdst, b=b: t.transpose(PT[b][:, k, :], hdst[:, k * 128:(k + 1) * 128], K["idb"][:, :]),
                     reads=[bh, K["b_idb"]], writes=[bPT[b]])
            if i % 2 == 0:
                P.op("act", lambda a, tt=tt, b=b: a.copy(out=HT[:, :, tt * 128:(tt + 1) * 128], in_=PT[b][:, :, :]), reads=[bPT[b]], writes=[bHT[tt]])
            else:
                P.op("dve", lambda v, tt=tt, b=b: v.tensor_copy(out=HT[:, :, tt * 128:(tt + 1) * 128], in_=PT[b][:, :, :]), reads=[bPT[b]], writes=[bHT[tt]])
        P.flush()


def phase_outproj(P, K, src, WO, X, bX, MODS, g_off, tiles=range(NT), mode="tm", norm2=None):
    with contextlib.ExitStack() as st:
        WOb = P.sb(st, "o_WO", [128, 8, D], BF16); bWO = [Buf() for _ in range(8)]
        STG = [P.sb(st, f"o_stg{i}", [128, D], F32) for i in range(2)]; bSTG = [Buf() for _ in range(2)]
        G1 = P.sb(st, "o_G1", [128, 2, D], F32); bG1 = Buf()
        XT = [P.sb(st, f"o_XT{i}", [128, D], F32) for i in range(2)]; bXT = [Buf() for _ in range(2)]
        TMP = [P.sb(st, f"o_TMP{i}", [128, 512], F32) for i in range(2)]; bTMP = [Buf() for _ in range(2)]
        YL = [P.sb(st, f"o_YL{i}", [128, D], BF16) for i in range(2)]; bYL = [Buf() for _ in range(2)]
        YTt = [P.sb(st, f"o_YTt{i}", [128, 8, 128], BF16) for i in range(2)]; bYTt = [Buf() for _ in range(2)]
        PT = [P.ps(st, f"o_PT{i}", [128, 8, 128], BF16) for i in range(2)]; bPT = [Buf(excl=True) for _ in range(2)]
        PO = [P.ps(st, f"o_PO{i}") for i in range(2)]; bPO = [Buf(excl=True) for _ in range(2)]
        cin = Buf()
        if norm2 is not None:
            n_off, n_g, nHT, nbHT, nHTM, nbHTM = norm2
            nG = P.sb(st, "on_G", [128, D], F32); nSC = P.sb(st, "on_SC", [128, 2, D], F32); nbC = Buf()
            nAV = P.sb(st, "on_AV", [128, 2, D], F32); nBV = P.sb(st, "on_BV", [128, 2, D], F32); nbAV = Buf()
            nJK = P.sb(st, "on_JK", [128, D], F32); nbJK = Buf()
            nTMP = [P.sb(st, f"on_TMP{i}", [128, D], F32) for i in range(2)]; nbTMP = [Buf() for _ in range(2)]
            nSS = P.sb(st, "on_SS", [128, 3 * NT], F32); nbSS = [Buf() for _ in range(NT)]
            nPT = [P.ps(st, f"on_PT{i}", [128, 8, 128], BF16) for i in range(2)]; nbPT = [Buf(excl=True) for _ in range(2)]
            P.dma(nG[:, :], n_g.partition_broadcast(128), reads=[cin], writes=[nbC])
            for r in range(2):
                P.dma(nSC[:, r, :], MODS[r, n_off + D:n_off + 2 * D].partition_broadcast(128), reads=[cin], writes=[nbC])
                P.dma(nBV[:, r, :], MODS[r, n_off:n_off + D].partition_broadcast(128), reads=[cin], writes=[nbAV])
            for r in range(2):
                P.op("dve", lambda v, r=r: v.scalar_tensor_tensor(out=nAV[:, r, :], in0=nSC[:, r, :], scalar=1.0, in1=nG[:, :], op0=ALU.add, op1=ALU.mult), reads=[nbC], writes=[nbAV])
        for k in range(8):
            s = k % 2
            P.dma(STG[s][:, :], WO[k * 128:(k + 1) * 128, :], reads=[cin], writes=[bSTG[s]])
            if k % 2 == 0:
                P.op("dve", lambda g, k=k, s=s: g.tensor_copy(out=WOb[:, k, :], in_=STG[s][:, :]), reads=[bSTG[s]], writes=[bWO[k]])
            else:
                P.op("act", lambda a, k=k, s=s: a.copy(out=WOb[:, k, :], in_=STG[s][:, :]), reads=[bSTG[s]], writes=[bWO[k]])
        for r in range(2):
            P.dma(G1[:, r, :], MODS[r, g_off:g_off + D].partition_broadcast(128), reads=[cin], writes=[bG1])
        pi = 0
        for i, tt in enumerate(tiles):
            r = 1 if tt < 2 else 0
            b = i % 2
            if mode == "tm":
                src(tt, YL[b], bYL[b])
                for k in range(8):
                    P.op("pe", lambda t, k=k, b=b: t.transpose(PT[b][:, k, :], YL[b][:, k * 128:(k + 1) * 128], K["idb"][:, :]),
                         reads=[bYL[b], K["b_idb"]], writes=[bPT[b]])
                P.op("act", lambda a, b=b: a.copy(out=YTt[b][:, :, :], in_=PT[b][:, :, :]), reads=[bPT[b]], writes=[bYTt[b]])
                lhs = lambda k, b=b: YTt[b][:, k, :]
                blhs = [bYTt[b]]
            else:
                YT, bYT = src
                lhs = lambda k, tt=tt: YT[:, k, tt * 128:(tt + 1) * 128]
                blhs = [bYT[tt]]
            P.dma(XT[b][:, :], X[tt * 128:(tt + 1) * 128, :], reads=[bX[tt]], writes=[bXT[b]])
            for dh in range(2):
                p = pi % 2; pi += 1
                for k in range(8):
                    P.op("pe", lambda t, k=k, p=p, dh=dh, lhs=lhs: t.matmul(PO[p][:, :], lhs(k), WOb[:, k, dh * 512:(dh + 1) * 512], start=(k == 0), stop=(k == 7)),
                         reads=blhs + [bWO[k]], writes=[bPO[p]])
                P.op("dve", lambda v, p=p, r=r, dh=dh: v.tensor_tensor(out=TMP[p][:, :], in0=PO[p][:, :], in1=G1[:, r, dh * 512:(dh + 1) * 512], op=ALU.mult),
                     reads=[bPO[p], bG1], writes=[bTMP[p]])
                P.op("dve", lambda g, p=p, b=b, dh=dh: g.tensor_tensor(out=XT[b][:, dh * 512:(dh + 1) * 512], in0=TMP[p][:, :], in1=XT[b][:, dh * 512:(dh + 1) * 512], op=ALU.add),
                     reads=[bTMP[p], bXT[b]], writes=[bXT[b]])
            P.dma(X[tt * 128:(tt + 1) * 128, :], XT[b][:, :], reads=[bXT[b]], writes=[bX[tt]])
            if norm2 is not None:
                P.op("act", lambda a, b=b, tt=tt: a.activation(out=nJK[:, :], in_=XT[b][:, :], func=AF.Square, accum_out=nSS[:, 3 * tt:3 * tt + 1]), reads=[bXT[b]], writes=[nbJK, nbSS[tt]])
                P.op("act", lambda a, tt=tt: a.activation(out=nSS[:, 3 * tt + 1:3 * tt + 2], in_=nSS[:, 3 * tt:3 * tt + 1], func=AF.Sqrt, scale=1.0 / D, bias=K["eps"][:, :]), reads=[nbSS[tt], K["b_eps"]], writes=[nbSS[tt]])
                P.op("dve", lambda v, tt=tt: v.reciprocal(out=nSS[:, 3 * tt + 2:3 * tt + 3], in_=nSS[:, 3 * tt + 1:3 * tt + 2]), reads=[nbSS[tt]], writes=[nbSS[tt]])
                P.op("dve", lambda v, b=b, tt=tt, r=r: v.scalar_tensor_tensor(out=nTMP[b][:, :], in0=XT[b][:, :], scalar=nSS[:, 3 * tt + 2:3 * tt + 3], in1=nAV[:, r, :], op0=ALU.mult, op1=ALU.mult),
                     reads=[bXT[b], nbSS[tt], nbAV], writes=[nbTMP[b]])
                P.op("dve", lambda v, b=b, tt=tt, r=r: v.tensor_tensor(out=nHTM[:, tt, :], in0=nTMP[b][:, :], in1=nBV[:, r, :], op=ALU.add), reads=[nbTMP[b], nbAV], writes=[nbHTM[tt]])
                for k in range(8):
                    P.op("pe", lambda t, k=k, b=b, tt=tt: t.transpose(nPT[b][:, k, :], nHTM[:, tt, k * 128:(k + 1) * 128], K["idb"][:, :]), reads=[nbHTM[tt], K["b_idb"]], writes=[nbPT[b]])
                P.op("act", lambda a, tt=tt, b=b: a.copy(out=nHT[:, :, tt * 128:(tt + 1) * 128], in_=nPT[b][:, :, :]), reads=[nbPT[b]], writes=[nbHT[tt]])
        P.flush()


def phase_route(P, K, C, HT, bHT, HTM, bHTM, RW, XG, GV, POSMT_out, dbg=None):
    with contextlib.ExitStack() as st:
        RWf = P.sb(st, "r_RWf", [128, 8, NE], F32); bRWf = Buf()
        RWb = P.sb(st, "r_RWb", [128, 8, NE], BF16); bRWb = Buf()
        AFF = P.sb(st, "r_AFF", [128, NT, NE], F32); bAFF = [Buf() for _ in range(NT)]
        AHI = P.sb(st, "r_AHI", [128, NT, NE], BF16)
        ALO = P.sb(st, "r_ALO", [128, NT, NE], BF16)
        AT32 = P.sb(st, "r_AT32", [128, NT, NE], F32)
        SM = P.sb(st, "r_SM", [128, NT, 4], F32); bSM = [Buf() for _ in range(NT)]
        EX = P.sb(st, "r_EX", [128, NT, NE], F32)
        AFFT = P.sb(st, "r_AFFT", [NE, T], F32); bAFFT = Buf()
        W = P.sb(st, "r_W", [NE, T], F32); bW = [Buf(), Buf()]
        M8 = P.sb(st, "r_M8", [NE, 8 * 36], F32); bM8 = [Buf(), Buf()]
        CA = P.sb(st, "r_CA", [NE, T], F32); bCA = [Buf(), Buf()]
        CB = P.sb(st, "r_CB", [NE, T], F32); bCB = [Buf(), Buf()]
        MASK = P.sb(st, "r_MASK", [NE, T], F32); bMASK = [Buf(), Buf()]
        POSM = P.sb(st, "r_POSM", [128, NT, NE], F32); bPOSM = [Buf() for _ in range(NT)]
        IOTA = P.sb(st, "r_IOTA", [128, NSLOT], F32); bIOTA = Buf()
        PSEL = [P.sb(st, f"r_PSEL{i}", [128, NT, 256], BF16) for i in range(2)]; bPSEL = [[Buf() for _ in range(NT)] for _ in range(2)]
        XGT = [P.sb(st, f"r_XGT{i}", [128, 8, NSLOT], BF16) for i in range(2)]; bXGT = [Buf() for _ in range(2)]
        GVR = [P.sb(st, f"r_GVR{i}", [1, NSLOT], F32) for i in range(2)]; bGVR = [Buf() for _ in range(2)]
        PL = [P.ps(st, f"r_PL{i}") for i in range(2)]; bPL = [Buf(excl=True) for _ in range(2)]
        PG = [P.ps(st, f"r_PG{i}") for i in range(3)]; bPG = [Buf(excl=True) for _ in range(3)]
        PV = [P.ps(st, f"r_PV{i}") for i in range(2)]; bPV = [Buf(excl=True) for _ in range(2)]
        cin = Buf(); cout = Buf()
        P.dma(RWf[:, :, :], RW.rearrange("(c p) e -> p c e", p=128), reads=[cin], writes=[bRWf])
        P.op("dve", lambda v: v.tensor_copy(out=RWb[:, :, :], in_=RWf[:, :, :]), reads=[bRWf], writes=[bRWb])
        P.dma(IOTA[:, :], C["iota"][0:NSLOT].partition_broadcast(128), reads=[cin], writes=[bIOTA])
        for tt in range(NT):
            p = tt % 2
            for k in range(8):
                P.op("pe", lambda t, k=k, tt=tt, p=p: t.matmul(PL[p][:, 0:NE], HT[:, k, tt * 128:(tt + 1) * 128], RWb[:, k, :], start=(k == 0), stop=(k == 7)),
                     reads=[bHT[tt], bRWb], writes=[bPL[p]])
            P.op("dve", lambda v, tt=tt, p=p: v.reduce_max(out=SM[:, tt, 0:1], in_=PL[p][:, 0:NE], axis=AX.X), reads=[bPL[p]], writes=[bSM[tt]])
            P.op("dve", lambda v, tt=tt: v.tensor_scalar(out=SM[:, tt, 1:2], in0=SM[:, tt, 0:1], scalar1=-1.0, scalar2=None, op0=ALU.mult), reads=[bSM[tt]], writes=[bSM[tt]])
            P.op("act", lambda a, tt=tt, p=p: a.activation(out=EX[:, tt, :], in_=PL[p][:, 0:NE], func=AF.Exp, bias=SM[:, tt, 1:2], accum_out=SM[:, tt, 2:3]),
                 reads=[bPL[p], bSM[tt]], writes=[bAFF[tt], bSM[tt]])
            P.op("dve", lambda v, tt=tt: v.reciprocal(out=SM[:, tt, 3:4], in_=SM[:, tt, 2:3]), reads=[bSM[tt]], writes=[bSM[tt]])
            P.op("dve", lambda v, tt=tt: v.tensor_scalar(out=AFF[:, tt, :], in0=EX[:, tt, :], scalar1=SM[:, tt, 3:4], scalar2=None, op0=ALU.mult), reads=[bSM[tt], bAFF[tt]], writes=[bAFF[tt]])
            P.op("dve", lambda v, tt=tt: v.tensor_copy(out=AHI[:, tt, :], in_=AFF[:, tt, :]), reads=[bAFF[tt]], writes=[bAFF[tt]])
            P.op("dve", lambda v, tt=tt: v.tensor_copy(out=AT32[:, tt, :], in_=AHI[:, tt, :]), reads=[bAFF[tt]], writes=[bAFF[tt]])
            P.op("dve", lambda v, tt=tt: v.tensor_tensor(out=ALO[:, tt, :], in0=AFF[:, tt, :], in1=AT32[:, tt, :], op=ALU.subtract), reads=[bAFF[tt]], writes=[bAFF[tt]])
            q = tt % 3
            P.op("pe", lambda t, tt=tt, q=q: t.transpose(PG[q][0:NE, 0:128], AFF[:, tt, :], K["idf"][:, :]), reads=[bAFF[tt], K["b_idf"]], writes=[bPG[q]])
            P.op("act", lambda a, tt=tt, q=q: a.copy(out=AFFT[:, tt * 128:(tt + 1) * 128], in_=PG[q][0:NE, 0:128]), reads=[bPG[q]], writes=[bAFFT])
        segs = [(0, 256, 32, 0.0), (256, T, 256, 32.0)]
        for si, (a0, a1, kk, base) in enumerate(segs):
            eng = "dve"
            P.op(eng, lambda v, a0=a0, a1=a1: v.tensor_copy(out=W[:, a0:a1], in_=AFFT[:, a0:a1]), reads=[bAFFT], writes=[bW[si]])
            nr = kk // 8
            for rnd in range(nr):
                mo = (si * 32 + rnd) * 8 if si == 0 else (4 + rnd) * 8
                P.op("dve", lambda v, a0=a0, a1=a1, mo=mo: v.max(out=M8[:, mo:mo + 8], in_=W[:, a0:a1]), reads=[bW[si]], writes=[bM8[si]])
                if rnd < nr - 1:
                    P.op("dve", lambda v, a0=a0, a1=a1, mo=mo: v.match_replace(out=W[:, a0:a1], in_to_replace=M8[:, mo:mo + 8], in_values=W[:, a0:a1], imm_value=-1.0),
                         reads=[bW[si], bM8[si]], writes=[bW[si]])
            thr = M8[:, mo + 7:mo + 8]
            P.op("dve", lambda v, a0=a0, a1=a1, thr=thr: v.tensor_scalar(out=MASK[:, a0:a1], in0=AFFT[:, a0:a1], scalar1=thr, scalar2=None, op0=ALU.is_ge),
                 reads=[bAFFT, bM8[si]], writes=[bMASK[si]])
            n = a1 - a0
            src, bsrc, dst, bdst = MASK, bMASK, CA, bCA
            sh = 1
            first = True
            while sh < n:
                P.op("dve", lambda v, src=src, dst=dst, sh=sh, a0=a0, a1=a1: v.tensor_tensor(out=dst[:, a0 + sh:a1], in0=src[:, a0 + sh:a1], in1=src[:, a0:a1 - sh], op=ALU.add),
                     reads=[bsrc[si]], writes=[bdst[si]])
                P.op("pool", lambda g, src=src, dst=dst, sh=sh, a0=a0: g.tensor_copy(out=dst[:, a0:a0 + sh], in_=src[:, a0:a0 + sh]),
                     reads=[bsrc[si]], writes=[bdst[si]])
                if first:
                    src, bsrc, dst, bdst = CA, bCA, CB, bCB
                    first = False
                else:
                    src, bsrc, dst, bdst = dst, bdst, src, bsrc
                sh *= 2
            incl, bincl = src, bsrc
            other, bother = (CB, bCB) if incl is CA else (CA, bCA)
            P.op("dve", lambda v, incl=incl, other=other, a0=a0, a1=a1, base=base: v.scalar_tensor_tensor(out=other[:, a0:a1], in0=incl[:, a0:a1], scalar=base, in1=MASK[:, a0:a1], op0=ALU.add, op1=ALU.mult),
                 reads=[bincl[si], bMASK[si]], writes=[bother[si]])
            P.op("dve", lambda v, other=other, a0=a0, a1=a1: v.tensor_scalar(out=W[:, a0:a1], in0=other[:, a0:a1], scalar1=-1.0, scalar2=None, op0=ALU.add),
                 reads=[bother[si]], writes=[bW[si]])
        P.dma(POSMT_out[:, :], W[:, :], reads=[bW[0], bW[1]], writes=[cout])
        if dbg is not None:
            P.dma(dbg["afft"][:, :], AFFT[:, :], reads=[bAFFT], writes=[cout])
        for tt in range(NT):
            q = tt % 3
            si = 0 if tt < 2 else 1
            P.op("pe", lambda t, tt=tt, q=q: t.transpose(PG[q][:, 0:NE], W[:, tt * 128:(tt + 1) * 128], K["idf"][0:NE, 0:NE]), reads=[bW[si], K["b_idf"]], writes=[bPG[q]])
            P.op("act", lambda a, tt=tt, q=q: a.copy(out=POSM[:, tt, :], in_=PG[q][:, 0:NE]), reads=[bPG[q]], writes=[bPOSM[tt]])
        for e in range(NE):
            b = e % 2
            for tt in range(NT):
                c0, nc_ = (0, 32) if tt < 2 else (32, 256)
                eng = "dve"
                P.op(eng, lambda v, tt=tt, e=e, b=b, c0=c0, nc_=nc_: v.tensor_scalar(out=PSEL[b][:, tt, 0:nc_], in0=IOTA[:, c0:c0 + nc_], scalar1=POSM[:, tt, e:e + 1], scalar2=None, op0=ALU.is_equal),
                     reads=[bIOTA, bPOSM[tt]], writes=[bPSEL[b][tt]])
            for dc in range(8):
                p = dc % 3
                for tt in range(NT):
                    c0, nc_ = (0, 32) if tt < 2 else (32, 256)
                    st_ = tt in (0, 2); sp_ = tt in (1, NT - 1)
                    P.op("pe", lambda t, tt=tt, dc=dc, p=p, b=b, c0=c0, nc_=nc_, st_=st_, sp_=sp_: t.matmul(PG[p][:, c0:c0 + nc_], HTM[:, tt, dc * 128:(dc + 1) * 128], PSEL[b][:, tt, 0:nc_], start=st_, stop=sp_),
                         reads=[bHTM[tt], bPSEL[b][tt]], writes=[bPG[p]])
                if dc % 2 == 0:
                    P.op("act", lambda a, dc=dc, p=p, b=b: a.copy(out=XGT[b][:, dc, :], in_=PG[p][:, 0:NSLOT]), reads=[bPG[p]], writes=[bXGT[b]])
                else:
                    P.op("dve", lambda v, dc=dc, p=p, b=b: v.tensor_copy(out=XGT[b][:, dc, :], in_=PG[p][:, 0:NSLOT]), reads=[bPG[p]], writes=[bXGT[b]])
            P.dma(XG[e].rearrange("(c p) s -> p c s", p=128), XGT[b][:, :, :], reads=[bXGT[b]], writes=[cout])
            for tt in range(NT):
                c0, nc_ = (0, 32) if tt < 2 else (32, 256)
                for hl, A_ in enumerate((AHI, ALO)):
                    st_ = (tt in (0, 2)) and hl == 0; sp_ = (tt in (1, NT - 1)) and hl == 1
                    P.op("pe", lambda t, tt=tt, e=e, b=b, c0=c0, nc_=nc_, st_=st_, sp_=sp_, A_=A_: t.matmul(PV[b][0:1, c0:c0 + nc_], A_[:, tt, e:e + 1], PSEL[b][:, tt, 0:nc_], start=st_, stop=sp_),
                         reads=[bAFF[tt], bPSEL[b][tt]], writes=[bPV[b]])
            P.op("act", lambda a, b=b: a.copy(out=GVR[b][:, :], in_=PV[b][0:1, 0:NSLOT]), reads=[bPV[b]], writes=[bGVR[b]])
            P.dma(GV[e:e + 1, :], GVR[b][:, :], reads=[bGVR[b]], writes=[cout])
        P.flush()


def phase_pro(P, K, C, Y, POSMT, MODS_prev, X_in, X, bX):
    with contextlib.ExitStack() as st:
        YG = P.sb(st, "p_YG", [128, NE * 2, D], BF16); bYG = [Buf() for _ in range(NE)]
        YGC = P.sb(st, "p_YGC", [128, 4, D], BF16); bYGC = Buf()
        PM = P.sb(st, "p_PM", [NE, T], F32); bPM = Buf()
        SEL = P.sb(st, "p_SEL", [NE, NE, 128], F32); bSEL = Buf()
        SELQ = P.sb(st, "p_SELQ", [NE, 4, 128], F32)
        SID = P.sb(st, "p_SID", [128, 4], F32); bSID = Buf()
        G2 = P.sb(st, "p_G2", [128, 2, D], F32); bG2 = Buf()
        PTt = [P.sb(st, f"p_PT{i}", [128, 384], BF16) for i in range(3)]; bPTt = [Buf() for _ in range(3)]
        XT = [P.sb(st, f"p_XT{i}", [128, D], F32) for i in range(2)]; bXT = [Buf() for _ in range(2)]
        TMP = [P.sb(st, f"p_TMP{i}", [128, 512], F32) for i in range(2)]; bTMP = [Buf() for _ in range(2)]
        ACC = [P.ps(st, f"p_ACC{i}") for i in range(6)]; bACC = [Buf(excl=True) for _ in range(6)]
        PB = [P.ps(st, f"p_PB{i}") for i in range(2)]; bPB = [Buf(excl=True) for _ in range(2)]
        cin = Buf()
        for e in range(NE):
            P.dma(YG[:, e * 2:e * 2 + 2, :], Y[e, 32:288, :].rearrange("(k p) d -> p k d", p=128), reads=[cin], writes=[bYG[e]])
            P.dma(YGC[(e % 4) * 32:(e % 4) * 32 + 32, e // 4, :], Y[e, 0:32, :], reads=[cin], writes=[bYGC])
        P.dma(PM[:, :], POSMT[:, :], reads=[cin], writes=[bPM])
        P.dma(SEL[:, :, :], C["sel16"][:, :, :], reads=[cin], writes=[bSEL])
        P.dma(SELQ[:, :, :], C["selq"][:, :, :], reads=[cin], writes=[bSEL])
        P.dma(SID[:, :], C["slotid"][:, :], reads=[cin], writes=[bSID])
        for r in range(2):
            P.dma(G2[:, r, :], MODS_prev[r, 5 * D:6 * D].partition_broadcast(128), reads=[cin], writes=[bG2])
        state = {"pti": 0, "xi": 0, "ti": 0}

        def finish(tiles_):
            for j, tt in enumerate(tiles_):
                r = 1 if tt < 2 else 0
                b = state["xi"] % 2; state["xi"] += 1
                P.dma(XT[b][:, :], X_in[tt * 128:(tt + 1) * 128, :], reads=[cin], writes=[bXT[b]])
                for dh in range(2):
                    a = j * 2 + dh
                    p = state["ti"] % 2; state["ti"] += 1
                    P.op("dve", lambda v, a=a, p=p, r=r, dh=dh: v.tensor_tensor(out=TMP[p][:, :], in0=ACC[a][:, :], in1=G2[:, r, dh * 512:(dh + 1) * 512], op=ALU.mult),
                         reads=[bACC[a], bG2], writes=[bTMP[p]])
                    P.op("dve", lambda g_, p=p, b=b, dh=dh: g_.tensor_tensor(out=XT[b][:, dh * 512:(dh + 1) * 512], in0=TMP[p][:, :], in1=XT[b][:, dh * 512:(dh + 1) * 512], op=ALU.add),
                         reads=[bTMP[p], bXT[b]], writes=[bXT[b]])
                P.dma(X[tt * 128:(tt + 1) * 128, :], XT[b][:, :], reads=[bXT[b]], writes=[bX[tt]])

        ntok = 256
        for g4 in range(4):
            pb = g4 % 2
            P.op("pe", lambda t, g4=g4, pb=pb: t.matmul(PB[pb][:, 0:ntok], SELQ[:, g4, :], PM[:, 0:ntok], start=True, stop=True), reads=[bSEL, bPM], writes=[bPB[pb]])
            pt = state["pti"] % 3; state["pti"] += 1
            P.op("dve", lambda v, pt=pt, pb=pb: v.tensor_scalar(out=PTt[pt][:, 0:ntok], in0=PB[pb][:, 0:ntok], scalar1=SID[:, 3:4], scalar2=None, op0=ALU.is_equal), reads=[bPB[pb], bSID], writes=[bPTt[pt]])
            for j in range(2):
                for dh in range(2):
                    a = j * 2 + dh
                    P.op("pe", lambda t, a=a, pt=pt, j=j, dh=dh, g4=g4: t.matmul(ACC[a][:, :], PTt[pt][:, j * 128:(j + 1) * 128], YGC[:, g4, dh * 512:(dh + 1) * 512], start=(g4 == 0), stop=(g4 == 3)),
                         reads=[bPTt[pt], bYGC], writes=[bACC[a]])
        finish([0, 1])
        groups = [list(range(2 + 3 * g, min(2 + 3 * g + 3, NT))) for g in range(6)]
        for tiles_ in groups:
            t0 = tiles_[0] * 128; ntk = len(tiles_) * 128

            def emit_sel(e, t0=t0, ntk=ntk):
                pb = e % 2
                P.op("pe", lambda t, e=e, pb=pb, t0=t0, ntk=ntk: t.matmul(PB[pb][:, 0:ntk], SEL[:, e, :], PM[:, t0:t0 + ntk], start=True, stop=True), reads=[bSEL, bPM], writes=[bPB[pb]])
            emit_sel(0)
            for e in range(NE):
                pb = e % 2
                if e + 1 < NE:
                    emit_sel(e + 1)
                for k in range(2):
                    pt = state["pti"] % 3; state["pti"] += 1
                    P.op("dve", lambda v, pt=pt, pb=pb, k=k, ntk=ntk: v.tensor_scalar(out=PTt[pt][:, 0:ntk], in0=PB[pb][:, 0:ntk], scalar1=SID[:, k:k + 1], scalar2=None, op0=ALU.is_equal),
                         reads=[bPB[pb], bSID], writes=[bPTt[pt]])
                    for j in range(len(tiles_)):
                        for dh in range(2):
                            a = j * 2 + dh
                            first = (e == 0 and k == 0); last = (e == NE - 1 and k == 1)
                            P.op("pe", lambda t, a=a, pt=pt, j=j, dh=dh, e=e, k=k, first=first, last=last: t.matmul(ACC[a][:, :], PTt[pt][:, j * 128:(j + 1) * 128], YG[:, e * 2 + k, dh * 512:(dh + 1) * 512], start=first, stop=last),
                                 reads=[bPTt[pt], bYG[e]], writes=[bACC[a]])
            finish(tiles_)
        P.flush()


NS = 2304
FF = 2816
D = 1024

def build_B(P, xgT, gv, wg, wu, wd, y, nexp=2):
    nc = P.nc
    with contextlib.ExitStack() as st:
        XT = P.sb(st, "XT", [128, 8, NS], BF16); bXT = Buf("XT")
        GV = P.sb(st, "GV", [128, 18], F32); bGV = Buf("GV")
        WD = P.sb(st, "WD", [128, 22, D], BF16); bWD = [Buf(f"WD{f}") for f in range(22)]
        ACTT = P.sb(st, "ACTT", [128, 22, NS // 2], BF16); bACTT = [Buf(f"ACTT{f}") for f in range(22)]
        stg = [P.sb(st, f"stg{i}", [128, 1024], F32) for i in range(4)]; bstg = [Buf(f"stg{i}") for i in range(4)]
        WG = [P.sb(st, f"WG{i}", [128, 8, 128], BF16) for i in range(2)]; bWG = [Buf() for _ in range(2)]
        WU = [P.sb(st, f"WU{i}", [128, 8, 128], BF16) for i in range(2)]; bWU = [Buf() for _ in range(2)]
        SA = [P.sb(st, f"SA{i}", [128, 384], F32) for i in range(2)]; bSA = [Buf() for _ in range(2)]
        YT = [P.sb(st, f"YT{i}", [128, D], BF16) for i in range(2)]; bYT = [Buf() for _ in range(2)]
        PA = [P.ps(st, f"PA{i}") for i in range(2)]; bPA = [Buf(excl=True) for _ in range(2)]
        PU = [P.ps(st, f"PU{i}") for i in range(2)]; bPU = [Buf(excl=True) for _ in range(2)]
        PY = [P.ps(st, f"PY{i}") for i in range(2)]; bPY = [Buf(excl=True) for _ in range(2)]
        bin_ = Buf("in"); bout = Buf("out")
        sgi = 0; ci = 0; pi = 0; yi = 0; pyi = 0
        for e in range(nexp):
            P.dma(XT[:, :, :], xgT[e].rearrange("(c p) s -> p c s", p=128), reads=[bin_], writes=[bXT])
            P.dma(GV[:, :], gv[e], reads=[bin_], writes=[bGV])
            for f in range(22):
                s = sgi % 4; sgi += 1
                P.dma(stg[s][:, :], wd[e, f * 128:(f + 1) * 128, :], reads=[bin_], writes=[bstg[s]])
                eng = "pool" if ci % 2 == 0 else "act"; ci += 1
                if eng == "pool":
                    P.op("pool", lambda g, o=WD[:, f, :], i=stg[s][:, :]: g.tensor_copy(out=o, in_=i), reads=[bstg[s]], writes=[bWD[f]])
                else:
                    P.op("act", lambda g, o=WD[:, f, :], i=stg[s][:, :]: g.copy(out=o, in_=i), reads=[bstg[s]], writes=[bWD[f]])
            for sh in range(2):
                for f in range(22):
                    w = f % 2
                    for (src, dst, bdst) in ((wg, WG[w], bWG[w]), (wu, WU[w], bWU[w])):
                        s = sgi % 4; sgi += 1
                        sv = stg[s][:, :].rearrange("p (c f) -> p c f", c=8)
                        P.dma(sv, src[e].rearrange("(c p) f -> p c f", p=128)[:, :, f * 128:(f + 1) * 128], reads=[bin_], writes=[bstg[s]])
                        eng = "pool" if ci % 2 == 0 else "act"; ci += 1
                        if eng == "pool":
                            P.op("pool", lambda g, o=dst[:, :, :], i=sv: g.tensor_copy(out=o, in_=i), reads=[bstg[s]], writes=[bdst])
                        else:
                            P.op("act", lambda g, o=dst[:, :, :], i=sv: g.copy(out=o, in_=i), reads=[bstg[s]], writes=[bdst])
                    for nb in range(3):
                        s0 = sh * (NS // 2) + nb * 384
                        p = pi % 2; pi += 1
                        for k in range(8):
                            P.op("pe", lambda t, o=PA[p][:, 0:384], l=WG[w][:, k, :], r=XT[:, k, s0:s0 + 384], k=k: t.matmul(o, l, r, start=(k == 0), stop=(k == 7)),
                                 reads=[bWG[w], bXT], writes=[bPA[p]])
                        for k in range(8):
                            P.op("pe", lambda t, o=PU[p][:, 0:384], l=WU[w][:, k, :], r=XT[:, k, s0:s0 + 384], k=k: t.matmul(o, l, r, start=(k == 0), stop=(k == 7)),
                                 reads=[bWU[w], bXT], writes=[bPU[p]])
                        P.op("act", lambda a, o=SA[p][:, :], i=PA[p][:, 0:384]: a.activation(out=o, in_=i, func=AF.Silu), reads=[bPA[p]], writes=[bSA[p]])
                        P.op("dve", lambda v, o=ACTT[:, f, nb * 384:(nb + 1) * 384], a=SA[p][:, :], b=PU[p][:, 0:384]: v.tensor_tensor(out=o, in0=a, in1=b, op=ALU.mult),
                             reads=[bSA[p], bPU[p]], writes=[bACTT[f]])
                for sc in range(9):
                    yb = yi % 2; yi += 1
                    chunk = sh * 9 + sc
                    for dh in range(2):
                        p = pyi % 2; pyi += 1
                        for f in range(22):
                            P.op("pe", lambda t, o=PY[p][:, :], l=ACTT[:, f, sc * 128:(sc + 1) * 128], r=WD[:, f, dh * 512:(dh + 1) * 512], f=f: t.matmul(o, l, r, start=(f == 0), stop=(f == 21)),
                                 reads=[bACTT[f], bWD[f]], writes=[bPY[p]])
                        if dh == 0:
                            P.op("dve", lambda v, o=YT[yb][:, 0:512], i=PY[p][:, :], s=GV[:, chunk:chunk + 1]: v.tensor_scalar(out=o, in0=i, scalar1=s, scalar2=None, op0=ALU.mult),
                                 reads=[bPY[p], bGV], writes=[bYT[yb]])
                        else:
                            P.op("act", lambda a, o=YT[yb][:, 512:1024], i=PY[p][:, :], s=GV[:, chunk:chunk + 1]: a.activation(out=o, in_=i, func=AF.Copy, scale=s),
                                 reads=[bPY[p], bGV], writes=[bYT[yb]])
                    P.dma(y[e, chunk * 128:(chunk + 1) * 128, :], YT[yb][:, :], reads=[bYT[yb]], writes=[bout])
        P.flush(final=True)


NEG = -30000.0
GW = 64

def na_tables(rpb):
    nh = rpb.shape[0]
    qc = np.arange(64); kc = np.arange(64)
    cs = np.clip(qc - 8, 0, 48)
    ok = (kc[:, None] >= cs[None, :]) & (kc[:, None] < cs[None, :] + 16)
    idx = np.clip(kc[:, None] - qc[None, :], -15, 15) + 15
    Tb = np.where(ok[None, None], rpb[:, :, idx], np.float32(NEG)).astype(np.float32)
    mask = np.full((nh, 64, 64), NEG, np.float32)
    tte = np.zeros((128, nh, 14, 64), np.float32)
    for d in range(14):
        tte[0:64, :, d, :] = Tb[:, d].transpose(1, 0, 2)
        tte[64:128, :, d, :] = Tb[:, d + 1].transpose(1, 0, 2)
    tto = np.zeros((128, nh, 5, 64), np.float32)
    pairs = [(None, 3), (4, 5), (6, 7), (8, 9), (10, None)]
    for j, (a, b) in enumerate(pairs):
        tto[0:64, :, j, :] = (mask if a is None else Tb[:, a]).transpose(1, 0, 2)
        tto[64:128, :, j, :] = (mask if b is None else Tb[:, b]).transpose(1, 0, 2)
    return tte, tto


def mixer_na(P, K, run_norm, WQKV, TTE, TTO, YATT, bY, last=False):
    with contextlib.ExitStack() as st:
        QT = P.sb(st, "a_QT", [128, 8, T], BF16); bQT = [Buf() for _ in range(8)]
        KT = P.sb(st, "a_KT", [128, 8, T], BF16); bKT = [Buf() for _ in range(8)]
        VA = P.sb(st, "a_VA", [128, NT, 16, 65], BF16); bVA = [Buf() for _ in range(NT)]
        cin = Buf()
        with contextlib.ExitStack() as st2:
            HT = P.sb(st2, "HT", [128, 8, T], BF16); bHT = [Buf() for _ in range(NT)]
            run_norm(HT, bHT)
            WB = P.sb(st2, "a_WB", [128, 8, 1024], BF16); bWB = [Buf() for _ in range(8)]
            STG = [P.sb(st2, f"a_stg{i}", [128, 1024], F32) for i in range(2)]; bSTG = [Buf() for _ in range(2)]
            PP = [P.ps(st2, f"a_PP{i}") for i in range(4)]; bPP = [Buf(excl=True) for _ in range(4)]
            P.op("dve", lambda v: v.memset(VA[:, :, :, 64:65], 1.0), writes=bVA)
            pi = 0
            for which in range(3):
                for k in range(8):
                    s = k % 2
                    P.dma(STG[s][:, :], WQKV[k * 128:(k + 1) * 128, which * 1024:(which + 1) * 1024], reads=[cin], writes=[bSTG[s]])
                    if k % 2 == 0:
                        P.op("dve", lambda g, k=k, s=s: g.tensor_copy(out=WB[:, k, :], in_=STG[s][:, :]), reads=[bSTG[s]], writes=[bWB[k]])
                    else:
                        P.op("act", lambda a, k=k, s=s: a.copy(out=WB[:, k, :], in_=STG[s][:, :]), reads=[bSTG[s]], writes=[bWB[k]])
                if which < 2:
                    DST, bD = (QT, bQT) if which == 0 else (KT, bKT)
                    for c in range(8):
                        for tb in range(6):
                            p = pi % 4; pi += 1
                            for k in range(8):
                                P.op("pe", lambda t, k=k, p=p, c=c, tb=tb: t.matmul(PP[p][:, 0:384], WB[:, k, c * 128:(c + 1) * 128], HT[:, k, tb * 384:(tb + 1) * 384], start=(k == 0), stop=(k == 7)),
                                     reads=[bWB[k]] + bHT[tb * 3:tb * 3 + 3], writes=[bPP[p]])
                            if which == 0:
                                P.op("act", lambda a, p=p, c=c, tb=tb: a.activation(out=QT[:, c, tb * 384:(tb + 1) * 384], in_=PP[p][:, 0:384], func=AF.Copy, scale=0.125), reads=[bPP[p]], writes=[bQT[c]])
                            else:
                                P.op("dve", lambda v, p=p, c=c, tb=tb: v.tensor_copy(out=KT[:, c, tb * 384:(tb + 1) * 384], in_=PP[p][:, 0:384]), reads=[bPP[p]], writes=[bKT[c]])
                else:
                    for tt in range(NT):
                        for dh in range(2):
                            p = pi % 4; pi += 1
                            for k in range(8):
                                P.op("pe", lambda t, k=k, p=p, tt=tt, dh=dh: t.matmul(PP[p][:, :], HT[:, k, tt * 128:(tt + 1) * 128], WB[:, k, dh * 512:(dh + 1) * 512], start=(k == 0), stop=(k == 7)),
                                     reads=[bWB[k], bHT[tt]], writes=[bPP[p]])
                            if dh == 0:
                                P.op("act", lambda a, p=p, tt=tt, dh=dh: a.copy(out=VA[:, tt, dh * 8:(dh + 1) * 8, 0:64], in_=PP[p][:, :].rearrange("p (h d) -> p h d", h=8)), reads=[bPP[p]], writes=[bVA[tt]])
                            else:
                                P.op("dve", lambda v, p=p, tt=tt, dh=dh: v.tensor_copy(out=VA[:, tt, dh * 8:(dh + 1) * 8, 0:64], in_=PP[p][:, :].rearrange("p (h d) -> p h d", h=8)), reads=[bPP[p]], writes=[bVA[tt]])
            P.flush()
        with contextlib.ExitStack() as st2:
            TTEb = P.sb(st2, "a_TTE", [128, 16, 14, 64], BF16); bTTE = Buf()
            TTOb = P.sb(st2, "a_TTO", [128, 16, 5, 64], BF16); bTTO = Buf()
            STG = [P.sb(st2, f"a_tstg{i}", [128, 14 * 64], F32) for i in range(2)]; bSTG = [Buf() for _ in range(2)]
            for h in range(16):
                s = h % 2
                P.dma(STG[s][:, :], TTE[:, h, :, :].rearrange("p a b -> p (a b)"), reads=[cin], writes=[bSTG[s]])
                P.op("dve", lambda g, h=h, s=s: g.tensor_copy(out=TTEb[:, h, :, :].rearrange("p a b -> p (a b)"), in_=STG[s][:, :]), reads=[bSTG[s]], writes=[bTTE])
            for h in range(16):
                s = h % 2
                P.dma(STG[s][:, 0:320], TTO[:, h, :, :].rearrange("p a b -> p (a b)"), reads=[cin], writes=[bSTG[s]])
                P.op("dve", lambda g, h=h, s=s: g.tensor_copy(out=TTOb[:, h, :, :].rearrange("p a b -> p (a b)"), in_=STG[s][:, 0:320]), reads=[bSTG[s]], writes=[bTTO])
            PS = [P.ps(st2, f"a_PS{i}") for i in range(4)]; bPS = [Buf(excl=True) for _ in range(4)]
            PO = [P.ps(st2, f"a_PO{i}") for i in range(4)]; bPO = [Buf(excl=True) for _ in range(4)]
            PT = [P.sb(st2, f"a_PT{i}", [128, 512], BF16) for i in range(4)]; bPT = [Buf() for _ in range(4)]
            REC = [P.sb(st2, f"a_REC{i}", [128, 2], F32) for i in range(4)]; bREC = [Buf() for _ in range(4)]
            YR = [P.sb(st2, f"a_YR{i}", [128, 2, D], BF16) for i in range(2)]; bYR = [Buf() for _ in range(2)]
            si = 0
            if not last:
                yb = 0
                for h in range(16):
                    c = h // 2; hp = (h % 2) * 64
                    s = si % 3; si += 1
                    for kc in range(2):
                        P.op("pe", lambda t, s=s, kc=kc, c=c, hp=hp: t.matmul(PS[s][:, kc * 256:(kc + 1) * 256], KT[hp:hp + 64, c, kc * 128:(kc + 1) * 128], QT[hp:hp + 64, c, 0:256], start=True, stop=True),
                             reads=[bKT[c], bQT[c]], writes=[bPS[s]])
                    P.op("act", lambda a, s=s: a.activation(out=PT[s][:, :], in_=PS[s][:, :], func=AF.Exp), reads=[bPS[s]], writes=[bPT[s]])
                    for qt in range(2):
                        for kc in range(2):
                            P.op("pe", lambda t, s=s, qt=qt, kc=kc, h=h: t.matmul(PO[s][:, qt * 65:(qt + 1) * 65], PT[s][:, kc * 256 + qt * 128:kc * 256 + (qt + 1) * 128], VA[:, kc, h, :], start=(kc == 0), stop=(kc == 1)),
                                 reads=[bPT[s], bVA[kc]], writes=[bPO[s]])
                    for qt in range(2):
                        P.op("dve", lambda v, s=s, qt=qt: v.reciprocal(out=REC[s][:, qt:qt + 1], in_=PO[s][:, qt * 65 + 64:qt * 65 + 65]), reads=[bPO[s]], writes=[bREC[s]])
                        P.op("act", lambda a, s=s, qt=qt, h=h: a.activation(out=YR[yb][:, qt, h * 64:(h + 1) * 64], in_=PO[s][:, qt * 65:qt * 65 + 64], func=AF.Copy, scale=REC[s][:, qt:qt + 1]),
                             reads=[bPO[s], bREC[s]], writes=[bYR[yb]])
                for qt in range(2):
                    P.dma(YATT[qt * 128:(qt + 1) * 128, :], YR[yb][:, qt, :], reads=[bYR[yb]], writes=[bY[qt]])
            LOOK = 3
            steps = []
            for r in range(32):
                rs = min(max(r - 4, 0), 24)
                if rs % 2 == 0:
                    m0 = rs // 2; nloc = 4; dr0 = rs - r + 7
                else:
                    m0 = (rs - 1) // 2; nloc = 5; dr0 = None
                tiles_ = [0, 1] + [2 + m0 + c for c in range(nloc)]
                for h in range(16):
                    steps.append((r, h, tiles_, nloc, dr0))
            def emit_S(idx):
                r, h, tiles_, nloc, dr0 = steps[idx]
                s = idx % 4; c = h // 2; hp = (h % 2) * 64; q0 = 256 + r * 64
                for ci, tile in enumerate(tiles_):
                    P.op("pe", lambda t, s=s, ci=ci, tile=tile, c=c, hp=hp, q0=q0: t.matmul(PS[s][:, ci * 64:(ci + 1) * 64], KT[hp:hp + 64, c, tile * 128:(tile + 1) * 128], QT[hp:hp + 64, c, q0:q0 + 64], start=True, stop=True),
                         reads=[bKT[c], bQT[c]], writes=[bPS[s]])
            for i0 in range(min(LOOK, len(steps))):
                emit_S(i0)
            for idx, (r, h, tiles_, nloc, dr0) in enumerate(steps):
                s = idx % 4
                yb = (r + 1) % 2
                nch = len(tiles_)
                q0 = 256 + r * 64
                if dr0 is not None:
                    tab = TTEb[:, h, dr0:dr0 + 7:2, :]; btab = bTTE
                else:
                    tab = TTOb[:, h, 0:5, :]; btab = bTTO
                P.op("dve", lambda v, s=s, nloc=nloc, tab=tab: v.tensor_tensor(out=PS[s][:, 128:128 + nloc * 64].rearrange("p (a b) -> p a b", b=64), in0=PS[s][:, 128:128 + nloc * 64].rearrange("p (a b) -> p a b", b=64), in1=tab, op=ALU.add),
                     reads=[bPS[s], btab], writes=[bPS[s]])
                P.op("act", lambda a, s=s, nch=nch: a.activation(out=PT[s][:, 0:nch * 64], in_=PS[s][:, 0:nch * 64], func=AF.Exp), reads=[bPS[s]], writes=[bPT[s]])
                if idx + LOOK < len(steps):
                    emit_S(idx + LOOK)
                for ci, tile in enumerate(tiles_):
                    P.op("pe", lambda t, s=s, ci=ci, tile=tile, h=h, nch=nch: t.matmul(PO[s][0:64, 0:65], PT[s][:, ci * 64:(ci + 1) * 64], VA[:, tile, h, :], start=(ci == 0), stop=(ci == nch - 1)),
                         reads=[bPT[s], bVA[tile]], writes=[bPO[s]])
                P.op("dve", lambda v, s=s: v.reciprocal(out=REC[s][0:64, 0:1], in_=PO[s][0:64, 64:65]), reads=[bPO[s]], writes=[bREC[s]])
                P.op("act", lambda a, s=s, h=h, yb=yb: a.activation(out=YR[yb][0:64, 0, h * 64:(h + 1) * 64], in_=PO[s][0:64, 0:64], func=AF.Copy, scale=REC[s][0:64, 0:1]),
                     reads=[bPO[s], bREC[s]], writes=[bYR[yb]])
                if h == 15:
                    P.dma(YATT[q0:q0 + 64, :], YR[yb][0:64, 0, :], reads=[bYR[yb]], writes=[bY[q0 // 128]])
            P.flush()


QR, KVR, RD = 384, 256, 32
SCALE = 96 ** -0.5

def mla_host(w_in, w_q_b, qg, kvg):
    wks = np.concatenate([w_in[:, 576:640], w_in[:, 656:672], w_in[:, 640:656]], 1).copy()
    wq = w_q_b.reshape(QR, 16, 96)
    wqs = np.concatenate([wq[:, :, 0:64], wq[:, :, 80:96], wq[:, :, 64:80]], 2).reshape(QR, 16 * 96).copy()
    gq = qg.reshape(3, 128).T.copy(); gkv = kvg.reshape(2, 128).T.copy()
    t = np.arange(2048)
    row = (t // 64).astype(np.float32); col = (t % 64).astype(np.float32)
    inv = (10000.0 ** (-np.arange(8, dtype=np.float32) / 8)).astype(np.float32)
    ang = np.concatenate([row[:, None] * inv, col[:, None] * inv], -1)
    cos = np.cos(ang).astype(np.float32); sin = np.sin(ang).astype(np.float32)
    cs = np.zeros((32, 2, T), np.float32)
    cs[:, 0, :256] = 1.0
    cs[0:16, 0, 256:] = cos.T; cs[16:32, 0, 256:] = cos.T
    cs[0:16, 1, 256:] = -sin.T; cs[16:32, 1, 256:] = sin.T
    return wks, wqs, gq, gkv, cs


STOP = 99
def mixer_mla(P, K, run_norm, WIN, WKS, GQ, GKV, WQB, WQS, WKVB, CS, YTM, bYTM):
    with contextlib.ExitStack() as st:
        CQN = P.sb(st, "m_CQN", [128, 3, T], BF16); bCQN = [Buf() for _ in range(6)]
        CKVN = P.sb(st, "m_CKVN", [128, 2, T], BF16); bCKVN = [Buf() for _ in range(6)]
        KRT = P.sb(st, "m_KRT", [128, T], BF16); bKRT = [Buf() for _ in range(6)]
        CSs = P.sb(st, "m_CS", [128, 2, T], F32); bCS = Buf()
        cin = Buf()
        P.dma(CSs[64:96, :, :], CS[:, :, :], reads=[cin], writes=[bCS])
        with contextlib.ExitStack() as st2:
            HT = P.sb(st2, "HT", [128, 8, T], BF16); bHT = [Buf() for _ in range(NT)]
            run_norm(HT, bHT)
            WINb = P.sb(st2, "m_WIN", [128, 8, 672], BF16); bWIN = [Buf() for _ in range(8)]
            WKSb = P.sb(st2, "m_WKS", [128, 8, 96], BF16); bWKS = Buf()
            STG = [P.sb(st2, f"m_stg{i}", [128, 672], F32) for i in range(2)]; bSTG = [Buf() for _ in range(2)]
            STK = P.sb(st2, "m_stk", [128, 8, 96], F32); bSTK = Buf()
            GQs = P.sb(st2, "m_GQ", [128, 3], F32); GKVs = P.sb(st2, "m_GKV", [128, 2], F32); bG = Buf()
            ONES = P.sb(st2, "m_ONES", [128, 128], BF16); bONES = Buf()
            ZF = [P.sb(st2, f"m_ZF{i}", [128, 5, 384], F32) for i in range(2)]; bZF = [Buf() for _ in range(2)]
            SQ = [P.sb(st2, f"m_SQ{i}", [128, 5, 384], BF16) for i in range(2)]; bSQ = [Buf() for _ in range(2)]
            RQ = [P.sb(st2, f"m_RQ{i}", [128, 2, 384], F32) for i in range(2)]; bRQ = [Buf() for _ in range(2)]
            T1 = [P.sb(st2, f"m_T1{i}", [128, 384], F32) for i in range(2)]; bT1 = [Buf() for _ in range(2)]
            T2 = [P.sb(st2, f"m_T2{i}", [128, 384], F32) for i in range(2)]; bT2 = [Buf() for _ in range(2)]
            PZ = [P.ps(st2, f"m_PZ{i}") for i in range(4)]; bPZ = [Buf(excl=True) for _ in range(4)]
            PSM = [P.ps(st2, f"m_PSM{i}") for i in range(2)]; bPSM = [Buf(excl=True) for _ in range(2)]
            PK = [P.ps(st2, f"m_PK{i}") for i in range(2)]; bPK = [Buf(excl=True) for _ in range(2)]
            for k in range(8):
                s = k % 2
                P.dma(STG[s][:, :], WIN[k * 128:(k + 1) * 128, :], reads=[cin], writes=[bSTG[s]])
                P.op("dve", lambda g, k=k, s=s: g.tensor_copy(out=WINb[:, k, :], in_=STG[s][:, :]), reads=[bSTG[s]], writes=[bWIN[k]])
            P.dma(STK[:, :, :], WKS.rearrange("(c p) f -> p c f", p=128), reads=[cin], writes=[bSTK])
            P.op("dve", lambda g: g.tensor_copy(out=WKSb[:, :, :], in_=STK[:, :, :]), reads=[bSTK], writes=[bWKS])
            P.dma(GQs[:, :], GQ[:, :], reads=[cin], writes=[bG])
            P.dma(GKVs[:, :], GKV[:, :], reads=[cin], writes=[bG])
            P.op("dve", lambda v: v.memset(ONES[:, :], 1.0), writes=[bONES])
            pz = 0
            for tb in range(6):
                b = tb % 2
                tsl = slice(tb * 384, (tb + 1) * 384)
                hts = bHT[tb * 3:tb * 3 + 3]
                for c in range(5):
                    p = pz % 4; pz += 1
                    for k in range(8):
                        P.op("pe", lambda t, k=k, p=p, c=c, tsl=tsl: t.matmul(PZ[p][:, 0:384], WINb[:, k, c * 128:(c + 1) * 128], HT[:, k, tsl], start=(k == 0), stop=(k == 7)),
                             reads=[bWIN[k]] + hts, writes=[bPZ[p]])
                    P.op("dve", lambda v, p=p, c=c, b=b: v.tensor_copy(out=ZF[b][:, c, :], in_=PZ[p][:, 0:384]), reads=[bPZ[p]], writes=[bZF[b]])
                    P.op("act", lambda a, p=p, c=c, b=b: a.activation(out=SQ[b][:, c, :], in_=PZ[p][:, 0:384], func=AF.Square), reads=[bPZ[p]], writes=[bSQ[b]])
                for which, cs_, n_ in ((0, (0, 1, 2), QR), (1, (3, 4), KVR)):
                    for i, c in enumerate(cs_):
                        P.op("pe", lambda t, which=which, c=c, b=b, i=i, cs_=cs_: t.matmul(PSM[which][:, 0:384], ONES[:, :], SQ[b][:, c, :], start=(i == 0), stop=(i == len(cs_) - 1)),
                             reads=[bONES, bSQ[b]], writes=[bPSM[which]])
                    P.op("act", lambda a, which=which, b=b, n_=n_: a.activation(out=RQ[b][:, which, :], in_=PSM[which][:, 0:384], func=AF.Sqrt, scale=1.0 / n_, bias=K["eps"][:, :]),
                         reads=[bPSM[which], K["b_eps"]], writes=[bRQ[b]])
                    P.op("dve", lambda v, which=which, b=b: v.reciprocal(out=RQ[b][:, which, :], in_=RQ[b][:, which, :]), reads=[bRQ[b]], writes=[bRQ[b]])
                for c in range(3):
                    P.op("dve", lambda v, c=c, b=b, tsl=tsl: v.scalar_tensor_tensor(out=CQN[:, c, tsl], in0=ZF[b][:, c, :], scalar=GQs[:, c:c + 1], in1=RQ[b][:, 0, :], op0=ALU.mult, op1=ALU.mult),
                         reads=[bZF[b], bG, bRQ[b]], writes=[bCQN[tb]])
                for c in range(2):
                    P.op("dve", lambda v, c=c, b=b, tsl=tsl: v.scalar_tensor_tensor(out=CKVN[:, c, tsl], in0=ZF[b][:, 3 + c, :], scalar=GKVs[:, c:c + 1], in1=RQ[b][:, 1, :], op0=ALU.mult, op1=ALU.mult),
                         reads=[bZF[b], bG, bRQ[b]], writes=[bCKVN[tb]])
                for k in range(8):
                    P.op("pe", lambda t, k=k, tsl=tsl: t.matmul(PK[0][0:96, 0:384], WINb[:, k, 576:672], HT[:, k, tsl], start=(k == 0), stop=(k == 7)), reads=[bWIN[k]] + hts, writes=[bPK[0]])
                for k in range(8):
                    P.op("pe", lambda t, k=k, tsl=tsl: t.matmul(PK[1][0:96, 0:384], WKSb[:, k, :], HT[:, k, tsl], start=(k == 0), stop=(k == 7)), reads=[bWKS] + hts, writes=[bPK[1]])
                P.op("dve", lambda v, b=b, tsl=tsl: v.tensor_tensor(out=T1[b][64:96, :], in0=PK[0][64:96, 0:384], in1=CSs[64:96, 0, tsl], op=ALU.mult), reads=[bPK[0], bCS], writes=[bT1[b]])
                P.op("dve", lambda v, b=b, tsl=tsl: v.tensor_tensor(out=T2[b][64:96, :], in0=PK[1][64:96, 0:384], in1=CSs[64:96, 1, tsl], op=ALU.mult), reads=[bPK[1], bCS], writes=[bT2[b]])
                P.op("dve", lambda g, b=b, tsl=tsl: g.tensor_tensor(out=KRT[64:96, tsl], in0=T1[b][64:96, :], in1=T2[b][64:96, :], op=ALU.add), reads=[bT1[b], bT2[b]], writes=[bKRT[tb]])
            P.flush()
        if STOP <= 1:
            return
        for hg in range(2):
            with contextlib.ExitStack() as st2:
                QT = P.sb(st2, "m_QT", [128, 8, T], BF16); bQT = [Buf() for _ in range(8)]
                KT = P.sb(st2, "m_KT", [128, 8, T], BF16); bKT = [Buf() for _ in range(8)]
                VA = P.sb(st2, "m_VA", [128, NT, 8, 65], BF16); bVA = [Buf() for _ in range(NT)]
                WQBb = P.sb(st2, "m_WQB", [128, 3, 768], BF16); WQSb = P.sb(st2, "m_WQS", [128, 3, 768], BF16); bWQ = Buf()
                WKVb = P.sb(st2, "m_WKV", [128, 2, 1024], BF16); bWKV = Buf()
                STG = [P.sb(st2, f"m_stg2{i}", [128, 1024], F32) for i in range(2)]; bSTG = [Buf() for _ in range(2)]
                T1 = [P.sb(st2, f"m_T1b{i}", [128, 384], F32) for i in range(2)]; bT1 = [Buf() for _ in range(2)]
                T2 = [P.sb(st2, f"m_T2b{i}", [128, 384], F32) for i in range(2)]; bT2 = [Buf() for _ in range(2)]
                PTs = [P.sb(st2, f"m_PT{i}", [128, 512], BF16) for i in range(4)]; bPT = [Buf() for _ in range(4)]
                REC = [P.sb(st2, f"m_REC{i}", [128, 4], F32) for i in range(2)]; bREC = [Buf() for _ in range(2)]
                PQ = [P.ps(st2, f"m_PQ{i}") for i in range(2)]; bPQ = [Buf(excl=True) for _ in range(2)]
                PQS = [P.ps(st2, f"m_PQS{i}") for i in range(2)]; bPQS = [Buf(excl=True) for _ in range(2)]
                PS = [P.ps(st2, f"m_PS{i}") for i in range(2)]; bPS = [Buf(excl=True) for _ in range(2)]
                PO = [P.ps(st2, f"m_PO{i}") for i in range(2)]; bPO = [Buf(excl=True) for _ in range(2)]
                si = 0
                for (SRC, DST) in ((WQB, WQBb), (WQS, WQSb)):
                    for k in range(3):
                        s = si % 2; si += 1
                        P.dma(STG[s][:, 0:768], SRC[k * 128:(k + 1) * 128, hg * 768:(hg + 1) * 768], reads=[cin], writes=[bSTG[s]])
                        P.op("dve", lambda g, k=k, s=s, DST=DST: g.tensor_copy(out=DST[:, k, :], in_=STG[s][:, 0:768]), reads=[bSTG[s]], writes=[bWQ])
                for k in range(2):
                    s = si % 2; si += 1
                    P.dma(STG[s][:, :], WKVB[k * 128:(k + 1) * 128, hg * 1024:(hg + 1) * 1024], reads=[cin], writes=[bSTG[s]])
                    P.op("dve", lambda g, k=k, s=s: g.tensor_copy(out=WKVb[:, k, :], in_=STG[s][:, :]), reads=[bSTG[s]], writes=[bWKV])
                P.op("dve", lambda v: v.memset(VA[:, :, :, 64:65], 1.0), writes=bVA)
                pq = 0
                for hl in range(8):
                    for tb in range(6):
                        tsl = slice(tb * 384, (tb + 1) * 384)
                        p = pq % 2; pq += 1
                        for k in range(3):
                            P.op("pe", lambda t, k=k, p=p, hl=hl, tsl=tsl: t.matmul(PQ[p][0:96, 0:384], WQBb[:, k, hl * 96:(hl + 1) * 96], CQN[:, k, tsl], start=(k == 0), stop=(k == 2)),
                                 reads=[bWQ, bCQN[tb]], writes=[bPQ[p]])
                        for k in range(3):
                            P.op("pe", lambda t, k=k, p=p, hl=hl, tsl=tsl: t.matmul(PQS[p][0:96, 0:384], WQSb[:, k, hl * 96:(hl + 1) * 96], CQN[:, k, tsl], start=(k == 0), stop=(k == 2)),
                                 reads=[bWQ, bCQN[tb]], writes=[bPQS[p]])
                        P.op("act", lambda a, p=p, hl=hl, tsl=tsl: a.copy(out=QT[0:64, hl, tsl], in_=PQ[p][0:64, 0:384]), reads=[bPQ[p]], writes=[bQT[hl]])
                        P.op("dve", lambda v, p=p, tsl=tsl: v.tensor_tensor(out=T1[p][64:96, :], in0=PQ[p][64:96, 0:384], in1=CSs[64:96, 0, tsl], op=ALU.mult), reads=[bPQ[p], bCS], writes=[bT1[p]])
                        P.op("dve", lambda v, p=p, tsl=tsl: v.tensor_tensor(out=T2[p][64:96, :], in0=PQS[p][64:96, 0:384], in1=CSs[64:96, 1, tsl], op=ALU.mult), reads=[bPQS[p], bCS], writes=[bT2[p]])
                        P.op("dve", lambda g, p=p, hl=hl, tsl=tsl: g.tensor_tensor(out=QT[64:96, hl, tsl], in0=T1[p][64:96, :], in1=T2[p][64:96, :], op=ALU.add), reads=[bT1[p], bT2[p]], writes=[bQT[hl]])
                        for k in range(2):
                            P.op("pe", lambda t, k=k, p=p, hl=hl, tsl=tsl: t.matmul(PS[p][0:64, 0:384], WKVb[:, k, hl * 128:hl * 128 + 64], CKVN[:, k, tsl], start=(k == 0), stop=(k == 1)),
                                 reads=[bWKV, bCKVN[tb]], writes=[bPS[p]])
                        P.op("act", lambda a, p=p, hl=hl, tsl=tsl: a.copy(out=KT[0:64, hl, tsl], in_=PS[p][0:64, 0:384]), reads=[bPS[p]], writes=[bKT[hl]])
                    P.op("dve", lambda g, hl=hl: g.tensor_copy(out=KT[64:96, hl, :], in_=KRT[64:96, :]), reads=bKRT, writes=[bKT[hl]])
                if STOP <= 2:
                    P.flush(); return
                for tt in range(NT):
                    p = tt % 2
                    for k in range(2):
                        P.op("pe", lambda t, k=k, p=p, tt=tt: t.matmul(PO[p][:, :], CKVN[:, k, tt * 128:(tt + 1) * 128], WKVb[:, k, :].rearrange("p (h d) -> p h d", d=128)[:, :, 64:128], start=(k == 0), stop=(k == 1)),
                             reads=[bWKV, bCKVN[tt // 3]], writes=[bPO[p]])
                    P.op("dve", lambda v, p=p, tt=tt: v.tensor_copy(out=VA[:, tt, :, 0:64], in_=PO[p][:, :].rearrange("p (h d) -> p h d", h=8)), reads=[bPO[p]], writes=[bVA[tt]])
                if STOP <= 3:
                    P.flush(); return
                POL = [PQ[0], PQ[1], PQS[0], PQS[1]]; bPOL = [bPQ[0], bPQ[1], bPQS[0], bPQS[1]]
                s3 = 0
                for hl in range(8):
                    h = hg * 8 + hl
                    s = s3 % 3; s3 += 1; ps = s % 2
                    for kc in range(2):
                        P.op("pe", lambda t, ps=ps, kc=kc, hl=hl: t.matmul(PS[ps][:, kc * 256:(kc + 1) * 256], KT[0:96, hl, kc * 128:(kc + 1) * 128], QT[0:96, hl, 0:256], start=True, stop=True),
                             reads=[bKT[hl], bQT[hl]], writes=[bPS[ps]])
                    P.op("act", lambda a, s=s, ps=ps: a.activation(out=PTs[s][:, :], in_=PS[ps][:, :], func=AF.Exp, scale=SCALE), reads=[bPS[ps]], writes=[bPT[s]])
                    for qt in range(2):
                        for kc in range(2):
                            P.op("pe", lambda t, s=s, qt=qt, kc=kc, hl=hl: t.matmul(POL[qt][:, 0:65], PTs[s][:, kc * 256 + qt * 128:kc * 256 + (qt + 1) * 128], VA[:, kc, hl, :], start=(kc == 0), stop=(kc == 1)),
                                 reads=[bPT[s], bVA[kc]], writes=[bPOL[qt]])
                    for qt in range(2):
                        P.op("dve", lambda v, qt=qt: v.reciprocal(out=REC[0][:, qt:qt + 1], in_=POL[qt][:, 64:65]), reads=[bPOL[qt]], writes=[bREC[0]])
                        P.op("act", lambda a, qt=qt, h=h: a.activation(out=YTM[:, qt, h * 64:(h + 1) * 64], in_=POL[qt][:, 0:64], func=AF.Copy, scale=REC[0][:, qt:qt + 1]),
                             reads=[bPOL[qt], bREC[0]], writes=[bYTM[qt]])
                steps = [(hl, qb, kc) for hl in range(8) for qb in range(4) for kc in range(NT)]
                PSA = [PS[0], PS[1], PO[0], PO[1]]; bPSA = [bPS[0], bPS[1], bPO[0], bPO[1]]
                LOOK = 2
                def emit_S(idx):
                    hl, qb, kc = steps[idx]; ps = idx % 4; q0 = 256 + qb * 512
                    P.op("pe", lambda t, ps=ps, kc=kc, hl=hl, q0=q0: t.matmul(PSA[ps][:, :], KT[0:96, hl, kc * 128:(kc + 1) * 128], QT[0:96, hl, q0:q0 + 512], start=True, stop=True),
                         reads=[bKT[hl], bQT[hl]], writes=[bPSA[ps]])
                for i0 in range(LOOK):
                    emit_S(i0)
                for idx, (hl, qb, kc) in enumerate(steps):
                    h = hg * 8 + hl
                    s = idx % 4; ps = idx % 4
                    if idx + LOOK < len(steps):
                        emit_S(idx + LOOK)
                    P.op("act", lambda a, s=s, ps=ps: a.activation(out=PTs[s][:, :], in_=PSA[ps][:, :], func=AF.Exp, scale=SCALE), reads=[bPSA[ps]], writes=[bPT[s]])
                    for j in range(4):
                        P.op("pe", lambda t, s=s, j=j, kc=kc, hl=hl: t.matmul(POL[j][:, 0:65], PTs[s][:, j * 128:(j + 1) * 128], VA[:, kc, hl, :], start=(kc == 0), stop=(kc == NT - 1)),
                             reads=[bPT[s], bVA[kc]], writes=[bPOL[j]])
                    if kc == NT - 1:
                        for j in range(4):
                            tile = 2 + qb * 4 + j
                            P.op("dve", lambda v, j=j: v.reciprocal(out=REC[1][:, j:j + 1], in_=POL[j][:, 64:65]), reads=[bPOL[j]], writes=[bREC[1]])
                            P.op("act", lambda a, j=j, h=h, tile=tile: a.activation(out=YTM[:, tile, h * 64:(h + 1) * 64], in_=POL[j][:, 0:64], func=AF.Copy, scale=REC[1][:, j:j + 1]),
                                 reads=[bPOL[j], bREC[1]], writes=[bYTM[tile]])
                P.flush()


PI = math.pi

def hy_host(conv_w, conv_b, b1, b2, b3, sin_freq, skip):
    H = {}
    H["cw"] = conv_w.reshape(3, 24, 128).transpose(2, 1, 0).copy()
    H["cb"] = conv_b.reshape(24, 128).T.copy()
    H["fb"] = np.stack([b1, b2, sin_freq[0], sin_freq[1]], 1).astype(np.float32).copy()
    max_decay = math.log(1e-2) / 0.3; min_decay = math.log(1e-2) / 1.5
    H["delta"] = np.abs(np.linspace(min_decay, max_decay, 1024, dtype=np.float32)).astype(np.float32)
    for L, tag in ((256, "c"), (2048, "l")):
        N = 2 * L
        t = np.linspace(0.0, 1.0, L, dtype=np.float32)
        w = (2.0 * np.float32(math.pi) / L) * np.arange(L, dtype=np.float32)
        f = np.linspace(1e-4, 15, 16, dtype=np.float32)
        ze = np.concatenate([t[:, None], np.cos(f[None, :] * w[:, None]), -np.sin(f[None, :] * w[:, None])], -1).astype(np.float32)
        H["ze" + tag] = ze.T.copy()
        nt = L // 128
        H["negt" + tag] = (-t).reshape(nt, 128).T.copy()
        idx = (np.arange(L, dtype=np.int64)[:, None] * np.arange(L, dtype=np.int64)[None, :]) % N
        ang = idx.astype(np.float64) * (2 * math.pi / N)
        Cm = np.cos(ang); Sm = -np.sin(ang)
        Sm[:, 0] = (-1.0) ** np.arange(L)
        H["C" + tag] = Cm.astype(NPBF); H["S" + tag] = Sm.astype(NPBF); H["ST" + tag] = Sm.T.copy().astype(NPBF)
        wsc = np.full((128, nt), 2.0 / N, np.float32); wsc[0, 0] = 1.0 / N
        H["wsc" + tag] = wsc
    return H


def mixer_hyena(P, K, run_norm, WIN, CW, CB, FW1, FW2, FW3, FB, FB3, SKIP, DELTA, TB, X0D, YTD, bYTD):
    cin = Buf(); bX0D = [Buf() for _ in range(8)]
    with contextlib.ExitStack() as st:
        GTM = P.sb(st, "h_GTM", [128, NT, D], BF16); bGTM = [Buf() for _ in range(NT)]
        with contextlib.ExitStack() as st2:
            HT = P.sb(st2, "HT", [128, 8, T], BF16); bHT = [Buf() for _ in range(NT)]
            run_norm(HT, bHT)
            X1T = P.sb(st2, "h_X1T", [128, 8, T], BF16); bX1T = [Buf() for _ in range(8)]
            WB = P.sb(st2, "h_WB", [128, 8, 1024], BF16); bWB = [Buf() for _ in range(8)]
            STG = [P.sb(st2, f"h_stg{i}", [128, 1024], F32) for i in range(2)]; bSTG = [Buf() for _ in range(2)]
            CWs = P.sb(st2, "h_CW", [128, 24, 3], F32); CBs = P.sb(st2, "h_CB", [128, 24], F32); bCW = Buf()
            ZCs = [P.sb(st2, f"h_ZC{i}", [128, T], F32) for i in range(2)]; bZCs = [Buf() for _ in range(2)]
            ACs = [P.sb(st2, f"h_AC{i}", [128, T], F32) for i in range(2)]; bACs = [Buf() for _ in range(2)]
            OB = [P.sb(st2, f"h_OB{i}", [128, T], BF16) for i in range(2)]; bOB = [Buf() for _ in range(2)]
            PP = [P.ps(st2, f"h_PP{i}") for i in range(4)]; bPP = [Buf(excl=True) for _ in range(4)]
            PT = [P.ps(st2, f"h_PT{i}", [128, 8, 128], BF16) for i in range(2)]; bPT = [Buf(excl=True) for _ in range(2)]
            P.dma(CWs[:, :, :], CW[:, :, :], reads=[cin], writes=[bCW])
            P.dma(CBs[:, :], CB[:, :], reads=[cin], writes=[bCW])
            pi = 0
            for which in range(3):
                for k in range(8):
                    s = k % 2
                    P.dma(STG[s][:, :], WIN[k * 128:(k + 1) * 128, which * 1024:(which + 1) * 1024], reads=[cin], writes=[bSTG[s]])
                    if k % 2 == 0:
                        P.op("dve", lambda g, k=k, s=s: g.tensor_copy(out=WB[:, k, :], in_=STG[s][:, :]), reads=[bSTG[s]], writes=[bWB[k]])
                    else:
                        P.op("act", lambda a, k=k, s=s: a.copy(out=WB[:, k, :], in_=STG[s][:, :]), reads=[bSTG[s]], writes=[bWB[k]])
                for c in range(8):
                    cc = which * 8 + c
                    ZC = ZCs[c % 2]; bZC = bZCs[c % 2]; AC = ACs[c % 2]; bAC = bACs[c % 2]
                    for tb in range(6):
                        p = pi % 4; pi += 1
                        for k in range(8):
                            P.op("pe", lambda t, k=k, p=p, c=c, tb=tb: t.matmul(PP[p][:, 0:384], WB[:, k, c * 128:(c + 1) * 128], HT[:, k, tb * 384:(tb + 1) * 384], start=(k == 0), stop=(k == 7)),
                                 reads=[bWB[k]] + bHT[tb * 3:tb * 3 + 3], writes=[bPP[p]])
                        P.op("act", lambda a, p=p, tb=tb, ZC=ZC: a.copy(out=ZC[:, tb * 384:(tb + 1) * 384], in_=PP[p][:, 0:384]), reads=[bPP[p]], writes=[bZC])
                        P.op("act", lambda a, p=p, tb=tb, AC=AC, cc=cc: a.activation(out=AC[:, tb * 384:(tb + 1) * 384], in_=PP[p][:, 0:384], func=AF.Identity, scale=CWs[:, cc, 1:2], bias=CBs[:, cc:cc + 1]), reads=[bPP[p], bCW], writes=[bAC])
                    for (o0, o1, i0, i1, tap) in ((1, 256, 0, 255, 0), (257, T, 256, T - 1, 0), (0, 255, 1, 256, 2), (256, T - 1, 257, T, 2)):
                        P.op("dve", lambda v, cc=cc, o0=o0, o1=o1, i0=i0, i1=i1, tap=tap, ZC=ZC, AC=AC: v.scalar_tensor_tensor(out=AC[:, o0:o1], in0=ZC[:, i0:i1], scalar=CWs[:, cc, tap:tap + 1], in1=AC[:, o0:o1], op0=ALU.mult, op1=ALU.add),
                             reads=[bZC, bAC, bCW], writes=[bAC])
                    if which == 0:
                        b = c % 2
                        P.op("act", lambda a, b=b, AC=AC: a.copy(out=OB[b][:, :], in_=AC[:, :]), reads=[bAC], writes=[bOB[b]])
                        P.dma(X0D[c * 128:(c + 1) * 128, :], OB[b][:, :], reads=[bOB[b]], writes=[bX0D[c]])
                    elif which == 1:
                        P.op("act", lambda a, c=c, AC=AC: a.copy(out=X1T[:, c, :], in_=AC[:, :]), reads=[bAC], writes=[bX1T[c]])
                    else:
                        b = c % 2
                        P.op("dve", lambda g, b=b, c=c, AC=AC: g.tensor_tensor(out=OB[b][:, :], in0=AC[:, :], in1=X1T[:, c, :], op=ALU.mult), reads=[bAC, bX1T[c]], writes=[bOB[b]])
                        for tt in range(NT):
                            q = tt // 8
                            P.op("pe", lambda t, b=b, tt=tt: t.transpose(PT[(tt // 8) % 2][:, tt % 8, :], OB[b][:, tt * 128:(tt + 1) * 128], K["idb"][:, :]), reads=[bOB[b], K["b_idb"]], writes=[bPT[q % 2]])
                            if tt % 8 == 7 or tt == NT - 1:
                                t0 = (tt // 8) * 8; n_ = tt - t0 + 1
                                P.op("act", lambda a, q=q, t0=t0, n_=n_, c=c: a.copy(out=GTM[:, t0:t0 + n_, c * 128:(c + 1) * 128], in_=PT[q % 2][:, 0:n_, :]), reads=[bPT[q % 2]], writes=bGTM[t0:t0 + n_])
            P.flush()
        with contextlib.ExitStack() as st2:
            W1 = P.sb(st2, "h_W1", [33, 64], F32); W2 = P.sb(st2, "h_W2", [64, 64], F32); W3 = P.sb(st2, "h_W3", [64, 2048], F32); bW = Buf()
            FBs = P.sb(st2, "h_FB", [64, 4], F32); B3 = P.sb(st2, "h_B3", [1, 2048], F32); ONES1 = P.sb(st2, "h_ON1", [1, 128], F32)
            SKs = P.sb(st2, "h_SK", [1, 1024], F32); DEL = P.sb(st2, "h_DEL", [128, 1024], F32)
            ARG = P.sb(st2, "h_ARG", [64, 512], F32); bARG = Buf(); MM = P.sb(st2, "h_MM", [64, 512], F32); bMM = Buf()
            H1 = P.sb(st2, "h_H1", [64, 2048], F32); bH1 = Buf(); H2 = P.sb(st2, "h_H2", [64, 2048], F32); bH2 = Buf()
            ZE = P.sb(st2, "h_ZE", [33, 2048], F32); bZE = Buf()
            NEGT = P.sb(st2, "h_NEGT", [128, 16], F32); WSC = P.sb(st2, "h_WSC", [128, 16], F32); bTBL = Buf()
            EE = P.sb(st2, "h_EE", [128, 512], F32); bEE = Buf()
            HF = P.sb(st2, "h_HF", [128, 16, 512], BF16); bHF = Buf(); HB = P.sb(st2, "h_HB", [128, 16, 512], BF16); bHB = Buf()
            YRE = P.sb(st2, "h_YRE", [128, 16, 512], BF16); bYRE = [Buf() for _ in range(16)]
            YIM = P.sb(st2, "h_YIM", [128, 16, 512], BF16); bYIM = [Buf() for _ in range(16)]
            CT = [P.sb(st2, f"h_CT{i}", [128, 16, 128], BF16) for i in range(1)] * 2; bCT = [Buf()] * 2
            STt = [P.sb(st2, f"h_ST{i}", [128, 16, 128], BF16) for i in range(1)] * 2; bST = [Buf()] * 2
            CI = P.sb(st2, "h_CI", [128, 16, 256], BF16); bCI = Buf(); SI = P.sb(st2, "h_SI", [128, 16, 256], BF16); bSI = Buf()
            TM = [P.sb(st2, f"h_TM{i}", [128, 512], F32) for i in range(9)]; bTM = [Buf() for _ in range(9)]
            F0 = TM[8]; bF0 = bTM[8]
            GcS = P.sb(st2, "h_GcS", [128, 512], F32); bGcS = Buf()
            X0L = [P.sb(st2, f"h_X0L{i}", [128, 512], BF16) for i in range(2)]; bX0L = [Buf() for _ in range(2)]
            YO = [P.sb(st2, f"h_YO{i}", [128, 512], BF16) for i in range(2)]; bYO = [Buf() for _ in range(2)]
            PF = [P.ps(st2, f"h_PF{i}") for i in range(6)]; bPF = [Buf(excl=True) for _ in range(6)]
            PI_ = [P.ps(st2, f"h_PI{i}") for i in range(2)]; bPI = [Buf(excl=True) for _ in range(2)]
            P.dma(W1[:, :], FW1[:, :], reads=[cin], writes=[bW]); P.dma(W2[:, :], FW2[:, :], reads=[cin], writes=[bW]); P.dma(W3[:, :], FW3[:, :], reads=[cin], writes=[bW])
            P.dma(FBs[:, :], FB[:, :], reads=[cin], writes=[bW]); P.dma(B3[:, :], FB3[:, :], reads=[cin], writes=[bW])
            P.dma(SKs[:, :], SKIP[:, :], reads=[cin], writes=[bW]); P.dma(DEL[:, :], DELTA.partition_broadcast(128), reads=[cin], writes=[bW])
            P.op("dve", lambda v: v.memset(ONES1[:, :], 1.0), writes=[bW])
            ci = 0; xi = 0
            for tag, L, tk0 in (("c", 256, 0), ("l", 2048, 256)):
                tb_ = TB[tag]; nt = L // 128; tile0 = tk0 // 128
                P.dma(ZE[:, 0:L], tb_["ze"][:, :], reads=[cin], writes=[bZE])
                P.dma(NEGT[:, 0:nt], tb_["negt"][:, :], reads=[cin], writes=[bTBL]); P.dma(WSC[:, 0:nt], tb_["wsc"][:, :], reads=[cin], writes=[bTBL])
                for (Wl, kin, SRC, bSRC, DST, bDST, bcol, scol) in ((W1, 33, ZE, bZE, H1, bH1, 0, 2), (W2, 64, H1, bH1, H2, bH2, 1, 3)):
                    for blk in range(0, L, 512):
                        n_ = min(512, L - blk)
                        P.op("pe", lambda t, Wl=Wl, kin=kin, SRC=SRC, blk=blk, n_=n_: t.matmul(PI_[0][0:64, 0:n_], Wl[0:kin, :], SRC[0:kin, blk:blk + n_], start=True, stop=True), reads=[bW, bSRC], writes=[bPI[0]])
                        P.op("dve", lambda v, n_=n_, bcol=bcol, scol=scol: v.tensor_scalar(out=ARG[:, 0:n_], in0=PI_[0][0:64, 0:n_], scalar1=FBs[:, bcol:bcol + 1], scalar2=FBs[:, scol:scol + 1], op0=ALU.add, op1=ALU.mult), reads=[bPI[0], bW], writes=[bARG])
                        P.op("dve", lambda v, n_=n_: v.tensor_scalar(out=MM[:, 0:n_], in0=ARG[:, 0:n_], scalar1=PI, scalar2=None, op0=ALU.is_gt), reads=[bARG], writes=[bMM])
                        P.op("dve", lambda v, n_=n_: v.scalar_tensor_tensor(out=ARG[:, 0:n_], in0=MM[:, 0:n_], scalar=-2 * PI, in1=ARG[:, 0:n_], op0=ALU.mult, op1=ALU.add), reads=[bMM, bARG], writes=[bARG])
                        P.op("dve", lambda v, n_=n_: v.tensor_scalar(out=MM[:, 0:n_], in0=ARG[:, 0:n_], scalar1=-PI, scalar2=None, op0=ALU.is_lt), reads=[bARG], writes=[bMM])
                        P.op("dve", lambda v, n_=n_: v.scalar_tensor_tensor(out=ARG[:, 0:n_], in0=MM[:, 0:n_], scalar=2 * PI, in1=ARG[:, 0:n_], op0=ALU.mult, op1=ALU.add), reads=[bMM, bARG], writes=[bARG])
                        P.op("act", lambda a, DST=DST, blk=blk, n_=n_: a.activation(out=DST[:, blk:blk + n_], in_=ARG[:, 0:n_], func=AF.Sin), reads=[bARG], writes=[bDST])
                for hh in range(2):
                    c0 = hh * 512
                    for tl in range(nt):
                        P.op("act", lambda a, tl=tl, c0=c0: a.activation(out=EE[:, 0:512], in_=DEL[:, c0:c0 + 512], func=AF.Exp, scale=NEGT[:, tl:tl + 1]), reads=[bW, bTBL], writes=[bEE])
                        for fi, (DSTF, bDSTF) in enumerate(((HF, bHF), (HB, bHB))):
                            w0 = fi * 1024 + c0
                            P.op("pe", lambda t, tl=tl, w0=w0: t.matmul(PI_[1][:, :], H2[:, tl * 128:(tl + 1) * 128], W3[:, w0:w0 + 512], start=True, stop=False), reads=[bH2, bW], writes=[bPI[1]])
                            P.op("pe", lambda t, w0=w0: t.matmul(PI_[1][:, :], ONES1[:, :], B3[:, w0:w0 + 512], start=False, stop=True), reads=[bW], writes=[bPI[1]])
                            if tl == 0:
                                P.op("dve", lambda v: v.scalar_tensor_tensor(out=F0[:, :], in0=EE[:, 0:512], scalar=0.05, in1=PI_[1][:, :], op0=ALU.add, op1=ALU.mult), reads=[bEE, bPI[1]], writes=[bF0])
                                if fi == 0:
                                    P.op("dve", lambda v, c0=c0: v.tensor_tensor(out=F0[0:1, :], in0=F0[0:1, :], in1=SKs[0:1, c0:c0 + 512], op=ALU.add), reads=[bF0, bW], writes=[bF0])
                                else:
                                    P.op("dve", lambda v: v.memset(F0[0:1, :], 0.0), reads=[bF0], writes=[bF0])
                                P.op("dve", lambda v, DSTF=DSTF: v.tensor_copy(out=DSTF[:, 0, :], in_=F0[:, :]), reads=[bF0], writes=[bDSTF])
                            else:
                                P.op("dve", lambda v, DSTF=DSTF, tl=tl: v.scalar_tensor_tensor(out=DSTF[:, tl, :], in0=EE[:, 0:512], scalar=0.05, in1=PI_[1][:, :], op0=ALU.add, op1=ALU.mult), reads=[bEE, bPI[1]], writes=[bDSTF])
                    for fc in range(nt):
                        cb_ = ci % 2; ci += 1
                        if cb_ == 0:
                            Cv = CT[0][:, 0:nt, :]; Sv = STt[0][:, 0:nt, :]; bCv = bCT[0]; bSv = bST[0]
                        else:
                            Cv = CI[:, 0:nt, 0:128]; Sv = SI[:, 0:nt, 0:128]; bCv = bCI; bSv = bSI
                        P.dma(Cv, tb_["C"].rearrange("(tc p) f -> p tc f", p=128)[:, :, fc * 128:(fc + 1) * 128], reads=[cin], writes=[bCv])
                        P.dma(Sv, tb_["S"].rearrange("(tc p) f -> p tc f", p=128)[:, :, fc * 128:(fc + 1) * 128], reads=[cin], writes=[bSv])
                        srcs = [(lambda tc, tile0=tile0, c0=c0: GTM[:, tile0 + tc, c0:c0 + 512], lambda tc, tile0=tile0: [bGTM[tile0 + tc]]), (lambda tc: HF[:, tc, :], lambda tc: [bHF]), (lambda tc: HB[:, tc, :], lambda tc: [bHB])]
                        for si_, (sf_, bf_) in enumerate(srcs):
                            for ti_, (TAB, bTAB) in enumerate(((Cv, bCv), (Sv, bSv))):
                                pf = si_ * 2 + ti_
                                for tc in range(nt):
                                    P.op("pe", lambda t, pf=pf, TAB=TAB, tc=tc, sf_=sf_, nt=nt: t.matmul(PF[pf][:, :], TAB[:, tc, :], sf_(tc), start=(tc == 0), stop=(tc == nt - 1)), reads=[bTAB] + bf_(tc), writes=[bPF[pf]])
                        Gc, Gs, Hfc, Hfs, Hbc, Hbs = PF; bGc, bGs, bHfc, bHfs, bHbc, bHbs = bPF
                        w_ = WSC[:, fc:fc + 1]
                        HbcW, HbsW, GsS, Kre, Kim, t1, t2, t3, t4 = TM; bHbcW, bHbsW, bGsS, bKre, bKim, bt1, bt2, bt3, bt4 = bTM
                        P.op("act", lambda a, w_=w_: a.activation(out=HbcW[:, :], in_=Hbc[:, :], func=AF.Copy, scale=w_), reads=[bHbc, bTBL], writes=[bHbcW])
                        P.op("act", lambda a, w_=w_: a.activation(out=HbsW[:, :], in_=Hbs[:, :], func=AF.Copy, scale=w_), reads=[bHbs, bTBL], writes=[bHbsW])
                        P.op("act", lambda a: a.copy(out=GsS[:, :], in_=Gs[:, :]), reads=[bGs], writes=[bGsS])
                        P.op("act", lambda a: a.copy(out=GcS[:, :], in_=Gc[:, :]), reads=[bGc], writes=[bGcS])
                        P.op("dve", lambda v, w_=w_: v.scalar_tensor_tensor(out=Kre[:, :], in0=Hfc[:, :], scalar=w_, in1=HbcW[:, :], op0=ALU.mult, op1=ALU.add), reads=[bHfc, bTBL, bHbcW], writes=[bKre])
                        P.op("dve", lambda v, w_=w_: v.scalar_tensor_tensor(out=Kim[:, :], in0=Hfs[:, :], scalar=w_, in1=HbsW[:, :], op0=ALU.mult, op1=ALU.subtract), reads=[bHfs, bTBL, bHbsW], writes=[bKim])
                        P.op("dve", lambda v: v.tensor_tensor(out=t1[:, :], in0=GcS[:, :], in1=Kre[:, :], op=ALU.mult), reads=[bGcS, bKre], writes=[bt1])
                        P.op("dve", lambda g: g.tensor_tensor(out=t2[:, :], in0=GsS[:, :], in1=Kim[:, :], op=ALU.mult), reads=[bGsS, bKim], writes=[bt2])
                        P.op("dve", lambda g, fc=fc: g.tensor_tensor(out=YRE[:, fc, :], in0=t1[:, :], in1=t2[:, :], op=ALU.subtract), reads=[bt1, bt2], writes=[bYRE[fc]])
                        P.op("dve", lambda v: v.tensor_tensor(out=t3[:, :], in0=GcS[:, :], in1=Kim[:, :], op=ALU.mult), reads=[bGcS, bKim], writes=[bt3])
                        P.op("dve", lambda g: g.tensor_tensor(out=t4[:, :], in0=GsS[:, :], in1=Kre[:, :], op=ALU.mult), reads=[bGsS, bKre], writes=[bt4])
                        P.op("dve", lambda g, fc=fc: g.tensor_tensor(out=YIM[:, fc, :], in0=t3[:, :], in1=t4[:, :], op=ALU.add), reads=[bt3, bt4], writes=[bYIM[fc]])
                        if fc == 0:
                            P.op("dve", lambda v: v.tensor_copy(out=YRE[0:1, 0, :], in_=t1[0:1, :]), reads=[bt1, bYRE[0]], writes=[bYRE[0]])
                            P.op("dve", lambda v, w_=w_: v.scalar_tensor_tensor(out=t2[0:1, :], in0=Hfs[0:1, :], scalar=WSC[0:1, 0:1], in1=HbsW[0:1, :], op0=ALU.mult, op1=ALU.add), reads=[bHfs, bTBL, bHbsW, bt2], writes=[bt2])
                            P.op("dve", lambda g: g.tensor_tensor(out=YIM[0:1, 0, :], in0=GsS[0:1, :], in1=t2[0:1, :], op=ALU.mult), reads=[bGsS, bt2, bYIM[0]], writes=[bYIM[0]])
                    for blk in range(0, L, 256):
                        n_ = min(256, L - blk)
                        P.dma(CI[:, 0:nt, 0:n_], tb_["C"].rearrange("(fc p) t -> p fc t", p=128)[:, :, blk:blk + n_], reads=[cin], writes=[bCI])
                        P.dma(SI[:, 0:nt, 0:n_], tb_["ST"].rearrange("(fc p) t -> p fc t", p=128)[:, :, blk:blk + n_], reads=[cin], writes=[bSI])
                        for cch in range(4):
                            pb = cch % 2
                            chan = hh * 4 + cch
                            xb = xi % 2; xi += 1
                            P.dma(X0L[xb][:, 0:n_], X0D[chan * 128:(chan + 1) * 128, tk0 + blk:tk0 + blk + n_], reads=[bX0D[chan]], writes=[bX0L[xb]])
                            for fc in range(nt):
                                P.op("pe", lambda t, pb=pb, fc=fc, cch=cch, n_=n_: t.matmul(PI_[pb][:, 0:n_], YRE[:, fc, cch * 128:(cch + 1) * 128], CI[:, fc, 0:n_], start=(fc == 0), stop=False), reads=[bYRE[fc], bCI], writes=[bPI[pb]])
                                P.op("pe", lambda t, pb=pb, fc=fc, cch=cch, n_=n_, nt=nt: t.matmul(PI_[pb][:, 0:n_], YIM[:, fc, cch * 128:(cch + 1) * 128], SI[:, fc, 0:n_], start=False, stop=(fc == nt - 1)), reads=[bYIM[fc], bSI], writes=[bPI[pb]])
                            P.op("dve", lambda v, pb=pb, xb=xb, n_=n_: v.tensor_tensor(out=YO[xb][:, 0:n_], in0=PI_[pb][:, 0:n_], in1=X0L[xb][:, 0:n_], op=ALU.mult), reads=[bPI[pb], bX0L[xb]], writes=[bYO[xb]])
                            P.dma(YTD[chan * 128:(chan + 1) * 128, tk0 + blk:tk0 + blk + n_], YO[xb][:, 0:n_], reads=[bYO[xb]], writes=[bYTD[chan]])
            P.flush()


def outproj_fm_dram(P, K, YTD, bYTD, WO, X, bX, MODS, g_off, tiles=range(NT), norm2=None):
    with contextlib.ExitStack() as st:
        YT = P.sb(st, "ofm_YT", [128, 8, T], BF16); bYT = [Buf() for _ in range(NT)]
        for k in range(8):
            P.dma(YT[:, k, :], YTD[k * 128:(k + 1) * 128, :], reads=[bYTD[k]], writes=bYT)
        phase_outproj(P, K, (YT, bYT), WO, X, bX, MODS, g_off, tiles=tiles, mode="fm", norm2=norm2)


LN8 = math.log(0.125)

def ml_host(conv_w, conv_b):
    cw = conv_w.reshape(3, 8, 128).transpose(2, 1, 0).copy()
    cb = conv_b.reshape(8, 128).T.copy()
    sel8 = np.zeros((8, 8, 128), np.float32)
    for h in range(8): sel8[h, h, :] = 1
    s = np.arange(128)
    tri = np.zeros((128, 2, 128), np.float32)
    tri[:, 0, :] = (s[:, None] <= s[None, :])
    tri[:, 1, :] = (s[:, None] >= s[None, :])
    return cw, cb, sel8, tri


def hs_scan(P, src, bsrc, A, bA, B, bB, a0, a1, op, reverse, nparts=8):
    n = a1 - a0
    sh = 1
    cur, bcur = src, bsrc
    nxt = [(A, bA), (B, bB)]
    i = 0
    while sh < n:
        dst, bdst = nxt[i % 2]; i += 1
        if not reverse:
            P.op("dve", lambda v, cur=cur, dst=dst, sh=sh: v.tensor_tensor(out=dst[0:nparts, a0 + sh:a1], in0=cur[0:nparts, a0 + sh:a1], in1=cur[0:nparts, a0:a1 - sh], op=op), reads=[bcur], writes=[bdst])
            P.op("pool", lambda g, cur=cur, dst=dst, sh=sh: g.tensor_copy(out=dst[0:nparts, a0:a0 + sh], in_=cur[0:nparts, a0:a0 + sh]), reads=[bcur], writes=[bdst])
        else:
            P.op("dve", lambda v, cur=cur, dst=dst, sh=sh: v.tensor_tensor(out=dst[0:nparts, a0:a1 - sh], in0=cur[0:nparts, a0:a1 - sh], in1=cur[0:nparts, a0 + sh:a1], op=op), reads=[bcur], writes=[bdst])
            P.op("pool", lambda g, cur=cur, dst=dst, sh=sh: g.tensor_copy(out=dst[0:nparts, a1 - sh:a1], in_=cur[0:nparts, a1 - sh:a1]), reads=[bcur], writes=[bdst])
        cur, bcur = dst, bdst
        sh *= 2
    return cur, bcur


def mixer_mlstm(P, K, run_norm, WIN, CW, CB, GB, ONG, SEL8, TRI, SIGO, YTM, bYTM):
    cin = Buf(); bSIGO = [Buf() for _ in range(NT)]
    with contextlib.ExitStack() as st:
        HT = P.sb(st, "HT", [128, 8, T], BF16); bHT = [Buf() for _ in range(NT)]
        run_norm(HT, bHT)
        NEGM = [P.sb(st, f"l_NEGM{d}", [8, T], F32) for d in range(2)]; bNEGM = [Buf(), Buf()]
        ATM = P.sb(st, "l_ATM", [128, NT, 16], F32); bATM = Buf()
        EMT = P.sb(st, "l_EMT", [128, NT, 16], F32); bEMT = Buf()
        if True:
            with contextlib.ExitStack() as st3:
                WG = P.sb(st3, "l_WG", [128, 8, 32], BF16); bWG = Buf()
                WGf = P.sb(st3, "l_WGf", [128, 8, 32], F32); bWGf = Buf()
                GBs = P.sb(st3, "l_GB", [128, 32], F32); bGB = Buf()
                ONE = P.sb(st3, "l_ONE", [128, 1], F32); bONE = Buf()
                GT = P.sb(st3, "l_GT", [128, NT, 32], F32); bGT = [Buf() for _ in range(NT)]
                TMPg = P.sb(st3, "l_TMPg", [128, NT, 32], F32)
                SC = [P.sb(st3, f"l_SC{i}", [8, T], F32) for i in range(10)]; bSC = [Buf() for _ in range(10)]
                PGt = [P.ps(st3, f"l_PG{i}") for i in range(2)]; bPG = [Buf(excl=True) for _ in range(2)]
                PTr = [P.ps(st3, f"l_PTr{i}") for i in range(4)]; bPTr = [Buf(excl=True) for _ in range(4)]
                P.dma(WGf[:, :, :], WIN.rearrange("(c p) f -> p c f", p=128)[:, :, 3072:3104], reads=[cin], writes=[bWGf])
                P.op("dve", lambda v: v.tensor_copy(out=WG[:, :, :], in_=WGf[:, :, :]), reads=[bWGf], writes=[bWG])
                P.dma(GBs[:, :], GB.partition_broadcast(128), reads=[cin], writes=[bGB])
                P.op("dve", lambda v: v.memset(ONE[:, :], 1.0), writes=[bONE])
                for tt in range(NT):
                    p = tt % 2
                    for k in range(8):
                        P.op("pe", lambda t, k=k, p=p, tt=tt: t.matmul(PGt[p][:, 0:32], HT[:, k, tt * 128:(tt + 1) * 128], WG[:, k, :], start=(k == 0), stop=(k == 7)), reads=[bHT[tt], bWG], writes=[bPG[p]])
                    P.op("dve", lambda v, p=p, tt=tt: v.tensor_tensor(out=GT[:, tt, :], in0=PGt[p][:, 0:32], in1=GBs[:, :], op=ALU.add), reads=[bPG[p], bGB], writes=[bGT[tt]])
                    for j in (1, 3):
                        sl = slice(j * 8, (j + 1) * 8)
                        P.op("act", lambda a, tt=tt, sl=sl: a.activation(out=TMPg[:, tt, sl], in_=GT[:, tt, sl], func=AF.Exp, scale=-1.0), reads=[bGT[tt]], writes=[bGT[tt]])
                        P.op("act", lambda a, tt=tt, sl=sl: a.activation(out=TMPg[:, tt, sl], in_=TMPg[:, tt, sl], func=AF.Ln, bias=ONE[:, :]), reads=[bGT[tt], bONE], writes=[bGT[tt]])
                        P.op("dve", lambda v, tt=tt, sl=sl: v.tensor_scalar(out=GT[:, tt, sl], in0=TMPg[:, tt, sl], scalar1=-1.0, scalar2=None, op0=ALU.mult), reads=[bGT[tt]], writes=[bGT[tt]])
                    for j in range(4):
                        P.op("pe", lambda t, j=j, tt=tt: t.transpose(PTr[j][0:8, 0:128], GT[:, tt, j * 8:(j + 1) * 8], K["idf"][:, :]), reads=[bGT[tt], K["b_idf"]], writes=[bPTr[j]])
                        P.op("act", lambda a, j=j, tt=tt: a.copy(out=SC[j][:, tt * 128:(tt + 1) * 128], in_=PTr[j][0:8, 0:128]), reads=[bPTr[j]], writes=[bSC[j]])
                for d in range(2):
                    IG, bIG, LF, bLF = SC[2 * d], bSC[2 * d], SC[2 * d + 1], bSC[2 * d + 1]
                    w = [(SC[4 + i], bSC[4 + i]) for i in range(6)]
                    if d == 0:
                        F_, bF = hs_scan(P, LF, bLF, w[0][0], w[0][1], w[1][0], w[1][1], 0, T, ALU.add, False)
                    else:
                        Fc, bFc = hs_scan(P, LF, bLF, w[0][0], w[0][1], w[1][0], w[1][1], 0, 256, ALU.add, True)
                        Fl, bFl = hs_scan(P, LF, bLF, w[2][0], w[2][1], w[3][0], w[3][1], 256, T, ALU.add, True)
                        F_, bF = w[4]
                        P.op("dve", lambda v, Fc=Fc: v.tensor_copy(out=F_[:, 0:256], in_=Fc[:, 0:256]), reads=[bFc], writes=[bF])
                        P.op("dve", lambda v, Fl=Fl, Fc=Fc: v.tensor_scalar(out=F_[:, 256:T], in0=Fl[:, 256:T], scalar1=Fc[:, 0:1], scalar2=None, op0=ALU.add), reads=[bFl, bFc], writes=[bF])
                    P.op("dve", lambda v, IG=IG, F_=F_: v.tensor_tensor(out=IG[:, :], in0=IG[:, :], in1=F_[:, :], op=ALU.subtract), reads=[bIG, bF], writes=[bIG])
                    if d == 0:
                        free = [x for x in w if x[0] is not F_]
                        CM, bCM = hs_scan(P, IG, bIG, free[0][0], free[0][1], free[1][0], free[1][1], 0, T, ALU.max, False)
                        Mt, bM = free[2]
                        P.op("dve", lambda v, CM=CM, Mt=Mt: v.tensor_scalar(out=Mt[:, :], in0=CM[:, :], scalar1=0.0, scalar2=None, op0=ALU.max), reads=[bCM], writes=[bM])
                    else:
                        CMc, bCMc = hs_scan(P, IG, bIG, w[0][0], w[0][1], w[1][0], w[1][1], 0, 256, ALU.max, True)
                        CMl, bCMl = hs_scan(P, IG, bIG, w[2][0], w[2][1], w[3][0], w[3][1], 256, T, ALU.max, True)
                        Mt, bM = w[5]
                        P.op("dve", lambda v, CMc=CMc, Mt=Mt: v.tensor_scalar(out=Mt[:, 0:256], in0=CMc[:, 0:256], scalar1=0.0, scalar2=None, op0=ALU.max), reads=[bCMc], writes=[bM])
                        P.op("dve", lambda v, CMl=CMl, CMc=CMc, Mt=Mt: v.tensor_scalar(out=Mt[:, 256:T], in0=CMl[:, 256:T], scalar1=CMc[:, 0:1], scalar2=0.0, op0=ALU.max, op1=ALU.max), reads=[bCMl, bCMc], writes=[bM])
                    P.op("dve", lambda v, d=d, Mt=Mt: v.tensor_scalar(out=NEGM[d][:, :], in0=Mt[:, :], scalar1=-1.0, scalar2=None, op0=ALU.mult), reads=[bM], writes=[bNEGM[d]])
                    P.op("dve", lambda v, LF=LF, F_=F_, Mt=Mt: v.tensor_tensor(out=LF[:, :], in0=F_[:, :], in1=Mt[:, :], op=ALU.add), reads=[bF, bM], writes=[bLF])
                    P.op("act", lambda a, LF=LF: a.activation(out=LF[:, :], in_=LF[:, :], func=AF.Exp, scale=-1.0), reads=[bLF], writes=[bLF])
                    P.op("dve", lambda v, IG=IG: v.tensor_scalar(out=IG[:, :], in0=IG[:, :], scalar1=LN8, scalar2=None, op0=ALU.add), reads=[bIG], writes=[bIG])
                    for tt in range(NT):
                        j = tt % 2
                        P.op("pe", lambda t, j=j, tt=tt, IG=IG: t.transpose(PTr[j][:, 0:8], IG[:, tt * 128:(tt + 1) * 128], K["idf"][0:8, 0:8]), reads=[bIG, K["b_idf"]], writes=[bPTr[j]])
                        P.op("act", lambda a, j=j, tt=tt, d=d: a.copy(out=ATM[:, tt, d * 8:(d + 1) * 8], in_=PTr[j][:, 0:8]), reads=[bPTr[j]], writes=[bATM])
                        P.op("pe", lambda t, j=j, tt=tt, LF=LF: t.transpose(PTr[2 + j][:, 0:8], LF[:, tt * 128:(tt + 1) * 128], K["idf"][0:8, 0:8]), reads=[bLF, K["b_idf"]], writes=[bPTr[2 + j]])
                        P.op("act", lambda a, j=j, tt=tt, d=d: a.copy(out=EMT[:, tt, d * 8:(d + 1) * 8], in_=PTr[2 + j][:, 0:8]), reads=[bPTr[2 + j]], writes=[bEMT])
                P.flush()
            QKT = P.sb(st, "l_QKT", [128, 8, T], BF16); bQKT = [Buf() for _ in range(8)]
            VA = P.sb(st, "l_VA", [128, NT, 8, 129], BF16); bVA = [Buf() for _ in range(NT)]
            with contextlib.ExitStack() as st3:
                WB = P.sb(st3, "l_WB", [128, 8, 1024], BF16); bWB = [Buf() for _ in range(8)]
                STG = [P.sb(st3, f"l_stg{i}", [128, 1024], F32) for i in range(2)]; bSTG = [Buf() for _ in range(2)]
                CWs = P.sb(st3, "l_CW", [128, 8, 3], F32); CBs = P.sb(st3, "l_CB", [128, 8], F32); bCW = Buf()
                ZC = [P.sb(st3, f"l_ZC{i}", [128, T], F32) for i in range(1)] * 2; bZC = [Buf()] * 2
                AC = [P.sb(st3, f"l_AC{i}", [128, T], F32) for i in range(1)] * 2; bAC = [Buf()] * 2
                SO = [P.sb(st3, f"l_SO{i}", [128, 1024], BF16) for i in range(2)]; bSO = [Buf() for _ in range(2)]
                PP = [P.ps(st3, f"l_PP{i}") for i in range(4)]; bPP = [Buf(excl=True) for _ in range(4)]
                P.dma(CWs[:, :, :], CW[:, :, :], reads=[cin], writes=[bCW])
                P.dma(CBs[:, :], CB[:, :], reads=[cin], writes=[bCW])
                P.op("dve", lambda v: v.memset(VA[:, :, :, 128:129], 1.0), writes=bVA)
                pi = 0
                for which in range(3):
                    for k in range(8):
                        s = k % 2
                        P.dma(STG[s][:, :], WIN[k * 128:(k + 1) * 128, which * 1024:(which + 1) * 1024], reads=[cin], writes=[bSTG[s]])
                        if k % 2 == 0:
                            P.op("dve", lambda g, k=k, s=s: g.tensor_copy(out=WB[:, k, :], in_=STG[s][:, :]), reads=[bSTG[s]], writes=[bWB[k]])
                        else:
                            P.op("act", lambda a, k=k, s=s: a.copy(out=WB[:, k, :], in_=STG[s][:, :]), reads=[bSTG[s]], writes=[bWB[k]])
                    if which == 0:
                        for c in range(8):
                            b = c % 2
                            for tb in range(6):
                                p = pi % 4; pi += 1
                                for k in range(8):
                                    P.op("pe", lambda t, k=k, p=p, c=c, tb=tb: t.matmul(PP[p][:, 0:384], WB[:, k, c * 128:(c + 1) * 128], HT[:, k, tb * 384:(tb + 1) * 384], start=(k == 0), stop=(k == 7)),
                                         reads=[bWB[k]] + bHT[tb * 3:tb * 3 + 3], writes=[bPP[p]])
                                P.op("act", lambda a, p=p, b=b, tb=tb: a.copy(out=ZC[b][:, tb * 384:(tb + 1) * 384], in_=PP[p][:, 0:384]), reads=[bPP[p]], writes=[bZC[b]])
                            z = ZC[b]; ac = AC[b]
                            P.op("dve", lambda v, z=z, ac=ac, c=c: v.tensor_scalar(out=ac[:, :], in0=z[:, :], scalar1=CWs[:, c, 1:2], scalar2=CBs[:, c:c + 1], op0=ALU.mult, op1=ALU.add), reads=[bZC[b], bCW], writes=[bAC[b]])
                            for (o0, o1, i0, i1, tap) in ((1, 256, 0, 255, 0), (257, T, 256, T - 1, 0), (0, 255, 1, 256, 2), (256, T - 1, 257, T, 2)):
                                P.op("dve", lambda v, z=z, ac=ac, c=c, o0=o0, o1=o1, i0=i0, i1=i1, tap=tap: v.scalar_tensor_tensor(out=ac[:, o0:o1], in0=z[:, i0:i1], scalar=CWs[:, c, tap:tap + 1], in1=ac[:, o0:o1], op0=ALU.mult, op1=ALU.add),
                                     reads=[bZC[b], bAC[b], bCW], writes=[bAC[b]])
                            P.op("act", lambda a, ac=ac, c=c: a.activation(out=QKT[:, c, :], in_=ac[:, :], func=AF.Silu), reads=[bAC[b]], writes=[bQKT[c]])
                    elif which == 1:
                        for tt in range(NT):
                            for dh in range(2):
                                p = pi % 4; pi += 1
                                for k in range(8):
                                    P.op("pe", lambda t, k=k, p=p, tt=tt, dh=dh: t.matmul(PP[p][:, :], HT[:, k, tt * 128:(tt + 1) * 128], WB[:, k, dh * 512:(dh + 1) * 512], start=(k == 0), stop=(k == 7)),
                                         reads=[bWB[k], bHT[tt]], writes=[bPP[p]])
                                if dh == 0:
                                    P.op("act", lambda a, p=p, tt=tt, dh=dh: a.copy(out=VA[:, tt, dh * 4:(dh + 1) * 4, 0:128], in_=PP[p][:, :].rearrange("p (h d) -> p h d", h=4)), reads=[bPP[p]], writes=[bVA[tt]])
                                else:
                                    P.op("dve", lambda v, p=p, tt=tt, dh=dh: v.tensor_copy(out=VA[:, tt, dh * 4:(dh + 1) * 4, 0:128], in_=PP[p][:, :].rearrange("p (h d) -> p h d", h=4)), reads=[bPP[p]], writes=[bVA[tt]])
                    else:
                        for tt in range(2, NT):
                            b = tt % 2
                            for dh in range(2):
                                p = pi % 4; pi += 1
                                for k in range(8):
                                    P.op("pe", lambda t, k=k, p=p, tt=tt, dh=dh: t.matmul(PP[p][:, :], HT[:, k, tt * 128:(tt + 1) * 128], WB[:, k, dh * 512:(dh + 1) * 512], start=(k == 0), stop=(k == 7)),
                                         reads=[bWB[k], bHT[tt]], writes=[bPP[p]])
                                P.op("act", lambda a, p=p, b=b, dh=dh: a.activation(out=SO[b][:, dh * 512:(dh + 1) * 512], in_=PP[p][:, :], func=AF.Sigmoid), reads=[bPP[p]], writes=[bSO[b]])
                            P.dma(SIGO[tt * 128:(tt + 1) * 128, :], SO[b][:, :], reads=[bSO[b]], writes=[bSIGO[tt]])
                P.flush()
        with contextlib.ExitStack() as st2:
            SEL = P.sb(st2, "l_SEL", [8, 8, 128], F32); bSEL = Buf()
            TRf = P.sb(st2, "l_TRf", [128, 2, 128], F32); TRb = P.sb(st2, "l_TRb", [128, 2, 128], BF16); bTR = Buf()
            ONGs = P.sb(st2, "l_ONG", [128, 1024], F32); bONG = Buf()
            HS = P.sb(st2, "l_HS", [128, 4, 1024], F32); bHS = [Buf() for _ in range(4)]
            DT = [P.sb(st2, f"l_DT{i}", [128, 512], F32) for i in range(3)]; bDT = [Buf() for _ in range(3)]
            WT = [P.sb(st2, f"l_WT{i}", [128, 512], BF16) for i in range(3)]; bWT = [Buf() for _ in range(3)]
            SM = [P.sb(st2, f"l_SM{i}", [128, 8], F32) for i in range(2)]; bSM = [Buf() for _ in range(2)]
            SG = [P.sb(st2, f"l_SG{i}", [128, 1024], BF16) for i in range(2)]; bSG = [Buf() for _ in range(2)]
            JK = P.sb(st2, "l_JK", [128, 128], F32); bJK = Buf()
            RS = P.sb(st2, "l_RS", [128, 4, 24], F32); bRS = [Buf() for _ in range(4)]
            HN = [P.sb(st2, f"l_HN{i}", [128, 1024], F32) for i in range(1)] * 2; bHN = [Buf()] * 2
            PO = [P.ps(st2, f"l_PO{i}") for i in range(4)]; bPO = [Buf(excl=True) for _ in range(4)]
            PS = [P.ps(st2, f"l_PS{i}") for i in range(3)]; bPS = [Buf(excl=True) for _ in range(3)]
            NB = [P.ps(st2, f"l_NB{i}") for i in range(1)]; bNB = [Buf(excl=True) for _ in range(1)]
            P.dma(SEL[:, :, :], SEL8[:, :, :], reads=[cin], writes=[bSEL])
            P.dma(TRf[:, :, :], TRI[:, :, :], reads=[cin], writes=[bTR])
            P.op("dve", lambda v: v.tensor_copy(out=TRb[:, :, :], in_=TRf[:, :, :]), reads=[bTR], writes=[bTR])
            P.dma(ONGs[:, :], ONG.partition_broadcast(128), reads=[cin], writes=[bONG])
            def kcs_of(qb, d):
                tiles_ = [2 + qb * 4 + j for j in range(4)]
                return list(range(0, tiles_[-1] + 1)) if d == 0 else [0, 1] + list(range(tiles_[0], NT))
            groups = [(qb, h, d) for qb in range(4) for h in range(8) for d in range(2)]
            def emit_NB(gi):
                qb, h, d = groups[gi]; nb = 0; q0 = 256 + qb * 512
                P.op("pe", lambda t, nb=nb, h=h, d=d, q0=q0: t.matmul(NB[nb][:, :], SEL[:, h, :], NEGM[d][:, q0:q0 + 512], start=True, stop=True), reads=[bSEL, bNEGM[d]], writes=[bNB[nb]])
            steps = []
            for gi, (qb, h, d) in enumerate(groups):
                ks = kcs_of(qb, d)
                for ki, kc in enumerate(ks):
                    steps.append((gi, kc, ki == 0, ki == len(ks) - 1))
            def emit_S(idx):
                gi, kc, _, _ = steps[idx]
                qb, h, d = groups[gi]; b = idx % 3; q0 = 256 + qb * 512
                cq = h // 2; ck = 4 + h // 2; hp = (h % 2) * 64
                P.op("pe", lambda t, b=b, kc=kc, ck=ck, cq=cq, hp=hp, q0=q0: t.matmul(PS[b][:, :], QKT[hp:hp + 64, ck, kc * 128:(kc + 1) * 128], QKT[hp:hp + 64, cq, q0:q0 + 512], start=True, stop=True),
                     reads=[bQKT[ck], bQKT[cq]], writes=[bPS[b]])
            emit_NB(0)
            emit_S(0)
            emit_S(1)
            for idx, (gi, kc, gfirst, glast) in enumerate(steps):
                qb, h, d = groups[gi]
                tiles_ = [2 + qb * 4 + j for j in range(4)]
                nb = 0; b = idx % 3
                col = d * 8 + h
                if gfirst and gi > 0:
                    emit_NB(gi)
                if tiles_[0] <= kc <= tiles_[-1]:
                    P.op("dve", lambda v, b=b, nb=nb, kc=kc, col=col: v.tensor_scalar(out=DT[b][:, :], in0=NB[nb][:, :], scalar1=ATM[:, kc, col:col + 1], scalar2=0.0, op0=ALU.add, op1=ALU.min),
                         reads=[bNB[nb], bATM], writes=[bDT[b]])
                    P.op("act", lambda a, b=b: a.activation(out=DT[b][:, :], in_=DT[b][:, :], func=AF.Exp), reads=[bDT[b]], writes=[bDT[b]])
                else:
                    P.op("act", lambda a, b=b, nb=nb, kc=kc, col=col: a.activation(out=DT[b][:, :], in_=NB[nb][:, :], func=AF.Exp, bias=ATM[:, kc, col:col + 1]), reads=[bNB[nb], bATM], writes=[bDT[b]])
                if idx + 2 < len(steps):
                    emit_S(idx + 2)
                P.op("dve", lambda v, b=b: v.tensor_tensor(out=WT[b][:, :], in0=PS[b][:, :], in1=DT[b][:, :], op=ALU.mult), reads=[bPS[b], bDT[b]], writes=[bWT[b]])
                for j, tj in enumerate(tiles_):
                    if d == 0:
                        ok = kc <= tj; first = (kc == 0); last = (kc == tj)
                    else:
                        ok = kc < 2 or kc >= tj; first = (kc == 0); last = (kc == NT - 1)
                    if not ok:
                        continue
                    if kc == tj:
                        P.op("dve", lambda g, b=b, j=j, d=d: g.tensor_tensor(out=WT[b][:, j * 128:(j + 1) * 128], in0=WT[b][:, j * 128:(j + 1) * 128], in1=TRb[:, d, :], op=ALU.mult), reads=[bWT[b], bTR], writes=[bWT[b]])
                    P.op("pe", lambda t, b=b, j=j, kc=kc, h=h, first=first, last=last: t.matmul(PO[j][:, 0:129], WT[b][:, j * 128:(j + 1) * 128], VA[:, kc, h, :], start=first, stop=last),
                         reads=[bWT[b], bVA[kc]], writes=[bPO[j]])
                if glast:
                    for j, tj in enumerate(tiles_):
                        sm = j % 2
                        P.op("act", lambda a, j=j, sm=sm: a.activation(out=SM[sm][:, 2:3], in_=PO[j][:, 128:129], func=AF.Abs), reads=[bPO[j]], writes=[bSM[sm]])
                        P.op("dve", lambda v, j=j, tj=tj, col=col, sm=sm: v.tensor_scalar(out=SM[sm][:, 0:1], in0=SM[sm][:, 2:3], scalar1=EMT[:, tj, col:col + 1], scalar2=None, op0=ALU.max), reads=[bSM[sm], bEMT], writes=[bSM[sm]])
                        P.op("dve", lambda v, sm=sm: v.reciprocal(out=SM[sm][:, 1:2], in_=SM[sm][:, 0:1]), reads=[bSM[sm]], writes=[bSM[sm]])
                        if d == 0:
                            P.op("dve", lambda v, j=j, h=h, sm=sm: v.tensor_scalar(out=HS[:, j, h * 128:(h + 1) * 128], in0=PO[j][:, 0:128], scalar1=SM[sm][:, 1:2], scalar2=None, op0=ALU.mult), reads=[bPO[j], bSM[sm]], writes=[bHS[j]])
                        else:
                            P.op("dve", lambda v, j=j, h=h, sm=sm: v.scalar_tensor_tensor(out=HS[:, j, h * 128:(h + 1) * 128], in0=PO[j][:, 0:128], scalar=SM[sm][:, 1:2], in1=HS[:, j, h * 128:(h + 1) * 128], op0=ALU.mult, op1=ALU.add),
                                 reads=[bPO[j], bSM[sm], bHS[j]], writes=[bHS[j]])
                if not (glast and h == 7 and d == 1):
                    continue
                for j, tj in enumerate(tiles_):
                    b = j % 2
                    P.dma(SG[b][:, :], SIGO[tj * 128:(tj + 1) * 128, :], reads=[bSIGO[tj]], writes=[bSG[b]])
                    for h in range(8):
                        P.op("act", lambda a, j=j, h=h: a.activation(out=JK[:, :], in_=HS[:, j, h * 128:(h + 1) * 128], func=AF.Square, accum_out=RS[:, j, h:h + 1]), reads=[bHS[j]], writes=[bJK, bRS[j]])
                    P.op("act", lambda a, j=j: a.activation(out=RS[:, j, 8:16], in_=RS[:, j, 0:8], func=AF.Sqrt, scale=1.0 / 128, bias=K["eps"][:, :]), reads=[bRS[j], K["b_eps"]], writes=[bRS[j]])
                    P.op("dve", lambda v, j=j: v.reciprocal(out=RS[:, j, 16:24], in_=RS[:, j, 8:16]), reads=[bRS[j]], writes=[bRS[j]])
                    for h in range(8):
                        P.op("dve", lambda v, j=j, h=h, b=b: v.scalar_tensor_tensor(out=HN[b][:, h * 128:(h + 1) * 128], in0=HS[:, j, h * 128:(h + 1) * 128], scalar=RS[:, j, 16 + h:17 + h], in1=ONGs[:, h * 128:(h + 1) * 128], op0=ALU.mult, op1=ALU.mult),
                             reads=[bHS[j], bRS[j], bONG], writes=[bHN[b]])
                    P.op("dve", lambda g, b=b, tj=tj: g.tensor_tensor(out=YTM[:, tj - 2, :], in0=HN[b][:, :], in1=SG[b][:, :], op=ALU.mult), reads=[bHN[b], bSG[b]], writes=[bYTM[tj - 2]])
            P.flush()


NCORES = 8


def _consts_np():
    sel16 = np.zeros((NE, NE, 128), np.float32)
    for e in range(NE):
        sel16[e, e, :] = 1
    slotid = np.zeros((128, 4), np.float32)
    slotid[:, 0] = 32 + np.arange(128); slotid[:, 1] = 160 + np.arange(128); slotid[:, 3] = np.arange(128) % 32
    selq = np.zeros((NE, 4, 128), np.float32)
    for e in range(NE):
        selq[e, e // 4, (e % 4) * 32:(e % 4) * 32 + 32] = 1
    return {"c_idf": np.eye(128, dtype=np.float32), "c_iota": np.arange(512, dtype=np.float32), "c_sel16": sel16, "c_slotid": slotid, "c_selq": selq}


def build_mod():
    P = Prog()
    CT = P.dram("ct", [128, 8, 9], F32, "ExternalInput")
    MW = P.dram("mw", [4, D, 768], F32, "ExternalInput")
    MB = P.dram("mb", [4, 1, 768], F32, "ExternalInput")
    OUT = P.dram("modo", [4, 9, 768], F32, "ExternalOutput")
    with contextlib.ExitStack() as st:
        C_ = P.sb(st, "C_", [128, 8, 9], F32); bC = Buf()
        S_ = P.sb(st, "S_", [128, 8, 9], F32); bS = Buf()
        ON = P.sb(st, "ON", [1, 9], F32); bON = Buf()
        W = [P.sb(st, f"W{i}", [128, 768], F32) for i in range(3)]; bW = [Buf() for _ in range(3)]
        Bb = [P.sb(st, f"Bb{i}", [1, 768], F32) for i in range(2)]; bBb = [Buf() for _ in range(2)]
        O_ = [P.sb(st, f"O{i}", [9, 768], F32) for i in range(2)]; bO = [Buf() for _ in range(2)]
        PS_ = [P.ps(st, f"PS{i}") for i in range(4)]; bPS = [Buf(excl=True) for _ in range(4)]
        cin = Buf(); cout = Buf()
        P.dma(C_[:, :, :], CT[:, :, :], reads=[cin], writes=[bC])
        P.op("act", lambda a: a.activation(out=S_[:, :, :], in_=C_[:, :, :], func=AF.Silu), reads=[bC], writes=[bS])
        P.op("dve", lambda v: v.memset(ON[:, :], 1.0), writes=[bON])
        wi = 0
        for l in range(4):
            b = l % 2
            P.dma(Bb[b][:, :], MB[l], reads=[cin], writes=[bBb[b]])
            for k in range(8):
                w = wi % 3; wi += 1
                P.dma(W[w][:, :], MW[l, k * 128:(k + 1) * 128, :], reads=[cin], writes=[bW[w]])
                for cb, (c0, c1) in enumerate(((0, 512), (512, 768))):
                    P.op("pe", lambda t, k=k, w=w, b=b, cb=cb, c0=c0, c1=c1: t.matmul(PS_[b * 2 + cb][0:9, 0:c1 - c0], S_[:, k, :], W[w][:, c0:c1], start=(k == 0), stop=False),
                         reads=[bS, bW[w]], writes=[bPS[b * 2 + cb]])
            for cb, (c0, c1) in enumerate(((0, 512), (512, 768))):
                P.op("pe", lambda t, b=b, cb=cb, c0=c0, c1=c1: t.matmul(PS_[b * 2 + cb][0:9, 0:c1 - c0], ON[:, :], Bb[b][:, c0:c1], start=False, stop=True),
                     reads=[bON, bBb[b]], writes=[bPS[b * 2 + cb]])
                P.op("dve", lambda v, b=b, cb=cb, c0=c0, c1=c1: v.tensor_copy(out=O_[b][:, c0:c1], in_=PS_[b * 2 + cb][0:9, 0:c1 - c0]), reads=[bPS[b * 2 + cb]], writes=[bO[b]])
            P.dma(OUT[l], O_[b][:, :], reads=[bO[b]], writes=[cout])
        P.flush(final=True)
    return P


def phase_copy_x(P, Xin, X, bX):
    with contextlib.ExitStack() as st1:
        XC = [P.sb(st1, f"XC{i}", [128, D], F32) for i in range(2)]; bXC = [Buf(), Buf()]
        cin = Buf()
        for tt in range(NT):
            P.dma(XC[tt % 2][:, :], Xin[tt * 128:(tt + 1) * 128, :], reads=[cin], writes=[bXC[tt % 2]])
            P.dma(X[tt * 128:(tt + 1) * 128, :], XC[tt % 2][:, :], reads=[bXC[tt % 2]], writes=[bX[tt]])
        P.flush()


FUSE_N2 = False


def build_A(i, fuse=None):
    fuse = FUSE_N2 if fuse is None else fuse
    P = Prog()
    C = {"idf": P.dram("c_idf", [128, 128], F32, "ExternalInput"), "iota": P.dram("c_iota", [512], F32, "ExternalInput")}
    Xin = P.dram("xin", [T, D], F32, "ExternalInput")
    MODS = P.dram("mods", [2, 6 * D], F32, "ExternalInput")
    NG1 = P.dram("ng1", [D], F32, "ExternalInput"); NG2 = P.dram("ng2", [D], F32, "ExternalInput")
    RW = P.dram("rw", [D, NE], F32, "ExternalInput")
    WO = P.dram("wo", [D, D], F32, "ExternalInput")
    X = P.dram("xmid", [T, D], F32, "ExternalOutput")
    XG = P.dram("xg", [NE, D, NSLOT], BF16, "ExternalOutput")
    GV = P.dram("gv", [NE, NSLOT], F32, "ExternalOutput")
    PM = P.dram("posmt", [NE, T], F32, "ExternalOutput")
    if i > 0:
        C["sel16"] = P.dram("c_sel16", [NE, NE, 128], F32, "ExternalInput"); C["slotid"] = P.dram("c_slotid", [128, 4], F32, "ExternalInput"); C["selq"] = P.dram("c_selq", [NE, 4, 128], F32, "ExternalInput")
        MODSP = P.dram("modsp", [2, 6 * D], F32, "ExternalInput")
        Yd = P.dram("y", [NE, NSLOT, D], BF16, "ExternalInput")
        PMin = P.dram("posmt_in", [NE, T], F32, "ExternalInput")
    with contextlib.ExitStack() as st:
        K = load_consts(P, st, C)
        bX = [Buf() for _ in range(NT)]

        def alloc_n2(stack):
            HT2 = P.sb(stack, "HT2", [128, 8, T], BF16); bHT2 = [Buf() for _ in range(NT)]
            HTM2 = P.sb(stack, "HTM2", [128, NT, D], BF16); bHTM2 = [Buf() for _ in range(NT)]
            return (3 * D, NG2, HT2, bHT2, HTM2, bHTM2)
        if i == 0:
            phase_copy_x(P, Xin, X, bX)
        else:
            phase_pro(P, K, C, Yd, PMin, MODSP, Xin, X, bX)
        rn = lambda HT, bHT: phase_norm(P, K, X, bX, MODS, 0, NG1, HT, bHT)
        if i == 0:
            WQKV = P.dram("wqkv", [D, 3 * D], F32, "ExternalInput")
            TTE = P.dram("tte", [128, 16, 14, 64], F32, "ExternalInput"); TTO = P.dram("tto", [128, 16, 5, 64], F32, "ExternalInput")
            YATT = P.dram("yatt", [T, D], BF16); bY = [Buf() for _ in range(NT)]
            mixer_na(P, K, rn, WQKV, TTE, TTO, YATT, bY)
            def src(tt, dst, bdst):
                P.dma(dst[:, :], YATT[tt * 128:(tt + 1) * 128, :], reads=[bY[tt]], writes=[bdst])
            n2 = alloc_n2(st) if fuse else None
            phase_outproj(P, K, src, WO, X, bX, MODS, 2 * D, norm2=n2)
        elif i == 1:
            WIN = P.dram("win", [D, 672], F32, "ExternalInput"); WKS = P.dram("wks", [D, 96], F32, "ExternalInput")
            GQ = P.dram("gq", [128, 3], F32, "ExternalInput"); GKV = P.dram("gkv", [128, 2], F32, "ExternalInput")
            WQB = P.dram("wqb", [QR, 1536], F32, "ExternalInput"); WQS = P.dram("wqs", [QR, 1536], F32, "ExternalInput")
            WKVB = P.dram("wkvb", [KVR, 2048], F32, "ExternalInput"); CS = P.dram("cs", [32, 2, T], F32, "ExternalInput")
            with contextlib.ExitStack() as stm:
                YTM = P.sb(stm, "YTM", [128, NT, D], BF16); bYTM = [Buf() for _ in range(NT)]
                mixer_mla(P, K, rn, WIN, WKS, GQ, GKV, WQB, WQS, WKVB, CS, YTM, bYTM)
                def src(tt, dst, bdst):
                    P.op("pool", lambda g, tt=tt, dst=dst: g.tensor_copy(out=dst[:, :], in_=YTM[:, tt, :]), reads=[bYTM[tt]], writes=[bdst])
                n2 = alloc_n2(stm) if fuse else None
                phase_outproj(P, K, src, WO, X, bX, MODS, 2 * D, norm2=n2)
                if fuse:
                    phase_route(P, K, C, n2[2], n2[3], n2[4], n2[5], RW, XG, GV, PM)
        elif i == 2:
            WIN = P.dram("win", [D, 3072], F32, "ExternalInput")
            CW = P.dram("cw", [128, 24, 3], F32, "ExternalInput"); CB = P.dram("cb", [128, 24], F32, "ExternalInput")
            FW1 = P.dram("fw1", [33, 64], F32, "ExternalInput"); FW2 = P.dram("fw2", [64, 64], F32, "ExternalInput"); FW3 = P.dram("fw3", [64, 2048], F32, "ExternalInput")
            FB = P.dram("fb", [64, 4], F32, "ExternalInput"); FB3 = P.dram("fb3", [1, 2048], F32, "ExternalInput")
            SKIP = P.dram("skip", [1, 1024], F32, "ExternalInput"); DELTA = P.dram("delta", [1024], F32, "ExternalInput")
            TB = {}
            for tag, L in (("c", 256), ("l", 2048)):
                nt = L // 128
                TB[tag] = {"ze": P.dram("ze" + tag, [33, L], F32, "ExternalInput"), "negt": P.dram("negt" + tag, [128, nt], F32, "ExternalInput"), "wsc": P.dram("wsc" + tag, [128, nt], F32, "ExternalInput"),
                           "C": P.dram("C" + tag, [L, L], BF16, "ExternalInput"), "S": P.dram("S" + tag, [L, L], BF16, "ExternalInput"), "ST": P.dram("ST" + tag, [L, L], BF16, "ExternalInput")}
            X0D = P.dram("x0d", [D, T], BF16); YTD = P.dram("ytd", [D, T], BF16); bYTD = [Buf() for _ in range(8)]
            mixer_hyena(P, K, rn, WIN, CW, CB, FW1, FW2, FW3, FB, FB3, SKIP, DELTA, TB, X0D, YTD, bYTD)
            n2 = alloc_n2(st) if fuse else None
            outproj_fm_dram(P, K, YTD, bYTD, WO, X, bX, MODS, 2 * D, norm2=n2)
        else:
            WIN = P.dram("win", [D, 3104], F32, "ExternalInput")
            CW = P.dram("cw", [128, 8, 3], F32, "ExternalInput"); CB = P.dram("cb", [128, 8], F32, "ExternalInput")
            GB = P.dram("gb", [32], F32, "ExternalInput"); ONG = P.dram("ong", [D], F32, "ExternalInput")
            SEL8 = P.dram("sel8", [8, 8, 128], F32, "ExternalInput"); TRI = P.dram("tri", [128, 2, 128], F32, "ExternalInput")
            SIGO = P.dram("sigo", [T, D], BF16)
            with contextlib.ExitStack() as stm:
                YTM = P.sb(stm, "YTM", [128, 16, D], BF16); bYTM = [Buf() for _ in range(16)]
                mixer_mlstm(P, K, rn, WIN, CW, CB, GB, ONG, SEL8, TRI, SIGO, YTM, bYTM)
                def src(tt, dst, bdst):
                    P.op("pool", lambda g, tt=tt, dst=dst: g.tensor_copy(out=dst[:, :], in_=YTM[:, tt - 2, :]), reads=[bYTM[tt - 2]], writes=[bdst])
                n2 = alloc_n2(stm) if fuse else None
                phase_outproj(P, K, src, WO, X, bX, MODS, 2 * D, tiles=range(2, NT), norm2=n2)
                if fuse:
                    phase_norm(P, K, X, bX, MODS, 3 * D, NG2, n2[2], n2[3], n2[4], n2[5], tiles=range(0, 2))
                    phase_route(P, K, C, n2[2], n2[3], n2[4], n2[5], RW, XG, GV, PM)
        if fuse and i in (0, 2):
            phase_route(P, K, C, n2[2], n2[3], n2[4], n2[5], RW, XG, GV, PM)
        if not fuse:
            with contextlib.ExitStack() as st2:
                HT = P.sb(st2, "HT2", [128, 8, T], BF16); bHT = [Buf() for _ in range(NT)]
                HTM = P.sb(st2, "HTM2", [128, NT, D], BF16); bHTM = [Buf() for _ in range(NT)]
                phase_norm(P, K, X, bX, MODS, 3 * D, NG2, HT, bHT, HTM, bHTM)
                phase_route(P, K, C, HT, bHT, HTM, bHTM, RW, XG, GV, PM)
        P.flush(final=True)
    return P


def build_Bprog():
    P = Prog()
    xgT = P.dram("xgT", [2, D, NS], BF16, "ExternalInput")
    gv = P.dram("gv", [2, 128, 18], F32, "ExternalInput")
    wg = P.dram("wg", [2, D, FF], F32, "ExternalInput")
    wu = P.dram("wu", [2, D, FF], F32, "ExternalInput")
    wd = P.dram("wd", [2, FF, D], F32, "ExternalInput")
    y = P.dram("y", [2, NS, D], BF16, "ExternalOutput")
    build_B(P, xgT, gv, wg, wu, wd, y, nexp=2)
    return P


def build_F():
    P = Prog()
    C = {"idf": P.dram("c_idf", [128, 128], F32, "ExternalInput"), "sel16": P.dram("c_sel16", [NE, NE, 128], F32, "ExternalInput"),
         "slotid": P.dram("c_slotid", [128, 4], F32, "ExternalInput"), "selq": P.dram("c_selq", [NE, 4, 128], F32, "ExternalInput")}
    Xin = P.dram("xin", [T, D], F32, "ExternalInput")
    MODSP = P.dram("modsp", [2, 6 * D], F32, "ExternalInput")
    Yd = P.dram("y", [NE, NSLOT, D], BF16, "ExternalInput")
    PMin = P.dram("posmt_in", [NE, T], F32, "ExternalInput")
    FNG = P.dram("fng", [D], F32, "ExternalInput")
    OUT = P.dram("out", [2048, D], F32, "ExternalOutput")
    X = P.dram("xfin", [T, D], F32)
    with contextlib.ExitStack() as st:
        K = load_consts(P, st, C)
        bX = [Buf() for _ in range(NT)]
        phase_pro(P, K, C, Yd, PMin, MODSP, Xin, X, bX)
        with contextlib.ExitStack() as st2:
            G = P.sb(st2, "f_G", [128, D], F32); bG = Buf()
            XT = [P.sb(st2, f"f_XT{i}", [128, D], F32) for i in range(2)]; bXT = [Buf() for _ in range(2)]
            OT = [P.sb(st2, f"f_OT{i}", [128, D], F32) for i in range(2)]; bOT = [Buf() for _ in range(2)]
            JK = P.sb(st2, "f_JK", [128, D], F32); bJK = Buf()
            SS = P.sb(st2, "f_SS", [128, 3 * NT], F32); bSS = [Buf() for _ in range(NT)]
            cin = Buf(); cout = Buf()
            P.dma(G[:, :], FNG.partition_broadcast(128), reads=[cin], writes=[bG])
            for tt in range(2, NT):
                b = tt % 2
                P.dma(XT[b][:, :], X[tt * 128:(tt + 1) * 128, :], reads=[bX[tt]], writes=[bXT[b]])
                P.op("act", lambda a, b=b, tt=tt: a.activation(out=JK[:, :], in_=XT[b][:, :], func=AF.Square, accum_out=SS[:, 3 * tt:3 * tt + 1]), reads=[bXT[b]], writes=[bJK, bSS[tt]])
                P.op("act", lambda a, tt=tt: a.activation(out=SS[:, 3 * tt + 1:3 * tt + 2], in_=SS[:, 3 * tt:3 * tt + 1], func=AF.Sqrt, scale=1.0 / D, bias=K["eps"][:, :]), reads=[bSS[tt], K["b_eps"]], writes=[bSS[tt]])
                P.op("dve", lambda v, tt=tt: v.reciprocal(out=SS[:, 3 * tt + 2:3 * tt + 3], in_=SS[:, 3 * tt + 1:3 * tt + 2]), reads=[bSS[tt]], writes=[bSS[tt]])
                P.op("dve", lambda v, b=b, tt=tt: v.scalar_tensor_tensor(out=OT[b][:, :], in0=XT[b][:, :], scalar=SS[:, 3 * tt + 2:3 * tt + 3], in1=G[:, :], op0=ALU.mult, op1=ALU.mult), reads=[bXT[b], bSS[tt], bG], writes=[bOT[b]])
                P.dma(OUT[(tt - 2) * 128:(tt - 1) * 128, :], OT[b][:, :], reads=[bOT[b]], writes=[cout])
        P.flush(final=True)
    return P


def _launch(P, maps):
    res = run_bass_kernel_spmd(P.nc, maps, core_ids=list(range(NCORES)))
    return res.results


def kernel(**inp):
    f32 = lambda a: np.ascontiguousarray(np.asarray(a, dtype=np.float32))
    x = f32(inp["x"]); c = f32(inp["c"]); ctx = f32(inp["ctx"]); c_ctx = f32(inp["c_ctx"])
    KC = _consts_np()
    cvec = np.concatenate([c, c_ctx[None, :]], 0)
    ct = np.ascontiguousarray(cvec.T.reshape(8, 128, 9).transpose(1, 0, 2))
    mod_w = inp["mod_w"]; mod_b = inp["mod_b"]
    Pm = build_mod()
    maps = [{"ct": ct, "mw": f32(mod_w[:, :, j * 768:(j + 1) * 768]), "mb": f32(mod_b[:, None, j * 768:(j + 1) * 768])} for j in range(NCORES)]
    r = _launch(Pm, maps)
    modall = np.concatenate([np.asarray(r[j]["modo"]) for j in range(NCORES)], axis=2)
    def mods_for(l, b):
        return np.ascontiguousarray(np.stack([modall[l, b], modall[l, 8]], 0))
    PB_ = build_Bprog()
    xcur = [np.ascontiguousarray(np.concatenate([ctx[b], x[b]], 0)) for b in range(NCORES)]
    ycur = None; pmcur = None
    for i in range(4):
        PA = build_A(i)
        maps = []
        if i == 0:
            tte, tto = na_tables(f32(inp["na_rpb"][0]))
            extra = {"wqkv": f32(inp["na_w_qkv"][0]), "tte": tte, "tto": tto, "wo": f32(inp["na_w_o"][0])}
        elif i == 1:
            wks, wqs, gq, gkv, cs = mla_host(f32(inp["mla_w_in"][0]), f32(inp["mla_w_q_b"][0]), f32(inp["mla_q_norm_g"][0]), f32(inp["mla_kv_norm_g"][0]))
            extra = {"win": f32(inp["mla_w_in"][0]), "wks": wks, "gq": gq, "gkv": gkv, "wqb": f32(inp["mla_w_q_b"][0]), "wqs": wqs,
                     "wkvb": f32(inp["mla_w_kv_b"][0]), "cs": cs, "wo": f32(inp["mla_w_o"][0])}
        elif i == 2:
            H = hy_host(f32(inp["hy_conv_w"][0]), f32(inp["hy_conv_b"][0]), f32(inp["hy_f_b1"][0]), f32(inp["hy_f_b2"][0]), f32(inp["hy_f_b3"][0]), f32(inp["hy_sin_freq"][0]), f32(inp["hy_skip"][0]))
            extra = {"win": f32(inp["hy_w_in"][0]), "cw": H["cw"], "cb": H["cb"], "fw1": f32(inp["hy_f_w1"][0]), "fw2": f32(inp["hy_f_w2"][0]), "fw3": f32(inp["hy_f_w3"][0]),
                     "fb": H["fb"], "fb3": f32(inp["hy_f_b3"][0])[None, :], "skip": f32(inp["hy_skip"][0])[None, :], "delta": H["delta"], "wo": f32(inp["hy_w_o"][0])}
            for tag in ("c", "l"):
                for kk in ("ze", "negt", "wsc", "C", "S", "ST"):
                    extra[kk + tag] = H[kk + tag]
        else:
            cw, cb, sel8, tri = ml_host(f32(inp["ml_conv_w"][0]), f32(inp["ml_conv_b"][0]))
            extra = {"win": f32(inp["ml_w_in"][0]), "cw": cw, "cb": cb, "gb": f32(inp["ml_gate_b"][0]), "ong": f32(inp["ml_out_norm_g"][0]), "sel8": sel8, "tri": tri, "wo": f32(inp["ml_w_o"][0])}
        for b in range(NCORES):
            m = {"c_idf": KC["c_idf"], "c_iota": KC["c_iota"], "xin": xcur[b], "mods": mods_for(i, b), "ng1": f32(inp["norm_mix_g"][i]), "ng2": f32(inp["norm_ffn_g"][i]),
                 "rw": f32(inp["router_w"][i])}
            m.update(extra)
            if i > 0:
                m.update({"c_sel16": KC["c_sel16"], "c_slotid": KC["c_slotid"], "c_selq": KC["c_selq"], "modsp": mods_for(i - 1, b), "y": ycur[b], "posmt_in": pmcur[b]})
            maps.append(m)
        r = _launch(PA, maps)
        xcur = [np.asarray(r[b]["xmid"]) for b in range(NCORES)]
        pmcur = [np.asarray(r[b]["posmt"]) for b in range(NCORES)]
        xg = [np.asarray(r[b]["xg"]) for b in range(NCORES)]
        gvv = [np.asarray(r[b]["gv"]) for b in range(NCORES)]
        maps = []
        for j in range(NCORES):
            xgT = np.ascontiguousarray(np.stack([np.concatenate([xg[b][2 * j + el] for b in range(NCORES)], axis=1) for el in range(2)], 0))
            gvj = np.stack([np.concatenate([gvv[b][2 * j + el] for b in range(NCORES)], 0) for el in range(2)], 0)
            gvl = np.ascontiguousarray(gvj.reshape(2, 18, 128).transpose(0, 2, 1))
            maps.append({"xgT": xgT, "gv": gvl, "wg": f32(inp["moe_w_gate"][i, 2 * j:2 * j + 2]), "wu": f32(inp["moe_w_up"][i, 2 * j:2 * j + 2]),
                         "wd": f32(inp["moe_w_down"][i, 2 * j:2 * j + 2])})
        r = _launch(PB_, maps)
        yb = [np.asarray(r[j]["y"]) for j in range(NCORES)]
        ycur = [np.ascontiguousarray(np.concatenate([yb[j][:, b * NSLOT:(b + 1) * NSLOT, :] for j in range(NCORES)], 0)) for b in range(NCORES)]
    PF_ = build_F()
    maps = [{"c_idf": KC["c_idf"], "c_sel16": KC["c_sel16"], "c_slotid": KC["c_slotid"], "c_selq": KC["c_selq"], "xin": xcur[b], "modsp": mods_for(3, b), "y": ycur[b], "posmt_in": pmcur[b],
             "fng": f32(inp["final_norm_g"])} for b in range(NCORES)]
    r = _launch(PF_, maps)
    return np.stack([np.asarray(r[b]["out"]) for b in range(NCORES)], 0).astype(np.float32)
```

```python
import contextlib
import math
import numpy as np
import concourse.bass as bass
import concourse.mybir as mybir
from concourse.bass_utils import run_bass_kernel_spmd

F32 = mybir.dt.float32
BF16 = mybir.dt.bfloat16
I32 = mybir.dt.int32
ALU = mybir.AluOpType
AF = mybir.ActivationFunctionType
AX = mybir.AxisListType
NPBF = mybir.dt.np(BF16)


class Buf:
    __slots__ = ("name", "w", "rs", "excl")

    def __init__(self, name="", excl=False):
        self.name = name
        self.w = None
        self.rs = []
        self.excl = excl


class Prog:
    EPOCH = 30000
    NRING = 6

    def __init__(self):
        self.nc = bass.Bass("TRN2", target_bir_lowering=False)
        nc = self.nc
        self.engs = ["pe", "act", "dve", "pool", "sp"]
        self.lists = {e: [] for e in self.engs}
        self.cnt = {e: 0 for e in self.engs}
        self.nsem = 0
        self.sem = {e: self._newsem(e) for e in ("pe", "act", "dve", "pool")}
        self.seen = {e: {} for e in self.engs}
        self.ring = {q: [self._newsem(f"r{q}{i}") for i in range(self.NRING)] for q in ("sp", "pool", "act")}
        self.ring_n = {q: [0] * self.NRING for q in self.ring}
        self.ring_i = {q: 0 for q in self.ring}
        self.nbuf = 0
        self.ninstr = 0
        self.split_stores = False
        self.limit = 1 << 60

    def _newsem(self, name):
        self.nsem += 1
        return self.nc.alloc_semaphore(name=f"s{self.nsem}_{name}")

    def dram(self, name, shape, dtype, kind="Internal"):
        return self.nc.dram_tensor(name, list(shape), dtype, kind=kind).ap()

    def sb(self, stack, name, shape, dtype):
        self.nbuf += 1
        return stack.enter_context(self.nc.sbuf_tensor(f"{name}_{self.nbuf}", list(shape), dtype))

    def ps(self, stack, name, shape=(128, 512), dtype=F32):
        self.nbuf += 1
        return stack.enter_context(self.nc.psum_tensor(f"{name}_{self.nbuf}", list(shape), dtype))

    def _deps(self, eng, reads, writes):
        evs = {}

        def add(ev, kind):
            if ev is None:
                return
            s, v, src = ev
            if src == eng:
                if eng == "pe":
                    return
            k = id(s)
            if self.seen[eng].get(k, -1) >= v:
                return
            if k not in evs or evs[k][1] < v:
                evs[k] = (s, v)

        for b in reads:
            add(b.w, "raw")
        for b in writes:
            add(b.w, "waw")
            for r in b.rs:
                add(r, "war")
        out = []
        for k, (s, v) in evs.items():
            self.seen[eng][k] = v
            out.append((s, v))
        return out

    def _commit(self, ev, reads, writes):
        for b in reads:
            b.rs.append(ev)
            if len(b.rs) > 64:
                best = {}
                for (s, v, src) in b.rs:
                    k = (id(s), src)
                    if k not in best or best[k][1] < v:
                        best[k] = (s, v, src)
                b.rs = list(best.values())
        for b in writes:
            b.w = ev
            b.rs = []

    def op(self, eng, fn, reads=(), writes=()):
        if self.ninstr >= self.limit:
            return
        ex = [b for b in reads if b.excl]
        if ex:
            writes = list(writes) + [b for b in ex if b not in writes]
        waits = self._deps(eng, reads, writes)
        if self.cnt[eng] >= self.EPOCH:
            self.sem[eng] = self._newsem(eng)
            self.cnt[eng] = 0
        self.cnt[eng] += 1
        s = self.sem[eng]
        ev = (s, self.cnt[eng], eng)
        self.lists[eng].append((waits, fn, s, 1))
        self._commit(ev, reads, writes)
        self.ninstr += 1

    def dma(self, out, in_, reads=(), writes=(), q=None, **kw):
        if self.ninstr >= self.limit:
            return
        if q is None:
            q = "pool" if (self.split_stores and "DRam" in type(out.tensor).__name__) else "sp"
        eng = q
        i = self.ring_i[q]
        self.ring_i[q] = (i + 1) % self.NRING
        s = self.ring[q][i]
        n = self.ring_n[q][i]
        waits = []
        if n > 0 and self.seen[eng].get(id(s), -1) < 16 * n:
            waits.append((s, 16 * n))
            self.seen[eng][id(s)] = 16 * n
        waits += self._deps(eng, reads, writes)
        self.ring_n[q][i] = n + 1
        ev = (s, 16 * (n + 1), "dma")
        self.lists[eng].append((waits, (lambda e, out=out, in_=in_, kw=kw: e.dma_start(out=out, in_=in_, **kw)), s, 16))
        self._commit(ev, reads, writes)
        self.ninstr += 1

    def barrier(self):
        targets = []
        for e in ("pe", "act", "dve", "pool"):
            if self.cnt[e] > 0:
                targets.append((self.sem[e], self.cnt[e], e))
        for q in self.ring:
            for i, s in enumerate(self.ring[q]):
                n = self.ring_n[q][i]
                if n > 0:
                    targets.append((s, 16 * n, "dma"))
        for eng in self.engs:
            waits = []
            for (s, v, src) in targets:
                if self.seen[eng].get(id(s), -1) >= v:
                    continue
                self.seen[eng][id(s)] = v
                waits.append((s, v))
            if waits:
                self.lists[eng].append((waits, None, None, 0))

    def flush(self, final=False):
        nc = self.nc
        self.barrier()
        if final:
            for q in self.ring:
                for i, s in enumerate(self.ring[q]):
                    n = self.ring_n[q][i]
                    if n > 0 and self.seen[q].get(id(s), -1) < 16 * n:
                        self.lists[q].append(([(s, 16 * n)], None, None, 0))
                        self.seen[q][id(s)] = 16 * n
        lists = self.lists
        self.lists = {e: [] for e in self.engs}

        def emit(e, items):
            for waits, fn, s, inc in items:
                for (ws, wv) in waits:
                    e.wait_ge(ws, wv)
                if fn is not None:
                    fn(e).then_inc(s, inc)

        with nc.Block() as block:
            @block.tensor
            def _(e):
                emit(e, lists["pe"])

            @block.scalar
            def _(e):
                emit(e, lists["act"])

            @block.vector
            def _(e):
                emit(e, lists["dve"])

            @block.gpsimd
            def _(e):
                emit(e, lists["pool"])

            @block.sync
            def _(e):
                emit(e, lists["sp"])


def run(prog, in_maps, trace=False):
    res = run_bass_kernel_spmd(prog.nc, in_maps, core_ids=list(range(len(in_maps))), trace=trace)
    return res


T = 2304
NT = 18
D = 1024
NE = 16
NSLOT = 288
EPS = 1e-6


def load_consts(P, st, C):
    K = {}
    K["idf"] = P.sb(st, "idf", [128, 128], F32); K["b_idf"] = Buf("idf")
    K["idb"] = P.sb(st, "idb", [128, 128], BF16); K["b_idb"] = Buf("idb")
    K["eps"] = P.sb(st, "epsc", [128, 1], F32); K["b_eps"] = Buf("eps")
    ci = Buf("cin")
    P.dma(K["idf"][:, :], C["idf"][:, :], reads=[ci], writes=[K["b_idf"]])
    P.op("dve", lambda v: v.tensor_copy(out=K["idb"][:, :], in_=K["idf"][:, :]), reads=[K["b_idf"]], writes=[K["b_idb"]])
    P.op("dve", lambda v: v.memset(K["eps"][:, :], EPS), writes=[K["b_eps"]])
    return K


def phase_norm(P, K, X, bX, MODS, sh_off, norm_g, HT, bHT, HTM=None, bHTM=None, tiles=range(NT)):
    with contextlib.ExitStack() as st:
        G = P.sb(st, "n_G", [128, D], F32); bG = Buf()
        SC = P.sb(st, "n_SC", [128, 2, D], F32); bSC = Buf()
        AV = P.sb(st, "n_AV", [128, 2, D], F32); bAV = Buf()
        BV = P.sb(st, "n_BV", [128, 2, D], F32); bBV = Buf()
        XT = [P.sb(st, f"n_XT{i}", [128, D], F32) for i in range(3)]; bXT = [Buf() for _ in range(3)]
        JK = P.sb(st, "n_JK", [128, D], F32); bJK = Buf()
        TMP = [P.sb(st, f"n_TMP{i}", [128, D], F32) for i in range(3)]; bTMP = [Buf() for _ in range(3)]
        SS = P.sb(st, "n_SS", [128, 3 * NT], F32); bSS = [Buf() for _ in range(NT)]
        HL = [P.sb(st, f"n_HL{i}", [128, D], BF16) for i in range(3)]; bHL = [Buf() for _ in range(3)]
        PT = [P.ps(st, f"n_PT{i}", [128, 8, 128], BF16) for i in range(3)]; bPT = [Buf(excl=True) for _ in range(3)]
        cin = Buf()
        P.dma(G[:, :], norm_g.partition_broadcast(128), reads=[cin], writes=[bG])
        for r in range(2):
            P.dma(SC[:, r, :], MODS[r, sh_off + D:sh_off + 2 * D].partition_broadcast(128), reads=[cin], writes=[bSC])
            P.dma(BV[:, r, :], MODS[r, sh_off:sh_off + D].partition_broadcast(128), reads=[cin], writes=[bBV])
        for r in range(2):
            P.op("dve", lambda v, r=r: v.scalar_tensor_tensor(out=AV[:, r, :], in0=SC[:, r, :], scalar=1.0, in1=G[:, :], op0=ALU.add, op1=ALU.mult),
                 reads=[bSC, bG], writes=[bAV])
        for i, tt in enumerate(tiles):
            r = 1 if tt < 2 else 0
            b = i % 3
            xt = XT[b]
            P.dma(xt[:, :], X[tt * 128:(tt + 1) * 128, :], reads=[bX[tt]], writes=[bXT[b]])
            P.op("act", lambda a, xt=xt, tt=tt: a.activation(out=JK[:, :], in_=xt[:, :], func=AF.Square, accum_out=SS[:, 3 * tt:3 * tt + 1]),
                 reads=[bXT[b]], writes=[bJK, bSS[tt]])
            P.op("act", lambda a, tt=tt: a.activation(out=SS[:, 3 * tt + 1:3 * tt + 2], in_=SS[:, 3 * tt:3 * tt + 1], func=AF.Sqrt, scale=1.0 / D, bias=K["eps"][:, :]),
                 reads=[bSS[tt], K["b_eps"]], writes=[bSS[tt]])
            P.op("dve", lambda v, tt=tt: v.reciprocal(out=SS[:, 3 * tt + 2:3 * tt + 3], in_=SS[:, 3 * tt + 1:3 * tt + 2]), reads=[bSS[tt]], writes=[bSS[tt]])
            P.op("dve", lambda v, xt=xt, tt=tt, r=r, b=b: v.scalar_tensor_tensor(out=TMP[b][:, :], in0=xt[:, :], scalar=SS[:, 3 * tt + 2:3 * tt + 3], in1=AV[:, r, :], op0=ALU.mult, op1=ALU.mult),
                 reads=[bXT[b], bSS[tt], bAV], writes=[bTMP[b]])
            if HTM is not None:
                hdst = HTM[:, tt, :]; bh = bHTM[tt]
            else:
                hdst = HL[b][:, :]; bh = bHL[b]
            P.op("dve", lambda g, hdst=hdst, r=r, b=b: g.tensor_tensor(out=hdst, in0=TMP[b][:, :], in1=BV[:, r, :], op=ALU.add),
                 reads=[bTMP[b], bBV], writes=[bh])
            for k in range(8):
                P.op("pe", lambda t, k=k, hdst=hdst, b=b: t.transpose(PT[b][:, k, :], hdst[:, k * 128:(k + 1) * 128], K["idb"][:, :]),
                     reads=[bh, K["b_idb"]], writes=[bPT[b]])
            if i % 2 == 0:
                P.op("act", lambda a, tt=tt, b=b: a.copy(out=HT[:, :, tt * 128:(tt + 1) * 128], in_=PT[b][:, :, :]), reads=[bPT[b]], writes=[bHT[tt]])
            else:
                P.op("dve", lambda v, tt=tt, b=b: v.tensor_copy(out=HT[:, :, tt * 128:(tt + 1) * 128], in_=PT[b][:, :, :]), reads=[bPT[b]], writes=[bHT[tt]])
        P.flush()


def phase_outproj(P, K, src, WO, X, bX, MODS, g_off, tiles=range(NT), mode="tm", norm2=None):
    with contextlib.ExitStack() as st:
        WOb = P.sb(st, "o_WO", [128, 8, D], BF16); bWO = [Buf() for _ in range(8)]
        STG = [P.sb(st, f"o_stg{i}", [128, D], F32) for i in range(2)]; bSTG = [Buf() for _ in range(2)]
        G1 = P.sb(st, "o_G1", [128, 2, D], F32); bG1 = Buf()
        XT = [P.sb(st, f"o_XT{i}", [128, D], F32) for i in range(3)]; bXT = [Buf() for _ in range(3)]
        TMP = [P.sb(st, f"o_TMP{i}", [128, 512], F32) for i in range(2)]; bTMP = [Buf() for _ in range(2)]
        YL = [P.sb(st, f"o_YL{i}", [128, D], BF16) for i in range(3)]; bYL = [Buf() for _ in range(3)]
        YTt = [P.sb(st, f"o_YTt{i}", [128, 8, 128], BF16) for i in range(3)]; bYTt = [Buf() for _ in range(3)]
        PT = [P.ps(st, f"o_PT{i}", [128, 8, 128], BF16) for i in range(3)]; bPT = [Buf(excl=True) for _ in range(3)]
        PO = [P.ps(st, f"o_PO{i}") for i in range(2)]; bPO = [Buf(excl=True) for _ in range(2)]
        cin = Buf()
        if norm2 is not None:
            n_off, n_g, nHT, nbHT, nHTM, nbHTM = norm2
            nG = P.sb(st, "on_G", [128, D], F32); nSC = P.sb(st, "on_SC", [128, 2, D], F32); nbC = Buf()
            nAV = P.sb(st, "on_AV", [128, 2, D], F32); nBV = P.sb(st, "on_BV", [128, 2, D], F32); nbAV = Buf()
            nJK = P.sb(st, "on_JK", [128, D], F32); nbJK = Buf()
            nTMP = [P.sb(st, f"on_TMP{i}", [128, D], F32) for i in range(3)]; nbTMP = [Buf() for _ in range(3)]
            nSS = P.sb(st, "on_SS", [128, 3 * NT], F32); nbSS = [Buf() for _ in range(NT)]
            nPT = [P.ps(st, f"on_PT{i}", [128, 8, 128], BF16) for i in range(3)]; nbPT = [Buf(excl=True) for _ in range(3)]
            P.dma(nG[:, :], n_g.partition_broadcast(128), reads=[cin], writes=[nbC])
            for r in range(2):
                P.dma(nSC[:, r, :], MODS[r, n_off + D:n_off + 2 * D].partition_broadcast(128), reads=[cin], writes=[nbC])
                P.dma(nBV[:, r, :], MODS[r, n_off:n_off + D].partition_broadcast(128), reads=[cin], writes=[nbAV])
            for r in range(2):
                P.op("dve", lambda v, r=r: v.scalar_tensor_tensor(out=nAV[:, r, :], in0=nSC[:, r, :], scalar=1.0, in1=nG[:, :], op0=ALU.add, op1=ALU.mult), reads=[nbC], writes=[nbAV])
        for k in range(8):
            s = k % 2
            P.dma(STG[s][:, :], WO[k * 128:(k + 1) * 128, :], reads=[cin], writes=[bSTG[s]])
            if k % 2 == 0:
                P.op("dve", lambda g, k=k, s=s: g.tensor_copy(out=WOb[:, k, :], in_=STG[s][:, :]), reads=[bSTG[s]], writes=[bWO[k]])
            else:
                P.op("act", lambda a, k=k, s=s: a.copy(out=WOb[:, k, :], in_=STG[s][:, :]), reads=[bSTG[s]], writes=[bWO[k]])
        for r in range(2):
            P.dma(G1[:, r, :], MODS[r, g_off:g_off + D].partition_broadcast(128), reads=[cin], writes=[bG1])
        pi = 0
        for i, tt in enumerate(tiles):
            r = 1 if tt < 2 else 0
            b = i % 3
            if mode == "tm":
                src(tt, YL[b], bYL[b])
                for k in range(8):
                    P.op("pe", lambda t, k=k, b=b: t.transpose(PT[b][:, k, :], YL[b][:, k * 128:(k + 1) * 128], K["idb"][:, :]),
                         reads=[bYL[b], K["b_idb"]], writes=[bPT[b]])
                P.op("act", lambda a, b=b: a.copy(out=YTt[b][:, :, :], in_=PT[b][:, :, :]), reads=[bPT[b]], writes=[bYTt[b]])
                lhs = lambda k, b=b: YTt[b][:, k, :]
                blhs = [bYTt[b]]
            else:
                YT, bYT = src
                lhs = lambda k, tt=tt: YT[:, k, tt * 128:(tt + 1) * 128]
                blhs = [bYT[tt]]
            P.dma(XT[b][:, :], X[tt * 128:(tt + 1) * 128, :], reads=[bX[tt]], writes=[bXT[b]])
            for dh in range(2):
                p = pi % 2; pi += 1
                for k in range(8):
                    P.op("pe", lambda t, k=k, p=p, dh=dh, lhs=lhs: t.matmul(PO[p][:, :], lhs(k), WOb[:, k, dh * 512:(dh + 1) * 512], start=(k == 0), stop=(k == 7)),
                         reads=blhs + [bWO[k]], writes=[bPO[p]])
                P.op("dve", lambda v, p=p, r=r, dh=dh: v.tensor_tensor(out=TMP[p][:, :], in0=PO[p][:, :], in1=G1[:, r, dh * 512:(dh + 1) * 512], op=ALU.mult),
                     reads=[bPO[p], bG1], writes=[bTMP[p]])
                P.op("dve", lambda g, p=p, b=b, dh=dh: g.tensor_tensor(out=XT[b][:, dh * 512:(dh + 1) * 512], in0=TMP[p][:, :], in1=XT[b][:, dh * 512:(dh + 1) * 512], op=ALU.add),
                     reads=[bTMP[p], bXT[b]], writes=[bXT[b]])
            P.dma(X[tt * 128:(tt + 1) * 128, :], XT[b][:, :], reads=[bXT[b]], writes=[bX[tt]])
            if norm2 is not None:
                P.op("act", lambda a, b=b, tt=tt: a.activation(out=nJK[:, :], in_=XT[b][:, :], func=AF.Square, accum_out=nSS[:, 3 * tt:3 * tt + 1]), reads=[bXT[b]], writes=[nbJK, nbSS[tt]])
                P.op("act", lambda a, tt=tt: a.activation(out=nSS[:, 3 * tt + 1:3 * tt + 2], in_=nSS[:, 3 * tt:3 * tt + 1], func=AF.Sqrt, scale=1.0 / D, bias=K["eps"][:, :]), reads=[nbSS[tt], K["b_eps"]], writes=[nbSS[tt]])
                P.op("dve", lambda v, tt=tt: v.reciprocal(out=nSS[:, 3 * tt + 2:3 * tt + 3], in_=nSS[:, 3 * tt + 1:3 * tt + 2]), reads=[nbSS[tt]], writes=[nbSS[tt]])
                P.op("dve", lambda v, b=b, tt=tt, r=r: v.scalar_tensor_tensor(out=nTMP[b][:, :], in0=XT[b][:, :], scalar=nSS[:, 3 * tt + 2:3 * tt + 3], in1=nAV[:, r, :], op0=ALU.mult, op1=ALU.mult),
                     reads=[bXT[b], nbSS[tt], nbAV], writes=[nbTMP[b]])
                P.op("dve", lambda v, b=b, tt=tt, r=r: v.tensor_tensor(out=nHTM[:, tt, :], in0=nTMP[b][:, :], in1=nBV[:, r, :], op=ALU.add), reads=[nbTMP[b], nbAV], writes=[nbHTM[tt]])
                for k in range(8):
                    P.op("pe", lambda t, k=k, b=b, tt=tt: t.transpose(nPT[b][:, k, :], nHTM[:, tt, k * 128:(k + 1) * 128], K["idb"][:, :]), reads=[nbHTM[tt], K["b_idb"]], writes=[nbPT[b]])
                P.op("act", lambda a, tt=tt, b=b: a.copy(out=nHT[:, :, tt * 128:(tt + 1) * 128], in_=nPT[b][:, :, :]), reads=[nbPT[b]], writes=[nbHT[tt]])
        P.flush()


def phase_route(P, K, C, HT, bHT, HTM, bHTM, RW, XG, GV, POSMT_out, dbg=None):
    with contextlib.ExitStack() as st:
        RWf = P.sb(st, "r_RWf", [128, 8, NE], F32); bRWf = Buf()
        RWb = P.sb(st, "r_RWb", [128, 8, NE], BF16); bRWb = Buf()
        AFF = P.sb(st, "r_AFF", [128, NT, NE], F32); bAFF = [Buf() for _ in range(NT)]
        AHI = P.sb(st, "r_AHI", [128, NT, NE], BF16)
        ALO = P.sb(st, "r_ALO", [128, NT, NE], BF16)
        AT32 = P.sb(st, "r_AT32", [128, NT, NE], F32)
        SM = P.sb(st, "r_SM", [128, NT, 4], F32); bSM = [Buf() for _ in range(NT)]
        EX = P.sb(st, "r_EX", [128, NT, NE], F32)
        AFFT = P.sb(st, "r_AFFT", [NE, T], F32); bAFFT = Buf()
        W = P.sb(st, "r_W", [NE, T], F32); bW = [Buf(), Buf()]
        M8 = P.sb(st, "r_M8", [NE, 8 * 36], F32); bM8 = [Buf(), Buf()]
        CA = P.sb(st, "r_CA", [NE, T], F32); bCA = [Buf(), Buf()]
        CB = P.sb(st, "r_CB", [NE, T], F32); bCB = [Buf(), Buf()]
        MASK = P.sb(st, "r_MASK", [NE, T], F32); bMASK = [Buf(), Buf()]
        POSM = P.sb(st, "r_POSM", [128, NT, NE], F32); bPOSM = [Buf() for _ in range(NT)]
        IOTA = P.sb(st, "r_IOTA", [128, NSLOT], F32); bIOTA = Buf()
        PSEL = [P.sb(st, f"r_PSEL{i}", [128, NT, 256], BF16) for i in range(2)]; bPSEL = [[Buf() for _ in range(NT)] for _ in range(2)]
        XGT = [P.sb(st, f"r_XGT{i}", [128, 8, NSLOT], BF16) for i in range(2)]; bXGT = [Buf() for _ in range(2)]
        GVR = [P.sb(st, f"r_GVR{i}", [1, NSLOT], F32) for i in range(2)]; bGVR = [Buf() for _ in range(2)]
        PL = [P.ps(st, f"r_PL{i}") for i in range(2)]; bPL = [Buf(excl=True) for _ in range(2)]
        PG = [P.ps(st, f"r_PG{i}") for i in range(3)]; bPG = [Buf(excl=True) for _ in range(3)]
        PV = [P.ps(st, f"r_PV{i}") for i in range(2)]; bPV = [Buf(excl=True) for _ in range(2)]
        cin = Buf(); cout = Buf()
        P.dma(RWf[:, :, :], RW.rearrange("(c p) e -> p c e", p=128), reads=[cin], writes=[bRWf])
        P.op("dve", lambda v: v.tensor_copy(out=RWb[:, :, :], in_=RWf[:, :, :]), reads=[bRWf], writes=[bRWb])
        P.dma(IOTA[:, :], C["iota"][0:NSLOT].partition_broadcast(128), reads=[cin], writes=[bIOTA])
        for tt in range(NT):
            p = tt % 2
            for k in range(8):
                P.op("pe", lambda t, k=k, tt=tt, p=p: t.matmul(PL[p][:, 0:NE], HT[:, k, tt * 128:(tt + 1) * 128], RWb[:, k, :], start=(k == 0), stop=(k == 7)),
                     reads=[bHT[tt], bRWb], writes=[bPL[p]])
            P.op("dve", lambda v, tt=tt, p=p: v.reduce_max(out=SM[:, tt, 0:1], in_=PL[p][:, 0:NE], axis=AX.X), reads=[bPL[p]], writes=[bSM[tt]])
            P.op("dve", lambda v, tt=tt: v.tensor_scalar(out=SM[:, tt, 1:2], in0=SM[:, tt, 0:1], scalar1=-1.0, scalar2=None, op0=ALU.mult), reads=[bSM[tt]], writes=[bSM[tt]])
            P.op("act", lambda a, tt=tt, p=p: a.activation(out=EX[:, tt, :], in_=PL[p][:, 0:NE], func=AF.Exp, bias=SM[:, tt, 1:2], accum_out=SM[:, tt, 2:3]),
                 reads=[bPL[p], bSM[tt]], writes=[bAFF[tt], bSM[tt]])
            P.op("dve", lambda v, tt=tt: v.reciprocal(out=SM[:, tt, 3:4], in_=SM[:, tt, 2:3]), reads=[bSM[tt]], writes=[bSM[tt]])
            P.op("dve", lambda v, tt=tt: v.tensor_scalar(out=AFF[:, tt, :], in0=EX[:, tt, :], scalar1=SM[:, tt, 3:4], scalar2=None, op0=ALU.mult), reads=[bSM[tt], bAFF[tt]], writes=[bAFF[tt]])
            P.op("dve", lambda v, tt=tt: v.tensor_copy(out=AHI[:, tt, :], in_=AFF[:, tt, :]), reads=[bAFF[tt]], writes=[bAFF[tt]])
            P.op("dve", lambda v, tt=tt: v.tensor_copy(out=AT32[:, tt, :], in_=AHI[:, tt, :]), reads=[bAFF[tt]], writes=[bAFF[tt]])
            P.op("dve", lambda v, tt=tt: v.tensor_tensor(out=ALO[:, tt, :], in0=AFF[:, tt, :], in1=AT32[:, tt, :], op=ALU.subtract), reads=[bAFF[tt]], writes=[bAFF[tt]])
            q = tt % 3
            P.op("pe", lambda t, tt=tt, q=q: t.transpose(PG[q][0:NE, 0:128], AFF[:, tt, :], K["idf"][:, :]), reads=[bAFF[tt], K["b_idf"]], writes=[bPG[q]])
            P.op("act", lambda a, tt=tt, q=q: a.copy(out=AFFT[:, tt * 128:(tt + 1) * 128], in_=PG[q][0:NE, 0:128]), reads=[bPG[q]], writes=[bAFFT])
        segs = [(0, 256, 32, 0.0), (256, T, 256, 32.0)]
        for si, (a0, a1, kk, base) in enumerate(segs):
            eng = "dve"
            P.op(eng, lambda v, a0=a0, a1=a1: v.tensor_copy(out=W[:, a0:a1], in_=AFFT[:, a0:a1]), reads=[bAFFT], writes=[bW[si]])
            nr = kk // 8
            for rnd in range(nr):
                mo = (si * 32 + rnd) * 8 if si == 0 else (4 + rnd) * 8
                P.op("dve", lambda v, a0=a0, a1=a1, mo=mo: v.max(out=M8[:, mo:mo + 8], in_=W[:, a0:a1]), reads=[bW[si]], writes=[bM8[si]])
                if rnd < nr - 1:
                    P.op("dve", lambda v, a0=a0, a1=a1, mo=mo: v.match_replace(out=W[:, a0:a1], in_to_replace=M8[:, mo:mo + 8], in_values=W[:, a0:a1], imm_value=-1.0),
                         reads=[bW[si], bM8[si]], writes=[bW[si]])
            thr = M8[:, mo + 7:mo + 8]
            P.op("dve", lambda v, a0=a0, a1=a1, thr=thr: v.tensor_scalar(out=MASK[:, a0:a1], in0=AFFT[:, a0:a1], scalar1=thr, scalar2=None, op0=ALU.is_ge),
                 reads=[bAFFT, bM8[si]], writes=[bMASK[si]])
            n = a1 - a0
            src, bsrc, dst, bdst = MASK, bMASK, CA, bCA
            sh = 1
            first = True
            while sh < n:
                P.op("dve", lambda v, src=src, dst=dst, sh=sh, a0=a0, a1=a1: v.tensor_tensor(out=dst[:, a0 + sh:a1], in0=src[:, a0 + sh:a1], in1=src[:, a0:a1 - sh], op=ALU.add),
                     reads=[bsrc[si]], writes=[bdst[si]])
                P.op("pool", lambda g, src=src, dst=dst, sh=sh, a0=a0: g.tensor_copy(out=dst[:, a0:a0 + sh], in_=src[:, a0:a0 + sh]),
                     reads=[bsrc[si]], writes=[bdst[si]])
                if first:
                    src, bsrc, dst, bdst = CA, bCA, CB, bCB
                    first = False
                else:
                    src, bsrc, dst, bdst = dst, bdst, src, bsrc
                sh *= 2
            incl, bincl = src, bsrc
            other, bother = (CB, bCB) if incl is CA else (CA, bCA)
            P.op("dve", lambda v, incl=incl, other=other, a0=a0, a1=a1, base=base: v.scalar_tensor_tensor(out=other[:, a0:a1], in0=incl[:, a0:a1], scalar=base, in1=MASK[:, a0:a1], op0=ALU.add, op1=ALU.mult),
                 reads=[bincl[si], bMASK[si]], writes=[bother[si]])
            P.op("dve", lambda v, other=other, a0=a0, a1=a1: v.tensor_scalar(out=W[:, a0:a1], in0=other[:, a0:a1], scalar1=-1.0, scalar2=None, op0=ALU.add),
                 reads=[bother[si]], writes=[bW[si]])
        P.dma(POSMT_out[:, :], W[:, :], reads=[bW[0], bW[1]], writes=[cout])
        if dbg is not None:
            P.dma(dbg["afft"][:, :], AFFT[:, :], reads=[bAFFT], writes=[cout])
        for tt in range(NT):
            q = tt % 3
            si = 0 if tt < 2 else 1
            P.op("pe", lambda t, tt=tt, q=q: t.transpose(PG[q][:, 0:NE], W[:, tt * 128:(tt + 1) * 128], K["idf"][0:NE, 0:NE]), reads=[bW[si], K["b_idf"]], writes=[bPG[q]])
            P.op("act", lambda a, tt=tt, q=q: a.copy(out=POSM[:, tt, :], in_=PG[q][:, 0:NE]), reads=[bPG[q]], writes=[bPOSM[tt]])
        for e in range(NE):
            b = e % 2
            for tt in range(NT):
                c0, nc_ = (0, 32) if tt < 2 else (32, 256)
                eng = "dve"
                P.op(eng, lambda v, tt=tt, e=e, b=b, c0=c0, nc_=nc_: v.tensor_scalar(out=PSEL[b][:, tt, 0:nc_], in0=IOTA[:, c0:c0 + nc_], scalar1=POSM[:, tt, e:e + 1], scalar2=None, op0=ALU.is_equal),
                     reads=[bIOTA, bPOSM[tt]], writes=[bPSEL[b][tt]])
            for dc in range(8):
                p = dc % 3
                for tt in range(NT):
                    c0, nc_ = (0, 32) if tt < 2 else (32, 256)
                    st_ = tt in (0, 2); sp_ = tt in (1, NT - 1)
                    P.op("pe", lambda t, tt=tt, dc=dc, p=p, b=b, c0=c0, nc_=nc_, st_=st_, sp_=sp_: t.matmul(PG[p][:, c0:c0 + nc_], HTM[:, tt, dc * 128:(dc + 1) * 128], PSEL[b][:, tt, 0:nc_], start=st_, stop=sp_),
                         reads=[bHTM[tt], bPSEL[b][tt]], writes=[bPG[p]])
                if dc % 2 == 0:
                    P.op("act", lambda a, dc=dc, p=p, b=b: a.copy(out=XGT[b][:, dc, :], in_=PG[p][:, 0:NSLOT]), reads=[bPG[p]], writes=[bXGT[b]])
                else:
                    P.op("dve", lambda v, dc=dc, p=p, b=b: v.tensor_copy(out=XGT[b][:, dc, :], in_=PG[p][:, 0:NSLOT]), reads=[bPG[p]], writes=[bXGT[b]])
            P.dma(XG[e].rearrange("(c p) s -> p c s", p=128), XGT[b][:, :, :], reads=[bXGT[b]], writes=[cout])
            for tt in range(NT):
                c0, nc_ = (0, 32) if tt < 2 else (32, 256)
                for hl, A_ in enumerate((AHI, ALO)):
                    st_ = (tt in (0, 2)) and hl == 0; sp_ = (tt in (1, NT - 1)) and hl == 1
                    P.op("pe", lambda t, tt=tt, e=e, b=b, c0=c0, nc_=nc_, st_=st_, sp_=sp_, A_=A_: t.matmul(PV[b][0:1, c0:c0 + nc_], A_[:, tt, e:e + 1], PSEL[b][:, tt, 0:nc_], start=st_, stop=sp_),
                         reads=[bAFF[tt], bPSEL[b][tt]], writes=[bPV[b]])
            P.op("act", lambda a, b=b: a.copy(out=GVR[b][:, :], in_=PV[b][0:1, 0:NSLOT]), reads=[bPV[b]], writes=[bGVR[b]])
            P.dma(GV[e:e + 1, :], GVR[b][:, :], reads=[bGVR[b]], writes=[cout])
        P.flush()


def phase_pro(P, K, C, Y, POSMT, MODS_prev, X_in, X, bX):
    with contextlib.ExitStack() as st:
        YG = P.sb(st, "p_YG", [128, NE * 2, D], BF16); bYG = [Buf() for _ in range(NE)]
        YGC = P.sb(st, "p_YGC", [128, 4, D], BF16); bYGC = Buf()
        PM = P.sb(st, "p_PM", [NE, T], F32); bPM = Buf()
        SEL = P.sb(st, "p_SEL", [NE, NE, 128], F32); bSEL = Buf()
        SELQ = P.sb(st, "p_SELQ", [NE, 4, 128], F32)
        SID = P.sb(st, "p_SID", [128, 4], F32); bSID = Buf()
        G2 = P.sb(st, "p_G2", [128, 2, D], F32); bG2 = Buf()
        PTt = [P.sb(st, f"p_PT{i}", [128, 384], BF16) for i in range(3)]; bPTt = [Buf() for _ in range(3)]
        XT = [P.sb(st, f"p_XT{i}", [128, D], F32) for i in range(2)]; bXT = [Buf() for _ in range(2)]
        TMP = [P.sb(st, f"p_TMP{i}", [128, 512], F32) for i in range(2)]; bTMP = [Buf() for _ in range(2)]
        ACC = [P.ps(st, f"p_ACC{i}") for i in range(6)]; bACC = [Buf(excl=True) for _ in range(6)]
        PB = [P.ps(st, f"p_PB{i}") for i in range(2)]; bPB = [Buf(excl=True) for _ in range(2)]
        cin = Buf()
        for e in range(NE):
            P.dma(YG[:, e * 2:e * 2 + 2, :], Y[e, 32:288, :].rearrange("(k p) d -> p k d", p=128), reads=[cin], writes=[bYG[e]])
            P.dma(YGC[(e % 4) * 32:(e % 4) * 32 + 32, e // 4, :], Y[e, 0:32, :], reads=[cin], writes=[bYGC])
        P.dma(PM[:, :], POSMT[:, :], reads=[cin], writes=[bPM])
        P.dma(SEL[:, :, :], C["sel16"][:, :, :], reads=[cin], writes=[bSEL])
        P.dma(SELQ[:, :, :], C["selq"][:, :, :], reads=[cin], writes=[bSEL])
        P.dma(SID[:, :], C["slotid"][:, :], reads=[cin], writes=[bSID])
        for r in range(2):
            P.dma(G2[:, r, :], MODS_prev[r, 5 * D:6 * D].partition_broadcast(128), reads=[cin], writes=[bG2])
        state = {"pti": 0, "xi": 0, "ti": 0}

        def finish(tiles_):
            for j, tt in enumerate(tiles_):
                r = 1 if tt < 2 else 0
                b = state["xi"] % 2; state["xi"] += 1
                P.dma(XT[b][:, :], X_in[tt * 128:(tt + 1) * 128, :], reads=[cin], writes=[bXT[b]])
                for dh in range(2):
                    a = j * 2 + dh
                    p = state["ti"] % 2; state["ti"] += 1
                    P.op("dve", lambda v, a=a, p=p, r=r, dh=dh: v.tensor_tensor(out=TMP[p][:, :], in0=ACC[a][:, :], in1=G2[:, r, dh * 512:(dh + 1) * 512], op=ALU.mult),
                         reads=[bACC[a], bG2], writes=[bTMP[p]])
                    P.op("dve", lambda g_, p=p, b=b, dh=dh: g_.tensor_tensor(out=XT[b][:, dh * 512:(dh + 1) * 512], in0=TMP[p][:, :], in1=XT[b][:, dh * 512:(dh + 1) * 512], op=ALU.add),
                         reads=[bTMP[p], bXT[b]], writes=[bXT[b]])
                P.dma(X[tt * 128:(tt + 1) * 128, :], XT[b][:, :], reads=[bXT[b]], writes=[bX[tt]])

        ntok = 256
        for g4 in range(4):
            pb = g4 % 2
            P.op("pe", lambda t, g4=g4, pb=pb: t.matmul(PB[pb][:, 0:ntok], SELQ[:, g4, :], PM[:, 0:ntok], start=True, stop=True), reads=[bSEL, bPM], writes=[bPB[pb]])
            pt = state["pti"] % 3; state["pti"] += 1
            P.op("dve", lambda v, pt=pt, pb=pb: v.tensor_scalar(out=PTt[pt][:, 0:ntok], in0=PB[pb][:, 0:ntok], scalar1=SID[:, 3:4], scalar2=None, op0=ALU.is_equal), reads=[bPB[pb], bSID], writes=[bPTt[pt]])
            for j in range(2):
                for dh in range(2):
                    a = j * 2 + dh
                    P.op("pe", lambda t, a=a, pt=pt, j=j, dh=dh, g4=g4: t.matmul(ACC[a][:, :], PTt[pt][:, j * 128:(j + 1) * 128], YGC[:, g4, dh * 512:(dh + 1) * 512], start=(g4 == 0), stop=(g4 == 3)),
                         reads=[bPTt[pt], bYGC], writes=[bACC[a]])
        finish([0, 1])
        groups = [list(range(2 + 3 * g, min(2 + 3 * g + 3, NT))) for g in range(6)]
        for tiles_ in groups:
            t0 = tiles_[0] * 128; ntk = len(tiles_) * 128

            def emit_sel(e, t0=t0, ntk=ntk):
                pb = e % 2
                P.op("pe", lambda t, e=e, pb=pb, t0=t0, ntk=ntk: t.matmul(PB[pb][:, 0:ntk], SEL[:, e, :], PM[:, t0:t0 + ntk], start=True, stop=True), reads=[bSEL, bPM], writes=[bPB[pb]])
            emit_sel(0)
            for e in range(NE):
                pb = e % 2
                if e + 1 < NE:
                    emit_sel(e + 1)
                for k in range(2):
                    pt = state["pti"] % 3; state["pti"] += 1
                    P.op("dve", lambda v, pt=pt, pb=pb, k=k, ntk=ntk: v.tensor_scalar(out=PTt[pt][:, 0:ntk], in0=PB[pb][:, 0:ntk], scalar1=SID[:, k:k + 1], scalar2=None, op0=ALU.is_equal),
                         reads=[bPB[pb], bSID], writes=[bPTt[pt]])
                    for j in range(len(tiles_)):
                        for dh in range(2):
                            a = j * 2 + dh
                            first = (e == 0 and k == 0); last = (e == NE - 1 and k == 1)
                            P.op("pe", lambda t, a=a, pt=pt, j=j, dh=dh, e=e, k=k, first=first, last=last: t.matmul(ACC[a][:, :], PTt[pt][:, j * 128:(j + 1) * 128], YG[:, e * 2 + k, dh * 512:(dh + 1) * 512], start=first, stop=last),
                                 reads=[bPTt[pt], bYG[e]], writes=[bACC[a]])
            finish(tiles_)
        P.flush()


NS = 2304
FF = 2816
D = 1024

def build_B(P, xgT, gv, wg, wu, wd, y, nexp=2):
    nc = P.nc
    with contextlib.ExitStack() as st:
        XT = P.sb(st, "XT", [128, 8, NS], BF16); bXT = Buf("XT")
        GV = P.sb(st, "GV", [128, 18], F32); bGV = Buf("GV")
        WD = P.sb(st, "WD", [128, 22, D], BF16); bWD = [Buf(f"WD{f}") for f in range(22)]
        ACTT = P.sb(st, "ACTT", [128, 22, NS // 2], BF16); bACTT = [Buf(f"ACTT{f}") for f in range(22)]
        stg = [P.sb(st, f"stg{i}", [128, 1024], F32) for i in range(4)]; bstg = [Buf(f"stg{i}") for i in range(4)]
        WG = [P.sb(st, f"WG{i}", [128, 8, 128], BF16) for i in range(2)]; bWG = [Buf() for _ in range(2)]
        WU = [P.sb(st, f"WU{i}", [128, 8, 128], BF16) for i in range(2)]; bWU = [Buf() for _ in range(2)]
        SA = [P.sb(st, f"SA{i}", [128, 384], F32) for i in range(2)]; bSA = [Buf() for _ in range(2)]
        YT = [P.sb(st, f"YT{i}", [128, D], BF16) for i in range(2)]; bYT = [Buf() for _ in range(2)]
        PA = [P.ps(st, f"PA{i}") for i in range(2)]; bPA = [Buf(excl=True) for _ in range(2)]
        PU = [P.ps(st, f"PU{i}") for i in range(2)]; bPU = [Buf(excl=True) for _ in range(2)]
        PY = [P.ps(st, f"PY{i}") for i in range(2)]; bPY = [Buf(excl=True) for _ in range(2)]
        bin_ = Buf("in"); bout = Buf("out")
        sgi = 0; ci = 0; pi = 0; yi = 0; pyi = 0
        for e in range(nexp):
            P.dma(XT[:, :, :], xgT[e].rearrange("(c p) s -> p c s", p=128), reads=[bin_], writes=[bXT])
            P.dma(GV[:, :], gv[e], reads=[bin_], writes=[bGV])
            for f in range(22):
                s = sgi % 4; sgi += 1
                P.dma(stg[s][:, :], wd[e, f * 128:(f + 1) * 128, :], reads=[bin_], writes=[bstg[s]])
                eng = "pool" if ci % 2 == 0 else "act"; ci += 1
                if eng == "pool":
                    P.op("pool", lambda g, o=WD[:, f, :], i=stg[s][:, :]: g.tensor_copy(out=o, in_=i), reads=[bstg[s]], writes=[bWD[f]])
                else:
                    P.op("act", lambda g, o=WD[:, f, :], i=stg[s][:, :]: g.copy(out=o, in_=i), reads=[bstg[s]], writes=[bWD[f]])
            for sh in range(2):
                for f in range(22):
                    w = f % 2
                    for (src, dst, bdst) in ((wg, WG[w], bWG[w]), (wu, WU[w], bWU[w])):
                        s = sgi % 4; sgi += 1
                        sv = stg[s][:, :].rearrange("p (c f) -> p c f", c=8)
                        P.dma(sv, src[e].rearrange("(c p) f -> p c f", p=128)[:, :, f * 128:(f + 1) * 128], reads=[bin_], writes=[bstg[s]])
                        eng = "pool" if ci % 2 == 0 else "act"; ci += 1
                        if eng == "pool":
                            P.op("pool", lambda g, o=dst[:, :, :], i=sv: g.tensor_copy(out=o, in_=i), reads=[bstg[s]], writes=[bdst])
                        else:
                            P.op("act", lambda g, o=dst[:, :, :], i=sv: g.copy(out=o, in_=i), reads=[bstg[s]], writes=[bdst])
                    for nb in range(3):
                        s0 = sh * (NS // 2) + nb * 384
                        p = pi % 2; pi += 1
                        for k in range(8):
                            P.op("pe", lambda t, o=PA[p][:, 0:384], l=WG[w][:, k, :], r=XT[:, k, s0:s0 + 384], k=k: t.matmul(o, l, r, start=(k == 0), stop=(k == 7)),
                                 reads=[bWG[w], bXT], writes=[bPA[p]])
                        for k in range(8):
                            P.op("pe", lambda t, o=PU[p][:, 0:384], l=WU[w][:, k, :], r=XT[:, k, s0:s0 + 384], k=k: t.matmul(o, l, r, start=(k == 0), stop=(k == 7)),
                                 reads=[bWU[w], bXT], writes=[bPU[p]])
                        P.op("act", lambda a, o=SA[p][:, :], i=PA[p][:, 0:384]: a.activation(out=o, in_=i, func=AF.Silu), reads=[bPA[p]], writes=[bSA[p]])
                        P.op("dve", lambda v, o=ACTT[:, f, nb * 384:(nb + 1) * 384], a=SA[p][:, :], b=PU[p][:, 0:384]: v.tensor_tensor(out=o, in0=a, in1=b, op=ALU.mult),
                             reads=[bSA[p], bPU[p]], writes=[bACTT[f]])
                for sc in range(9):
                    yb = yi % 2; yi += 1
                    chunk = sh * 9 + sc
                    for dh in range(2):
                        p = pyi % 2; pyi += 1
                        for f in range(22):
                            P.op("pe", lambda t, o=PY[p][:, :], l=ACTT[:, f, sc * 128:(sc + 1) * 128], r=WD[:, f, dh * 512:(dh + 1) * 512], f=f: t.matmul(o, l, r, start=(f == 0), stop=(f == 21)),
                                 reads=[bACTT[f], bWD[f]], writes=[bPY[p]])
                        if dh == 0:
                            P.op("dve", lambda v, o=YT[yb][:, 0:512], i=PY[p][:, :], s=GV[:, chunk:chunk + 1]: v.tensor_scalar(out=o, in0=i, scalar1=s, scalar2=None, op0=ALU.mult),
                                 reads=[bPY[p], bGV], writes=[bYT[yb]])
                        else:
                            P.op("act", lambda a, o=YT[yb][:, 512:1024], i=PY[p][:, :], s=GV[:, chunk:chunk + 1]: a.activation(out=o, in_=i, func=AF.Copy, scale=s),
                                 reads=[bPY[p], bGV], writes=[bYT[yb]])
                    P.dma(y[e, chunk * 128:(chunk + 1) * 128, :], YT[yb][:, :], reads=[bYT[yb]], writes=[bout])
        P.flush(final=True)


NEG = -30000.0
GW = 64

def na_tables(rpb):
    nh = rpb.shape[0]
    qc = np.arange(64); kc = np.arange(64)
    cs = np.clip(qc - 8, 0, 48)
    ok = (kc[:, None] >= cs[None, :]) & (kc[:, None] < cs[None, :] + 16)
    idx = np.clip(kc[:, None] - qc[None, :], -15, 15) + 15
    Tb = np.where(ok[None, None], rpb[:, :, idx], np.float32(NEG)).astype(np.float32)
    mask = np.full((nh, 64, 64), NEG, np.float32)
    tte = np.zeros((128, nh, 14, 64), np.float32)
    for d in range(14):
        tte[0:64, :, d, :] = Tb[:, d].transpose(1, 0, 2)
        tte[64:128, :, d, :] = Tb[:, d + 1].transpose(1, 0, 2)
    tto = np.zeros((128, nh, 5, 64), np.float32)
    pairs = [(None, 3), (4, 5), (6, 7), (8, 9), (10, None)]
    for j, (a, b) in enumerate(pairs):
        tto[0:64, :, j, :] = (mask if a is None else Tb[:, a]).transpose(1, 0, 2)
        tto[64:128, :, j, :] = (mask if b is None else Tb[:, b]).transpose(1, 0, 2)
    return tte, tto


def mixer_na(P, K, run_norm, WQKV, TTE, TTO, YATT, bY, last=False):
    with contextlib.ExitStack() as st:
        QT = P.sb(st, "a_QT", [128, 8, T], BF16); bQT = [Buf() for _ in range(8)]
        KT = P.sb(st, "a_KT", [128, 8, T], BF16); bKT = [Buf() for _ in range(8)]
        VA = P.sb(st, "a_VA", [128, NT, 16, 65], BF16); bVA = [Buf() for _ in range(NT)]
        cin = Buf()
        with contextlib.ExitStack() as st2:
            HT = P.sb(st2, "HT", [128, 8, T], BF16); bHT = [Buf() for _ in range(NT)]
            run_norm(HT, bHT)
            WB = P.sb(st2, "a_WB", [128, 8, 1024], BF16); bWB = [Buf() for _ in range(8)]
            STG = [P.sb(st2, f"a_stg{i}", [128, 1024], F32) for i in range(2)]; bSTG = [Buf() for _ in range(2)]
            PP = [P.ps(st2, f"a_PP{i}") for i in range(4)]; bPP = [Buf(excl=True) for _ in range(4)]
            P.op("dve", lambda v: v.memset(VA[:, :, :, 64:65], 1.0), writes=bVA)
            pi = 0
            for which in range(3):
                for k in range(8):
                    s = k % 2
                    P.dma(STG[s][:, :], WQKV[k * 128:(k + 1) * 128, which * 1024:(which + 1) * 1024], reads=[cin], writes=[bSTG[s]])
                    if k % 2 == 0:
                        P.op("dve", lambda g, k=k, s=s: g.tensor_copy(out=WB[:, k, :], in_=STG[s][:, :]), reads=[bSTG[s]], writes=[bWB[k]])
                    else:
                        P.op("act", lambda a, k=k, s=s: a.copy(out=WB[:, k, :], in_=STG[s][:, :]), reads=[bSTG[s]], writes=[bWB[k]])
                if which < 2:
                    DST, bD = (QT, bQT) if which == 0 else (KT, bKT)
                    for c in range(8):
                        for tb in range(6):
                            p = pi % 4; pi += 1
                            for k in range(8):
                                P.op("pe", lambda t, k=k, p=p, c=c, tb=tb: t.matmul(PP[p][:, 0:384], WB[:, k, c * 128:(c + 1) * 128], HT[:, k, tb * 384:(tb + 1) * 384], start=(k == 0), stop=(k == 7)),
                                     reads=[bWB[k]] + bHT[tb * 3:tb * 3 + 3], writes=[bPP[p]])
                            if which == 0:
                                P.op("act", lambda a, p=p, c=c, tb=tb: a.activation(out=QT[:, c, tb * 384:(tb + 1) * 384], in_=PP[p][:, 0:384], func=AF.Copy, scale=0.125), reads=[bPP[p]], writes=[bQT[c]])
                            else:
                                P.op("dve", lambda v, p=p, c=c, tb=tb: v.tensor_copy(out=KT[:, c, tb * 384:(tb + 1) * 384], in_=PP[p][:, 0:384]), reads=[bPP[p]], writes=[bKT[c]])
                else:
                    for tt in range(NT):
                        for dh in range(2):
                            p = pi % 4; pi += 1
                            for k in range(8):
                                P.op("pe", lambda t, k=k, p=p, tt=tt, dh=dh: t.matmul(PP[p][:, :], HT[:, k, tt * 128:(tt + 1) * 128], WB[:, k, dh * 512:(dh + 1) * 512], start=(k == 0), stop=(k == 7)),
                                     reads=[bWB[k], bHT[tt]], writes=[bPP[p]])
                            if dh == 0:
                                P.op("act", lambda a, p=p, tt=tt, dh=dh: a.copy(out=VA[:, tt, dh * 8:(dh + 1) * 8, 0:64], in_=PP[p][:, :].rearrange("p (h d) -> p h d", h=8)), reads=[bPP[p]], writes=[bVA[tt]])
                            else:
                                P.op("dve", lambda v, p=p, tt=tt, dh=dh: v.tensor_copy(out=VA[:, tt, dh * 8:(dh + 1) * 8, 0:64], in_=PP[p][:, :].rearrange("p (h d) -> p h d", h=8)), reads=[bPP[p]], writes=[bVA[tt]])
            P.flush()
        with contextlib.ExitStack() as st2:
            TTEb = P.sb(st2, "a_TTE", [128, 16, 14, 64], BF16); bTTE = Buf()
            TTOb = P.sb(st2, "a_TTO", [128, 16, 5, 64], BF16); bTTO = Buf()
            STG = [P.sb(st2, f"a_tstg{i}", [128, 14 * 64], F32) for i in range(2)]; bSTG = [Buf() for _ in range(2)]
            for h in range(16):
                s = h % 2
                P.dma(STG[s][:, :], TTE[:, h, :, :].rearrange("p a b -> p (a b)"), reads=[cin], writes=[bSTG[s]])
                P.op("dve", lambda g, h=h, s=s: g.tensor_copy(out=TTEb[:, h, :, :].rearrange("p a b -> p (a b)"), in_=STG[s][:, :]), reads=[bSTG[s]], writes=[bTTE])
            for h in range(16):
                s = h % 2
                P.dma(STG[s][:, 0:320], TTO[:, h, :, :].rearrange("p a b -> p (a b)"), reads=[cin], writes=[bSTG[s]])
                P.op("dve", lambda g, h=h, s=s: g.tensor_copy(out=TTOb[:, h, :, :].rearrange("p a b -> p (a b)"), in_=STG[s][:, 0:320]), reads=[bSTG[s]], writes=[bTTO])
            PS = [P.ps(st2, f"a_PS{i}") for i in range(4)]; bPS = [Buf(excl=True) for _ in range(4)]
            PO = [P.ps(st2, f"a_PO{i}") for i in range(4)]; bPO = [Buf(excl=True) for _ in range(4)]
            PT = [P.sb(st2, f"a_PT{i}", [128, 512], BF16) for i in range(4)]; bPT = [Buf() for _ in range(4)]
            REC = [P.sb(st2, f"a_REC{i}", [128, 2], F32) for i in range(4)]; bREC = [Buf() for _ in range(4)]
            YR = [P.sb(st2, f"a_YR{i}", [128, 2, D], BF16) for i in range(2)]; bYR = [Buf() for _ in range(2)]
            si = 0
            if not last:
                yb = 0
                for h in range(16):
                    c = h // 2; hp = (h % 2) * 64
                    s = si % 3; si += 1
                    for kc in range(2):
                        P.op("pe", lambda t, s=s, kc=kc, c=c, hp=hp: t.matmul(PS[s][:, kc * 256:(kc + 1) * 256], KT[hp:hp + 64, c, kc * 128:(kc + 1) * 128], QT[hp:hp + 64, c, 0:256], start=True, stop=True),
                             reads=[bKT[c], bQT[c]], writes=[bPS[s]])
                    P.op("act", lambda a, s=s: a.activation(out=PT[s][:, :], in_=PS[s][:, :], func=AF.Exp), reads=[bPS[s]], writes=[bPT[s]])
                    for qt in range(2):
                        for kc in range(2):
                            P.op("pe", lambda t, s=s, qt=qt, kc=kc, h=h: t.matmul(PO[s][:, qt * 65:(qt + 1) * 65], PT[s][:, kc * 256 + qt * 128:kc * 256 + (qt + 1) * 128], VA[:, kc, h, :], start=(kc == 0), stop=(kc == 1)),
                                 reads=[bPT[s], bVA[kc]], writes=[bPO[s]])
                    for qt in range(2):
                        P.op("dve", lambda v, s=s, qt=qt: v.reciprocal(out=REC[s][:, qt:qt + 1], in_=PO[s][:, qt * 65 + 64:qt * 65 + 65]), reads=[bPO[s]], writes=[bREC[s]])
                        P.op("act", lambda a, s=s, qt=qt, h=h: a.activation(out=YR[yb][:, qt, h * 64:(h + 1) * 64], in_=PO[s][:, qt * 65:qt * 65 + 64], func=AF.Copy, scale=REC[s][:, qt:qt + 1]),
                             reads=[bPO[s], bREC[s]], writes=[bYR[yb]])
                for qt in range(2):
                    P.dma(YATT[qt * 128:(qt + 1) * 128, :], YR[yb][:, qt, :], reads=[bYR[yb]], writes=[bY[qt]])
            LOOK = 3
            steps = []
            for r in range(32):
                rs = min(max(r - 4, 0), 24)
                if rs % 2 == 0:
                    m0 = rs // 2; nloc = 4; dr0 = rs - r + 7
                else:
                    m0 = (rs - 1) // 2; nloc = 5; dr0 = None
                tiles_ = [0, 1] + [2 + m0 + c for c in range(nloc)]
                for h in range(16):
                    steps.append((r, h, tiles_, nloc, dr0))
            def emit_S(idx):
                r, h, tiles_, nloc, dr0 = steps[idx]
                s = idx % 4; c = h // 2; hp = (h % 2) * 64; q0 = 256 + r * 64
                for ci, tile in enumerate(tiles_):
                    P.op("pe", lambda t, s=s, ci=ci, tile=tile, c=c, hp=hp, q0=q0: t.matmul(PS[s][:, ci * 64:(ci + 1) * 64], KT[hp:hp + 64, c, tile * 128:(tile + 1) * 128], QT[hp:hp + 64, c, q0:q0 + 64], start=True, stop=True),
                         reads=[bKT[c], bQT[c]], writes=[bPS[s]])
            for i0 in range(min(LOOK, len(steps))):
                emit_S(i0)
            for idx, (r, h, tiles_, nloc, dr0) in enumerate(steps):
                s = idx % 4
                yb = (r + 1) % 2
                nch = len(tiles_)
                q0 = 256 + r * 64
                if dr0 is not None:
                    tab = TTEb[:, h, dr0:dr0 + 7:2, :]; btab = bTTE
                else:
                    tab = TTOb[:, h, 0:5, :]; btab = bTTO
                P.op("dve", lambda v, s=s, nloc=nloc, tab=tab: v.tensor_tensor(out=PS[s][:, 128:128 + nloc * 64].rearrange("p (a b) -> p a b", b=64), in0=PS[s][:, 128:128 + nloc * 64].rearrange("p (a b) -> p a b", b=64), in1=tab, op=ALU.add),
                     reads=[bPS[s], btab], writes=[bPS[s]])
                P.op("act", lambda a, s=s, nch=nch: a.activation(out=PT[s][:, 0:nch * 64], in_=PS[s][:, 0:nch * 64], func=AF.Exp), reads=[bPS[s]], writes=[bPT[s]])
                if idx + LOOK < len(steps):
                    emit_S(idx + LOOK)
                for ci, tile in enumerate(tiles_):
                    P.op("pe", lambda t, s=s, ci=ci, tile=tile, h=h, nch=nch: t.matmul(PO[s][0:64, 0:65], PT[s][:, ci * 64:(ci + 1) * 64], VA[:, tile, h, :], start=(ci == 0), stop=(ci == nch - 1)),
                         reads=[bPT[s], bVA[tile]], writes=[bPO[s]])
                P.op("dve", lambda v, s=s: v.reciprocal(out=REC[s][0:64, 0:1], in_=PO[s][0:64, 64:65]), reads=[bPO[s]], writes=[bREC[s]])
                P.op("act", lambda a, s=s, h=h, yb=yb: a.activation(out=YR[yb][0:64, 0, h * 64:(h + 1) * 64], in_=PO[s][0:64, 0:64], func=AF.Copy, scale=REC[s][0:64, 0:1]),
                     reads=[bPO[s], bREC[s]], writes=[bYR[yb]])
                if h == 15:
                    P.dma(YATT[q0:q0 + 64, :], YR[yb][0:64, 0, :], reads=[bYR[yb]], writes=[bY[q0 // 128]])
            P.flush()


QR, KVR, RD = 384, 256, 32
SCALE = 96 ** -0.5

def mla_host(w_in, w_q_b, qg, kvg):
    wks = np.concatenate([w_in[:, 576:640], w_in[:, 656:672], w_in[:, 640:656]], 1).copy()
    wq = w_q_b.reshape(QR, 16, 96)
    wqs = np.concatenate([wq[:, :, 0:64], wq[:, :, 80:96], wq[:, :, 64:80]], 2).reshape(QR, 16 * 96).copy()
    gq = qg.reshape(3, 128).T.copy(); gkv = kvg.reshape(2, 128).T.copy()
    t = np.arange(2048)
    row = (t // 64).astype(np.float32); col = (t % 64).astype(np.float32)
    inv = (10000.0 ** (-np.arange(8, dtype=np.float32) / 8)).astype(np.float32)
    ang = np.concatenate([row[:, None] * inv, col[:, None] * inv], -1)
    cos = np.cos(ang).astype(np.float32); sin = np.sin(ang).astype(np.float32)
    cs = np.zeros((32, 2, T), np.float32)
    cs[:, 0, :256] = 1.0
    cs[0:16, 0, 256:] = cos.T; cs[16:32, 0, 256:] = cos.T
    cs[0:16, 1, 256:] = -sin.T; cs[16:32, 1, 256:] = sin.T
    return wks, wqs, gq, gkv, cs


STOP = 99
def mixer_mla(P, K, run_norm, WIN, WKS, GQ, GKV, WQB, WQS, WKVB, CS, YTM, bYTM):
    with contextlib.ExitStack() as st:
        CQN = P.sb(st, "m_CQN", [128, 3, T], BF16); bCQN = [Buf() for _ in range(6)]
        CKVN = P.sb(st, "m_CKVN", [128, 2, T], BF16); bCKVN = [Buf() for _ in range(6)]
        KRT = P.sb(st, "m_KRT", [128, T], BF16); bKRT = [Buf() for _ in range(6)]
        CSs = P.sb(st, "m_CS", [128, 2, T], F32); bCS = Buf()
        cin = Buf()
        P.dma(CSs[64:96, :, :], CS[:, :, :], reads=[cin], writes=[bCS])
        with contextlib.ExitStack() as st2:
            HT = P.sb(st2, "HT", [128, 8, T], BF16); bHT = [Buf() for _ in range(NT)]
            run_norm(HT, bHT)
            WINb = P.sb(st2, "m_WIN", [128, 8, 672], BF16); bWIN = [Buf() for _ in range(8)]
            WKSb = P.sb(st2, "m_WKS", [128, 8, 96], BF16); bWKS = Buf()
            STG = [P.sb(st2, f"m_stg{i}", [128, 672], F32) for i in range(2)]; bSTG = [Buf() for _ in range(2)]
            STK = P.sb(st2, "m_stk", [128, 8, 96], F32); bSTK = Buf()
            GQs = P.sb(st2, "m_GQ", [128, 3], F32); GKVs = P.sb(st2, "m_GKV", [128, 2], F32); bG = Buf()
            ONES = P.sb(st2, "m_ONES", [128, 128], BF16); bONES = Buf()
            ZF = [P.sb(st2, f"m_ZF{i}", [128, 5, 384], F32) for i in range(2)]; bZF = [Buf() for _ in range(2)]
            SQ = [P.sb(st2, f"m_SQ{i}", [128, 5, 384], BF16) for i in range(2)]; bSQ = [Buf() for _ in range(2)]
            RQ = [P.sb(st2, f"m_RQ{i}", [128, 2, 384], F32) for i in range(2)]; bRQ = [Buf() for _ in range(2)]
            T1 = [P.sb(st2, f"m_T1{i}", [128, 384], F32) for i in range(2)]; bT1 = [Buf() for _ in range(2)]
            T2 = [P.sb(st2, f"m_T2{i}", [128, 384], F32) for i in range(2)]; bT2 = [Buf() for _ in range(2)]
            PZ = [P.ps(st2, f"m_PZ{i}") for i in range(4)]; bPZ = [Buf(excl=True) for _ in range(4)]
            PSM = [P.ps(st2, f"m_PSM{i}") for i in range(2)]; bPSM = [Buf(excl=True) for _ in range(2)]
            PK = [P.ps(st2, f"m_PK{i}") for i in range(2)]; bPK = [Buf(excl=True) for _ in range(2)]
            for k in range(8):
                s = k % 2
                P.dma(STG[s][:, :], WIN[k * 128:(k + 1) * 128, :], reads=[cin], writes=[bSTG[s]])
                P.op("dve", lambda g, k=k, s=s: g.tensor_copy(out=WINb[:, k, :], in_=STG[s][:, :]), reads=[bSTG[s]], writes=[bWIN[k]])
            P.dma(STK[:, :, :], WKS.rearrange("(c p) f -> p c f", p=128), reads=[cin], writes=[bSTK])
            P.op("dve", lambda g: g.tensor_copy(out=WKSb[:, :, :], in_=STK[:, :, :]), reads=[bSTK], writes=[bWKS])
            P.dma(GQs[:, :], GQ[:, :], reads=[cin], writes=[bG])
            P.dma(GKVs[:, :], GKV[:, :], reads=[cin], writes=[bG])
            P.op("dve", lambda v: v.memset(ONES[:, :], 1.0), writes=[bONES])
            pz = 0
            for tb in range(6):
                b = tb % 2
                tsl = slice(tb * 384, (tb + 1) * 384)
                hts = bHT[tb * 3:tb * 3 + 3]
                for c in range(5):
                    p = pz % 4; pz += 1
                    for k in range(8):
                        P.op("pe", lambda t, k=k, p=p, c=c, tsl=tsl: t.matmul(PZ[p][:, 0:384], WINb[:, k, c * 128:(c + 1) * 128], HT[:, k, tsl], start=(k == 0), stop=(k == 7)),
                             reads=[bWIN[k]] + hts, writes=[bPZ[p]])
                    P.op("dve", lambda v, p=p, c=c, b=b: v.tensor_copy(out=ZF[b][:, c, :], in_=PZ[p][:, 0:384]), reads=[bPZ[p]], writes=[bZF[b]])
                    P.op("act", lambda a, p=p, c=c, b=b: a.activation(out=SQ[b][:, c, :], in_=PZ[p][:, 0:384], func=AF.Square), reads=[bPZ[p]], writes=[bSQ[b]])
                for which, cs_, n_ in ((0, (0, 1, 2), QR), (1, (3, 4), KVR)):
                    for i, c in enumerate(cs_):
                        P.op("pe", lambda t, which=which, c=c, b=b, i=i, cs_=cs_: t.matmul(PSM[which][:, 0:384], ONES[:, :], SQ[b][:, c, :], start=(i == 0), stop=(i == len(cs_) - 1)),
                             reads=[bONES, bSQ[b]], writes=[bPSM[which]])
                    P.op("act", lambda a, which=which, b=b, n_=n_: a.activation(out=RQ[b][:, which, :], in_=PSM[which][:, 0:384], func=AF.Sqrt, scale=1.0 / n_, bias=K["eps"][:, :]),
                         reads=[bPSM[which], K["b_eps"]], writes=[bRQ[b]])
                    P.op("dve", lambda v, which=which, b=b: v.reciprocal(out=RQ[b][:, which, :], in_=RQ[b][:, which, :]), reads=[bRQ[b]], writes=[bRQ[b]])
                for c in range(3):
                    P.op("dve", lambda v, c=c, b=b, tsl=tsl: v.scalar_tensor_tensor(out=CQN[:, c, tsl], in0=ZF[b][:, c, :], scalar=GQs[:, c:c + 1], in1=RQ[b][:, 0, :], op0=ALU.mult, op1=ALU.mult),
                         reads=[bZF[b], bG, bRQ[b]], writes=[bCQN[tb]])
                for c in range(2):
                    P.op("dve", lambda v, c=c, b=b, tsl=tsl: v.scalar_tensor_tensor(out=CKVN[:, c, tsl], in0=ZF[b][:, 3 + c, :], scalar=GKVs[:, c:c + 1], in1=RQ[b][:, 1, :], op0=ALU.mult, op1=ALU.mult),
                         reads=[bZF[b], bG, bRQ[b]], writes=[bCKVN[tb]])
                for k in range(8):
                    P.op("pe", lambda t, k=k, tsl=tsl: t.matmul(PK[0][0:96, 0:384], WINb[:, k, 576:672], HT[:, k, tsl], start=(k == 0), stop=(k == 7)), reads=[bWIN[k]] + hts, writes=[bPK[0]])
                for k in range(8):
                    P.op("pe", lambda t, k=k, tsl=tsl: t.matmul(PK[1][0:96, 0:384], WKSb[:, k, :], HT[:, k, tsl], start=(k == 0), stop=(k == 7)), reads=[bWKS] + hts, writes=[bPK[1]])
                P.op("dve", lambda v, b=b, tsl=tsl: v.tensor_tensor(out=T1[b][64:96, :], in0=PK[0][64:96, 0:384], in1=CSs[64:96, 0, tsl], op=ALU.mult), reads=[bPK[0], bCS], writes=[bT1[b]])
                P.op("dve", lambda v, b=b, tsl=tsl: v.tensor_tensor(out=T2[b][64:96, :], in0=PK[1][64:96, 0:384], in1=CSs[64:96, 1, tsl], op=ALU.mult), reads=[bPK[1], bCS], writes=[bT2[b]])
                P.op("dve", lambda g, b=b, tsl=tsl: g.tensor_tensor(out=KRT[64:96, tsl], in0=T1[b][64:96, :], in1=T2[b][64:96, :], op=ALU.add), reads=[bT1[b], bT2[b]], writes=[bKRT[tb]])
            P.flush()
        if STOP <= 1:
            return
        for hg in range(2):
            with contextlib.ExitStack() as st2:
                QT = P.sb(st2, "m_QT", [128, 8, T], BF16); bQT = [Buf() for _ in range(8)]
                KT = P.sb(st2, "m_KT", [128, 8, T], BF16); bKT = [Buf() for _ in range(8)]
                VA = P.sb(st2, "m_VA", [128, NT, 8, 65], BF16); bVA = [Buf() for _ in range(NT)]
                WQBb = P.sb(st2, "m_WQB", [128, 3, 768], BF16); WQSb = P.sb(st2, "m_WQS", [128, 3, 768], BF16); bWQ = Buf()
                WKVb = P.sb(st2, "m_WKV", [128, 2, 1024], BF16); bWKV = Buf()
                STG = [P.sb(st2, f"m_stg2{i}", [128, 1024], F32) for i in range(2)]; bSTG = [Buf() for _ in range(2)]
                T1 = [P.sb(st2, f"m_T1b{i}", [128, 384], F32) for i in range(2)]; bT1 = [Buf() for _ in range(2)]
                T2 = [P.sb(st2, f"m_T2b{i}", [128, 384], F32) for i in range(2)]; bT2 = [Buf() for _ in range(2)]
                PTs = [P.sb(st2, f"m_PT{i}", [128, 512], BF16) for i in range(4)]; bPT = [Buf() for _ in range(4)]
                REC = [P.sb(st2, f"m_REC{i}", [128, 4], F32) for i in range(2)]; bREC = [Buf() for _ in range(2)]
                PQ = [P.ps(st2, f"m_PQ{i}") for i in range(2)]; bPQ = [Buf(excl=True) for _ in range(2)]
                PQS = [P.ps(st2, f"m_PQS{i}") for i in range(2)]; bPQS = [Buf(excl=True) for _ in range(2)]
                PS = [P.ps(st2, f"m_PS{i}") for i in range(2)]; bPS = [Buf(excl=True) for _ in range(2)]
                PO = [P.ps(st2, f"m_PO{i}") for i in range(2)]; bPO = [Buf(excl=True) for _ in range(2)]
                si = 0
                for (SRC, DST) in ((WQB, WQBb), (WQS, WQSb)):
                    for k in range(3):
                        s = si % 2; si += 1
                        P.dma(STG[s][:, 0:768], SRC[k * 128:(k + 1) * 128, hg * 768:(hg + 1) * 768], reads=[cin], writes=[bSTG[s]])
                        P.op("dve", lambda g, k=k, s=s, DST=DST: g.tensor_copy(out=DST[:, k, :], in_=STG[s][:, 0:768]), reads=[bSTG[s]], writes=[bWQ])
                for k in range(2):
                    s = si % 2; si += 1
                    P.dma(STG[s][:, :], WKVB[k * 128:(k + 1) * 128, hg * 1024:(hg + 1) * 1024], reads=[cin], writes=[bSTG[s]])
                    P.op("dve", lambda g, k=k, s=s: g.tensor_copy(out=WKVb[:, k, :], in_=STG[s][:, :]), reads=[bSTG[s]], writes=[bWKV])
                P.op("dve", lambda v: v.memset(VA[:, :, :, 64:65], 1.0), writes=bVA)
                pq = 0
                for hl in range(8):
                    for tb in range(6):
                        tsl = slice(tb * 384, (tb + 1) * 384)
                        p = pq % 2; pq += 1
                        for k in range(3):
                            P.op("pe", lambda t, k=k, p=p, hl=hl, tsl=tsl: t.matmul(PQ[p][0:96, 0:384], WQBb[:, k, hl * 96:(hl + 1) * 96], CQN[:, k, tsl], start=(k == 0), stop=(k == 2)),
                                 reads=[bWQ, bCQN[tb]], writes=[bPQ[p]])
                        for k in range(3):
                            P.op("pe", lambda t, k=k, p=p, hl=hl, tsl=tsl: t.matmul(PQS[p][0:96, 0:384], WQSb[:, k, hl * 96:(hl + 1) * 96], CQN[:, k, tsl], start=(k == 0), stop=(k == 2)),
                                 reads=[bWQ, bCQN[tb]], writes=[bPQS[p]])
                        P.op("act", lambda a, p=p, hl=hl, tsl=tsl: a.copy(out=QT[0:64, hl, tsl], in_=PQ[p][0:64, 0:384]), reads=[bPQ[p]], writes=[bQT[hl]])
                        P.op("dve", lambda v, p=p, tsl=tsl: v.tensor_tensor(out=T1[p][64:96, :], in0=PQ[p][64:96, 0:384], in1=CSs[64:96, 0, tsl], op=ALU.mult), reads=[bPQ[p], bCS], writes=[bT1[p]])
                        P.op("dve", lambda v, p=p, tsl=tsl: v.tensor_tensor(out=T2[p][64:96, :], in0=PQS[p][64:96, 0:384], in1=CSs[64:96, 1, tsl], op=ALU.mult), reads=[bPQS[p], bCS], writes=[bT2[p]])
                        P.op("dve", lambda g, p=p, hl=hl, tsl=tsl: g.tensor_tensor(out=QT[64:96, hl, tsl], in0=T1[p][64:96, :], in1=T2[p][64:96, :], op=ALU.add), reads=[bT1[p], bT2[p]], writes=[bQT[hl]])
                        for k in range(2):
                            P.op("pe", lambda t, k=k, p=p, hl=hl, tsl=tsl: t.matmul(PS[p][0:64, 0:384], WKVb[:, k, hl * 128:hl * 128 + 64], CKVN[:, k, tsl], start=(k == 0), stop=(k == 1)),
                                 reads=[bWKV, bCKVN[tb]], writes=[bPS[p]])
                        P.op("act", lambda a, p=p, hl=hl, tsl=tsl: a.copy(out=KT[0:64, hl, tsl], in_=PS[p][0:64, 0:384]), reads=[bPS[p]], writes=[bKT[hl]])
                    P.op("dve", lambda g, hl=hl: g.tensor_copy(out=KT[64:96, hl, :], in_=KRT[64:96, :]), reads=bKRT, writes=[bKT[hl]])
                if STOP <= 2:
                    P.flush(); return
                for tt in range(NT):
                    p = tt % 2
                    for k in range(2):
                        P.op("pe", lambda t, k=k, p=p, tt=tt: t.matmul(PO[p][:, :], CKVN[:, k, tt * 128:(tt + 1) * 128], WKVb[:, k, :].rearrange("p (h d) -> p h d", d=128)[:, :, 64:128], start=(k == 0), stop=(k == 1)),
                             reads=[bWKV, bCKVN[tt // 3]], writes=[bPO[p]])
                    P.op("dve", lambda v, p=p, tt=tt: v.tensor_copy(out=VA[:, tt, :, 0:64], in_=PO[p][:, :].rearrange("p (h d) -> p h d", h=8)), reads=[bPO[p]], writes=[bVA[tt]])
                if STOP <= 3:
                    P.flush(); return
                POL = [PQ[0], PQ[1], PQS[0], PQS[1]]; bPOL = [bPQ[0], bPQ[1], bPQS[0], bPQS[1]]
                s3 = 0
                for hl in range(8):
                    h = hg * 8 + hl
                    s = s3 % 3; s3 += 1; ps = s % 2
                    for kc in range(2):
                        P.op("pe", lambda t, ps=ps, kc=kc, hl=hl: t.matmul(PS[ps][:, kc * 256:(kc + 1) * 256], KT[0:96, hl, kc * 128:(kc + 1) * 128], QT[0:96, hl, 0:256], start=True, stop=True),
                             reads=[bKT[hl], bQT[hl]], writes=[bPS[ps]])
                    P.op("act", lambda a, s=s, ps=ps: a.activation(out=PTs[s][:, :], in_=PS[ps][:, :], func=AF.Exp, scale=SCALE), reads=[bPS[ps]], writes=[bPT[s]])
                    for qt in range(2):
                        for kc in range(2):
                            P.op("pe", lambda t, s=s, qt=qt, kc=kc, hl=hl: t.matmul(POL[qt][:, 0:65], PTs[s][:, kc * 256 + qt * 128:kc * 256 + (qt + 1) * 128], VA[:, kc, hl, :], start=(kc == 0), stop=(kc == 1)),
                                 reads=[bPT[s], bVA[kc]], writes=[bPOL[qt]])
                    for qt in range(2):
                        P.op("dve", lambda v, qt=qt: v.reciprocal(out=REC[0][:, qt:qt + 1], in_=POL[qt][:, 64:65]), reads=[bPOL[qt]], writes=[bREC[0]])
                        P.op("act", lambda a, qt=qt, h=h: a.activation(out=YTM[:, qt, h * 64:(h + 1) * 64], in_=POL[qt][:, 0:64], func=AF.Copy, scale=REC[0][:, qt:qt + 1]),
                             reads=[bPOL[qt], bREC[0]], writes=[bYTM[qt]])
                steps = [(hl, qb, kc) for hl in range(8) for qb in range(4) for kc in range(NT)]
                PSA = [PS[0], PS[1], PO[0], PO[1]]; bPSA = [bPS[0], bPS[1], bPO[0], bPO[1]]
                LOOK = 2
                def emit_S(idx):
                    hl, qb, kc = steps[idx]; ps = idx % 4; q0 = 256 + qb * 512
                    P.op("pe", lambda t, ps=ps, kc=kc, hl=hl, q0=q0: t.matmul(PSA[ps][:, :], KT[0:96, hl, kc * 128:(kc + 1) * 128], QT[0:96, hl, q0:q0 + 512], start=True, stop=True),
                         reads=[bKT[hl], bQT[hl]], writes=[bPSA[ps]])
                for i0 in range(LOOK):
                    emit_S(i0)
                for idx, (hl, qb, kc) in enumerate(steps):
                    h = hg * 8 + hl
                    s = idx % 4; ps = idx % 4
                    if idx + LOOK < len(steps):
                        emit_S(idx + LOOK)
                    P.op("act", lambda a, s=s, ps=ps: a.activation(out=PTs[s][:, :], in_=PSA[ps][:, :], func=AF.Exp, scale=SCALE), reads=[bPSA[ps]], writes=[bPT[s]])
                    for j in range(4):
                        P.op("pe", lambda t, s=s, j=j, kc=kc, hl=hl: t.matmul(POL[j][:, 0:65], PTs[s][:, j * 128:(j + 1) * 128], VA[:, kc, hl, :], start=(kc == 0), stop=(kc == NT - 1)),
                             reads=[bPT[s], bVA[kc]], writes=[bPOL[j]])
                    if kc == NT - 1:
                        for j in range(4):
                            tile = 2 + qb * 4 + j
                            P.op("dve", lambda v, j=j: v.reciprocal(out=REC[1][:, j:j + 1], in_=POL[j][:, 64:65]), reads=[bPOL[j]], writes=[bREC[1]])
                            P.op("act", lambda a, j=j, h=h, tile=tile: a.activation(out=YTM[:, tile, h * 64:(h + 1) * 64], in_=POL[j][:, 0:64], func=AF.Copy, scale=REC[1][:, j:j + 1]),
                                 reads=[bPOL[j], bREC[1]], writes=[bYTM[tile]])
                P.flush()


PI = math.pi

def hy_host(conv_w, conv_b, b1, b2, b3, sin_freq, skip):
    H = {}
    H["cw"] = conv_w.reshape(3, 24, 128).transpose(2, 1, 0).copy()
    H["cb"] = conv_b.reshape(24, 128).T.copy()
    H["fb"] = np.stack([b1, b2, sin_freq[0], sin_freq[1]], 1).astype(np.float32).copy()
    max_decay = math.log(1e-2) / 0.3; min_decay = math.log(1e-2) / 1.5
    H["delta"] = np.abs(np.linspace(min_decay, max_decay, 1024, dtype=np.float32)).astype(np.float32)
    for L, tag in ((256, "c"), (2048, "l")):
        N = 2 * L
        t = np.linspace(0.0, 1.0, L, dtype=np.float32)
        w = (2.0 * np.float32(math.pi) / L) * np.arange(L, dtype=np.float32)
        f = np.linspace(1e-4, 15, 16, dtype=np.float32)
        ze = np.concatenate([t[:, None], np.cos(f[None, :] * w[:, None]), -np.sin(f[None, :] * w[:, None])], -1).astype(np.float32)
        H["ze" + tag] = ze.T.copy()
        nt = L // 128
        H["negt" + tag] = (-t).reshape(nt, 128).T.copy()
        idx = (np.arange(L, dtype=np.int64)[:, None] * np.arange(L, dtype=np.int64)[None, :]) % N
        ang = idx.astype(np.float64) * (2 * math.pi / N)
        Cm = np.cos(ang); Sm = -np.sin(ang)
        Sm[:, 0] = (-1.0) ** np.arange(L)
        H["C" + tag] = Cm.astype(NPBF); H["S" + tag] = Sm.astype(NPBF); H["ST" + tag] = Sm.T.copy().astype(NPBF)
        wsc = np.full((128, nt), 2.0 / N, np.float32); wsc[0, 0] = 1.0 / N
        H["wsc" + tag] = wsc
    return H


def mixer_hyena(P, K, run_norm, WIN, CW, CB, FW1, FW2, FW3, FB, FB3, SKIP, DELTA, TB, X0D, YTD, bYTD):
    cin = Buf(); bX0D = [Buf() for _ in range(8)]
    with contextlib.ExitStack() as st:
        GTM = P.sb(st, "h_GTM", [128, NT, D], BF16); bGTM = [Buf() for _ in range(NT)]
        with contextlib.ExitStack() as st2:
            HT = P.sb(st2, "HT", [128, 8, T], BF16); bHT = [Buf() for _ in range(NT)]
            run_norm(HT, bHT)
            X1T = P.sb(st2, "h_X1T", [128, 8, T], BF16); bX1T = [Buf() for _ in range(8)]
            WB = P.sb(st2, "h_WB", [128, 8, 1024], BF16); bWB = [Buf() for _ in range(8)]
            STG = [P.sb(st2, f"h_stg{i}", [128, 1024], F32) for i in range(2)]; bSTG = [Buf() for _ in range(2)]
            CWs = P.sb(st2, "h_CW", [128, 24, 3], F32); CBs = P.sb(st2, "h_CB", [128, 24], F32); bCW = Buf()
            ZCs = [P.sb(st2, f"h_ZC{i}", [128, T], F32) for i in range(2)]; bZCs = [Buf() for _ in range(2)]
            ACs = [P.sb(st2, f"h_AC{i}", [128, T], F32) for i in range(2)]; bACs = [Buf() for _ in range(2)]
            OB = [P.sb(st2, f"h_OB{i}", [128, T], BF16) for i in range(2)]; bOB = [Buf() for _ in range(2)]
            PP = [P.ps(st2, f"h_PP{i}") for i in range(4)]; bPP = [Buf(excl=True) for _ in range(4)]
            PT = [P.ps(st2, f"h_PT{i}", [128, 8, 128], BF16) for i in range(2)]; bPT = [Buf(excl=True) for _ in range(2)]
            P.dma(CWs[:, :, :], CW[:, :, :], reads=[cin], writes=[bCW])
            P.dma(CBs[:, :], CB[:, :], reads=[cin], writes=[bCW])
            pi = 0
            for which in range(3):
                for k in range(8):
                    s = k % 2
                    P.dma(STG[s][:, :], WIN[k * 128:(k + 1) * 128, which * 1024:(which + 1) * 1024], reads=[cin], writes=[bSTG[s]])
                    if k % 2 == 0:
                        P.op("dve", lambda g, k=k, s=s: g.tensor_copy(out=WB[:, k, :], in_=STG[s][:, :]), reads=[bSTG[s]], writes=[bWB[k]])
                    else:
                        P.op("act", lambda a, k=k, s=s: a.copy(out=WB[:, k, :], in_=STG[s][:, :]), reads=[bSTG[s]], writes=[bWB[k]])
                for c in range(8):
                    cc = which * 8 + c
                    ZC = ZCs[c % 2]; bZC = bZCs[c % 2]; AC = ACs[c % 2]; bAC = bACs[c % 2]
                    for tb in range(6):
                        p = pi % 4; pi += 1
                        for k in range(8):
                            P.op("pe", lambda t, k=k, p=p, c=c, tb=tb: t.matmul(PP[p][:, 0:384], WB[:, k, c * 128:(c + 1) * 128], HT[:, k, tb * 384:(tb + 1) * 384], start=(k == 0), stop=(k == 7)),
                                 reads=[bWB[k]] + bHT[tb * 3:tb * 3 + 3], writes=[bPP[p]])
                        P.op("act", lambda a, p=p, tb=tb, ZC=ZC: a.copy(out=ZC[:, tb * 384:(tb + 1) * 384], in_=PP[p][:, 0:384]), reads=[bPP[p]], writes=[bZC])
                        P.op("act", lambda a, p=p, tb=tb, AC=AC, cc=cc: a.activation(out=AC[:, tb * 384:(tb + 1) * 384], in_=PP[p][:, 0:384], func=AF.Identity, scale=CWs[:, cc, 1:2], bias=CBs[:, cc:cc + 1]), reads=[bPP[p], bCW], writes=[bAC])
                    for (o0, o1, i0, i1, tap) in ((1, 256, 0, 255, 0), (257, T, 256, T - 1, 0), (0, 255, 1, 256, 2), (256, T - 1, 257, T, 2)):
                        P.op("dve", lambda v, cc=cc, o0=o0, o1=o1, i0=i0, i1=i1, tap=tap, ZC=ZC, AC=AC: v.scalar_tensor_tensor(out=AC[:, o0:o1], in0=ZC[:, i0:i1], scalar=CWs[:, cc, tap:tap + 1], in1=AC[:, o0:o1], op0=ALU.mult, op1=ALU.add),
                             reads=[bZC, bAC, bCW], writes=[bAC])
                    if which == 0:
                        b = c % 2
                        P.op("act", lambda a, b=b, AC=AC: a.copy(out=OB[b][:, :], in_=AC[:, :]), reads=[bAC], writes=[bOB[b]])
                        P.dma(X0D[c * 128:(c + 1) * 128, :], OB[b][:, :], reads=[bOB[b]], writes=[bX0D[c]])
                    elif which == 1:
                        P.op("act", lambda a, c=c, AC=AC: a.copy(out=X1T[:, c, :], in_=AC[:, :]), reads=[bAC], writes=[bX1T[c]])
                    else:
                        b = c % 2
                        P.op("dve", lambda g, b=b, c=c, AC=AC: g.tensor_tensor(out=OB[b][:, :], in0=AC[:, :], in1=X1T[:, c, :], op=ALU.mult), reads=[bAC, bX1T[c]], writes=[bOB[b]])
                        for tt in range(NT):
                            q = tt // 8
                            P.op("pe", lambda t, b=b, tt=tt: t.transpose(PT[(tt // 8) % 2][:, tt % 8, :], OB[b][:, tt * 128:(tt + 1) * 128], K["idb"][:, :]), reads=[bOB[b], K["b_idb"]], writes=[bPT[q % 2]])
                            if tt % 8 == 7 or tt == NT - 1:
                                t0 = (tt // 8) * 8; n_ = tt - t0 + 1
                                P.op("act", lambda a, q=q, t0=t0, n_=n_, c=c: a.copy(out=GTM[:, t0:t0 + n_, c * 128:(c + 1) * 128], in_=PT[q % 2][:, 0:n_, :]), reads=[bPT[q % 2]], writes=bGTM[t0:t0 + n_])
            P.flush()
        with contextlib.ExitStack() as st2:
            W1 = P.sb(st2, "h_W1", [33, 64], F32); W2 = P.sb(st2, "h_W2", [64, 64], F32); W3 = P.sb(st2, "h_W3", [64, 2048], F32); bW = Buf()
            FBs = P.sb(st2, "h_FB", [64, 4], F32); B3 = P.sb(st2, "h_B3", [1, 2048], F32); ONES1 = P.sb(st2, "h_ON1", [1, 128], F32)
            SKs = P.sb(st2, "h_SK", [1, 1024], F32); DEL = P.sb(st2, "h_DEL", [128, 1024], F32)
            ARG = P.sb(st2, "h_ARG", [64, 512], F32); bARG = Buf(); MM = P.sb(st2, "h_MM", [64, 512], F32); bMM = Buf()
            H1 = P.sb(st2, "h_H1", [64, 2048], F32); bH1 = Buf(); H2 = P.sb(st2, "h_H2", [64, 2048], F32); bH2 = Buf()
            ZE = P.sb(st2, "h_ZE", [33, 2048], F32); bZE = Buf()
            NEGT = P.sb(st2, "h_NEGT", [128, 16], F32); WSC = P.sb(st2, "h_WSC", [128, 16], F32); bTBL = Buf()
            EE = P.sb(st2, "h_EE", [128, 512], F32); bEE = Buf()
            HF = P.sb(st2, "h_HF", [128, 16, 512], BF16); bHF = Buf(); HB = P.sb(st2, "h_HB", [128, 16, 512], BF16); bHB = Buf()
            YRE = P.sb(st2, "h_YRE", [128, 16, 512], BF16); bYRE = [Buf() for _ in range(16)]
            YIM = P.sb(st2, "h_YIM", [128, 16, 512], BF16); bYIM = [Buf() for _ in range(16)]
            CT = [P.sb(st2, f"h_CT{i}", [128, 16, 128], BF16) for i in range(1)] * 2; bCT = [Buf()] * 2
            STt = [P.sb(st2, f"h_ST{i}", [128, 16, 128], BF16) for i in range(1)] * 2; bST = [Buf()] * 2
            CI = P.sb(st2, "h_CI", [128, 16, 256], BF16); bCI = Buf(); SI = P.sb(st2, "h_SI", [128, 16, 256], BF16); bSI = Buf()
            TM = [P.sb(st2, f"h_TM{i}", [128, 512], F32) for i in range(9)]; bTM = [Buf() for _ in range(9)]
            F0 = TM[8]; bF0 = bTM[8]
            GcS = P.sb(st2, "h_GcS", [128, 512], F32); bGcS = Buf()
            X0L = [P.sb(st2, f"h_X0L{i}", [128, 512], BF16) for i in range(2)]; bX0L = [Buf() for _ in range(2)]
            YO = [P.sb(st2, f"h_YO{i}", [128, 512], BF16) for i in range(2)]; bYO = [Buf() for _ in range(2)]
            PF = [P.ps(st2, f"h_PF{i}") for i in range(6)]; bPF = [Buf(excl=True) for _ in range(6)]
            PI_ = [P.ps(st2, f"h_PI{i}") for i in range(2)]; bPI = [Buf(excl=True) for _ in range(2)]
            P.dma(W1[:, :], FW1[:, :], reads=[cin], writes=[bW]); P.dma(W2[:, :], FW2[:, :], reads=[cin], writes=[bW]); P.dma(W3[:, :], FW3[:, :], reads=[cin], writes=[bW])
            P.dma(FBs[:, :], FB[:, :], reads=[cin], writes=[bW]); P.dma(B3[:, :], FB3[:, :], reads=[cin], writes=[bW])
            P.dma(SKs[:, :], SKIP[:, :], reads=[cin], writes=[bW]); P.dma(DEL[:, :], DELTA.partition_broadcast(128), reads=[cin], writes=[bW])
            P.op("dve", lambda v: v.memset(ONES1[:, :], 1.0), writes=[bW])
            ci = 0; xi = 0
            for tag, L, tk0 in (("c", 256, 0), ("l", 2048, 256)):
                tb_ = TB[tag]; nt = L // 128; tile0 = tk0 // 128
                P.dma(ZE[:, 0:L], tb_["ze"][:, :], reads=[cin], writes=[bZE])
                P.dma(NEGT[:, 0:nt], tb_["negt"][:, :], reads=[cin], writes=[bTBL]); P.dma(WSC[:, 0:nt], tb_["wsc"][:, :], reads=[cin], writes=[bTBL])
                for (Wl, kin, SRC, bSRC, DST, bDST, bcol, scol) in ((W1, 33, ZE, bZE, H1, bH1, 0, 2), (W2, 64, H1, bH1, H2, bH2, 1, 3)):
                    for blk in range(0, L, 512):
                        n_ = min(512, L - blk)
                        P.op("pe", lambda t, Wl=Wl, kin=kin, SRC=SRC, blk=blk, n_=n_: t.matmul(PI_[0][0:64, 0:n_], Wl[0:kin, :], SRC[0:kin, blk:blk + n_], start=True, stop=True), reads=[bW, bSRC], writes=[bPI[0]])
                        P.op("dve", lambda v, n_=n_, bcol=bcol, scol=scol: v.tensor_scalar(out=ARG[:, 0:n_], in0=PI_[0][0:64, 0:n_], scalar1=FBs[:, bcol:bcol + 1], scalar2=FBs[:, scol:scol + 1], op0=ALU.add, op1=ALU.mult), reads=[bPI[0], bW], writes=[bARG])
                        P.op("dve", lambda v, n_=n_: v.tensor_scalar(out=MM[:, 0:n_], in0=ARG[:, 0:n_], scalar1=PI, scalar2=None, op0=ALU.is_gt), reads=[bARG], writes=[bMM])
                        P.op("dve", lambda v, n_=n_: v.scalar_tensor_tensor(out=ARG[:, 0:n_], in0=MM[:, 0:n_], scalar=-2 * PI, in1=ARG[:, 0:n_], op0=ALU.mult, op1=ALU.add), reads=[bMM, bARG], writes=[bARG])
                        P.op("dve", lambda v, n_=n_: v.tensor_scalar(out=MM[:, 0:n_], in0=ARG[:, 0:n_], scalar1=-PI, scalar2=None, op0=ALU.is_lt), reads=[bARG], writes=[bMM])
                        P.op("dve", lambda v, n_=n_: v.scalar_tensor_tensor(out=ARG[:, 0:n_], in0=MM[:, 0:n_], scalar=2 * PI, in1=ARG[:, 0:n_], op0=ALU.mult, op1=ALU.add), reads=[bMM, bARG], writes=[bARG])
                        P.op("act", lambda a, DST=DST, blk=blk, n_=n_: a.activation(out=DST[:, blk:blk + n_], in_=ARG[:, 0:n_], func=AF.Sin), reads=[bARG], writes=[bDST])
                for hh in range(2):
                    c0 = hh * 512
                    for tl in range(nt):
                        P.op("act", lambda a, tl=tl, c0=c0: a.activation(out=EE[:, 0:512], in_=DEL[:, c0:c0 + 512], func=AF.Exp, scale=NEGT[:, tl:tl + 1]), reads=[bW, bTBL], writes=[bEE])
                        for fi, (DSTF, bDSTF) in enumerate(((HF, bHF), (HB, bHB))):
                            w0 = fi * 1024 + c0
                            P.op("pe", lambda t, tl=tl, w0=w0: t.matmul(PI_[1][:, :], H2[:, tl * 128:(tl + 1) * 128], W3[:, w0:w0 + 512], start=True, stop=False), reads=[bH2, bW], writes=[bPI[1]])
                            P.op("pe", lambda t, w0=w0: t.matmul(PI_[1][:, :], ONES1[:, :], B3[:, w0:w0 + 512], start=False, stop=True), reads=[bW], writes=[bPI[1]])
                            if tl == 0:
                                P.op("dve", lambda v: v.scalar_tensor_tensor(out=F0[:, :], in0=EE[:, 0:512], scalar=0.05, in1=PI_[1][:, :], op0=ALU.add, op1=ALU.mult), reads=[bEE, bPI[1]], writes=[bF0])
                                if fi == 0:
                                    P.op("dve", lambda v, c0=c0: v.tensor_tensor(out=F0[0:1, :], in0=F0[0:1, :], in1=SKs[0:1, c0:c0 + 512], op=ALU.add), reads=[bF0, bW], writes=[bF0])
                                else:
                                    P.op("dve", lambda v: v.memset(F0[0:1, :], 0.0), reads=[bF0], writes=[bF0])
                                P.op("dve", lambda v, DSTF=DSTF: v.tensor_copy(out=DSTF[:, 0, :], in_=F0[:, :]), reads=[bF0], writes=[bDSTF])
                            else:
                                P.op("dve", lambda v, DSTF=DSTF, tl=tl: v.scalar_tensor_tensor(out=DSTF[:, tl, :], in0=EE[:, 0:512], scalar=0.05, in1=PI_[1][:, :], op0=ALU.add, op1=ALU.mult), reads=[bEE, bPI[1]], writes=[bDSTF])
                    for fc in range(nt):
                        cb_ = ci % 2; ci += 1
                        if cb_ == 0:
                            Cv = CT[0][:, 0:nt, :]; Sv = STt[0][:, 0:nt, :]; bCv = bCT[0]; bSv = bST[0]
                        else:
                            Cv = CI[:, 0:nt, 0:128]; Sv = SI[:, 0:nt, 0:128]; bCv = bCI; bSv = bSI
                        P.dma(Cv, tb_["C"].rearrange("(tc p) f -> p tc f", p=128)[:, :, fc * 128:(fc + 1) * 128], reads=[cin], writes=[bCv])
                        P.dma(Sv, tb_["S"].rearrange("(tc p) f -> p tc f", p=128)[:, :, fc * 128:(fc + 1) * 128], reads=[cin], writes=[bSv])
                        srcs = [(lambda tc, tile0=tile0, c0=c0: GTM[:, tile0 + tc, c0:c0 + 512], lambda tc, tile0=tile0: [bGTM[tile0 + tc]]), (lambda tc: HF[:, tc, :], lambda tc: [bHF]), (lambda tc: HB[:, tc, :], lambda tc: [bHB])]
                        for si_, (sf_, bf_) in enumerate(srcs):
                            for ti_, (TAB, bTAB) in enumerate(((Cv, bCv), (Sv, bSv))):
                                pf = si_ * 2 + ti_
                                for tc in range(nt):
                                    P.op("pe", lambda t, pf=pf, TAB=TAB, tc=tc, sf_=sf_, nt=nt: t.matmul(PF[pf][:, :], TAB[:, tc, :], sf_(tc), start=(tc == 0), stop=(tc == nt - 1)), reads=[bTAB] + bf_(tc), writes=[bPF[pf]])
                        Gc, Gs, Hfc, Hfs, Hbc, Hbs = PF; bGc, bGs, bHfc, bHfs, bHbc, bHbs = bPF
                        w_ = WSC[:, fc:fc + 1]
                        HbcW, HbsW, GsS, Kre, Kim, t1, t2, t3, t4 = TM; bHbcW, bHbsW, bGsS, bKre, bKim, bt1, bt2, bt3, bt4 = bTM
                        P.op("act", lambda a, w_=w_: a.activation(out=HbcW[:, :], in_=Hbc[:, :], func=AF.Copy, scale=w_), reads=[bHbc, bTBL], writes=[bHbcW])
                        P.op("act", lambda a, w_=w_: a.activation(out=HbsW[:, :], in_=Hbs[:, :], func=AF.Copy, scale=w_), reads=[bHbs, bTBL], writes=[bHbsW])
                        P.op("act", lambda a: a.copy(out=GsS[:, :], in_=Gs[:, :]), reads=[bGs], writes=[bGsS])
                        P.op("act", lambda a: a.copy(out=GcS[:, :], in_=Gc[:, :]), reads=[bGc], writes=[bGcS])
                        P.op("dve", lambda v, w_=w_: v.scalar_tensor_tensor(out=Kre[:, :], in0=Hfc[:, :], scalar=w_, in1=HbcW[:, :], op0=ALU.mult, op1=ALU.add), reads=[bHfc, bTBL, bHbcW], writes=[bKre])
                        P.op("dve", lambda v, w_=w_: v.scalar_tensor_tensor(out=Kim[:, :], in0=Hfs[:, :], scalar=w_, in1=HbsW[:, :], op0=ALU.mult, op1=ALU.subtract), reads=[bHfs, bTBL, bHbsW], writes=[bKim])
                        P.op("dve", lambda v: v.tensor_tensor(out=t1[:, :], in0=GcS[:, :], in1=Kre[:, :], op=ALU.mult), reads=[bGcS, bKre], writes=[bt1])
                        P.op("dve", lambda g: g.tensor_tensor(out=t2[:, :], in0=GsS[:, :], in1=Kim[:, :], op=ALU.mult), reads=[bGsS, bKim], writes=[bt2])
                        P.op("dve", lambda g, fc=fc: g.tensor_tensor(out=YRE[:, fc, :], in0=t1[:, :], in1=t2[:, :], op=ALU.subtract), reads=[bt1, bt2], writes=[bYRE[fc]])
                        P.op("dve", lambda v: v.tensor_tensor(out=t3[:, :], in0=GcS[:, :], in1=Kim[:, :], op=ALU.mult), reads=[bGcS, bKim], writes=[bt3])
                        P.op("dve", lambda g: g.tensor_tensor(out=t4[:, :], in0=GsS[:, :], in1=Kre[:, :], op=ALU.mult), reads=[bGsS, bKre], writes=[bt4])
                        P.op("dve", lambda g, fc=fc: g.tensor_tensor(out=YIM[:, fc, :], in0=t3[:, :], in1=t4[:, :], op=ALU.add), reads=[bt3, bt4], writes=[bYIM[fc]])
                        if fc == 0:
                            P.op("dve", lambda v: v.tensor_copy(out=YRE[0:1, 0, :], in_=t1[0:1, :]), reads=[bt1, bYRE[0]], writes=[bYRE[0]])
                            P.op("dve", lambda v, w_=w_: v.scalar_tensor_tensor(out=t2[0:1, :], in0=Hfs[0:1, :], scalar=WSC[0:1, 0:1], in1=HbsW[0:1, :], op0=ALU.mult, op1=ALU.add), reads=[bHfs, bTBL, bHbsW, bt2], writes=[bt2])
                            P.op("dve", lambda g: g.tensor_tensor(out=YIM[0:1, 0, :], in0=GsS[0:1, :], in1=t2[0:1, :], op=ALU.mult), reads=[bGsS, bt2, bYIM[0]], writes=[bYIM[0]])
                    for blk in range(0, L, 256):
                        n_ = min(256, L - blk)
                        P.dma(CI[:, 0:nt, 0:n_], tb_["C"].rearrange("(fc p) t -> p fc t", p=128)[:, :, blk:blk + n_], reads=[cin], writes=[bCI])
                        P.dma(SI[:, 0:nt, 0:n_], tb_["ST"].rearrange("(fc p) t -> p fc t", p=128)[:, :, blk:blk + n_], reads=[cin], writes=[bSI])
                        for cch in range(4):
                            pb = cch % 2
                            chan = hh * 4 + cch
                            xb = xi % 2; xi += 1
                            P.dma(X0L[xb][:, 0:n_], X0D[chan * 128:(chan + 1) * 128, tk0 + blk:tk0 + blk + n_], reads=[bX0D[chan]], writes=[bX0L[xb]])
                            for fc in range(nt):
                                P.op("pe", lambda t, pb=pb, fc=fc, cch=cch, n_=n_: t.matmul(PI_[pb][:, 0:n_], YRE[:, fc, cch * 128:(cch + 1) * 128], CI[:, fc, 0:n_], start=(fc == 0), stop=False), reads=[bYRE[fc], bCI], writes=[bPI[pb]])
                                P.op("pe", lambda t, pb=pb, fc=fc, cch=cch, n_=n_, nt=nt: t.matmul(PI_[pb][:, 0:n_], YIM[:, fc, cch * 128:(cch + 1) * 128], SI[:, fc, 0:n_], start=False, stop=(fc == nt - 1)), reads=[bYIM[fc], bSI], writes=[bPI[pb]])
                            P.op("dve", lambda v, pb=pb, xb=xb, n_=n_: v.tensor_tensor(out=YO[xb][:, 0:n_], in0=PI_[pb][:, 0:n_], in1=X0L[xb][:, 0:n_], op=ALU.mult), reads=[bPI[pb], bX0L[xb]], writes=[bYO[xb]])
                            P.dma(YTD[chan * 128:(chan + 1) * 128, tk0 + blk:tk0 + blk + n_], YO[xb][:, 0:n_], reads=[bYO[xb]], writes=[bYTD[chan]])
            P.flush()


def outproj_fm_dram(P, K, YTD, bYTD, WO, X, bX, MODS, g_off, tiles=range(NT), norm2=None):
    with contextlib.ExitStack() as st:
        YT = P.sb(st, "ofm_YT", [128, 8, T], BF16); bYT = [Buf() for _ in range(NT)]
        for k in range(8):
            P.dma(YT[:, k, :], YTD[k * 128:(k + 1) * 128, :], reads=[bYTD[k]], writes=bYT)
        phase_outproj(P, K, (YT, bYT), WO, X, bX, MODS, g_off, tiles=tiles, mode="fm", norm2=norm2)


LN8 = math.log(0.125)

def ml_host(conv_w, conv_b):
    cw = conv_w.reshape(3, 8, 128).transpose(2, 1, 0).copy()
    cb = conv_b.reshape(8, 128).T.copy()
    sel8 = np.zeros((8, 8, 128), np.float32)
    for h in range(8): sel8[h, h, :] = 1
    s = np.arange(128)
    tri = np.zeros((128, 2, 128), np.float32)
    tri[:, 0, :] = (s[:, None] <= s[None, :])
    tri[:, 1, :] = (s[:, None] >= s[None, :])
    return cw, cb, sel8, tri


def hs_scan(P, src, bsrc, A, bA, B, bB, a0, a1, op, reverse, nparts=8):
    n = a1 - a0
    sh = 1
    cur, bcur = src, bsrc
    nxt = [(A, bA), (B, bB)]
    i = 0
    while sh < n:
        dst, bdst = nxt[i % 2]; i += 1
        if not reverse:
            P.op("dve", lambda v, cur=cur, dst=dst, sh=sh: v.tensor_tensor(out=dst[0:nparts, a0 + sh:a1], in0=cur[0:nparts, a0 + sh:a1], in1=cur[0:nparts, a0:a1 - sh], op=op), reads=[bcur], writes=[bdst])
            P.op("pool", lambda g, cur=cur, dst=dst, sh=sh: g.tensor_copy(out=dst[0:nparts, a0:a0 + sh], in_=cur[0:nparts, a0:a0 + sh]), reads=[bcur], writes=[bdst])
        else:
            P.op("dve", lambda v, cur=cur, dst=dst, sh=sh: v.tensor_tensor(out=dst[0:nparts, a0:a1 - sh], in0=cur[0:nparts, a0:a1 - sh], in1=cur[0:nparts, a0 + sh:a1], op=op), reads=[bcur], writes=[bdst])
            P.op("pool", lambda g, cur=cur, dst=dst, sh=sh: g.tensor_copy(out=dst[0:nparts, a1 - sh:a1], in_=cur[0:nparts, a1 - sh:a1]), reads=[bcur], writes=[bdst])
        cur, bcur = dst, bdst
        sh *= 2
    return cur, bcur


def mixer_mlstm(P, K, run_norm, WIN, CW, CB, GB, ONG, SEL8, TRI, SIGO, YTM, bYTM):
    cin = Buf(); bSIGO = [Buf() for _ in range(NT)]
    with contextlib.ExitStack() as st:
        HT = P.sb(st, "HT", [128, 8, T], BF16); bHT = [Buf() for _ in range(NT)]
        run_norm(HT, bHT)
        NEGM = [P.sb(st, f"l_NEGM{d}", [8, T], F32) for d in range(2)]; bNEGM = [Buf(), Buf()]
        ATM = P.sb(st, "l_ATM", [128, NT, 16], F32); bATM = Buf()
        EMT = P.sb(st, "l_EMT", [128, NT, 16], F32); bEMT = Buf()
        if True:
            with contextlib.ExitStack() as st3:
                WG = P.sb(st3, "l_WG", [128, 8, 32], BF16); bWG = Buf()
                WGf = P.sb(st3, "l_WGf", [128, 8, 32], F32); bWGf = Buf()
                GBs = P.sb(st3, "l_GB", [128, 32], F32); bGB = Buf()
                ONE = P.sb(st3, "l_ONE", [128, 1], F32); bONE = Buf()
                GT = P.sb(st3, "l_GT", [128, NT, 32], F32); bGT = [Buf() for _ in range(NT)]
                TMPg = P.sb(st3, "l_TMPg", [128, NT, 32], F32)
                SC = [P.sb(st3, f"l_SC{i}", [8, T], F32) for i in range(10)]; bSC = [Buf() for _ in range(10)]
                PGt = [P.ps(st3, f"l_PG{i}") for i in range(2)]; bPG = [Buf(excl=True) for _ in range(2)]
                PTr = [P.ps(st3, f"l_PTr{i}") for i in range(4)]; bPTr = [Buf(excl=True) for _ in range(4)]
                P.dma(WGf[:, :, :], WIN.rearrange("(c p) f -> p c f", p=128)[:, :, 3072:3104], reads=[cin], writes=[bWGf])
                P.op("dve", lambda v: v.tensor_copy(out=WG[:, :, :], in_=WGf[:, :, :]), reads=[bWGf], writes=[bWG])
                P.dma(GBs[:, :], GB.partition_broadcast(128), reads=[cin], writes=[bGB])
                P.op("dve", lambda v: v.memset(ONE[:, :], 1.0), writes=[bONE])
                for tt in range(NT):
                    p = tt % 2
                    for k in range(8):
                        P.op("pe", lambda t, k=k, p=p, tt=tt: t.matmul(PGt[p][:, 0:32], HT[:, k, tt * 128:(tt + 1) * 128], WG[:, k, :], start=(k == 0), stop=(k == 7)), reads=[bHT[tt], bWG], writes=[bPG[p]])
                    P.op("dve", lambda v, p=p, tt=tt: v.tensor_tensor(out=GT[:, tt, :], in0=PGt[p][:, 0:32], in1=GBs[:, :], op=ALU.add), reads=[bPG[p], bGB], writes=[bGT[tt]])
                    for j in (1, 3):
                        sl = slice(j * 8, (j + 1) * 8)
                        P.op("act", lambda a, tt=tt, sl=sl: a.activation(out=TMPg[:, tt, sl], in_=GT[:, tt, sl], func=AF.Exp, scale=-1.0), reads=[bGT[tt]], writes=[bGT[tt]])
                        P.op("act", lambda a, tt=tt, sl=sl: a.activation(out=TMPg[:, tt, sl], in_=TMPg[:, tt, sl], func=AF.Ln, bias=ONE[:, :]), reads=[bGT[tt], bONE], writes=[bGT[tt]])
                        P.op("dve", lambda v, tt=tt, sl=sl: v.tensor_scalar(out=GT[:, tt, sl], in0=TMPg[:, tt, sl], scalar1=-1.0, scalar2=None, op0=ALU.mult), reads=[bGT[tt]], writes=[bGT[tt]])
                    for j in range(4):
                        P.op("pe", lambda t, j=j, tt=tt: t.transpose(PTr[j][0:8, 0:128], GT[:, tt, j * 8:(j + 1) * 8], K["idf"][:, :]), reads=[bGT[tt], K["b_idf"]], writes=[bPTr[j]])
                        P.op("act", lambda a, j=j, tt=tt: a.copy(out=SC[j][:, tt * 128:(tt + 1) * 128], in_=PTr[j][0:8, 0:128]), reads=[bPTr[j]], writes=[bSC[j]])
                for d in range(2):
                    IG, bIG, LF, bLF = SC[2 * d], bSC[2 * d], SC[2 * d + 1], bSC[2 * d + 1]
                    w = [(SC[4 + i], bSC[4 + i]) for i in range(6)]
                    if d == 0:
                        F_, bF = hs_scan(P, LF, bLF, w[0][0], w[0][1], w[1][0], w[1][1], 0, T, ALU.add, False)
                    else:
                        Fc, bFc = hs_scan(P, LF, bLF, w[0][0], w[0][1], w[1][0], w[1][1], 0, 256, ALU.add, True)
                        Fl, bFl = hs_scan(P, LF, bLF, w[2][0], w[2][1], w[3][0], w[3][1], 256, T, ALU.add, True)
                        F_, bF = w[4]
                        P.op("dve", lambda v, Fc=Fc: v.tensor_copy(out=F_[:, 0:256], in_=Fc[:, 0:256]), reads=[bFc], writes=[bF])
                        P.op("dve", lambda v, Fl=Fl, Fc=Fc: v.tensor_scalar(out=F_[:, 256:T], in0=Fl[:, 256:T], scalar1=Fc[:, 0:1], scalar2=None, op0=ALU.add), reads=[bFl, bFc], writes=[bF])
                    P.op("dve", lambda v, IG=IG, F_=F_: v.tensor_tensor(out=IG[:, :], in0=IG[:, :], in1=F_[:, :], op=ALU.subtract), reads=[bIG, bF], writes=[bIG])
                    if d == 0:
                        free = [x for x in w if x[0] is not F_]
                        CM, bCM = hs_scan(P, IG, bIG, free[0][0], free[0][1], free[1][0], free[1][1], 0, T, ALU.max, False)
                        Mt, bM = free[2]
                        P.op("dve", lambda v, CM=CM, Mt=Mt: v.tensor_scalar(out=Mt[:, :], in0=CM[:, :], scalar1=0.0, scalar2=None, op0=ALU.max), reads=[bCM], writes=[bM])
                    else:
                        CMc, bCMc = hs_scan(P, IG, bIG, w[0][0], w[0][1], w[1][0], w[1][1], 0, 256, ALU.max, True)
                        CMl, bCMl = hs_scan(P, IG, bIG, w[2][0], w[2][1], w[3][0], w[3][1], 256, T, ALU.max, True)
                        Mt, bM = w[5]
                        P.op("dve", lambda v, CMc=CMc, Mt=Mt: v.tensor_scalar(out=Mt[:, 0:256], in0=CMc[:, 0:256], scalar1=0.0, scalar2=None, op0=ALU.max), reads=[bCMc], writes=[bM])
                        P.op("dve", lambda v, CMl=CMl, CMc=CMc, Mt=Mt: v.tensor_scalar(out=Mt[:, 256:T], in0=CMl[:, 256:T], scalar1=CMc[:, 0:1], scalar2=0.0, op0=ALU.max, op1=ALU.max), reads=[bCMl, bCMc], writes=[bM])
                    P.op("dve", lambda v, d=d, Mt=Mt: v.tensor_scalar(out=NEGM[d][:, :], in0=Mt[:, :], scalar1=-1.0, scalar2=None, op0=ALU.mult), reads=[bM], writes=[bNEGM[d]])
                    P.op("dve", lambda v, LF=LF, F_=F_, Mt=Mt: v.tensor_tensor(out=LF[:, :], in0=F_[:, :], in1=Mt[:, :], op=ALU.add), reads=[bF, bM], writes=[bLF])
                    P.op("act", lambda a, LF=LF: a.activation(out=LF[:, :], in_=LF[:, :], func=AF.Exp, scale=-1.0), reads=[bLF], writes=[bLF])
                    P.op("dve", lambda v, IG=IG: v.tensor_scalar(out=IG[:, :], in0=IG[:, :], scalar1=LN8, scalar2=None, op0=ALU.add), reads=[bIG], writes=[bIG])
                    for tt in range(NT):
                        j = tt % 2
                        P.op("pe", lambda t, j=j, tt=tt, IG=IG: t.transpose(PTr[j][:, 0:8], IG[:, tt * 128:(tt + 1) * 128], K["idf"][0:8, 0:8]), reads=[bIG, K["b_idf"]], writes=[bPTr[j]])
                        P.op("act", lambda a, j=j, tt=tt, d=d: a.copy(out=ATM[:, tt, d * 8:(d + 1) * 8], in_=PTr[j][:, 0:8]), reads=[bPTr[j]], writes=[bATM])
                        P.op("pe", lambda t, j=j, tt=tt, LF=LF: t.transpose(PTr[2 + j][:, 0:8], LF[:, tt * 128:(tt + 1) * 128], K["idf"][0:8, 0:8]), reads=[bLF, K["b_idf"]], writes=[bPTr[2 + j]])
                        P.op("act", lambda a, j=j, tt=tt, d=d: a.copy(out=EMT[:, tt, d * 8:(d + 1) * 8], in_=PTr[2 + j][:, 0:8]), reads=[bPTr[2 + j]], writes=[bEMT])
                P.flush()
            QKT = P.sb(st, "l_QKT", [128, 8, T], BF16); bQKT = [Buf() for _ in range(8)]
            VA = P.sb(st, "l_VA", [128, NT, 8, 129], BF16); bVA = [Buf() for _ in range(NT)]
            with contextlib.ExitStack() as st3:
                WB = P.sb(st3, "l_WB", [128, 8, 1024], BF16); bWB = [Buf() for _ in range(8)]
                STG = [P.sb(st3, f"l_stg{i}", [128, 1024], F32) for i in range(2)]; bSTG = [Buf() for _ in range(2)]
                CWs = P.sb(st3, "l_CW", [128, 8, 3], F32); CBs = P.sb(st3, "l_CB", [128, 8], F32); bCW = Buf()
                ZC = [P.sb(st3, f"l_ZC{i}", [128, T], F32) for i in range(1)] * 2; bZC = [Buf()] * 2
                AC = [P.sb(st3, f"l_AC{i}", [128, T], F32) for i in range(1)] * 2; bAC = [Buf()] * 2
                SO = [P.sb(st3, f"l_SO{i}", [128, 1024], BF16) for i in range(2)]; bSO = [Buf() for _ in range(2)]
                PP = [P.ps(st3, f"l_PP{i}") for i in range(4)]; bPP = [Buf(excl=True) for _ in range(4)]
                P.dma(CWs[:, :, :], CW[:, :, :], reads=[cin], writes=[bCW])
                P.dma(CBs[:, :], CB[:, :], reads=[cin], writes=[bCW])
                P.op("dve", lambda v: v.memset(VA[:, :, :, 128:129], 1.0), writes=bVA)
                pi = 0
                for which in range(3):
                    for k in range(8):
                        s = k % 2
                        P.dma(STG[s][:, :], WIN[k * 128:(k + 1) * 128, which * 1024:(which + 1) * 1024], reads=[cin], writes=[bSTG[s]])
                        if k % 2 == 0:
                            P.op("dve", lambda g, k=k, s=s: g.tensor_copy(out=WB[:, k, :], in_=STG[s][:, :]), reads=[bSTG[s]], writes=[bWB[k]])
                        else:
                            P.op("act", lambda a, k=k, s=s: a.copy(out=WB[:, k, :], in_=STG[s][:, :]), reads=[bSTG[s]], writes=[bWB[k]])
                    if which == 0:
                        for c in range(8):
                            b = c % 2
                            for tb in range(6):
                                p = pi % 4; pi += 1
                                for k in range(8):
                                    P.op("pe", lambda t, k=k, p=p, c=c, tb=tb: t.matmul(PP[p][:, 0:384], WB[:, k, c * 128:(c + 1) * 128], HT[:, k, tb * 384:(tb + 1) * 384], start=(k == 0), stop=(k == 7)),
                                         reads=[bWB[k]] + bHT[tb * 3:tb * 3 + 3], writes=[bPP[p]])
                                P.op("act", lambda a, p=p, b=b, tb=tb: a.copy(out=ZC[b][:, tb * 384:(tb + 1) * 384], in_=PP[p][:, 0:384]), reads=[bPP[p]], writes=[bZC[b]])
                            z = ZC[b]; ac = AC[b]
                            P.op("dve", lambda v, z=z, ac=ac, c=c: v.tensor_scalar(out=ac[:, :], in0=z[:, :], scalar1=CWs[:, c, 1:2], scalar2=CBs[:, c:c + 1], op0=ALU.mult, op1=ALU.add), reads=[bZC[b], bCW], writes=[bAC[b]])
                            for (o0, o1, i0, i1, tap) in ((1, 256, 0, 255, 0), (257, T, 256, T - 1, 0), (0, 255, 1, 256, 2), (256, T - 1, 257, T, 2)):
                                P.op("dve", lambda v, z=z, ac=ac, c=c, o0=o0, o1=o1, i0=i0, i1=i1, tap=tap: v.scalar_tensor_tensor(out=ac[:, o0:o1], in0=z[:, i0:i1], scalar=CWs[:, c, tap:tap + 1], in1=ac[:, o0:o1], op0=ALU.mult, op1=ALU.add),
                                     reads=[bZC[b], bAC[b], bCW], writes=[bAC[b]])
                            P.op("act", lambda a, ac=ac, c=c: a.activation(out=QKT[:, c, :], in_=ac[:, :], func=AF.Silu), reads=[bAC[b]], writes=[bQKT[c]])
                    elif which == 1:
                        for tt in range(NT):
                            for dh in range(2):
                                p = pi % 4; pi += 1
                                for k in range(8):
                                    P.op("pe", lambda t, k=k, p=p, tt=tt, dh=dh: t.matmul(PP[p][:, :], HT[:, k, tt * 128:(tt + 1) * 128], WB[:, k, dh * 512:(dh + 1) * 512], start=(k == 0), stop=(k == 7)),
                                         reads=[bWB[k], bHT[tt]], writes=[bPP[p]])
                                if dh == 0:
                                    P.op("act", lambda a, p=p, tt=tt, dh=dh: a.copy(out=VA[:, tt, dh * 4:(dh + 1) * 4, 0:128], in_=PP[p][:, :].rearrange("p (h d) -> p h d", h=4)), reads=[bPP[p]], writes=[bVA[tt]])
                                else:
                                    P.op("dve", lambda v, p=p, tt=tt, dh=dh: v.tensor_copy(out=VA[:, tt, dh * 4:(dh + 1) * 4, 0:128], in_=PP[p][:, :].rearrange("p (h d) -> p h d", h=4)), reads=[bPP[p]], writes=[bVA[tt]])
                    else:
                        for tt in range(2, NT):
                            b = tt % 2
                            for dh in range(2):
                                p = pi % 4; pi += 1
                                for k in range(8):
                                    P.op("pe", lambda t, k=k, p=p, tt=tt, dh=dh: t.matmul(PP[p][:, :], HT[:, k, tt * 128:(tt + 1) * 128], WB[:, k, dh * 512:(dh + 1) * 512], start=(k == 0), stop=(k == 7)),
                                         reads=[bWB[k], bHT[tt]], writes=[bPP[p]])
                                P.op("act", lambda a, p=p, b=b, dh=dh: a.activation(out=SO[b][:, dh * 512:(dh + 1) * 512], in_=PP[p][:, :], func=AF.Sigmoid), reads=[bPP[p]], writes=[bSO[b]])
                            P.dma(SIGO[tt * 128:(tt + 1) * 128, :], SO[b][:, :], reads=[bSO[b]], writes=[bSIGO[tt]])
                P.flush()
        with contextlib.ExitStack() as st2:
            SEL = P.sb(st2, "l_SEL", [8, 8, 128], F32); bSEL = Buf()
            TRf = P.sb(st2, "l_TRf", [128, 2, 128], F32); TRb = P.sb(st2, "l_TRb", [128, 2, 128], BF16); bTR = Buf()
            ONGs = P.sb(st2, "l_ONG", [128, 1024], F32); bONG = Buf()
            HS = P.sb(st2, "l_HS", [128, 4, 1024], F32); bHS = [Buf() for _ in range(4)]
            DT = [P.sb(st2, f"l_DT{i}", [128, 512], F32) for i in range(3)]; bDT = [Buf() for _ in range(3)]
            WT = [P.sb(st2, f"l_WT{i}", [128, 512], BF16) for i in range(3)]; bWT = [Buf() for _ in range(3)]
            SM = [P.sb(st2, f"l_SM{i}", [128, 8], F32) for i in range(2)]; bSM = [Buf() for _ in range(2)]
            SG = [P.sb(st2, f"l_SG{i}", [128, 1024], BF16) for i in range(2)]; bSG = [Buf() for _ in range(2)]
            JK = P.sb(st2, "l_JK", [128, 128], F32); bJK = Buf()
            RS = P.sb(st2, "l_RS", [128, 4, 24], F32); bRS = [Buf() for _ in range(4)]
            HN = [P.sb(st2, f"l_HN{i}", [128, 1024], F32) for i in range(1)] * 2; bHN = [Buf()] * 2
            PO = [P.ps(st2, f"l_PO{i}") for i in range(4)]; bPO = [Buf(excl=True) for _ in range(4)]
            PS = [P.ps(st2, f"l_PS{i}") for i in range(3)]; bPS = [Buf(excl=True) for _ in range(3)]
            NB = [P.ps(st2, f"l_NB{i}") for i in range(1)]; bNB = [Buf(excl=True) for _ in range(1)]
            P.dma(SEL[:, :, :], SEL8[:, :, :], reads=[cin], writes=[bSEL])
            P.dma(TRf[:, :, :], TRI[:, :, :], reads=[cin], writes=[bTR])
            P.op("dve", lambda v: v.tensor_copy(out=TRb[:, :, :], in_=TRf[:, :, :]), reads=[bTR], writes=[bTR])
            P.dma(ONGs[:, :], ONG.partition_broadcast(128), reads=[cin], writes=[bONG])
            def kcs_of(qb, d):
                tiles_ = [2 + qb * 4 + j for j in range(4)]
                return list(range(0, tiles_[-1] + 1)) if d == 0 else [0, 1] + list(range(tiles_[0], NT))
            groups = [(qb, h, d) for qb in range(4) for h in range(8) for d in range(2)]
            def emit_NB(gi):
                qb, h, d = groups[gi]; nb = 0; q0 = 256 + qb * 512
                P.op("pe", lambda t, nb=nb, h=h, d=d, q0=q0: t.matmul(NB[nb][:, :], SEL[:, h, :], NEGM[d][:, q0:q0 + 512], start=True, stop=True), reads=[bSEL, bNEGM[d]], writes=[bNB[nb]])
            steps = []
            for gi, (qb, h, d) in enumerate(groups):
                ks = kcs_of(qb, d)
                for ki, kc in enumerate(ks):
                    steps.append((gi, kc, ki == 0, ki == len(ks) - 1))
            def emit_S(idx):
                gi, kc, _, _ = steps[idx]
                qb, h, d = groups[gi]; b = idx % 3; q0 = 256 + qb * 512
                cq = h // 2; ck = 4 + h // 2; hp = (h % 2) * 64
                P.op("pe", lambda t, b=b, kc=kc, ck=ck, cq=cq, hp=hp, q0=q0: t.matmul(PS[b][:, :], QKT[hp:hp + 64, ck, kc * 128:(kc + 1) * 128], QKT[hp:hp + 64, cq, q0:q0 + 512], start=True, stop=True),
                     reads=[bQKT[ck], bQKT[cq]], writes=[bPS[b]])
            emit_NB(0)
            emit_S(0)
            emit_S(1)
            for idx, (gi, kc, gfirst, glast) in enumerate(steps):
                qb, h, d = groups[gi]
                tiles_ = [2 + qb * 4 + j for j in range(4)]
                nb = 0; b = idx % 3
                col = d * 8 + h
                if gfirst and gi > 0:
                    emit_NB(gi)
                if tiles_[0] <= kc <= tiles_[-1]:
                    P.op("dve", lambda v, b=b, nb=nb, kc=kc, col=col: v.tensor_scalar(out=DT[b][:, :], in0=NB[nb][:, :], scalar1=ATM[:, kc, col:col + 1], scalar2=0.0, op0=ALU.add, op1=ALU.min),
                         reads=[bNB[nb], bATM], writes=[bDT[b]])
                    P.op("act", lambda a, b=b: a.activation(out=DT[b][:, :], in_=DT[b][:, :], func=AF.Exp), reads=[bDT[b]], writes=[bDT[b]])
                else:
                    P.op("act", lambda a, b=b, nb=nb, kc=kc, col=col: a.activation(out=DT[b][:, :], in_=NB[nb][:, :], func=AF.Exp, bias=ATM[:, kc, col:col + 1]), reads=[bNB[nb], bATM], writes=[bDT[b]])
                if idx + 2 < len(steps):
                    emit_S(idx + 2)
                P.op("dve", lambda v, b=b: v.tensor_tensor(out=WT[b][:, :], in0=PS[b][:, :], in1=DT[b][:, :], op=ALU.mult), reads=[bPS[b], bDT[b]], writes=[bWT[b]])
                for j, tj in enumerate(tiles_):
                    if d == 0:
                        ok = kc <= tj; first = (kc == 0); last = (kc == tj)
                    else:
                        ok = kc < 2 or kc >= tj; first = (kc == 0); last = (kc == NT - 1)
                    if not ok:
                        continue
                    if kc == tj:
                        P.op("dve", lambda g, b=b, j=j, d=d: g.tensor_tensor(out=WT[b][:, j * 128:(j + 1) * 128], in0=WT[b][:, j * 128:(j + 1) * 128], in1=TRb[:, d, :], op=ALU.mult), reads=[bWT[b], bTR], writes=[bWT[b]])
                    P.op("pe", lambda t, b=b, j=j, kc=kc, h=h, first=first, last=last: t.matmul(PO[j][:, 0:129], WT[b][:, j * 128:(j + 1) * 128], VA[:, kc, h, :], start=first, stop=last),
                         reads=[bWT[b], bVA[kc]], writes=[bPO[j]])
                if glast:
                    for j, tj in enumerate(tiles_):
                        sm = j % 2
                        P.op("act", lambda a, j=j, sm=sm: a.activation(out=SM[sm][:, 2:3], in_=PO[j][:, 128:129], func=AF.Abs), reads=[bPO[j]], writes=[bSM[sm]])
                        P.op("dve", lambda v, j=j, tj=tj, col=col, sm=sm: v.tensor_scalar(out=SM[sm][:, 0:1], in0=SM[sm][:, 2:3], scalar1=EMT[:, tj, col:col + 1], scalar2=None, op0=ALU.max), reads=[bSM[sm], bEMT], writes=[bSM[sm]])
                        P.op("dve", lambda v, sm=sm: v.reciprocal(out=SM[sm][:, 1:2], in_=SM[sm][:, 0:1]), reads=[bSM[sm]], writes=[bSM[sm]])
                        if d == 0:
                            P.op("dve", lambda v, j=j, h=h, sm=sm: v.tensor_scalar(out=HS[:, j, h * 128:(h + 1) * 128], in0=PO[j][:, 0:128], scalar1=SM[sm][:, 1:2], scalar2=None, op0=ALU.mult), reads=[bPO[j], bSM[sm]], writes=[bHS[j]])
                        else:
                            P.op("dve", lambda v, j=j, h=h, sm=sm: v.scalar_tensor_tensor(out=HS[:, j, h * 128:(h + 1) * 128], in0=PO[j][:, 0:128], scalar=SM[sm][:, 1:2], in1=HS[:, j, h * 128:(h + 1) * 128], op0=ALU.mult, op1=ALU.add),
                                 reads=[bPO[j], bSM[sm], bHS[j]], writes=[bHS[j]])
                if not (glast and h == 7 and d == 1):
                    continue
                for j, tj in enumerate(tiles_):
                    b = j % 2
                    P.dma(SG[b][:, :], SIGO[tj * 128:(tj + 1) * 128, :], reads=[bSIGO[tj]], writes=[bSG[b]])
                    for h in range(8):
                        P.op("act", lambda a, j=j, h=h: a.activation(out=JK[:, :], in_=HS[:, j, h * 128:(h + 1) * 128], func=AF.Square, accum_out=RS[:, j, h:h + 1]), reads=[bHS[j]], writes=[bJK, bRS[j]])
                    P.op("act", lambda a, j=j: a.activation(out=RS[:, j, 8:16], in_=RS[:, j, 0:8], func=AF.Sqrt, scale=1.0 / 128, bias=K["eps"][:, :]), reads=[bRS[j], K["b_eps"]], writes=[bRS[j]])
                    P.op("dve", lambda v, j=j: v.reciprocal(out=RS[:, j, 16:24], in_=RS[:, j, 8:16]), reads=[bRS[j]], writes=[bRS[j]])
                    for h in range(8):
                        P.op("dve", lambda v, j=j, h=h, b=b: v.scalar_tensor_tensor(out=HN[b][:, h * 128:(h + 1) * 128], in0=HS[:, j, h * 128:(h + 1) * 128], scalar=RS[:, j, 16 + h:17 + h], in1=ONGs[:, h * 128:(h + 1) * 128], op0=ALU.mult, op1=ALU.mult),
                             reads=[bHS[j], bRS[j], bONG], writes=[bHN[b]])
                    P.op("dve", lambda g, b=b, tj=tj: g.tensor_tensor(out=YTM[:, tj - 2, :], in0=HN[b][:, :], in1=SG[b][:, :], op=ALU.mult), reads=[bHN[b], bSG[b]], writes=[bYTM[tj - 2]])
            P.flush()


NCORES = 8


def _consts_np():
    sel16 = np.zeros((NE, NE, 128), np.float32)
    for e in range(NE):
        sel16[e, e, :] = 1
    slotid = np.zeros((128, 4), np.float32)
    slotid[:, 0] = 32 + np.arange(128); slotid[:, 1] = 160 + np.arange(128); slotid[:, 3] = np.arange(128) % 32
    selq = np.zeros((NE, 4, 128), np.float32)
    for e in range(NE):
        selq[e, e // 4, (e % 4) * 32:(e % 4) * 32 + 32] = 1
    return {"c_idf": np.eye(128, dtype=np.float32), "c_iota": np.arange(512, dtype=np.float32), "c_sel16": sel16, "c_slotid": slotid, "c_selq": selq}


def build_mod():
    P = Prog()
    CT = P.dram("ct", [128, 8, 9], F32, "ExternalInput")
    MW = P.dram("mw", [4, D, 768], F32, "ExternalInput")
    MB = P.dram("mb", [4, 1, 768], F32, "ExternalInput")
    OUT = P.dram("modo", [4, 9, 768], F32, "ExternalOutput")
    with contextlib.ExitStack() as st:
        C_ = P.sb(st, "C_", [128, 8, 9], F32); bC = Buf()
        S_ = P.sb(st, "S_", [128, 8, 9], F32); bS = Buf()
        ON = P.sb(st, "ON", [1, 9], F32); bON = Buf()
        W = [P.sb(st, f"W{i}", [128, 768], F32) for i in range(3)]; bW = [Buf() for _ in range(3)]
        Bb = [P.sb(st, f"Bb{i}", [1, 768], F32) for i in range(2)]; bBb = [Buf() for _ in range(2)]
        O_ = [P.sb(st, f"O{i}", [9, 768], F32) for i in range(2)]; bO = [Buf() for _ in range(2)]
        PS_ = [P.ps(st, f"PS{i}") for i in range(4)]; bPS = [Buf(excl=True) for _ in range(4)]
        cin = Buf(); cout = Buf()
        P.dma(C_[:, :, :], CT[:, :, :], reads=[cin], writes=[bC])
        P.op("act", lambda a: a.activation(out=S_[:, :, :], in_=C_[:, :, :], func=AF.Silu), reads=[bC], writes=[bS])
        P.op("dve", lambda v: v.memset(ON[:, :], 1.0), writes=[bON])
        wi = 0
        for l in range(4):
            b = l % 2
            P.dma(Bb[b][:, :], MB[l], reads=[cin], writes=[bBb[b]])
            for k in range(8):
                w = wi % 3; wi += 1
                P.dma(W[w][:, :], MW[l, k * 128:(k + 1) * 128, :], reads=[cin], writes=[bW[w]])
                for cb, (c0, c1) in enumerate(((0, 512), (512, 768))):
                    P.op("pe", lambda t, k=k, w=w, b=b, cb=cb, c0=c0, c1=c1: t.matmul(PS_[b * 2 + cb][0:9, 0:c1 - c0], S_[:, k, :], W[w][:, c0:c1], start=(k == 0), stop=False),
                         reads=[bS, bW[w]], writes=[bPS[b * 2 + cb]])
            for cb, (c0, c1) in enumerate(((0, 512), (512, 768))):
                P.op("pe", lambda t, b=b, cb=cb, c0=c0, c1=c1: t.matmul(PS_[b * 2 + cb][0:9, 0:c1 - c0], ON[:, :], Bb[b][:, c0:c1], start=False, stop=True),
                     reads=[bON, bBb[b]], writes=[bPS[b * 2 + cb]])
                P.op("dve", lambda v, b=b, cb=cb, c0=c0, c1=c1: v.tensor_copy(out=O_[b][:, c0:c1], in_=PS_[b * 2 + cb][0:9, 0:c1 - c0]), reads=[bPS[b * 2 + cb]], writes=[bO[b]])
            P.dma(OUT[l], O_[b][:, :], reads=[bO[b]], writes=[cout])
        P.flush(final=True)
    return P


def phase_copy_x(P, Xin, X, bX):
    with contextlib.ExitStack() as st1:
        XC = [P.sb(st1, f"XC{i}", [128, D], F32) for i in range(2)]; bXC = [Buf(), Buf()]
        cin = Buf()
        for tt in range(NT):
            P.dma(XC[tt % 2][:, :], Xin[tt * 128:(tt + 1) * 128, :], reads=[cin], writes=[bXC[tt % 2]])
            P.dma(X[tt * 128:(tt + 1) * 128, :], XC[tt % 2][:, :], reads=[bXC[tt % 2]], writes=[bX[tt]])
        P.flush()


FUSE_N2 = False


def build_A(i, fuse=None):
    fuse = FUSE_N2 if fuse is None else fuse
    P = Prog()
    P.split_stores = i in (0, 2)
    C = {"idf": P.dram("c_idf", [128, 128], F32, "ExternalInput"), "iota": P.dram("c_iota", [512], F32, "ExternalInput")}
    Xin = P.dram("xin", [T, D], F32, "ExternalInput")
    MODS = P.dram("mods", [2, 6 * D], F32, "ExternalInput")
    NG1 = P.dram("ng1", [D], F32, "ExternalInput"); NG2 = P.dram("ng2", [D], F32, "ExternalInput")
    RW = P.dram("rw", [D, NE], F32, "ExternalInput")
    WO = P.dram("wo", [D, D], F32, "ExternalInput")
    X = P.dram("xmid", [T, D], F32, "ExternalOutput")
    XG = P.dram("xg", [NE, D, NSLOT], BF16, "ExternalOutput")
    GV = P.dram("gv", [NE, NSLOT], F32, "ExternalOutput")
    PM = P.dram("posmt", [NE, T], F32, "ExternalOutput")
    if i > 0:
        C["sel16"] = P.dram("c_sel16", [NE, NE, 128], F32, "ExternalInput"); C["slotid"] = P.dram("c_slotid", [128, 4], F32, "ExternalInput"); C["selq"] = P.dram("c_selq", [NE, 4, 128], F32, "ExternalInput")
        MODSP = P.dram("modsp", [2, 6 * D], F32, "ExternalInput")
        Yd = P.dram("y", [NE, NSLOT, D], BF16, "ExternalInput")
        PMin = P.dram("posmt_in", [NE, T], F32, "ExternalInput")
    with contextlib.ExitStack() as st:
        K = load_consts(P, st, C)
        bX = [Buf() for _ in range(NT)]

        def alloc_n2(stack):
            HT2 = P.sb(stack, "HT2", [128, 8, T], BF16); bHT2 = [Buf() for _ in range(NT)]
            HTM2 = P.sb(stack, "HTM2", [128, NT, D], BF16); bHTM2 = [Buf() for _ in range(NT)]
            return (3 * D, NG2, HT2, bHT2, HTM2, bHTM2)
        if i == 0:
            phase_copy_x(P, Xin, X, bX)
        else:
            phase_pro(P, K, C, Yd, PMin, MODSP, Xin, X, bX)
        rn = lambda HT, bHT: phase_norm(P, K, X, bX, MODS, 0, NG1, HT, bHT)
        if i == 0:
            WQKV = P.dram("wqkv", [D, 3 * D], F32, "ExternalInput")
            TTE = P.dram("tte", [128, 16, 14, 64], F32, "ExternalInput"); TTO = P.dram("tto", [128, 16, 5, 64], F32, "ExternalInput")
            YATT = P.dram("yatt", [T, D], BF16); bY = [Buf() for _ in range(NT)]
            mixer_na(P, K, rn, WQKV, TTE, TTO, YATT, bY)
            def src(tt, dst, bdst):
                P.dma(dst[:, :], YATT[tt * 128:(tt + 1) * 128, :], reads=[bY[tt]], writes=[bdst])
            n2 = alloc_n2(st) if fuse else None
            phase_outproj(P, K, src, WO, X, bX, MODS, 2 * D, norm2=n2)
        elif i == 1:
            WIN = P.dram("win", [D, 672], F32, "ExternalInput"); WKS = P.dram("wks", [D, 96], F32, "ExternalInput")
            GQ = P.dram("gq", [128, 3], F32, "ExternalInput"); GKV = P.dram("gkv", [128, 2], F32, "ExternalInput")
            WQB = P.dram("wqb", [QR, 1536], F32, "ExternalInput"); WQS = P.dram("wqs", [QR, 1536], F32, "ExternalInput")
            WKVB = P.dram("wkvb", [KVR, 2048], F32, "ExternalInput"); CS = P.dram("cs", [32, 2, T], F32, "ExternalInput")
            with contextlib.ExitStack() as stm:
                YTM = P.sb(stm, "YTM", [128, NT, D], BF16); bYTM = [Buf() for _ in range(NT)]
                mixer_mla(P, K, rn, WIN, WKS, GQ, GKV, WQB, WQS, WKVB, CS, YTM, bYTM)
                def src(tt, dst, bdst):
                    P.op("pool", lambda g, tt=tt, dst=dst: g.tensor_copy(out=dst[:, :], in_=YTM[:, tt, :]), reads=[bYTM[tt]], writes=[bdst])
                n2 = alloc_n2(stm) if fuse else None
                phase_outproj(P, K, src, WO, X, bX, MODS, 2 * D, norm2=n2)
                if fuse:
                    phase_route(P, K, C, n2[2], n2[3], n2[4], n2[5], RW, XG, GV, PM)
        elif i == 2:
            WIN = P.dram("win", [D, 3072], F32, "ExternalInput")
            CW = P.dram("cw", [128, 24, 3], F32, "ExternalInput"); CB = P.dram("cb", [128, 24], F32, "ExternalInput")
            FW1 = P.dram("fw1", [33, 64], F32, "ExternalInput"); FW2 = P.dram("fw2", [64, 64], F32, "ExternalInput"); FW3 = P.dram("fw3", [64, 2048], F32, "ExternalInput")
            FB = P.dram("fb", [64, 4], F32, "ExternalInput"); FB3 = P.dram("fb3", [1, 2048], F32, "ExternalInput")
            SKIP = P.dram("skip", [1, 1024], F32, "ExternalInput"); DELTA = P.dram("delta", [1024], F32, "ExternalInput")
            TB = {}
            for tag, L in (("c", 256), ("l", 2048)):
                nt = L // 128
                TB[tag] = {"ze": P.dram("ze" + tag, [33, L], F32, "ExternalInput"), "negt": P.dram("negt" + tag, [128, nt], F32, "ExternalInput"), "wsc": P.dram("wsc" + tag, [128, nt], F32, "ExternalInput"),
                           "C": P.dram("C" + tag, [L, L], BF16, "ExternalInput"), "S": P.dram("S" + tag, [L, L], BF16, "ExternalInput"), "ST": P.dram("ST" + tag, [L, L], BF16, "ExternalInput")}
            X0D = P.dram("x0d", [D, T], BF16); YTD = P.dram("ytd", [D, T], BF16); bYTD = [Buf() for _ in range(8)]
            mixer_hyena(P, K, rn, WIN, CW, CB, FW1, FW2, FW3, FB, FB3, SKIP, DELTA, TB, X0D, YTD, bYTD)
            n2 = alloc_n2(st) if fuse else None
            outproj_fm_dram(P, K, YTD, bYTD, WO, X, bX, MODS, 2 * D, norm2=n2)
        else:
            WIN = P.dram("win", [D, 3104], F32, "ExternalInput")
            CW = P.dram("cw", [128, 8, 3], F32, "ExternalInput"); CB = P.dram("cb", [128, 8], F32, "ExternalInput")
            GB = P.dram("gb", [32], F32, "ExternalInput"); ONG = P.dram("ong", [D], F32, "ExternalInput")
            SEL8 = P.dram("sel8", [8, 8, 128], F32, "ExternalInput"); TRI = P.dram("tri", [128, 2, 128], F32, "ExternalInput")
            SIGO = P.dram("sigo", [T, D], BF16)
            with contextlib.ExitStack() as stm:
                YTM = P.sb(stm, "YTM", [128, 16, D], BF16); bYTM = [Buf() for _ in range(16)]
                mixer_mlstm(P, K, rn, WIN, CW, CB, GB, ONG, SEL8, TRI, SIGO, YTM, bYTM)
                def src(tt, dst, bdst):
                    P.op("pool", lambda g, tt=tt, dst=dst: g.tensor_copy(out=dst[:, :], in_=YTM[:, tt - 2, :]), reads=[bYTM[tt - 2]], writes=[bdst])
                n2 = alloc_n2(stm) if fuse else None
                phase_outproj(P, K, src, WO, X, bX, MODS, 2 * D, tiles=range(2, NT), norm2=n2)
                if fuse:
                    phase_norm(P, K, X, bX, MODS, 3 * D, NG2, n2[2], n2[3], n2[4], n2[5], tiles=range(0, 2))
                    phase_route(P, K, C, n2[2], n2[3], n2[4], n2[5], RW, XG, GV, PM)
        if fuse and i in (0, 2):
            phase_route(P, K, C, n2[2], n2[3], n2[4], n2[5], RW, XG, GV, PM)
        if not fuse:
            with contextlib.ExitStack() as st2:
                HT = P.sb(st2, "HT2", [128, 8, T], BF16); bHT = [Buf() for _ in range(NT)]
                HTM = P.sb(st2, "HTM2", [128, NT, D], BF16); bHTM = [Buf() for _ in range(NT)]
                phase_norm(P, K, X, bX, MODS, 3 * D, NG2, HT, bHT, HTM, bHTM)
                phase_route(P, K, C, HT, bHT, HTM, bHTM, RW, XG, GV, PM)
        P.flush(final=True)
    return P


def build_Bprog():
    P = Prog()
    xgT = P.dram("xgT", [2, D, NS], BF16, "ExternalInput")
    gv = P.dram("gv", [2, 128, 18], F32, "ExternalInput")
    wg = P.dram("wg", [2, D, FF], F32, "ExternalInput")
    wu = P.dram("wu", [2, D, FF], F32, "ExternalInput")
    wd = P.dram("wd", [2, FF, D], F32, "ExternalInput")
    y = P.dram("y", [2, NS, D], BF16, "ExternalOutput")
    build_B(P, xgT, gv, wg, wu, wd, y, nexp=2)
    return P


def build_F():
    P = Prog()
    C = {"idf": P.dram("c_idf", [128, 128], F32, "ExternalInput"), "sel16": P.dram("c_sel16", [NE, NE, 128], F32, "ExternalInput"),
         "slotid": P.dram("c_slotid", [128, 4], F32, "ExternalInput"), "selq": P.dram("c_selq", [NE, 4, 128], F32, "ExternalInput")}
    Xin = P.dram("xin", [T, D], F32, "ExternalInput")
    MODSP = P.dram("modsp", [2, 6 * D], F32, "ExternalInput")
    Yd = P.dram("y", [NE, NSLOT, D], BF16, "ExternalInput")
    PMin = P.dram("posmt_in", [NE, T], F32, "ExternalInput")
    FNG = P.dram("fng", [D], F32, "ExternalInput")
    OUT = P.dram("out", [2048, D], F32, "ExternalOutput")
    X = P.dram("xfin", [T, D], F32)
    with contextlib.ExitStack() as st:
        K = load_consts(P, st, C)
        bX = [Buf() for _ in range(NT)]
        phase_pro(P, K, C, Yd, PMin, MODSP, Xin, X, bX)
        with contextlib.ExitStack() as st2:
            G = P.sb(st2, "f_G", [128, D], F32); bG = Buf()
            XT = [P.sb(st2, f"f_XT{i}", [128, D], F32) for i in range(2)]; bXT = [Buf() for _ in range(2)]
            OT = [P.sb(st2, f"f_OT{i}", [128, D], F32) for i in range(2)]; bOT = [Buf() for _ in range(2)]
            JK = P.sb(st2, "f_JK", [128, D], F32); bJK = Buf()
            SS = P.sb(st2, "f_SS", [128, 3 * NT], F32); bSS = [Buf() for _ in range(NT)]
            cin = Buf(); cout = Buf()
            P.dma(G[:, :], FNG.partition_broadcast(128), reads=[cin], writes=[bG])
            for tt in range(2, NT):
                b = tt % 2
                P.dma(XT[b][:, :], X[tt * 128:(tt + 1) * 128, :], reads=[bX[tt]], writes=[bXT[b]])
                P.op("act", lambda a, b=b, tt=tt: a.activation(out=JK[:, :], in_=XT[b][:, :], func=AF.Square, accum_out=SS[:, 3 * tt:3 * tt + 1]), reads=[bXT[b]], writes=[bJK, bSS[tt]])
                P.op("act", lambda a, tt=tt: a.activation(out=SS[:, 3 * tt + 1:3 * tt + 2], in_=SS[:, 3 * tt:3 * tt + 1], func=AF.Sqrt, scale=1.0 / D, bias=K["eps"][:, :]), reads=[bSS[tt], K["b_eps"]], writes=[bSS[tt]])
                P.op("dve", lambda v, tt=tt: v.reciprocal(out=SS[:, 3 * tt + 2:3 * tt + 3], in_=SS[:, 3 * tt + 1:3 * tt + 2]), reads=[bSS[tt]], writes=[bSS[tt]])
                P.op("dve", lambda v, b=b, tt=tt: v.scalar_tensor_tensor(out=OT[b][:, :], in0=XT[b][:, :], scalar=SS[:, 3 * tt + 2:3 * tt + 3], in1=G[:, :], op0=ALU.mult, op1=ALU.mult), reads=[bXT[b], bSS[tt], bG], writes=[bOT[b]])
                P.dma(OUT[(tt - 2) * 128:(tt - 1) * 128, :], OT[b][:, :], reads=[bOT[b]], writes=[cout])
        P.flush(final=True)
    return P


def _launch(P, maps):
    res = run_bass_kernel_spmd(P.nc, maps, core_ids=list(range(NCORES)))
    return res.results


def kernel(**inp):
    f32 = lambda a: np.ascontiguousarray(np.asarray(a, dtype=np.float32))
    x = f32(inp["x"]); c = f32(inp["c"]); ctx = f32(inp["ctx"]); c_ctx = f32(inp["c_ctx"])
    KC = _consts_np()
    cvec = np.concatenate([c, c_ctx[None, :]], 0)
    ct = np.ascontiguousarray(cvec.T.reshape(8, 128, 9).transpose(1, 0, 2))
    mod_w = inp["mod_w"]; mod_b = inp["mod_b"]
    Pm = build_mod()
    maps = [{"ct": ct, "mw": f32(mod_w[:, :, j * 768:(j + 1) * 768]), "mb": f32(mod_b[:, None, j * 768:(j + 1) * 768])} for j in range(NCORES)]
    r = _launch(Pm, maps)
    modall = np.concatenate([np.asarray(r[j]["modo"]) for j in range(NCORES)], axis=2)
    def mods_for(l, b):
        return np.ascontiguousarray(np.stack([modall[l, b], modall[l, 8]], 0))
    PB_ = build_Bprog()
    xcur = [np.ascontiguousarray(np.concatenate([ctx[b], x[b]], 0)) for b in range(NCORES)]
    ycur = None; pmcur = None
    for i in range(4):
        PA = build_A(i)
        maps = []
        if i == 0:
            tte, tto = na_tables(f32(inp["na_rpb"][0]))
            extra = {"wqkv": f32(inp["na_w_qkv"][0]), "tte": tte, "tto": tto, "wo": f32(inp["na_w_o"][0])}
        elif i == 1:
            wks, wqs, gq, gkv, cs = mla_host(f32(inp["mla_w_in"][0]), f32(inp["mla_w_q_b"][0]), f32(inp["mla_q_norm_g"][0]), f32(inp["mla_kv_norm_g"][0]))
            extra = {"win": f32(inp["mla_w_in"][0]), "wks": wks, "gq": gq, "gkv": gkv, "wqb": f32(inp["mla_w_q_b"][0]), "wqs": wqs,
                     "wkvb": f32(inp["mla_w_kv_b"][0]), "cs": cs, "wo": f32(inp["mla_w_o"][0])}
        elif i == 2:
            H = hy_host(f32(inp["hy_conv_w"][0]), f32(inp["hy_conv_b"][0]), f32(inp["hy_f_b1"][0]), f32(inp["hy_f_b2"][0]), f32(inp["hy_f_b3"][0]), f32(inp["hy_sin_freq"][0]), f32(inp["hy_skip"][0]))
            extra = {"win": f32(inp["hy_w_in"][0]), "cw": H["cw"], "cb": H["cb"], "fw1": f32(inp["hy_f_w1"][0]), "fw2": f32(inp["hy_f_w2"][0]), "fw3": f32(inp["hy_f_w3"][0]),
                     "fb": H["fb"], "fb3": f32(inp["hy_f_b3"][0])[None, :], "skip": f32(inp["hy_skip"][0])[None, :], "delta": H["delta"], "wo": f32(inp["hy_w_o"][0])}
            for tag in ("c", "l"):
                for kk in ("ze", "negt", "wsc", "C", "S", "ST"):
                    extra[kk + tag] = H[kk + tag]
        else:
            cw, cb, sel8, tri = ml_host(f32(inp["ml_conv_w"][0]), f32(inp["ml_conv_b"][0]))
            extra = {"win": f32(inp["ml_w_in"][0]), "cw": cw, "cb": cb, "gb": f32(inp["ml_gate_b"][0]), "ong": f32(inp["ml_out_norm_g"][0]), "sel8": sel8, "tri": tri, "wo": f32(inp["ml_w_o"][0])}
        for b in range(NCORES):
            m = {"c_idf": KC["c_idf"], "c_iota": KC["c_iota"], "xin": xcur[b], "mods": mods_for(i, b), "ng1": f32(inp["norm_mix_g"][i]), "ng2": f32(inp["norm_ffn_g"][i]),
                 "rw": f32(inp["router_w"][i])}
            m.update(extra)
            if i > 0:
                m.update({"c_sel16": KC["c_sel16"], "c_slotid": KC["c_slotid"], "c_selq": KC["c_selq"], "modsp": mods_for(i - 1, b), "y": ycur[b], "posmt_in": pmcur[b]})
            maps.append(m)
        r = _launch(PA, maps)
        xcur = [np.asarray(r[b]["xmid"]) for b in range(NCORES)]
        pmcur = [np.asarray(r[b]["posmt"]) for b in range(NCORES)]
        xg = [np.asarray(r[b]["xg"]) for b in range(NCORES)]
        gvv = [np.asarray(r[b]["gv"]) for b in range(NCORES)]
        maps = []
        for j in range(NCORES):
            xgT = np.ascontiguousarray(np.stack([np.concatenate([xg[b][2 * j + el] for b in range(NCORES)], axis=1) for el in range(2)], 0))
            gvj = np.stack([np.concatenate([gvv[b][2 * j + el] for b in range(NCORES)], 0) for el in range(2)], 0)
            gvl = np.ascontiguousarray(gvj.reshape(2, 18, 128).transpose(0, 2, 1))
            maps.append({"xgT": xgT, "gv": gvl, "wg": f32(inp["moe_w_gate"][i, 2 * j:2 * j + 2]), "wu": f32(inp["moe_w_up"][i, 2 * j:2 * j + 2]),
                         "wd": f32(inp["moe_w_down"][i, 2 * j:2 * j + 2])})
        r = _launch(PB_, maps)
        yb = [np.asarray(r[j]["y"]) for j in range(NCORES)]
        ycur = [np.ascontiguousarray(np.concatenate([yb[j][:, b * NSLOT:(b + 1) * NSLOT, :] for j in range(NCORES)], 0)) for b in range(NCORES)]
    PF_ = build_F()
    maps = [{"c_idf": KC["c_idf"], "c_sel16": KC["c_sel16"], "c_slotid": KC["c_slotid"], "c_selq": KC["c_selq"], "xin": xcur[b], "modsp": mods_for(3, b), "y": ycur[b], "posmt_in": pmcur[b],
             "fng": f32(inp["final_norm_g"])} for b in range(NCORES)]
    r = _launch(PF_, maps)
    return np.stack([np.asarray(r[b]["out"]) for b in range(NCORES)], 0).astype(np.float32)
```
